# Optimizing a Trainium2 kernel written in Bass

```python
import jax
import jax.numpy as jnp
from jax import lax
import numpy as np


D_MODEL = 1024
BATCH = 8
SEQ = 2048
DEPTH = 4

GRID_W = 64
HEAD_DIM = 64
ATT_HEADS = 8
ATT_KV_HEADS = 2
ATT_WIDTH = ATT_HEADS * HEAD_DIM
KV_WIDTH = ATT_KV_HEADS * HEAD_DIM
Q_BLOCK = 128
ROPE_THETA = 10000.0
LRU_WIDTH = D_MODEL // 4
LRU_BLOCKS = 4
LRU_BLOCK_DIM = LRU_WIDTH // LRU_BLOCKS
CONV_WIDTH = 4
LRU_C = 8.0
MLSTM_HEADS = 4
MLSTM_DIM = 64
MLSTM_WIDTH = MLSTM_HEADS * MLSTM_DIM
MLSTM_CHUNK = 128
D_MIX = ATT_WIDTH + LRU_WIDTH + MLSTM_WIDTH
IN_SPLITS = (ATT_WIDTH, KV_WIDTH, KV_WIDTH, LRU_WIDTH, LRU_WIDTH, MLSTM_WIDTH, MLSTM_WIDTH, MLSTM_WIDTH, MLSTM_WIDTH, 4 * MLSTM_HEADS)
D_IN = sum(IN_SPLITS)
N_EXPERTS = 16
CAPACITY_FACTOR = 2
D_EXPERT = 2 * D_MODEL
EPS = 1e-6

kernel_name = 'hybrid_head_group_encoder_ec_moe'

F32 = jnp.float32


def rms_norm(x, g):
    xf = x.astype(F32)
    y = xf * lax.rsqrt(jnp.mean(xf * xf, axis=-1, keepdims=True) + EPS)
    return (y * g.astype(F32)).astype(x.dtype)


def split_columns(p):
    idx = [int(i) for i in np.cumsum(np.array(IN_SPLITS))[:-1]]
    return jnp.split(p, idx, axis=-1)


def axial_rope_tables(seq_len):
    rows = seq_len // GRID_W
    t = jnp.arange(rows * GRID_W)
    row = (t // GRID_W).astype(F32)
    col = (t % GRID_W).astype(F32)
    n_freq = HEAD_DIM // 4
    inv = 1.0 / (ROPE_THETA ** (jnp.arange(n_freq, dtype=F32) / n_freq))
    ang = jnp.concatenate([row[:, None] * inv, col[:, None] * inv], axis=-1)
    return jnp.cos(ang), jnp.sin(ang)


def apply_rope(x, cos, sin):
    half = HEAD_DIM // 2
    x1, x2 = x[..., :half], x[..., half:]
    return jnp.concatenate([x1 * cos - x2 * sin, x2 * cos + x1 * sin], axis=-1)


def axial_gqa_attention(q, k, v, q_g, k_g):
    B, S, _ = q.shape
    dt = q.dtype
    q = rms_norm(q.reshape(B, S, ATT_HEADS, HEAD_DIM), q_g)
    k = rms_norm(k.reshape(B, S, ATT_KV_HEADS, HEAD_DIM), k_g)
    v = v.reshape(B, S, ATT_KV_HEADS, HEAD_DIM)
    cos, sin = axial_rope_tables(S)
    cos, sin = cos[:, None, :], sin[:, None, :]
    q = apply_rope(q.astype(F32), cos, sin).astype(dt)
    k = apply_rope(k.astype(F32), cos, sin).astype(dt)
    grp = ATT_HEADS // ATT_KV_HEADS
    qb = q.reshape(B, S // Q_BLOCK, Q_BLOCK, ATT_KV_HEADS, grp, HEAD_DIM)
    qb = jnp.moveaxis(qb, 1, 0)
    scale = HEAD_DIM ** -0.5

    def one_block(qblk):
        s = jnp.einsum('bqhgd,bkhd->bhgqk', qblk, k).astype(F32) * scale
        p = jax.nn.softmax(s, axis=-1).astype(v.dtype)
        return jnp.einsum('bhgqk,bkhd->bqhgd', p, v)

    o = lax.map(one_block, qb)
    return jnp.moveaxis(o, 0, 1).reshape(B, S, ATT_WIDTH)


def centred_depthwise_conv(x, w, b):
    S = x.shape[1]
    left = CONV_WIDTH // 2
    xp = jnp.pad(x, ((0, 0), (left, CONV_WIDTH - 1 - left), (0, 0)))
    y = b
    for j in range(CONV_WIDTH):
        y = y + xp[:, j:j + S, :] * w[j]
    return y


def block_diag_linear(x, w, b):
    B, S, C = x.shape
    xb = x.reshape(B, S, LRU_BLOCKS, LRU_BLOCK_DIM)
    return jnp.einsum('bsnd,nde->bsne', xb, w).reshape(B, S, C) + b


def linear_scan(a, u):
    def comb(l, r):
        al, ul = l
        ar, ur = r
        return al * ar, ar * ul + ur
    _, h = lax.associative_scan(comb, (a, u), axis=1)
    return h


def rglru_direction(xc, wa, ba, wx, bx, lam, reverse):
    if reverse:
        xc = jnp.flip(xc, axis=1)
    r = jax.nn.sigmoid(block_diag_linear(xc, wa, ba).astype(F32))
    i = jax.nn.sigmoid(block_diag_linear(xc, wx, bx).astype(F32))
    log_a = -LRU_C * r * jax.nn.softplus(-lam.astype(F32))
    u = jnp.sqrt(-jnp.expm1(2.0 * log_a)) * (i * xc.astype(F32))
    h = linear_scan(jnp.exp(log_a), u)
    if reverse:
        h = jnp.flip(h, axis=1)
    return h


def rglru_mixer(xb, gate, conv_w, conv_b, wa, ba, wx, bx, lam):
    xc = centred_depthwise_conv(xb, conv_w, conv_b)
    h = rglru_direction(xc, wa[0], ba[0], wx[0], bx[0], lam[0], False) \
        + rglru_direction(xc, wa[1], ba[1], wx[1], bx[1], lam[1], True)
    return h.astype(xb.dtype) * jax.nn.gelu(gate)


def mlstm_chunkwise(q, k, v, i_pre, f_pre):
    B, H, S, D = q.shape
    L = MLSTM_CHUNK
    NC = S // L
    qc = q.reshape(B, H, NC, L, D)
    kc = k.reshape(B, H, NC, L, D) * (D ** -0.5)
    vc = v.reshape(B, H, NC, L, D)
    ig = i_pre.reshape(B, H, NC, L)
    bcum = jnp.cumsum(jax.nn.log_sigmoid(f_pre).reshape(B, H, NC, L), axis=-1)
    mask = jnp.tril(jnp.ones((L, L), dtype=bool))
    log_d = jnp.where(mask, bcum[..., :, None] - bcum[..., None, :] + ig[..., None, :], -jnp.inf)
    g = bcum[..., -1]
    log_w_end = g[..., None] - bcum + ig
    m_loc = jnp.max(log_w_end, axis=-1)
    w_end = jnp.exp(log_w_end - m_loc[..., None])
    c_loc = jnp.einsum('bhcl,bhcld,bhcle->bhcde', w_end, vc, kc)
    n_loc = jnp.einsum('bhcl,bhcle->bhce', w_end, kc)

    def step(carry, inp):
        c_st, n_st, m_st = carry
        c_l, n_l, m_l, g_c = inp
        m_new = jnp.maximum(g_c + m_st, m_l)
        a = jnp.exp(g_c + m_st - m_new)
        bb = jnp.exp(m_l - m_new)
        c_new = a[..., None, None] * c_st + bb[..., None, None] * c_l
        n_new = a[..., None] * n_st + bb[..., None] * n_l
        return (c_new, n_new, m_new), (c_st, n_st, m_st)

    init = (jnp.zeros((B, H, D, D), F32), jnp.zeros((B, H, D), F32), jnp.zeros((B, H), F32))
    xs = (jnp.moveaxis(c_loc, 2, 0), jnp.moveaxis(n_loc, 2, 0), jnp.moveaxis(m_loc, 2, 0), jnp.moveaxis(g, 2, 0))
    _, (c_prev, n_prev, m_prev) = lax.scan(step, init, xs)
    c_prev = jnp.moveaxis(c_prev, 0, 2)
    n_prev = jnp.moveaxis(n_prev, 0, 2)
    m_prev = jnp.moveaxis(m_prev, 0, 2)

    log_inter = bcum + m_prev[..., None]
    m_out = jnp.maximum(jnp.max(log_d, axis=-1), log_inter)
    s = jnp.einsum('bhcid,bhcjd->bhcij', qc, kc) * jnp.exp(log_d - m_out[..., None])
    inter = jnp.exp(log_inter - m_out)
    num = jnp.einsum('bhcij,bhcjd->bhcid', s, vc) + inter[..., None] * jnp.einsum('bhcde,bhcie->bhcid', c_prev, qc)
    den = jnp.sum(s, axis=-1) + inter * jnp.einsum('bhce,bhcie->bhci', n_prev, qc)
    h = num / jnp.maximum(jnp.abs(den), jnp.exp(-m_out))[..., None]
    return h.reshape(B, H, S, D)


def mlstm_mixer(q, k, v, o_pre, gates, f_bias, head_g):
    B, S, _ = q.shape
    dt = q.dtype

    def heads(t):
        return t.reshape(B, S, MLSTM_HEADS, MLSTM_DIM).transpose(0, 2, 1, 3).astype(F32)

    qh, kh, vh = heads(q), heads(k), heads(v)
    gt = gates.astype(F32).reshape(B, S, 4, MLSTM_HEADS).transpose(2, 0, 3, 1)
    fb = f_bias.astype(F32)
    h_fw = mlstm_chunkwise(qh, kh, vh, gt[0], gt[1] + fb[0][None, :, None])
    flip = lambda t: jnp.flip(t, axis=2)
    h_bw = jnp.flip(mlstm_chunkwise(flip(qh), flip(kh), flip(vh), jnp.flip(gt[2], axis=-1),
                                    jnp.flip(gt[3] + fb[1][None, :, None], axis=-1)), axis=2)
    h = rms_norm((h_fw + h_bw).transpose(0, 2, 1, 3), head_g)
    return h.reshape(B, S, MLSTM_WIDTH).astype(dt) * jax.nn.sigmoid(o_pre)


def expert_choice_moe(h, w_router, w_gate, w_up, w_down):
    B, S, D = h.shape
    cap = CAPACITY_FACTOR * S // N_EXPERTS
    aff = jax.nn.softmax(jnp.einsum('bsd,de->bse', h, w_router).astype(F32), axis=-1)
    gate, idx = lax.top_k(jnp.swapaxes(aff, 1, 2), cap)
    xg = jax.vmap(lambda hb, ib: hb[ib])(h, idx)
    a = jnp.einsum('becd,edf->becf', xg, w_gate)
    u = jnp.einsum('becd,edf->becf', xg, w_up)
    y = jnp.einsum('becf,efd->becd', jax.nn.silu(a) * u, w_down) * gate[..., None].astype(h.dtype)
    return jax.vmap(lambda yb, ib: jnp.zeros((S, D), yb.dtype).at[ib.reshape(-1)].add(yb.reshape(-1, D)))(y, idx)


def setup_inputs(seed: int = 0) -> dict:
    key = jax.random.key(seed)
    ks = jax.random.split(key, 24)
    nrm = lambda k, shape, s: jax.random.normal(k, shape, F32) * s
    gain = lambda k, shape: 1.0 + 0.02 * jax.random.normal(k, shape, F32)
    u = jax.random.uniform(ks[11], (DEPTH, 2, LRU_WIDTH), F32, 0.9, 0.999)
    a0 = u ** (1.0 / LRU_C)
    lam = jnp.log(a0) - jnp.log1p(-a0)
    f_bias = jnp.broadcast_to(jnp.linspace(3.0, 6.0, MLSTM_HEADS), (DEPTH, 2, MLSTM_HEADS)) \
        + 0.1 * jax.random.normal(ks[12], (DEPTH, 2, MLSTM_HEADS), F32)
    return {
        'x': jax.random.normal(ks[0], (BATCH, SEQ, D_MODEL), F32),
        'norm1_g': gain(ks[1], (DEPTH, D_MODEL)),
        'w_in': nrm(ks[2], (DEPTH, D_MODEL, D_IN), D_MODEL ** -0.5),
        'q_norm_g': gain(ks[3], (DEPTH, HEAD_DIM)),
        'k_norm_g': gain(ks[4], (DEPTH, HEAD_DIM)),
        'conv_w': nrm(ks[5], (DEPTH, CONV_WIDTH, LRU_WIDTH), CONV_WIDTH ** -0.5),
        'conv_b': nrm(ks[6], (DEPTH, LRU_WIDTH), 0.02),
        'lru_wa': nrm(ks[7], (DEPTH, 2, LRU_BLOCKS, LRU_BLOCK_DIM, LRU_BLOCK_DIM), LRU_BLOCK_DIM ** -0.5),
        'lru_ba': nrm(ks[8], (DEPTH, 2, LRU_WIDTH), 0.02),
        'lru_wx': nrm(ks[9], (DEPTH, 2, LRU_BLOCKS, LRU_BLOCK_DIM, LRU_BLOCK_DIM), LRU_BLOCK_DIM ** -0.5),
        'lru_bx': nrm(ks[10], (DEPTH, 2, LRU_WIDTH), 0.02),
        'lru_lambda': lam,
        'mlstm_f_bias': f_bias,
        'mlstm_norm_g': gain(ks[13], (DEPTH, MLSTM_HEADS, MLSTM_DIM)),
        'att_out_g': gain(ks[14], (DEPTH, ATT_WIDTH)),
        'lru_out_g': gain(ks[15], (DEPTH, LRU_WIDTH)),
        'w_out': nrm(ks[16], (DEPTH, D_MIX, D_MODEL), D_MIX ** -0.5),
        'norm2_g': gain(ks[17], (DEPTH, D_MODEL)),
        'w_router': nrm(ks[18], (DEPTH, D_MODEL, N_EXPERTS), D_MODEL ** -0.5),
        'w_expert_gate': nrm(ks[19], (DEPTH, N_EXPERTS, D_MODEL, D_EXPERT), D_MODEL ** -0.5),
        'w_expert_up': nrm(ks[20], (DEPTH, N_EXPERTS, D_MODEL, D_EXPERT), D_MODEL ** -0.5),
        'w_expert_down': nrm(ks[21], (DEPTH, N_EXPERTS, D_EXPERT, D_MODEL), D_EXPERT ** -0.5),
    }


def reference(x, norm1_g, w_in, q_norm_g, k_norm_g, conv_w, conv_b, lru_wa, lru_ba, lru_wx, lru_bx,
              lru_lambda, mlstm_f_bias, mlstm_norm_g, att_out_g, lru_out_g, w_out, norm2_g, w_router,
              w_expert_gate, w_expert_up, w_expert_down):
    for l in range(DEPTH):
        h = rms_norm(x, norm1_g[l])
        aq, ak, av, lx, lg, mq, mk, mv, mo, mg = split_columns(h @ w_in[l])
        y_att = axial_gqa_attention(aq, ak, av, q_norm_g[l], k_norm_g[l])
        y_lru = rglru_mixer(lx, lg, conv_w[l], conv_b[l], lru_wa[l], lru_ba[l], lru_wx[l], lru_bx[l], lru_lambda[l])
        y_mls = mlstm_mixer(mq, mk, mv, mo, mg, mlstm_f_bias[l], mlstm_norm_g[l])
        y = jnp.concatenate([rms_norm(y_att, att_out_g[l]), rms_norm(y_lru, lru_out_g[l]), y_mls], axis=-1)
        x = x + y @ w_out[l]
        h2 = rms_norm(x, norm2_g[l])
        x = x + expert_choice_moe(h2, w_router[l], w_expert_gate[l], w_expert_up[l], w_expert_down[l])
    return x
```

```python
import numpy as np
from contextlib import ExitStack
import concourse.bass as bass
import concourse.mybir as mybir
from concourse.bass_utils import run_bass_kernel_spmd

F32 = mybir.dt.float32
F32R = mybir.dt.float32r
AF = mybir.ActivationFunctionType
ALU = mybir.AluOpType
AX = mybir.AxisListType

S = 2048
D = 1024
DEPTH = 4
D_IN = 2320
NT = S // 128
EPS = 1e-6

ENG = ["pe", "act", "dve", "pool", "sp"]
BLK = {"pe": "tensor", "act": "scalar", "dve": "vector", "pool": "gpsimd", "sp": "sync"}


class Sched:
    def __init__(self, nc, es, n_dma=24):
        self.nc = nc
        self.sem = {e: es.enter_context(nc.semaphore("s_" + e)) for e in ENG}
        self.dsem = [es.enter_context(nc.semaphore("s_dma%d" % i)) for i in range(n_dma)]
        self.cnt = {e: 0 for e in ENG}
        self.duse = [0] * n_dma
        self.drr = 0
        self.ops = {e: [] for e in ENG}
        self.waited = {e: {} for e in ENG}
        self.last_w = {}
        self.readers = {}
        self.nops = 0

    def semobj(self, s):
        return self.sem[s] if isinstance(s, str) else self.dsem[s[1]]

    def add(self, eng, fn, reads=(), writes=(), dma=False):
        deps = {}
        def need(tok):
            s, v = tok
            if deps.get(s, 0) < v:
                deps[s] = v
        for r in reads:
            if r in self.last_w:
                need(self.last_w[r])
        for w in writes:
            if w in self.last_w:
                need(self.last_w[w])
            for t in self.readers.get(w, ()):
                need(t)
        if dma:
            k = self.drr
            self.drr = (self.drr + 1) % len(self.dsem)
            self.duse[k] += 1
            n = self.duse[k]
            if n > 1:
                need((("dma", k), 16 * (n - 1)))
            token = (("dma", k), 16 * n)
        else:
            self.cnt[eng] += 1
            token = (eng, self.cnt[eng])
        waits = []
        wd = self.waited[eng]
        for s, v in deps.items():
            if s == "pe" and eng == "pe" and not dma:
                continue
            if wd.get(s, 0) < v:
                wd[s] = v
                waits.append((s, v))
        self.ops[eng].append((waits, fn, token, dma))
        self.nops += 1
        for r in reads:
            self.readers.setdefault(r, []).append(token)
        for w in writes:
            self.last_w[w] = token
            self.readers[w] = []
        return token

    def flush(self):
        waits = []
        for k, n in enumerate(self.duse):
            if n and self.waited["sp"].get(("dma", k), 0) < 16 * n:
                waits.append((("dma", k), 16 * n))
        self.ops["sp"].append((waits, None, None, False))
        ops = self.ops
        self.ops = {e: [] for e in ENG}
        sched = self
        with self.nc.Block() as block:
            for e in ENG:
                lst = ops[e]

                def body(eng, lst=lst):
                    for waits, fn, token, isdma in lst:
                        for (s, v) in waits:
                            eng.wait_ge(sched.semobj(s), v)
                        if fn is None:
                            continue
                        inst = fn(eng)
                        inst.then_inc(sched.semobj(token[0]), 16 if isdma else 1)

                getattr(block, BLK[e])(body)
        for e in ENG:
            for e2 in ENG:
                self.waited[e][e2] = self.cnt[e2]
            for k, n in enumerate(self.duse):
                self.waited[e][("dma", k)] = 16 * n
        self.last_w = {}
        self.readers = {}

    def dma(self, q, out, in_, reads=(), writes=(), slow=False):
        if slow:
            return self.add(q, lambda e: e.dma_start(out=out, in_=in_, allow_slow_non_contiguous=True), reads, writes, dma=True)
        return self.add(q, lambda e: e.dma_start(out=out, in_=in_), reads, writes, dma=True)

    def mm(self, out, lhsT, rhs, start, stop, reads=(), writes=()):
        return self.add("pe", lambda e: e.matmul(out, lhsT, rhs, start=start, stop=stop), reads, writes)

    def tr(self, out, in_, ident, reads=(), writes=()):
        return self.add("pe", lambda e: e.transpose(out, in_, ident), reads, writes)

    def act(self, out, in_, func, reads=(), writes=(), eng="act", **kw):
        return self.add(eng, lambda e: e.activation(out, in_, func, **kw), reads, writes)

    def ts(self, eng, out, in0, s1, s2, op0, op1=None, reads=(), writes=(), **kw):
        if op1 is None:
            return self.add(eng, lambda e: e.tensor_scalar(out, in0, s1, None, op0, **kw), reads, writes)
        return self.add(eng, lambda e: e.tensor_scalar(out, in0, s1, s2, op0, op1, **kw), reads, writes)

    def tt(self, eng, out, in0, in1, op, reads=(), writes=()):
        return self.add(eng, lambda e: e.tensor_tensor(out, in0, in1, op), reads, writes)

    def stt(self, eng, out, in0, scalar, in1, op0, op1, reads=(), writes=()):
        return self.add(eng, lambda e: e.scalar_tensor_tensor(out, in0, scalar, in1, op0, op1), reads, writes)

    def copy(self, eng, out, in_, reads=(), writes=()):
        if eng == "act":
            return self.add(eng, lambda e: e.copy(out, in_), reads, writes)
        return self.add(eng, lambda e: e.tensor_copy(out, in_), reads, writes)


def r32(ap):
    return ap.bitcast(F32R)


class Ctx:
    pass


def phase_inproj(nc, sc, c, l, x_src):
    with ExitStack() as es:
        T = lambda name, shape, dt=F32: es.enter_context(nc.sbuf_tensor("%s_L%d" % (name, l), shape, dt))
        P = lambda name, shape, dt=F32: es.enter_context(nc.psum_tensor("%s_L%d" % (name, l), shape, dt))
        wt = T("a_wt", [128, 8, D_IN])
        g = T("a_g", [128, 8])
        ident = T("a_id", [128, 128])
        xt = [T("a_xt%d" % i, [128, D]) for i in range(2)]
        junk = T("a_junk", [128, D])
        st = [T("a_st%d" % i, [128, 4]) for i in range(2)]
        hT = [T("a_hT%d" % i, [128, 8, 512]) for i in range(2)]
        og = [T("a_og%d" % i, [128, D_IN]) for i in range(2)]
        of = [T("a_of%d" % i, [128, 512]) for i in range(2)]
        pT = [P("a_pT%d" % i, [128, 512]) for i in range(4)]
        pm = [P("a_pm%d" % i, [128, 512]) for i in range(4)]

        sc.dma("sp", ident[:], c.ident, writes=["ident"])
        sc.dma("sp", g[:], c.norm1_g[l].rearrange("(c p) -> p c", p=128), writes=["g"], slow=True)
        for ch in range(8):
            sc.dma("sp", r32(wt[:, ch, :]), r32(c.w_in[l][ch * 128:(ch + 1) * 128, :]), writes=[("wt", ch)])
        for ch in range(8):
            eng = "dve" if ch % 2 == 0 else "pool"
            sc.ts(eng, r32(wt[:, ch, :]), wt[:, ch, :], g[:, ch:ch + 1], None, ALU.mult,
                  reads=["g", ("wt", ch)], writes=[("wt", ch)])

        pmi = 0
        for b in range(4):
            hb = hT[b % 2]
            hkey = ("hT", b % 2)
            for tl in range(4):
                i = b * 4 + tl
                xb = xt[i % 2]
                sb = st[i % 2]
                xk = ("xt", i % 2)
                sk = ("st", i % 2)
                sc.dma("sp", xb[:], x_src[i * 128:(i + 1) * 128, :], writes=[xk])
                sc.act(junk[:], xb[:], AF.Square, reads=[xk], writes=["junk", sk], accum_out=sb[:, 0:1])
                sc.ts("dve", sb[:, 1:2], sb[:, 0:1], 1.0 / D, EPS, ALU.mult, ALU.add, reads=[sk], writes=[sk])
                sc.act(sb[:, 2:3], sb[:, 1:2], AF.Sqrt, reads=[sk], writes=[sk])
                sc.add("dve", lambda e, o=sb[:, 3:4], i_=sb[:, 2:3]: e.reciprocal(o, i_), reads=[sk], writes=[sk])
                sc.ts("dve", xb[:], xb[:], sb[:, 3:4], None, ALU.mult, reads=[xk, sk], writes=[xk])
                for half in range(2):
                    pt = pT[(i % 2) * 2 + half]
                    pk = ("pT", (i % 2) * 2 + half)
                    for cc in range(4):
                        ch = half * 4 + cc
                        sc.tr(pt[:, cc * 128:(cc + 1) * 128], xb[:, ch * 128:(ch + 1) * 128], ident[:],
                              reads=[xk, "ident"], writes=[pk])
                    eng = "act" if half == 0 else "dve"
                    sc.copy(eng, r32(hb[:, half * 4:(half + 1) * 4, tl * 128:(tl + 1) * 128]),
                            pt[:].rearrange("p (c t) -> p c t", c=4), reads=[pk], writes=[(hkey, tl)])
            for tl in range(4):
                i = b * 4 + tl
                ob = og[i % 2]
                ok = ("og", i % 2)
                for n in range(5):
                    n0 = n * 512
                    nw = min(512, D_IN - n0)
                    pmm = pm[pmi % 4]
                    pk = ("pm", pmi % 4)
                    pmi += 1
                    for ch in range(8):
                        sc.mm(pmm[:, 0:nw], r32(hb[:, ch, tl * 128:(tl + 1) * 128]), r32(wt[:, ch, n0:n0 + nw]),
                              ch == 0, ch == 7, reads=[(hkey, tl), ("wt", ch)], writes=[pk])
                    eng = "act" if n % 2 == 0 else "dve"
                    sc.copy(eng, ob[:, n0:n0 + nw], pmm[:, 0:nw], reads=[pk], writes=[ok])
                sc.dma("pool", c.p_tm[i * 128:(i + 1) * 128, :], ob[:], reads=[ok])
            for j in range(8):
                c0 = 768 + j * 128
                pmm = pm[pmi % 4]
                pk = ("pm", pmi % 4)
                pmi += 1
                ob = of[j % 2]
                ok = ("of", j % 2)
                for ch in range(8):
                    sc.mm(pmm[:, :], r32(wt[:, ch, c0:c0 + 128]), r32(hb[:, ch, :]), ch == 0, ch == 7,
                          reads=[(hkey, 0), (hkey, 1), (hkey, 2), (hkey, 3), ("wt", ch)], writes=[pk])
                eng = "act" if j % 2 == 0 else "dve"
                sc.copy(eng, ob[:], pmm[:], reads=[pk], writes=[ok])
                sc.dma("pool", c.p_fm[j * 128:(j + 1) * 128, b * 512:(b + 1) * 512], ob[:], reads=[ok])
        sc.flush()


def phase_attn(nc, sc, c, l):
    with ExitStack() as es:
        T = lambda name, shape, dt=F32: es.enter_context(nc.sbuf_tensor("%s_L%d" % (name, l), shape, dt))
        P = lambda name, shape, dt=F32: es.enter_context(nc.psum_tensor("%s_L%d" % (name, l), shape, dt))
        ident = T("b_id", [128, 128])
        ones = T("b_ones", [128, 128])
        gg = T("b_gg", [128, 640])
        qT = T("b_qT", [128, 4, S])
        kT = T("b_kT", [128, S])
        vx = T("b_vx", [128, NT, 2, 65])
        qkv = [T("b_qkv%d" % i, [128, 768]) for i in range(2)]
        cs = [T("b_cs%d" % i, [128, 2, 320]) for i in range(2)]
        sq = T("b_sq", [128, 640])
        st = T("b_st", [128, 4, 10])
        qn = T("b_qn", [128, 640])
        tmp = T("b_tmp", [128, 4, 320])
        qr = T("b_qr", [128, 640])
        PT = [T("b_PT%d" % i, [128, 512]) for i in range(3)]
        rd = [T("b_rd%d" % i, [128, 512]) for i in range(2)]
        bcs = [T("b_bcs%d" % i, [64, 512]) for i in range(2)]
        yo = [T("b_yo%d" % i, [64, 512]) for i in range(2)]
        ptA = [P("b_ptA%d" % i, [128, 512]) for i in range(1)]
        ptB = [P("b_ptB%d" % i, [128, 512]) for i in range(1)]
        sps = [P("b_sps%d" % i, [128, 512]) for i in range(3)]
        ops_ = [P("b_ops%d" % i, [128, 512]) for i in range(2)]
        bcp = P("b_bcp", [128, 512])

        sc.dma("sp", ident[:], c.ident, writes=["ident"])
        sc.dma("sp", ones[:], c.ones, writes=["ones"])
        sc.dma("sp", gg[:], c.ggrep[l], writes=["gg"])
        for i in range(NT):
            qb_ = qkv[i % 2]; qk = ("qkv", i % 2)
            cb = cs[i % 2]; ck = ("cs", i % 2)
            for half in range(2):
                sc.dma("sp", qb_[:, 0:512].rearrange("p (pr half d) -> p pr half d", pr=4, half=2)[:, :, half, :],
                       c.p_tm[i * 128:(i + 1) * 128, half * 256:(half + 1) * 256].rearrange("t (pr d) -> t pr d", pr=4),
                       writes=[qk])
            sc.dma("sp", qb_[:, 512:768], c.p_tm[i * 128:(i + 1) * 128, 512:768], writes=[qk])
            sc.dma("sp", cb[:, 0, :], c.cos_rep[i * 128:(i + 1) * 128, :], writes=[ck])
            sc.dma("sp", cb[:, 1, :], c.sin_rep[i * 128:(i + 1) * 128, :], writes=[ck])
            sc.tt("pool", sq[:], qb_[:, 0:640], qb_[:, 0:640], ALU.mult, reads=[qk], writes=["sq"])
            sc.add("dve", lambda e, o=st[:, 0, :], i_=sq[:].rearrange("p (h d) -> p h d", d=64):
                   e.tensor_reduce(o, i_, AX.X, ALU.add), reads=["sq"], writes=["st"])
            sc.ts("dve", st[:, 1, :], st[:, 0, :], 1.0 / 64, EPS, ALU.mult, ALU.add, reads=["st"], writes=["st"])
            sc.act(st[:, 2, :], st[:, 1, :], AF.Sqrt, reads=["st"], writes=["st"])
            sc.add("dve", lambda e, o=st[:, 3, :], i_=st[:, 2, :]: e.reciprocal(o, i_), reads=["st"], writes=["st"])
            sc.tt("dve", qn[:].rearrange("p (h d) -> p h d", d=64), qb_[:, 0:640].rearrange("p (h d) -> p h d", d=64),
                  st[:, 3, :].unsqueeze(2).broadcast_to([128, 10, 64]), ALU.mult, reads=[qk, "st"], writes=["qn"])
            sc.tt("pool", qn[:], qn[:], gg[:], ALU.mult, reads=["qn", "gg"], writes=["qn"])
            q3 = qn[:].rearrange("p (h d) -> p h d", d=64)
            x1 = q3[:, :, 0:32]; x2 = q3[:, :, 32:64]
            co = cb[:, 0, :].rearrange("p (h d) -> p h d", d=32); si = cb[:, 1, :].rearrange("p (h d) -> p h d", d=32)
            t = [tmp[:, k, :].rearrange("p (h d) -> p h d", d=32) for k in range(4)]
            r3 = qr[:].rearrange("p (h d) -> p h d", d=64)
            sc.tt("dve", t[0], x1, co, ALU.mult, reads=["qn", ck], writes=[("tmp", 0)])
            sc.tt("pool", t[1], x2, si, ALU.mult, reads=["qn", ck], writes=[("tmp", 1)])
            sc.tt("dve", t[2], x2, co, ALU.mult, reads=["qn", ck], writes=[("tmp", 2)])
            sc.tt("pool", t[3], x1, si, ALU.mult, reads=["qn", ck], writes=[("tmp", 3)])
            sc.tt("dve", r3[:, :, 0:32], t[0], t[1], ALU.subtract, reads=[("tmp", 0), ("tmp", 1)], writes=["qr"])
            sc.tt("pool", r3[:, :, 32:64], t[2], t[3], ALU.add, reads=[("tmp", 2), ("tmp", 3)], writes=["qr"])
            for pr in range(4):
                sc.tr(ptA[0][:, pr * 128:(pr + 1) * 128], qr[:, pr * 128:(pr + 1) * 128], ident[:], reads=["qr", "ident"], writes=["ptA"])
            sc.tr(ptB[0][:, 0:128], qr[:, 512:640], ident[:], reads=["qr", "ident"], writes=["ptB"])
            sc.copy("act", r32(qT[:, :, i * 128:(i + 1) * 128]), ptA[0][:].rearrange("p (c t) -> p c t", c=4),
                    reads=["ptA"], writes=["qT"])
            sc.copy("dve", r32(kT[:, i * 128:(i + 1) * 128]), ptB[0][:, 0:128], reads=["ptB"], writes=["kT"])
            sc.copy("act", r32(vx[:, i, :, 0:64]), qb_[:, 640:768].rearrange("p (g d) -> p g d", d=64),
                    reads=[qk], writes=["vx"])
            sc.ts("pool", r32(vx[:, i, :, 64:65]), qb_[:, 0:2].unsqueeze(2), 0.0, 1.0, ALU.mult, ALU.add,
                  reads=[qk], writes=["vx"])

        steps = [(h, qb, kt) for h in range(8) for qb in range(4) for kt in range(NT)]
        def fin(gi, h, qb):
            O = ops_[gi % 2]; ok = ("O", gi % 2)
            r = rd[gi % 2]; rk = ("rd", gi % 2)
            sc.act(r[64:65, :], O[64:65, :], AF.Ln, reads=[ok], writes=[rk])
            sc.act(r[64:65, :], r[64:65, :], AF.Exp, reads=[rk], writes=[rk], scale=-1.0)
            sc.mm(bcp[0:64, :], ones[64:65, 0:64], r[64:65, :], True, True, reads=[rk, "ones"], writes=["bcp"])
            sc.copy("act", bcs[gi % 2][:], bcp[0:64, :], reads=["bcp"], writes=[("bcs", gi % 2)])
            sc.tt("dve", yo[gi % 2][:], O[0:64, :], bcs[gi % 2][:], ALU.mult, reads=[ok, ("bcs", gi % 2)],
                  writes=[("yo", gi % 2)])
            sc.dma("pool", c.yT[h * 64:(h + 1) * 64, qb * 512:(qb + 1) * 512], yo[gi % 2][:], reads=[("yo", gi % 2)])
        for s_ in range(len(steps) + 1):
            if s_ < len(steps):
                h, qb, kt = steps[s_]
                pr = h % 4; half = h // 4
                p0 = half * 64
                sp_ = sps[s_ % 3]; sk = ("sps", s_ % 3)
                sc.mm(sp_[:, :], r32(kT[p0:p0 + 64, kt * 128:(kt + 1) * 128]),
                      r32(qT[p0:p0 + 64, pr, qb * 512:(qb + 1) * 512]), True, True,
                      reads=["kT", "qT"], writes=[sk])
                sc.act(r32(PT[s_ % 3][:]), sp_[:], AF.Exp, reads=[sk], writes=[("PT", s_ % 3)], scale=0.125)
            if s_ >= 1:
                h, qb, kt = steps[s_ - 1]
                gi = (s_ - 1) // NT
                half = h // 4
                O = ops_[gi % 2]; ok = ("O", gi % 2)
                sc.mm(O[0:65, :], r32(vx[:, kt, half, :]), r32(PT[(s_ - 1) % 3][:]), kt == 0, kt == NT - 1,
                      reads=["vx", ("PT", (s_ - 1) % 3)], writes=[ok])
                if kt == NT - 1:
                    fin(gi, h, qb)
        sc.flush()


def phase_lru(nc, sc, c, l):
    with ExitStack() as es:
        T = lambda name, shape, dt=F32: es.enter_context(nc.sbuf_tensor("%s_L%d" % (name, l), shape, dt))
        P = lambda name, shape, dt=F32: es.enter_context(nc.psum_tensor("%s_L%d" % (name, l), shape, dt))
        sm = T("c_sm", [128, 2, 11])
        W = T("c_W", [128, 8, 128])
        X = T("c_X", [128, S]); G = T("c_G", [128, S]); XF = T("c_XF", [128, S]); XC = T("c_XC", [128, S])
        R = T("c_R", [128, S]); I_ = T("c_I", [128, S]); A = T("c_A", [128, S]); U = T("c_U", [128, S])
        H = [T("c_H%d" % i, [128, S]) for i in range(2)]
        cf = T("c_cf", [128, 8])
        pp = [P("c_pp%d" % i, [128, 512]) for i in range(4)]
        sc.dma("sp", sm[:], c.lru_small[l], writes=["sm"])
        sc.dma("sp", r32(W[:]), r32(c.lru_w[l]), writes=["W"])
        ppi = 0
        for cc in range(2):
            sc.dma("sp", X[:], c.p_fm[cc * 128:(cc + 1) * 128, :], writes=["X"])
            sc.dma("sp", G[:], c.p_fm[256 + cc * 128:256 + (cc + 1) * 128, :], writes=["G"])
            w = lambda j: sm[:, cc, j:j + 1]
            sc.ts("dve", XF[:], X[:], w(2), w(4), ALU.mult, ALU.add, reads=["X", "sm"], writes=["XF"])
            sc.stt("dve", XF[:, 2:S], X[:, 0:S - 2], w(0), XF[:, 2:S], ALU.mult, ALU.add, reads=["X", "XF", "sm"], writes=["XF"])
            sc.stt("dve", XF[:, 1:S], X[:, 0:S - 1], w(1), XF[:, 1:S], ALU.mult, ALU.add, reads=["X", "XF", "sm"], writes=["XF"])
            sc.copy("pool", r32(XC[:, S - 1:S]), XF[:, S - 1:S], reads=["XF"], writes=["XC"])
            sc.stt("dve", r32(XC[:, 0:S - 1]), X[:, 1:S], w(3), XF[:, 0:S - 1], ALU.mult, ALU.add, reads=["X", "XF", "sm"], writes=["XC"])
            for d in range(2):
                ba = sm[:, cc, 5 + d:6 + d]; bx = sm[:, cc, 7 + d:8 + d]; lam = sm[:, cc, 9 + d:10 + d]
                ck = ("cf", d)
                c0 = cf[:, 4 * d:4 * d + 1]; c1 = cf[:, 4 * d + 1:4 * d + 2]; c2 = cf[:, 4 * d + 2:4 * d + 3]
                sc.act(c0, lam, AF.Exp, reads=["sm"], writes=[ck], scale=-1.0)
                sc.act(c0, c0, AF.Ln, reads=[ck], writes=[ck], bias=1.0)
                sc.ts("dve", c1, c0, -8.0, None, ALU.mult, reads=[ck], writes=[ck])
                sc.ts("dve", c2, c0, -16.0, None, ALU.mult, reads=[ck], writes=[ck])
                for tb in range(4):
                    sl = slice(tb * 512, (tb + 1) * 512)
                    pa = pp[ppi % 4]; pak = ("pp", ppi % 4); ppi += 1
                    px = pp[ppi % 4]; pxk = ("pp", ppi % 4); ppi += 1
                    sc.mm(pa[:, :], r32(W[:, d * 4 + 0 * 2 + cc, :]), r32(XC[:, sl]), True, True, reads=["W", "XC"], writes=[pak])
                    sc.mm(px[:, :], r32(W[:, d * 4 + 1 * 2 + cc, :]), r32(XC[:, sl]), True, True, reads=["W", "XC"], writes=[pxk])
                    sc.act(R[:, sl], pa[:, :], AF.Sigmoid, reads=[pak, "sm"], writes=["R"], bias=ba)
                    sc.act(I_[:, sl], px[:, :], AF.Sigmoid, reads=[pxk, "sm"], writes=["I"], bias=bx)
                sc.act(A[:], R[:], AF.Exp, reads=["R", ck], writes=["A"], scale=c1)
                sc.act(U[:], R[:], AF.Exp, reads=["R", ck], writes=["U"], scale=c2)
                sc.ts("pool", U[:], U[:], -1.0, 1.0, ALU.mult, ALU.add, reads=["U"], writes=["U"])
                sc.act(U[:], U[:], AF.Sqrt, reads=["U"], writes=["U"])
                sc.tt("pool", I_[:], I_[:], XC[:], ALU.mult, reads=["I", "XC"], writes=["I"])
                sc.tt("dve", U[:], U[:], I_[:], ALU.mult, reads=["U", "I"], writes=["U"])
                if d == 0:
                    sc.add("dve", lambda e, o=H[0][:], a=A[:], u=U[:]: e.tensor_tensor_scan(o, a, u, 0.0, ALU.mult, ALU.add),
                           reads=["A", "U"], writes=[("H", 0)])
                else:
                    sc.add("dve", lambda e, o=H[1][:, ::-1], a=A[:, ::-1], u=U[:, ::-1]: e.tensor_tensor_scan(o, a, u, 0.0, ALU.mult, ALU.add),
                           reads=["A", "U"], writes=[("H", 1)])
            sc.tt("pool", H[0][:], H[0][:], H[1][:], ALU.add, reads=[("H", 0), ("H", 1)], writes=[("H", 0)])
            sc.tt("dve", R[:], G[:], G[:], ALU.mult, reads=["G", "R"], writes=["R"])
            sc.ts("dve", R[:], R[:], 0.044715, 1.0, ALU.mult, ALU.add, reads=["R"], writes=["R"])
            sc.tt("dve", R[:], R[:], G[:], ALU.mult, reads=["R", "G"], writes=["R"])
            sc.act(R[:], R[:], AF.Sigmoid, reads=["R"], writes=["R"], scale=1.5957691216057308)
            sc.tt("pool", H[0][:], H[0][:], G[:], ALU.mult, reads=[("H", 0), "G"], writes=[("H", 0)])
            sc.tt("dve", H[0][:], H[0][:], R[:], ALU.mult, reads=[("H", 0), "R"], writes=[("H", 0)])
            sc.dma("pool", c.yT[512 + cc * 128:512 + (cc + 1) * 128, :], H[0][:], reads=[("H", 0)])
        sc.flush()


def phase_mlstm(nc, sc, c, l):
    with ExitStack() as es:
        T = lambda name, shape, dt=F32: es.enter_context(nc.sbuf_tensor("%s_L%d" % (name, l), shape, dt))
        P = lambda name, shape, dt=F32: es.enter_context(nc.psum_tensor("%s_L%d" % (name, l), shape, dt))
        ident = T("d_id", [128, 128]); ones = T("d_ones", [128, 128])
        mask = T("d_mask", [128, 2, 128])
        fb = T("d_fb", [128, 8]); ng = T("d_ng", [128, 256])
        qT = T("d_qT", [128, 2, S]); kT = T("d_kT", [128, 2, S])
        tm = T("d_tm", [128, NT, 784])
        vp = T("d_vp", [128, NT, 8, 65])
        wA = T("d_wA", [128, NT, 8]); wB = T("d_wB", [128, NT, 8]); eg = T("d_eg", [128, NT, 8])
        nlf = T("d_nlf", [128, NT, 8]); tg = T("d_tg", [128, NT, 8])
        hs = T("d_hs", [128, NT, 256])
        Cst = T("d_C", [128, 2, 4, 65])
        tC = T("d_tC", [128, 65])
        PT = [T("d_PT%d" % i, [128, 128]) for i in range(3)]
        s4 = [T("d_s4%d" % i, [128, 4, 4]) for i in range(2)]
        sq = T("d_sq", [128, 256]); st = T("d_st", [128, 4, 4]); ym = [T("d_ym%d" % i, [128, 256]) for i in range(2)]
        yo = [T("d_yo%d" % i, [128, 256]) for i in range(2)]
        sg = T("d_sg", [128, 256])
        pg = P("d_pg", [128, 512])
        psS = [P("d_pS%d" % i, [128, 512]) for i in range(2)]
        pacc = [P("d_pa%d" % i, [128, 512]) for i in range(2)]
        pkv = [P("d_pk%d" % i, [128, 512]) for i in range(2)]
        ptr = P("d_ptr", [128, 512])

        sc.dma("sp", ident[:], c.ident, writes=["ident"])
        sc.dma("sp", ones[:], c.ones, writes=["ones"])
        sc.dma("sp", mask[:, 0, :], c.triu, writes=["mask"])
        sc.dma("sp", mask[:, 1, :], c.tril, writes=["mask"])
        sc.dma("sp", fb[:], c.fbrep[l], writes=["fb"])
        sc.dma("sp", ng[:], c.ngrep[l], writes=["ng"])
        for ch in range(2):
            sc.dma("sp", qT[:, ch, :], c.p_fm[512 + ch * 128:512 + (ch + 1) * 128, :], writes=["qT"])
            sc.dma("sp", kT[:, ch, :], c.p_fm[768 + ch * 128:768 + (ch + 1) * 128, :], writes=["kT"])
        for i in range(NT):
            sc.dma("sp", tm[:, i, :], c.p_tm[i * 128:(i + 1) * 128, 1536:2320], writes=[("tm", i)])
        sc.add("pool", lambda e: e.memset(Cst[:], 0.0), writes=["C"])
        for i in range(NT):
            gt = tm[:, i, 768:784]
            fview = gt.rearrange("p (d k h) -> p d k h", d=2, k=2)[:, :, 1, :]
            iview = gt.rearrange("p (d k h) -> p d k h", d=2, k=2)[:, :, 0, :]
            tk = ("tg", i)
            sc.tt("dve", tg[:, i, :].rearrange("p (d h) -> p d h", d=2), fview, fb[:].rearrange("p (d h) -> p d h", d=2),
                  ALU.add, reads=[("tm", i), "fb"], writes=[tk])
            sc.act(tg[:, i, :], tg[:, i, :], AF.Exp, reads=[tk], writes=[tk], scale=-1.0)
            sc.act(nlf[:, i, :], tg[:, i, :], AF.Ln, reads=[tk], writes=[("nlf", i)], bias=1.0)
            sc.mm(pg[:, 0:4], mask[:, 0, :], nlf[:, i, 0:4], True, True, reads=["mask", ("nlf", i)], writes=["pg"])
            sc.mm(pg[:, 4:8], mask[:, 1, :], nlf[:, i, 4:8], True, True, reads=["mask", ("nlf", i)], writes=["pg"])
            sc.mm(pg[:, 8:16], ones[:, :], nlf[:, i, :], True, True, reads=["ones", ("nlf", i)], writes=["pg"])
            sc.act(wA[:, i, :], pg[:, 0:8], AF.Exp, reads=["pg"], writes=[("wA", i)], scale=-1.0)
            sc.act(eg[:, i, :], pg[:, 8:16], AF.Exp, reads=["pg"], writes=[("eg", i)], scale=-1.0)
            sc.tt("dve", wB[:, i, :].rearrange("p (d h) -> p d h", d=2), iview, pg[:, 0:8].rearrange("p (d h) -> p d h", d=2),
                  ALU.add, reads=[("tm", i), "pg"], writes=[("wB", i)])
            sc.act(wB[:, i, :], wB[:, i, :], AF.Exp, reads=[("wB", i)], writes=[("wB", i)])
            v3 = tm[:, i, 256:512].rearrange("p (h d) -> p h d", d=64)
            for d in range(2):
                eng = "dve" if d == 0 else "pool"
                sc.tt(eng, vp[:, i, d * 4:(d + 1) * 4, 0:64], v3, wB[:, i, d * 4:(d + 1) * 4].unsqueeze(2).broadcast_to([128, 4, 64]),
                      ALU.mult, reads=[("tm", i), ("wB", i)], writes=[("vp", i)])
            sc.copy("pool", vp[:, i, :, 64:65], wB[:, i, :].unsqueeze(2), reads=[("wB", i)], writes=[("vp", i)])
        ui = 0
        for s_ in range(NT):
            for d in range(2):
                i = s_ if d == 0 else NT - 1 - s_
                tsl = slice(i * 128, (i + 1) * 128)
                acc = pacc[d]; ak = ("pacc", d)
                for h in range(4):
                    p0 = (h % 2) * 64; ch = h // 2
                    pS = psS[ui % 2]; pk_ = ("pS", ui % 2)
                    pt = PT[ui % 3]; ptk = ("PT", ui % 3)
                    kv = pkv[ui % 2]; kvk = ("pkv", ui % 2)
                    ui += 1
                    sc.mm(pS[:, 0:128], kT[p0:p0 + 64, ch, tsl], qT[p0:p0 + 64, ch, tsl], True, True,
                          reads=["kT", "qT"], writes=[pk_])
                    sc.stt("dve", pt[:], pS[:, 0:128], 0.125, mask[:, d, :], ALU.mult, ALU.mult,
                           reads=[pk_, "mask"], writes=[ptk])
                    sc.mm(acc[:, h * 65:(h + 1) * 65], pt[:], vp[:, i, d * 4 + h, :], True, False,
                          reads=[ptk, ("vp", i)], writes=[ak])
                    sc.mm(acc[:, h * 65:(h + 1) * 65], qT[p0:p0 + 64, ch, tsl], Cst[p0:p0 + 64, d, h, :], False, True,
                          reads=["qT", ("C", d, h)], writes=[ak])
                    if s_ < NT - 1:
                        sc.mm(kv[p0:p0 + 64, 0:65], tm[:, i, h * 64:(h + 1) * 64], vp[:, i, d * 4 + h, :], True, True,
                              reads=[("tm", i), ("vp", i)], writes=[kvk])
                        sc.stt("dve", tC[p0:p0 + 64, :], kv[p0:p0 + 64, 0:65], 0.125, Cst[p0:p0 + 64, d, h, :], ALU.mult, ALU.add,
                               reads=[kvk, ("C", d, h)], writes=["tC"])
                        sc.ts("dve", Cst[p0:p0 + 64, d, h, :], tC[p0:p0 + 64, :], eg[p0:p0 + 64, i, d * 4 + h:d * 4 + h + 1], None, ALU.mult,
                              reads=["tC", ("eg", i)], writes=[("C", d, h)])
                a3 = acc[:, 0:260].rearrange("p (h e) -> p h e", e=65)
                sb = s4[d]; sk = ("s4", d)
                sc.tt("dve", sb[:, 0, :], a3[:, :, 64], wA[:, i, d * 4:(d + 1) * 4], ALU.mult, reads=[ak, ("wA", i)], writes=[sk])
                sc.act(sb[:, 1, :], sb[:, 0, :], AF.Abs, reads=[sk], writes=[sk])
                sc.ts("dve", sb[:, 1, :], sb[:, 1, :], 1.0, None, ALU.max, reads=[sk], writes=[sk])
                sc.add("dve", lambda e, o=sb[:, 2, :], i_=sb[:, 1, :]: e.reciprocal(o, i_), reads=[sk], writes=[sk])
                sc.tt("dve", sb[:, 3, :], sb[:, 2, :], wA[:, i, d * 4:(d + 1) * 4], ALU.mult, reads=[sk, ("wA", i)], writes=[sk])
                h3 = hs[:, i, :].rearrange("p (h e) -> p h e", e=64)
                if s_ < NT // 2:
                    sc.tt("dve", h3, a3[:, :, 0:64], sb[:, 3, :].unsqueeze(2).broadcast_to([128, 4, 64]), ALU.mult,
                          reads=[ak, sk], writes=[("hs", i)])
                else:
                    sc.tt("dve", sq[:].rearrange("p (h e) -> p h e", e=64), a3[:, :, 0:64],
                          sb[:, 3, :].unsqueeze(2).broadcast_to([128, 4, 64]), ALU.mult, reads=[ak, sk], writes=["sq"])
                    sc.tt("pool", hs[:, i, :], hs[:, i, :], sq[:], ALU.add, reads=[("hs", i), "sq"], writes=[("hs", i)])
        for i in range(NT):
            y = ym[i % 2]; yk = ("ym", i % 2)
            sc.tt("pool", sq[:], hs[:, i, :], hs[:, i, :], ALU.mult, reads=[("hs", i)], writes=["sq"])
            sc.add("dve", lambda e, o=st[:, 0, :], i_=sq[:].rearrange("p (h d) -> p h d", d=64):
                   e.tensor_reduce(o, i_, AX.X, ALU.add), reads=["sq"], writes=["st"])
            sc.ts("dve", st[:, 1, :], st[:, 0, :], 1.0 / 64, EPS, ALU.mult, ALU.add, reads=["st"], writes=["st"])
            sc.act(st[:, 2, :], st[:, 1, :], AF.Sqrt, reads=["st"], writes=["st"])
            sc.add("dve", lambda e, o=st[:, 3, :], i_=st[:, 2, :]: e.reciprocal(o, i_), reads=["st"], writes=["st"])
            sc.tt("dve", y[:].rearrange("p (h d) -> p h d", d=64), hs[:, i, :].rearrange("p (h d) -> p h d", d=64),
                  st[:, 3, :].unsqueeze(2).broadcast_to([128, 4, 64]), ALU.mult, reads=[("hs", i), "st"], writes=[yk])
            sc.tt("pool", y[:], y[:], ng[:], ALU.mult, reads=[yk, "ng"], writes=[yk])
            sc.act(sg[:], tm[:, i, 512:768], AF.Sigmoid, reads=[("tm", i)], writes=["sg"])
            sc.tt("dve", y[:], y[:], sg[:], ALU.mult, reads=[yk, "sg"], writes=[yk])
            for ch in range(2):
                sc.tr(ptr[:, ch * 128:(ch + 1) * 128], y[:, ch * 128:(ch + 1) * 128], ident[:], reads=[yk, "ident"], writes=["ptr"])
            o = yo[i % 2]; ok = ("yo", i % 2)
            sc.copy("act", o[:], ptr[:, 0:256], reads=["ptr"], writes=[ok])
            sc.dma("pool", c.yT[768:1024, i * 128:(i + 1) * 128].rearrange("(ch p) t -> p ch t", p=128),
                   o[:].rearrange("p (ch t) -> p ch t", ch=2), reads=[ok])
        sc.flush()


BF16 = mybir.dt.bfloat16
F16 = mybir.dt.float16
NE = 16
CAP = 256


def phase_outproj(nc, sc, c, l, x_src):
    with ExitStack() as es:
        T = lambda name, shape, dt=F32: es.enter_context(nc.sbuf_tensor("%s_L%d" % (name, l), shape, dt))
        P = lambda name, shape, dt=F32: es.enter_context(nc.psum_tensor("%s_L%d" % (name, l), shape, dt))
        wt = T("e_wt", [128, 8, D]); g = T("e_g", [128, 8]); ones = T("e_ones", [128, 128])
        yb = [T("e_yb%d" % i, [128, 8, 512]) for i in range(2)]
        ysq = T("e_ysq", [128, 6, 512])
        xt = [T("e_xt%d" % i, [128, D]) for i in range(2)]
        st = [T("e_st%d" % i, [128, 8]) for i in range(2)]
        pm = [P("e_pm%d" % i, [128, 512]) for i in range(6)]
        pst = P("e_pst", [128, 512])
        sc.dma("sp", ones[:], c.ones, writes=["ones"])
        sc.dma("sp", g[:], c.gout[l], writes=["g"])
        for ch in range(8):
            sc.dma("sp", r32(wt[:, ch, :]), r32(c.w_out[l][ch * 128:(ch + 1) * 128, :]), writes=[("wt", ch)])
        for ch in range(8):
            eng = "dve" if ch % 2 == 0 else "pool"
            sc.ts(eng, r32(wt[:, ch, :]), wt[:, ch, :], g[:, ch:ch + 1], None, ALU.mult, reads=["g", ("wt", ch)], writes=[("wt", ch)])
        pi = 0
        for b in range(4):
            y = yb[b % 2]; ykey = ("yb", b % 2)
            for ch in range(8):
                sc.dma("sp", r32(y[:, ch, :]), r32(c.yT[ch * 128:(ch + 1) * 128, b * 512:(b + 1) * 512]), writes=[ykey])
            sc.act(ysq[:], y[:, 0:6, :], AF.Square, reads=[ykey], writes=["ysq"])
            for tl in range(4):
                i = b * 4 + tl
                tsl = slice(tl * 128, (tl + 1) * 128)
                x = xt[i % 2]; xk = ("xt", i % 2); sb = st[i % 2]; sk = ("st", i % 2)
                sc.dma("sp", x[:], x_src[i * 128:(i + 1) * 128, :], writes=[xk])
                for ch in range(6):
                    col = 0 if ch < 4 else 1
                    sc.mm(pst[:, col:col + 1], ysq[:, ch, tsl], ones[:, 0:1], ch in (0, 4), ch in (3, 5), reads=["ysq", "ones"], writes=["pst"])
                sc.ts("dve", sb[:, 0:1], pst[:, 0:1], 1.0 / 512, EPS, ALU.mult, ALU.add, reads=["pst"], writes=[sk])
                sc.ts("dve", sb[:, 1:2], pst[:, 1:2], 1.0 / 256, EPS, ALU.mult, ALU.add, reads=["pst"], writes=[sk])
                sc.act(sb[:, 2:4], sb[:, 0:2], AF.Sqrt, reads=[sk], writes=[sk])
                sc.add("dve", lambda e, o=sb[:, 4:6], i_=sb[:, 2:4]: e.reciprocal(o, i_), reads=[sk], writes=[sk])
                for n in range(2):
                    nsl = slice(n * 512, (n + 1) * 512)
                    ps = []
                    for (c0, c1) in ((0, 4), (4, 6), (6, 8)):
                        p_ = pm[pi % 6]; pk = ("pm", pi % 6); pi += 1
                        for ch in range(c0, c1):
                            sc.mm(p_[:, :], r32(y[:, ch, tsl]), r32(wt[:, ch, nsl]), ch == c0, ch == c1 - 1,
                                  reads=[ykey, ("wt", ch)], writes=[pk])
                        ps.append((p_, pk))
                    sc.stt("dve", x[:, nsl], ps[0][0][:, :], sb[:, 4:5], x[:, nsl], ALU.mult, ALU.add, reads=[ps[0][1], sk, xk], writes=[xk])
                    sc.stt("dve", x[:, nsl], ps[1][0][:, :], sb[:, 5:6], x[:, nsl], ALU.mult, ALU.add, reads=[ps[1][1], sk, xk], writes=[xk])
                    sc.tt("dve", x[:, nsl], x[:, nsl], ps[2][0][:, :], ALU.add, reads=[ps[2][1], xk], writes=[xk])
                sc.dma("pool", c.out[i * 128:(i + 1) * 128, :], x[:], reads=[xk])
        sc.flush()


def phase_router(nc, sc, c, l):
    with ExitStack() as es:
        T = lambda name, shape, dt=F32: es.enter_context(nc.sbuf_tensor("%s_L%d" % (name, l), shape, dt))
        P = lambda name, shape, dt=F32: es.enter_context(nc.psum_tensor("%s_L%d" % (name, l), shape, dt))
        ident = T("f_id", [128, 128]); g2 = T("f_g2", [128, 8]); wr = T("f_wr", [128, 8, NE])
        xt = [T("f_xt%d" % i, [128, D]) for i in range(2)]
        junk = T("f_junk", [128, D])
        hb = [T("f_hb%d" % i, [128, D], BF16) for i in range(2)]
        hT = [T("f_hT%d" % i, [128, 8, 128]) for i in range(2)]
        st = [T("f_st%d" % i, [128, 8]) for i in range(2)]
        aff = T("f_aff", [128, NT, NE]); ex = T("f_ex", [128, NE])
        affT = T("f_affT", [NE, S]); work = T("f_work", [NE, S]); msk = T("f_msk", [NE, S]); pos = T("f_pos", [NE, S])
        mx8 = T("f_mx8", [NE, 8]); onesr = T("f_onesr", [NE, S])
        slot = T("f_slot", [128, NT, NE])
        ga = T("f_ga", [128, NT, NE, 4], BF16); gf = T("f_gf", [128, NT, NE]); tki = T("f_tki", [128, NT, 2], BF16)
        pT = [P("f_pT%d" % i, [128, 512]) for i in range(4)]
        pl = [P("f_pl%d" % i, [128, 512]) for i in range(2)]
        pa = [P("f_pa%d" % i, [128, 512]) for i in range(2)]
        sc.dma("sp", ident[:], c.ident, writes=["ident"])
        sc.dma("sp", g2[:], c.g2rep[l], writes=["g2"])
        sc.dma("sp", wr[:], c.w_router[l].rearrange("(c p) e -> p c e", p=128), writes=["wr"])
        sc.dma("sp", tki[:], c.tokhl, writes=["tki"])
        sc.tt("dve", wr[:], wr[:], g2[:].unsqueeze(2).broadcast_to([128, 8, NE]), ALU.mult, reads=["wr", "g2"], writes=["wr"])
        sc.add("pool", lambda e: e.memset(onesr[:], 1.0), writes=["onesr"])
        for i in range(NT):
            x = xt[i % 2]; xk = ("xt", i % 2); sb = st[i % 2]; sk = ("st", i % 2)
            sc.dma("sp", x[:], c.out[i * 128:(i + 1) * 128, :], writes=[xk])
            sc.act(junk[:], x[:], AF.Square, reads=[xk], writes=["junk", sk], accum_out=sb[:, 0:1])
            sc.ts("dve", sb[:, 1:2], sb[:, 0:1], 1.0 / D, EPS, ALU.mult, ALU.add, reads=[sk], writes=[sk])
            sc.act(sb[:, 2:3], sb[:, 1:2], AF.Sqrt, reads=[sk], writes=[sk])
            sc.add("dve", lambda e, o=sb[:, 3:4], i_=sb[:, 2:3]: e.reciprocal(o, i_), reads=[sk], writes=[sk])
            sc.ts("dve", x[:], x[:], sb[:, 3:4], None, ALU.mult, reads=[xk, sk], writes=[xk])
            sc.copy("pool", hb[i % 2][:], x[:], reads=[xk], writes=[("hb", i % 2)])
            sc.dma("pool", c.h2n[i * 128:(i + 1) * 128, :], hb[i % 2][:], reads=[("hb", i % 2)])
            for half in range(2):
                pt = pT[(i % 2) * 2 + half]; pk = ("pT", (i % 2) * 2 + half)
                for cc in range(4):
                    ch = half * 4 + cc
                    sc.tr(pt[:, cc * 128:(cc + 1) * 128], x[:, ch * 128:(ch + 1) * 128], ident[:], reads=[xk, "ident"], writes=[pk])
                sc.copy("act" if half == 0 else "dve", hT[i % 2][:, half * 4:(half + 1) * 4, :],
                        pt[:].rearrange("p (c t) -> p c t", c=4), reads=[pk], writes=[("hT", i % 2)])
            lg = pl[i % 2]; lk = ("pl", i % 2)
            for ch in range(8):
                sc.mm(lg[:, 0:NE], hT[i % 2][:, ch, :], wr[:, ch, :], ch == 0, ch == 7, reads=[("hT", i % 2), "wr"], writes=[lk])
            sc.add("dve", lambda e, o=sb[:, 4:5], i_=lg[:, 0:NE]: e.tensor_reduce(o, i_, AX.X, ALU.max), reads=[lk], writes=[sk])
            sc.ts("dve", sb[:, 5:6], sb[:, 4:5], -1.0, None, ALU.mult, reads=[sk], writes=[sk])
            sc.act(ex[:], lg[:, 0:NE], AF.Exp, reads=[lk, sk], writes=["ex", sk], bias=sb[:, 5:6], accum_out=sb[:, 6:7])
            sc.add("dve", lambda e, o=sb[:, 7:8], i_=sb[:, 6:7]: e.reciprocal(o, i_), reads=[sk], writes=[sk])
            sc.ts("dve", aff[:, i, :], ex[:], sb[:, 7:8], None, ALU.mult, reads=["ex", sk], writes=[("aff", i)])
            pa_ = pa[i % 2]; pak = ("pa", i % 2)
            sc.tr(pa_[0:NE, 0:128], aff[:, i, :], ident[:], reads=[("aff", i), "ident"], writes=[pak])
            sc.copy("act", affT[:, i * 128:(i + 1) * 128], pa_[0:NE, 0:128], reads=[pak], writes=["affT"])
        sc.copy("dve", work[:], affT[:], reads=["affT"], writes=["work"])
        for r in range(CAP // 8):
            sc.add("dve", lambda e: e.max(mx8[:], work[:]), reads=["work"], writes=["mx8"])
            if r < CAP // 8 - 1:
                sc.add("dve", lambda e: e.match_replace(work[:], mx8[:], work[:], -1e30), reads=["mx8", "work"], writes=["work"])
        sc.ts("dve", msk[:], affT[:], mx8[:, 7:8], None, ALU.is_ge, reads=["affT", "mx8"], writes=["msk"])
        sc.add("dve", lambda e: e.tensor_tensor_scan(pos[:], onesr[:], msk[:], 0.0, ALU.mult, ALU.add), reads=["onesr", "msk"], writes=["pos"])
        sc.tt("dve", pos[:], pos[:], msk[:], ALU.mult, reads=["pos", "msk"], writes=["pos"])
        sc.ts("dve", pos[:], pos[:], -1.0, None, ALU.add, reads=["pos"], writes=["pos"])
        for i in range(NT):
            pa_ = pa[i % 2]; pak = ("pa", i % 2)
            sc.tr(pa_[:, 0:NE], pos[:, i * 128:(i + 1) * 128], ident[0:NE, 0:NE], reads=["pos", "ident"], writes=[pak])
            sc.copy("act", slot[:, i, :], pa_[:, 0:NE], reads=[pak], writes=["slot"])
        sc.copy("dve", ga[:, :, :, 0], aff[:], reads=[("aff", i) for i in range(NT)], writes=["ga"])
        sc.copy("dve", gf[:], ga[:, :, :, 0], reads=["ga"], writes=["gf"])
        sc.tt("dve", gf[:], aff[:], gf[:], ALU.subtract, reads=["gf"] + [("aff", i) for i in range(NT)], writes=["gf"])
        sc.copy("dve", ga[:, :, :, 1], gf[:], reads=["gf"], writes=["ga"])
        sc.copy("pool", ga[:, :, :, 2], tki[:, :, 0:1].broadcast_to([128, NT, NE]), reads=["tki"], writes=["ga"])
        sc.copy("pool", ga[:, :, :, 3], tki[:, :, 1:2].broadcast_to([128, NT, NE]), reads=["tki"], writes=["ga"])
        sc.dma("pool", c.slot_d, slot[:].rearrange("p a b -> p (a b)"), reads=["slot"])
        sc.dma("pool", c.ga_d, ga[:].rearrange("p a b c -> p (a b c)"), reads=["ga"])
        sc.flush()


def phase_experts(nc, sc, c, l):
    with ExitStack() as es:
        T = lambda name, shape, dt=F32: es.enter_context(nc.sbuf_tensor("%s_L%d" % (name, l), shape, dt))
        P = lambda name, shape, dt=F32: es.enter_context(nc.psum_tensor("%s_L%d" % (name, l), shape, dt))
        h2 = T("g_h2", [128, NT, D], BF16)
        acc = T("g_acc", [128, NT, D])
        Wg = [T("g_Wg%d" % i, [128, 8, 256]) for i in range(2)]
        Wu = [T("g_Wu%d" % i, [128, 8, 256]) for i in range(2)]
        Wd = [T("g_Wd%d" % i, [128, 2, D]) for i in range(2)]
        Sel = T("g_Sel", [128, NT, CAP], BF16)
        SelT = T("g_SelT", [128, 2, S])
        xg = T("g_xg", [128, 8, CAP])
        H = [T("g_H%d" % i, [128, 2, CAP]) for i in range(2)]
        sil = [T("g_sil%d" % i, [128, CAP]) for i in range(2)]
        ye = T("g_ye", [128, 2, D])
        slot = T("g_slot", [128, NT * NE]); ga = T("g_ga", [128, NT * NE * 4], BF16)
        io256 = T("g_io256", [128, CAP], F16); ioT = T("g_ioT", [128, S], F16)
        g2 = T("g_g2", [128, 8]); gt2 = T("g_gt2", [128, 2, 2]); tks = T("g_tks", [128, 8])
        yacc = [P("g_ya%d" % i, [128, 512]) for i in range(4)]
        au = [P("g_au%d" % i, [128, 512]) for i in range(2)]
        gs = [P("g_gs%d" % i, [128, 512]) for i in range(2)]
        sc.dma("sp", slot[:], c.slot_d, writes=["slot"])
        sc.dma("sp", ga[:], c.ga_d, writes=["ga"])
        sc.dma("sp", io256[:], c.iota256, writes=["io256"])
        sc.dma("sp", ioT[:], c.iotaT, writes=["ioT"])
        sc.dma("sp", g2[:], c.g2rep[l], writes=["g2"])
        for i in range(NT):
            sc.dma("sp", h2[:, i, :], c.h2n[i * 128:(i + 1) * 128, :], writes=[("h2", i)])
            sc.dma("sp", acc[:, i, :], c.out[i * 128:(i + 1) * 128, :], writes=[("acc", i)])
        ga4 = ga[:].rearrange("p (i e k) -> p i e k", i=NT, e=NE)
        wq = [0]
        def load_w(e, wb):
            b = wq[0] % 2; wq[0] += 1
            hs_ = slice(wb * 256, (wb + 1) * 256)
            sc.dma("sp", r32(Wg[b][:]), r32(c.w_gate[l, e][:, hs_].rearrange("(c p) h -> p c h", p=128)), writes=[("Wg", b)])
            sc.dma("sp", r32(Wu[b][:]), r32(c.w_up[l, e][:, hs_].rearrange("(c p) h -> p c h", p=128)), writes=[("Wu", b)])
            sc.dma("sp", r32(Wd[b][:]), r32(c.w_down[l, e][wb * 256:(wb + 1) * 256, :].rearrange("(c p) f -> p c f", p=128)), writes=[("Wd", b)])
            return b
        def build_sel(e):
            for i in range(NT):
                eng = "dve" if i % 2 == 0 else "pool"
                sc.ts(eng, Sel[:, i, :], io256[:], slot[:, i * NE + e:i * NE + e + 1], None, ALU.is_equal,
                      reads=["io256", "slot"], writes=[("Sel", i)])
        build_sel(0)
        seq = [(e, wb) for e in range(NE) for wb in range(8)]
        nxt = load_w(0, 0)
        gi = 0; ai = 0; hi = 0
        for e in range(NE):
            tk = gs[gi % 2]; tkk = ("gs", gi % 2); gi += 1
            for ct in range(2):
                for i in range(NT):
                    sc.mm(tk[:, ct * 4:(ct + 1) * 4], Sel[:, i, ct * 128:(ct + 1) * 128], ga4[:, i, e, :], i == 0, i == NT - 1,
                          reads=[("Sel", i), "ga"], writes=[tkk])
            sc.copy("act", tks[:], tk[:, 0:8], reads=[tkk], writes=["tks"])
            t4 = tks[:].rearrange("p (ct k two) -> p ct k two", ct=2, k=2)
            sc.tt("dve", gt2[:], t4[:, :, :, 0], t4[:, :, :, 1], ALU.add, reads=["tks"], writes=["gt2"])
            for fp in range(4):
                gp = gs[gi % 2]; gk = ("gs", gi % 2); gi += 1
                for k in range(2):
                    fc = fp * 2 + k
                    for i in range(NT):
                        sc.mm(gp[:, k * 256:(k + 1) * 256], h2[:, i, fc * 128:(fc + 1) * 128], Sel[:, i, :], i == 0, i == NT - 1,
                              reads=[("h2", i), ("Sel", i)], writes=[gk])
                    if k == 0:
                        sc.act(r32(xg[:, fc, :]), gp[:, 0:256], AF.Copy, reads=[gk, "g2"], writes=[("xg", fc)], scale=g2[:, fc:fc + 1])
                    else:
                        sc.ts("dve", r32(xg[:, fc, :]), gp[:, 256:512], g2[:, fc:fc + 1], None, ALU.mult, reads=[gk, "g2"], writes=[("xg", fc)])
            if e + 1 < NE:
                build_sel(e + 1)
            for wb in range(8):
                b = nxt
                si = e * 8 + wb + 1
                if si < len(seq):
                    nxt = load_w(*seq[si])
                hb_ = H[hi % 2]; hk = ("H", hi % 2); hi += 1
                for jj in range(2):
                    p_ = au[ai % 2]; pk = ("au", ai % 2)
                    sl_ = sil[ai % 2]; slk = ("sil", ai % 2); ai += 1
                    for ch in range(8):
                        sc.mm(p_[:, 0:256], r32(Wg[b][:, ch, jj * 128:(jj + 1) * 128]), r32(xg[:, ch, :]), ch == 0, ch == 7,
                              reads=[("Wg", b), ("xg", ch)], writes=[pk])
                    for ch in range(8):
                        sc.mm(p_[:, 256:512], r32(Wu[b][:, ch, jj * 128:(jj + 1) * 128]), r32(xg[:, ch, :]), ch == 0, ch == 7,
                              reads=[("Wu", b), ("xg", ch)], writes=[pk])
                    sc.act(sl_[:], p_[:, 0:256], AF.Silu, reads=[pk], writes=[slk])
                    sc.tt("dve", r32(hb_[:, jj, :]), sl_[:], p_[:, 256:512], ALU.mult, reads=[slk, pk], writes=[hk])
                for ct in range(2):
                    for fb in range(2):
                        for jj in range(2):
                            sc.mm(yacc[ct * 2 + fb][:, :], r32(hb_[:, jj, ct * 128:(ct + 1) * 128]), r32(Wd[b][:, jj, fb * 512:(fb + 1) * 512]),
                                  wb == 0 and jj == 0, wb == 7 and jj == 1, reads=[hk, ("Wd", b)], writes=[("ya", ct * 2 + fb)])
            for ct in range(2):
                for fb in range(2):
                    if fb == 0:
                        sc.act(r32(ye[:, ct, fb * 512:(fb + 1) * 512]), yacc[ct * 2 + fb][:, :], AF.Copy, reads=[("ya", ct * 2 + fb), "gt2"],
                               writes=[("ye", ct)], scale=gt2[:, ct, 0:1])
                    else:
                        sc.ts("dve", r32(ye[:, ct, fb * 512:(fb + 1) * 512]), yacc[ct * 2 + fb][:, :], gt2[:, ct, 0:1], None, ALU.mult,
                              reads=[("ya", ct * 2 + fb), "gt2"], writes=[("ye", ct)])
                sc.ts("pool" if ct == 0 else "dve", r32(SelT[:, ct, :]), ioT[:], gt2[:, ct, 1:2], None, ALU.is_equal,
                      reads=["ioT", "gt2"], writes=[("SelT", ct)])
            for i in range(NT):
                for fb in range(2):
                    sp_ = gs[gi % 2]; spk = ("gs", gi % 2); gi += 1
                    for ct in range(2):
                        sc.mm(sp_[:, :], r32(SelT[:, ct, i * 128:(i + 1) * 128]), r32(ye[:, ct, fb * 512:(fb + 1) * 512]), ct == 0, ct == 1,
                              reads=[("SelT", ct), ("ye", ct)], writes=[spk])
                    sc.tt("dve", acc[:, i, fb * 512:(fb + 1) * 512], acc[:, i, fb * 512:(fb + 1) * 512], sp_[:, :], ALU.add,
                          reads=[spk, ("acc", i)], writes=[("acc", i)])
        for i in range(NT):
            sc.dma("pool", c.out[i * 128:(i + 1) * 128, :], acc[:, i, :], reads=[("acc", i)])
        sc.flush()


def build(n_layers=DEPTH, debug=None):
    nc = bass.Bass("TRN2", target_bir_lowering=False)
    nc.dge_precook = False
    c = Ctx()
    def inp(name, shape):
        return nc.dram_tensor(name, list(shape), F32, kind="ExternalInput").ap()
    c.x = inp("x", [S, D])
    c.norm1_g = inp("norm1_g", [DEPTH, D])
    c.w_in = inp("w_in", [DEPTH, D, D_IN])
    c.ident = inp("ident", [128, 128])
    c.ones = inp("ones", [128, 128])
    c.cos_rep = inp("cos_rep", [S, 320])
    c.sin_rep = inp("sin_rep", [S, 320])
    c.ggrep = inp("ggrep", [DEPTH, 128, 640])
    c.lru_small = inp("lru_small", [DEPTH, 128, 2, 11])
    c.lru_w = inp("lru_w", [DEPTH, 128, 8, 128])
    c.w_out = inp("w_out", [DEPTH, D, D])
    c.gout = inp("gout", [DEPTH, 128, 8])
    c.g2rep = inp("g2rep", [DEPTH, 128, 8])
    c.w_router = inp("w_router", [DEPTH, D, NE])
    c.w_gate = inp("w_expert_gate", [DEPTH, NE, D, 2 * D])
    c.w_up = inp("w_expert_up", [DEPTH, NE, D, 2 * D])
    c.w_down = inp("w_expert_down", [DEPTH, NE, 2 * D, D])
    c.tokhl = nc.dram_tensor("tokhl", [128, NT, 2], BF16, kind="ExternalInput").ap()
    c.iota256 = nc.dram_tensor("iota256", [128, CAP], F16, kind="ExternalInput").ap()
    c.iotaT = nc.dram_tensor("iotaT", [128, S], F16, kind="ExternalInput").ap()
    c.h2n = nc.dram_tensor("h2n", [S, D], BF16, kind="Internal").ap()
    c.slot_d = nc.dram_tensor("slot_d", [128, NT * NE], F32, kind="ExternalOutput" if debug == "R" else "Internal").ap()
    c.ga_d = nc.dram_tensor("ga_d", [128, NT * NE * 4], BF16, kind="Internal").ap()
    c.triu = inp("triu", [128, 128])
    c.tril = inp("tril", [128, 128])
    c.fbrep = inp("fbrep", [DEPTH, 128, 8])
    c.ngrep = inp("ngrep", [DEPTH, 128, 256])
    c.yT = nc.dram_tensor("yT", [1024, S], F32, kind="ExternalOutput" if debug in ("B", "C", "D") else "Internal").ap()
    c.out = nc.dram_tensor("out", [S, D], F32, kind="ExternalOutput").ap()
    c.p_tm = nc.dram_tensor("p_tm", [S, D_IN], F32, kind="ExternalOutput" if debug == "A" else "Internal").ap()
    c.p_fm = nc.dram_tensor("p_fm", [1024, S], F32, kind="ExternalOutput" if debug == "A" else "Internal").ap()
    with ExitStack() as es:
        sc = Sched(nc, es)
        for l in range(n_layers):
            phase_inproj(nc, sc, c, l, c.x if l == 0 else c.out)
            if debug == "A":
                continue
            if debug != "C" and debug != "D":
                phase_attn(nc, sc, c, l)
            if debug == "B":
                continue
            if debug != "D":
                phase_lru(nc, sc, c, l)
            if debug == "C":
                continue
            phase_mlstm(nc, sc, c, l)
            if debug == "D":
                continue
            phase_outproj(nc, sc, c, l, c.x if l == 0 else c.out)
            if debug == "E":
                continue
            phase_router(nc, sc, c, l)
            if debug == "R":
                continue
            phase_experts(nc, sc, c, l)
    return nc


def consts():
    t = np.arange(S)
    row = (t // 64).astype(np.float32)
    col = (t % 64).astype(np.float32)
    inv = (1.0 / (np.float32(10000.0) ** (np.arange(16, dtype=np.float32) / np.float32(16)))).astype(np.float32)
    ang = np.concatenate([row[:, None] * inv, col[:, None] * inv], axis=-1).astype(np.float32)
    cos = np.cos(ang).astype(np.float32)
    sin = np.sin(ang).astype(np.float32)
    import ml_dtypes
    tok = (np.arange(NT)[None, :] * 128 + np.arange(128)[:, None]).astype(np.float32)
    thi = tok.astype(ml_dtypes.bfloat16)
    tlo = (tok - thi.astype(np.float32)).astype(ml_dtypes.bfloat16)
    return {"tokhl": np.ascontiguousarray(np.stack([thi, tlo], axis=-1)),
            "iota256": np.ascontiguousarray(np.broadcast_to(np.arange(CAP, dtype=np.float16)[None, :], (128, CAP))),
            "iotaT": np.ascontiguousarray(np.broadcast_to(np.arange(S, dtype=np.float16)[None, :], (128, S))),
            "ident": np.eye(128, dtype=np.float32), "ones": np.ones((128, 128), np.float32),
            "triu": np.triu(np.ones((128, 128), np.float32)), "tril": np.tril(np.ones((128, 128), np.float32)),
            "cos_rep": np.ascontiguousarray(np.tile(cos, (1, 10))), "sin_rep": np.ascontiguousarray(np.tile(sin, (1, 10)))}


def layout_small(inputs):
    o = {}
    qg = inputs["q_norm_g"]; kg = inputs["k_norm_g"]
    gg = np.concatenate([np.tile(qg, (1, 8)), np.tile(kg, (1, 2))], axis=1)
    o["ggrep"] = np.ascontiguousarray(np.broadcast_to(gg[:, None, :], (DEPTH, 128, 640))).astype(np.float32)
    go = np.concatenate([inputs["att_out_g"], inputs["lru_out_g"], np.ones((DEPTH, 256), np.float32)], axis=1)
    o["gout"] = np.ascontiguousarray(go.reshape(DEPTH, 8, 128).transpose(0, 2, 1)).astype(np.float32)
    o["g2rep"] = np.ascontiguousarray(inputs["norm2_g"].reshape(DEPTH, 8, 128).transpose(0, 2, 1)).astype(np.float32)
    o["fbrep"] = np.ascontiguousarray(np.broadcast_to(inputs["mlstm_f_bias"].reshape(DEPTH, 1, 8), (DEPTH, 128, 8))).astype(np.float32)
    o["ngrep"] = np.ascontiguousarray(np.broadcast_to(inputs["mlstm_norm_g"].reshape(DEPTH, 1, 256), (DEPTH, 128, 256))).astype(np.float32)
    cols = [inputs["conv_w"][:, j, :] for j in range(4)] + [inputs["conv_b"]]
    cols += [inputs["lru_ba"][:, d, :] for d in range(2)] + [inputs["lru_bx"][:, d, :] for d in range(2)]
    cols += [inputs["lru_lambda"][:, d, :] for d in range(2)]
    sm = np.stack(cols, axis=-1)
    o["lru_small"] = np.ascontiguousarray(sm.reshape(DEPTH, 2, 128, 11).transpose(0, 2, 1, 3)).astype(np.float32)
    lw = np.zeros((DEPTH, 128, 2, 2, 2, 128), np.float32)
    for kind, nm in enumerate(["lru_wa", "lru_wx"]):
        wsrc = inputs[nm]
        for d in range(2):
            for cc in range(2):
                for b in range(2):
                    lw[:, b * 64:(b + 1) * 64, d, kind, cc, b * 64:(b + 1) * 64] = wsrc[:, d, 2 * cc + b]
    o["lru_w"] = lw.reshape(DEPTH, 128, 8, 128)
    return o


SMALL = ["norm1_g", "q_norm_g", "k_norm_g", "conv_w", "conv_b", "lru_wa", "lru_ba", "lru_wx", "lru_bx", "lru_lambda",
         "mlstm_f_bias", "mlstm_norm_g", "att_out_g", "lru_out_g", "norm2_g"]
BIG = ["w_in", "w_out", "w_router", "w_expert_gate", "w_expert_up", "w_expert_down", "norm1_g"]


def kernel(**inputs):
    inputs = {k: np.asarray(v) for k, v in inputs.items()}
    nc = build()
    shared = {k: np.ascontiguousarray(inputs[k], dtype=np.float32) for k in BIG}
    shared.update(consts())
    shared.update(layout_small(inputs))
    x = np.ascontiguousarray(inputs["x"], dtype=np.float32)
    in_maps = []
    for b in range(8):
        m = dict(shared)
        m["x"] = x[b]
        in_maps.append(m)
    res = run_bass_kernel_spmd(nc, in_maps, core_ids=list(range(8)))
    return np.stack([r["out"] for r in res.results], axis=0).astype(np.float32)
```

```python
import numpy as np
from contextlib import ExitStack
import concourse.bass as bass
import concourse.mybir as mybir
from concourse.bass_utils import run_bass_kernel_spmd

F32 = mybir.dt.float32
F32R = mybir.dt.float32r
BF16 = mybir.dt.bfloat16
F16 = mybir.dt.float16
AF = mybir.ActivationFunctionType
ALU = mybir.AluOpType
AX = mybir.AxisListType

S = 2048
D = 1024
DEPTH = 4
D_IN = 2320
NT = S // 128
EPS = 1e-6

ENG = ["pe", "act", "dve", "pool", "sp"]
BLK = {"pe": "tensor", "act": "scalar", "dve": "vector", "pool": "gpsimd", "sp": "sync"}


class Sched:
    def __init__(self, nc, es, n_dma=24):
        self.nc = nc
        self.sem = {e: es.enter_context(nc.semaphore("s_" + e)) for e in ENG}
        self.dsem = [es.enter_context(nc.semaphore("s_dma%d" % i)) for i in range(n_dma)]
        self.cnt = {e: 0 for e in ENG}
        self.duse = [0] * n_dma
        self.drr = 0
        self.ops = {e: [] for e in ENG}
        self.waited = {e: {} for e in ENG}
        self.last_w = {}
        self.readers = {}
        self.nops = 0

    def semobj(self, s):
        return self.sem[s] if isinstance(s, str) else self.dsem[s[1]]

    def add(self, eng, fn, reads=(), writes=(), dma=False):
        deps = {}
        def need(tok):
            s, v = tok
            if deps.get(s, 0) < v:
                deps[s] = v
        for r in reads:
            if r in self.last_w:
                need(self.last_w[r])
        for w in writes:
            if w in self.last_w:
                need(self.last_w[w])
            for t in self.readers.get(w, ()):
                need(t)
        if dma:
            k = self.drr
            self.drr = (self.drr + 1) % len(self.dsem)
            self.duse[k] += 1
            n = self.duse[k]
            if n > 1:
                need((("dma", k), 16 * (n - 1)))
            token = (("dma", k), 16 * n)
        else:
            self.cnt[eng] += 1
            token = (eng, self.cnt[eng])
        waits = []
        wd = self.waited[eng]
        for s, v in deps.items():
            if s == "pe" and eng == "pe" and not dma:
                continue
            if wd.get(s, 0) < v:
                wd[s] = v
                waits.append((s, v))
        self.ops[eng].append((waits, fn, token, dma))
        self.nops += 1
        for r in reads:
            self.readers.setdefault(r, []).append(token)
        for w in writes:
            self.last_w[w] = token
            self.readers[w] = []
        return token

    def flush(self):
        waits = []
        for k, n in enumerate(self.duse):
            if n and self.waited["sp"].get(("dma", k), 0) < 16 * n:
                waits.append((("dma", k), 16 * n))
        self.ops["sp"].append((waits, None, None, False))
        ops = self.ops
        self.ops = {e: [] for e in ENG}
        sched = self
        with self.nc.Block() as block:
            for e in ENG:
                lst = ops[e]

                def body(eng, lst=lst):
                    for waits, fn, token, isdma in lst:
                        for (s, v) in waits:
                            eng.wait_ge(sched.semobj(s), v)
                        if fn is None:
                            continue
                        inst = fn(eng)
                        inst.then_inc(sched.semobj(token[0]), 16 if isdma else 1)

                getattr(block, BLK[e])(body)
        for e in ENG:
            for e2 in ENG:
                self.waited[e][e2] = self.cnt[e2]
            for k, n in enumerate(self.duse):
                self.waited[e][("dma", k)] = 16 * n
        self.last_w = {}
        self.readers = {}

    def dma(self, q, out, in_, reads=(), writes=(), slow=False):
        if slow:
            return self.add(q, lambda e: e.dma_start(out=out, in_=in_, allow_slow_non_contiguous=True), reads, writes, dma=True)
        return self.add(q, lambda e: e.dma_start(out=out, in_=in_), reads, writes, dma=True)

    def mm(self, out, lhsT, rhs, start, stop, reads=(), writes=()):
        return self.add("pe", lambda e: e.matmul(out, lhsT, rhs, start=start, stop=stop), reads, writes)

    def tr(self, out, in_, ident, reads=(), writes=()):
        return self.add("pe", lambda e: e.transpose(out, in_, ident), reads, writes)

    def act(self, out, in_, func, reads=(), writes=(), eng="act", **kw):
        return self.add(eng, lambda e: e.activation(out, in_, func, **kw), reads, writes)

    def ts(self, eng, out, in0, s1, s2, op0, op1=None, reads=(), writes=(), **kw):
        if op1 is None:
            return self.add(eng, lambda e: e.tensor_scalar(out, in0, s1, None, op0, **kw), reads, writes)
        return self.add(eng, lambda e: e.tensor_scalar(out, in0, s1, s2, op0, op1, **kw), reads, writes)

    def tt(self, eng, out, in0, in1, op, reads=(), writes=()):
        return self.add(eng, lambda e: e.tensor_tensor(out, in0, in1, op), reads, writes)

    def stt(self, eng, out, in0, scalar, in1, op0, op1, reads=(), writes=()):
        return self.add(eng, lambda e: e.scalar_tensor_tensor(out, in0, scalar, in1, op0, op1), reads, writes)

    def copy(self, eng, out, in_, reads=(), writes=()):
        if eng == "act":
            return self.add(eng, lambda e: e.copy(out, in_), reads, writes)
        return self.add(eng, lambda e: e.tensor_copy(out, in_), reads, writes)


def r32(ap):
    return ap.bitcast(F32R)


class Ctx:
    pass


def phase_inproj(nc, sc, c, l, x_src):
    with ExitStack() as es:
        T = lambda name, shape, dt=F32: es.enter_context(nc.sbuf_tensor("%s_L%d" % (name, l), shape, dt))
        P = lambda name, shape, dt=F32: es.enter_context(nc.psum_tensor("%s_L%d" % (name, l), shape, dt))
        wt = T("a_wt", [128, 8, D_IN], BF16)
        g = T("a_g", [128, 8])
        ident = T("a_id", [128, 128])
        xt = [T("a_xt%d" % i, [128, D]) for i in range(2)]
        junk = T("a_junk", [128, D])
        st = [T("a_st%d" % i, [128, 4]) for i in range(2)]
        hT = [T("a_hT%d" % i, [128, 8, 512], BF16) for i in range(2)]
        og = [T("a_og%d" % i, [128, D_IN]) for i in range(2)]
        of = [T("a_of%d" % i, [128, 512]) for i in range(2)]
        pT = [P("a_pT%d" % i, [128, 512]) for i in range(4)]
        pm = [P("a_pm%d" % i, [128, 512]) for i in range(4)]

        sc.dma("sp", ident[:], c.ident, writes=["ident"])
        sc.dma("sp", g[:], c.norm1_g[l].rearrange("(c p) -> p c", p=128), writes=["g"], slow=True)
        for ch in range(8):
            sc.dma("pool", wt[:, ch, :], c.w_in[l][ch * 128:(ch + 1) * 128, :], writes=[("wt", ch)])

        pmi = 0
        for b in range(4):
            hb = hT[b % 2]
            hkey = ("hT", b % 2)
            for tl in range(4):
                i = b * 4 + tl
                xb = xt[i % 2]
                sb = st[i % 2]
                xk = ("xt", i % 2)
                sk = ("st", i % 2)
                sc.dma("sp", xb[:], x_src[i * 128:(i + 1) * 128, :], writes=[xk])
                sc.act(junk[:], xb[:], AF.Square, reads=[xk], writes=["junk", sk], accum_out=sb[:, 0:1])
                sc.ts("dve", sb[:, 1:2], sb[:, 0:1], 1.0 / D, EPS, ALU.mult, ALU.add, reads=[sk], writes=[sk])
                sc.act(sb[:, 2:3], sb[:, 1:2], AF.Sqrt, reads=[sk], writes=[sk])
                sc.add("dve", lambda e, o=sb[:, 3:4], i_=sb[:, 2:3]: e.reciprocal(o, i_), reads=[sk], writes=[sk])
                sc.ts("dve", xb[:], xb[:], sb[:, 3:4], None, ALU.mult, reads=[xk, sk], writes=[xk])
                for half in range(2):
                    pt = pT[(i % 2) * 2 + half]
                    pk = ("pT", (i % 2) * 2 + half)
                    for cc in range(4):
                        ch = half * 4 + cc
                        sc.tr(pt[:, cc * 128:(cc + 1) * 128], xb[:, ch * 128:(ch + 1) * 128], ident[:],
                              reads=[xk, "ident"], writes=[pk])
                    sc.tt("dve", hb[:, half * 4:(half + 1) * 4, tl * 128:(tl + 1) * 128],
                          pt[:].rearrange("p (c t) -> p c t", c=4),
                          g[:, half * 4:(half + 1) * 4].unsqueeze(2).broadcast_to([128, 4, 128]), ALU.mult,
                          reads=[pk, "g"], writes=[(hkey, tl)])
            for tl in range(4):
                i = b * 4 + tl
                ob = og[i % 2]
                ok = ("og", i % 2)
                for n in range(5):
                    n0 = n * 512
                    nw = min(512, D_IN - n0)
                    pmm = pm[pmi % 4]
                    pk = ("pm", pmi % 4)
                    pmi += 1
                    for ch in range(8):
                        sc.mm(pmm[:, 0:nw], hb[:, ch, tl * 128:(tl + 1) * 128], wt[:, ch, n0:n0 + nw],
                              ch == 0, ch == 7, reads=[(hkey, tl), ("wt", ch)], writes=[pk])
                    eng = "act" if n % 2 == 0 else "dve"
                    sc.copy(eng, ob[:, n0:n0 + nw], pmm[:, 0:nw], reads=[pk], writes=[ok])
                sc.dma("pool", c.p_tm[i * 128:(i + 1) * 128, :], ob[:], reads=[ok])
            for j in range(8):
                c0 = 768 + j * 128
                pmm = pm[pmi % 4]
                pk = ("pm", pmi % 4)
                pmi += 1
                ob = of[j % 2]
                ok = ("of", j % 2)
                for ch in range(8):
                    sc.mm(pmm[:, :], wt[:, ch, c0:c0 + 128], hb[:, ch, :], ch == 0, ch == 7,
                          reads=[(hkey, 0), (hkey, 1), (hkey, 2), (hkey, 3), ("wt", ch)], writes=[pk])
                eng = "act" if j % 2 == 0 else "dve"
                sc.copy(eng, ob[:], pmm[:], reads=[pk], writes=[ok])
                sc.dma("pool", c.p_fm[j * 128:(j + 1) * 128, b * 512:(b + 1) * 512], ob[:], reads=[ok])
        sc.flush()


def phase_attn(nc, sc, c, l):
    with ExitStack() as es:
        T = lambda name, shape, dt=F32: es.enter_context(nc.sbuf_tensor("%s_L%d" % (name, l), shape, dt))
        P = lambda name, shape, dt=F32: es.enter_context(nc.psum_tensor("%s_L%d" % (name, l), shape, dt))
        ident = T("b_id", [128, 128])
        ones = T("b_ones", [128, 128])
        gg = T("b_gg", [128, 640])
        qT = T("b_qT", [128, 4, S], BF16)
        kT = T("b_kT", [128, S], BF16)
        vx = T("b_vx", [128, NT, 2, 65], BF16)
        qkv = [T("b_qkv%d" % i, [128, 768]) for i in range(2)]
        cs = [T("b_cs%d" % i, [128, 2, 320]) for i in range(2)]
        sq = T("b_sq", [128, 640])
        st = T("b_st", [128, 4, 10])
        qn = T("b_qn", [128, 640])
        tmp = T("b_tmp", [128, 4, 320])
        qr = T("b_qr", [128, 640])
        PT = [T("b_PT%d" % i, [128, 512], BF16) for i in range(3)]
        rd = [T("b_rd%d" % i, [128, 512]) for i in range(2)]
        bcs = [T("b_bcs%d" % i, [64, 512]) for i in range(2)]
        yo = [T("b_yo%d" % i, [64, 512]) for i in range(2)]
        ptA = [P("b_ptA%d" % i, [128, 512]) for i in range(1)]
        ptB = [P("b_ptB%d" % i, [128, 512]) for i in range(1)]
        sps = [P("b_sps%d" % i, [128, 512]) for i in range(3)]
        ops_ = [P("b_ops%d" % i, [128, 512]) for i in range(2)]
        bcp = P("b_bcp", [128, 512])

        sc.dma("sp", ident[:], c.ident, writes=["ident"])
        sc.dma("sp", ones[:], c.ones, writes=["ones"])
        sc.dma("sp", gg[:], c.ggrep[l], writes=["gg"])
        for i in range(NT):
            qb_ = qkv[i % 2]; qk = ("qkv", i % 2)
            cb = cs[i % 2]; ck = ("cs", i % 2)
            for half in range(2):
                sc.dma("sp", qb_[:, 0:512].rearrange("p (pr half d) -> p pr half d", pr=4, half=2)[:, :, half, :],
                       c.p_tm[i * 128:(i + 1) * 128, half * 256:(half + 1) * 256].rearrange("t (pr d) -> t pr d", pr=4),
                       writes=[qk])
            sc.dma("sp", qb_[:, 512:768], c.p_tm[i * 128:(i + 1) * 128, 512:768], writes=[qk])
            sc.dma("sp", cb[:, 0, :], c.cos_rep[i * 128:(i + 1) * 128, :], writes=[ck])
            sc.dma("sp", cb[:, 1, :], c.sin_rep[i * 128:(i + 1) * 128, :], writes=[ck])
            sc.tt("pool", sq[:], qb_[:, 0:640], qb_[:, 0:640], ALU.mult, reads=[qk], writes=["sq"])
            sc.add("dve", lambda e, o=st[:, 0, :], i_=sq[:].rearrange("p (h d) -> p h d", d=64):
                   e.tensor_reduce(o, i_, AX.X, ALU.add), reads=["sq"], writes=["st"])
            sc.ts("dve", st[:, 1, :], st[:, 0, :], 1.0 / 64, EPS, ALU.mult, ALU.add, reads=["st"], writes=["st"])
            sc.act(st[:, 2, :], st[:, 1, :], AF.Sqrt, reads=["st"], writes=["st"])
            sc.add("dve", lambda e, o=st[:, 3, :], i_=st[:, 2, :]: e.reciprocal(o, i_), reads=["st"], writes=["st"])
            sc.tt("dve", qn[:].rearrange("p (h d) -> p h d", d=64), qb_[:, 0:640].rearrange("p (h d) -> p h d", d=64),
                  st[:, 3, :].unsqueeze(2).broadcast_to([128, 10, 64]), ALU.mult, reads=[qk, "st"], writes=["qn"])
            sc.tt("pool", qn[:], qn[:], gg[:], ALU.mult, reads=["qn", "gg"], writes=["qn"])
            q3 = qn[:].rearrange("p (h d) -> p h d", d=64)
            x1 = q3[:, :, 0:32]; x2 = q3[:, :, 32:64]
            co = cb[:, 0, :].rearrange("p (h d) -> p h d", d=32); si = cb[:, 1, :].rearrange("p (h d) -> p h d", d=32)
            t = [tmp[:, k, :].rearrange("p (h d) -> p h d", d=32) for k in range(4)]
            r3 = qr[:].rearrange("p (h d) -> p h d", d=64)
            sc.tt("dve", t[0], x1, co, ALU.mult, reads=["qn", ck], writes=[("tmp", 0)])
            sc.tt("pool", t[1], x2, si, ALU.mult, reads=["qn", ck], writes=[("tmp", 1)])
            sc.tt("dve", t[2], x2, co, ALU.mult, reads=["qn", ck], writes=[("tmp", 2)])
            sc.tt("pool", t[3], x1, si, ALU.mult, reads=["qn", ck], writes=[("tmp", 3)])
            sc.tt("dve", r3[:, :, 0:32], t[0], t[1], ALU.subtract, reads=[("tmp", 0), ("tmp", 1)], writes=["qr"])
            sc.tt("pool", r3[:, :, 32:64], t[2], t[3], ALU.add, reads=[("tmp", 2), ("tmp", 3)], writes=["qr"])
            for pr in range(4):
                sc.tr(ptA[0][:, pr * 128:(pr + 1) * 128], qr[:, pr * 128:(pr + 1) * 128], ident[:], reads=["qr", "ident"], writes=["ptA"])
            sc.tr(ptB[0][:, 0:128], qr[:, 512:640], ident[:], reads=["qr", "ident"], writes=["ptB"])
            sc.copy("act", qT[:, :, i * 128:(i + 1) * 128], ptA[0][:].rearrange("p (c t) -> p c t", c=4),
                    reads=["ptA"], writes=["qT"])
            sc.copy("dve", kT[:, i * 128:(i + 1) * 128], ptB[0][:, 0:128], reads=["ptB"], writes=["kT"])
            sc.copy("act", vx[:, i, :, 0:64], qb_[:, 640:768].rearrange("p (g d) -> p g d", d=64),
                    reads=[qk], writes=["vx"])
            sc.ts("pool", vx[:, i, :, 64:65], qb_[:, 0:2].unsqueeze(2), 0.0, 1.0, ALU.mult, ALU.add,
                  reads=[qk], writes=["vx"])

        steps = [(h, qb, kt) for h in range(8) for qb in range(4) for kt in range(NT)]
        def fin(gi, h, qb):
            O = ops_[gi % 2]; ok = ("O", gi % 2)
            r = rd[gi % 2]; rk = ("rd", gi % 2)
            sc.act(r[64:65, :], O[64:65, :], AF.Ln, reads=[ok], writes=[rk])
            sc.act(r[64:65, :], r[64:65, :], AF.Exp, reads=[rk], writes=[rk], scale=-1.0)
            sc.mm(bcp[0:64, :], ones[64:65, 0:64], r[64:65, :], True, True, reads=[rk, "ones"], writes=["bcp"])
            sc.copy("act", bcs[gi % 2][:], bcp[0:64, :], reads=["bcp"], writes=[("bcs", gi % 2)])
            sc.tt("dve", yo[gi % 2][:], O[0:64, :], bcs[gi % 2][:], ALU.mult, reads=[ok, ("bcs", gi % 2)],
                  writes=[("yo", gi % 2)])
            sc.dma("pool", c.yT[h * 64:(h + 1) * 64, qb * 512:(qb + 1) * 512], yo[gi % 2][:], reads=[("yo", gi % 2)])
        for s_ in range(len(steps) + 1):
            if s_ < len(steps):
                h, qb, kt = steps[s_]
                pr = h % 4; half = h // 4
                p0 = half * 64
                sp_ = sps[s_ % 3]; sk = ("sps", s_ % 3)
                sc.mm(sp_[:, :], kT[p0:p0 + 64, kt * 128:(kt + 1) * 128],
                      qT[p0:p0 + 64, pr, qb * 512:(qb + 1) * 512], True, True,
                      reads=["kT", "qT"], writes=[sk])
                sc.act(PT[s_ % 3][:], sp_[:], AF.Exp, reads=[sk], writes=[("PT", s_ % 3)], scale=0.125)
            if s_ >= 1:
                h, qb, kt = steps[s_ - 1]
                gi = (s_ - 1) // NT
                half = h // 4
                O = ops_[gi % 2]; ok = ("O", gi % 2)
                sc.mm(O[0:65, :], vx[:, kt, half, :], PT[(s_ - 1) % 3][:], kt == 0, kt == NT - 1,
                      reads=["vx", ("PT", (s_ - 1) % 3)], writes=[ok])
                if kt == NT - 1:
                    fin(gi, h, qb)
        sc.flush()


def phase_lru(nc, sc, c, l):
    with ExitStack() as es:
        T = lambda name, shape, dt=F32: es.enter_context(nc.sbuf_tensor("%s_L%d" % (name, l), shape, dt))
        P = lambda name, shape, dt=F32: es.enter_context(nc.psum_tensor("%s_L%d" % (name, l), shape, dt))
        sm = T("c_sm", [128, 2, 11])
        W = T("c_W", [128, 8, 128])
        X = T("c_X", [128, S]); G = T("c_G", [128, S]); XF = T("c_XF", [128, S]); XC = T("c_XC", [128, S])
        R = T("c_R", [128, S]); I_ = T("c_I", [128, S]); A = T("c_A", [128, S]); U = T("c_U", [128, S])
        H = [T("c_H%d" % i, [128, S]) for i in range(2)]
        cf = T("c_cf", [128, 8])
        pp = [P("c_pp%d" % i, [128, 512]) for i in range(4)]
        sc.dma("sp", sm[:], c.lru_small[l], writes=["sm"])
        sc.dma("sp", r32(W[:]), r32(c.lru_w[l]), writes=["W"])
        ppi = 0
        for cc in range(2):
            sc.dma("sp", X[:], c.p_fm[cc * 128:(cc + 1) * 128, :], writes=["X"])
            sc.dma("sp", G[:], c.p_fm[256 + cc * 128:256 + (cc + 1) * 128, :], writes=["G"])
            w = lambda j: sm[:, cc, j:j + 1]
            sc.ts("dve", XF[:], X[:], w(2), w(4), ALU.mult, ALU.add, reads=["X", "sm"], writes=["XF"])
            sc.stt("dve", XF[:, 2:S], X[:, 0:S - 2], w(0), XF[:, 2:S], ALU.mult, ALU.add, reads=["X", "XF", "sm"], writes=["XF"])
            sc.stt("dve", XF[:, 1:S], X[:, 0:S - 1], w(1), XF[:, 1:S], ALU.mult, ALU.add, reads=["X", "XF", "sm"], writes=["XF"])
            sc.copy("pool", r32(XC[:, S - 1:S]), XF[:, S - 1:S], reads=["XF"], writes=["XC"])
            sc.stt("dve", r32(XC[:, 0:S - 1]), X[:, 1:S], w(3), XF[:, 0:S - 1], ALU.mult, ALU.add, reads=["X", "XF", "sm"], writes=["XC"])
            for d in range(2):
                ba = sm[:, cc, 5 + d:6 + d]; bx = sm[:, cc, 7 + d:8 + d]; lam = sm[:, cc, 9 + d:10 + d]
                ck = ("cf", d)
                c0 = cf[:, 4 * d:4 * d + 1]; c1 = cf[:, 4 * d + 1:4 * d + 2]; c2 = cf[:, 4 * d + 2:4 * d + 3]
                sc.act(c0, lam, AF.Exp, reads=["sm"], writes=[ck], scale=-1.0)
                sc.act(c0, c0, AF.Ln, reads=[ck], writes=[ck], bias=1.0)
                sc.ts("dve", c1, c0, -8.0, None, ALU.mult, reads=[ck], writes=[ck])
                sc.ts("dve", c2, c0, -16.0, None, ALU.mult, reads=[ck], writes=[ck])
                for tb in range(4):
                    sl = slice(tb * 512, (tb + 1) * 512)
                    pa = pp[ppi % 4]; pak = ("pp", ppi % 4); ppi += 1
                    px = pp[ppi % 4]; pxk = ("pp", ppi % 4); ppi += 1
                    sc.mm(pa[:, :], r32(W[:, d * 4 + 0 * 2 + cc, :]), r32(XC[:, sl]), True, True, reads=["W", "XC"], writes=[pak])
                    sc.mm(px[:, :], r32(W[:, d * 4 + 1 * 2 + cc, :]), r32(XC[:, sl]), True, True, reads=["W", "XC"], writes=[pxk])
                    sc.act(R[:, sl], pa[:, :], AF.Sigmoid, reads=[pak, "sm"], writes=["R"], bias=ba)
                    sc.act(I_[:, sl], px[:, :], AF.Sigmoid, reads=[pxk, "sm"], writes=["I"], bias=bx)
                sc.act(A[:], R[:], AF.Exp, reads=["R", ck], writes=["A"], scale=c1)
                sc.act(U[:], R[:], AF.Exp, reads=["R", ck], writes=["U"], scale=c2)
                sc.ts("pool", U[:], U[:], -1.0, 1.0, ALU.mult, ALU.add, reads=["U"], writes=["U"])
                sc.act(U[:], U[:], AF.Sqrt, reads=["U"], writes=["U"])
                sc.tt("pool", I_[:], I_[:], XC[:], ALU.mult, reads=["I", "XC"], writes=["I"])
                sc.tt("dve", U[:], U[:], I_[:], ALU.mult, reads=["U", "I"], writes=["U"])
                if d == 0:
                    sc.add("dve", lambda e, o=H[0][:], a=A[:], u=U[:]: e.tensor_tensor_scan(o, a, u, 0.0, ALU.mult, ALU.add),
                           reads=["A", "U"], writes=[("H", 0)])
                else:
                    sc.add("dve", lambda e, o=H[1][:, ::-1], a=A[:, ::-1], u=U[:, ::-1]: e.tensor_tensor_scan(o, a, u, 0.0, ALU.mult, ALU.add),
                           reads=["A", "U"], writes=[("H", 1)])
            sc.tt("pool", H[0][:], H[0][:], H[1][:], ALU.add, reads=[("H", 0), ("H", 1)], writes=[("H", 0)])
            sc.tt("dve", R[:], G[:], G[:], ALU.mult, reads=["G", "R"], writes=["R"])
            sc.ts("dve", R[:], R[:], 0.044715, 1.0, ALU.mult, ALU.add, reads=["R"], writes=["R"])
            sc.tt("dve", R[:], R[:], G[:], ALU.mult, reads=["R", "G"], writes=["R"])
            sc.act(R[:], R[:], AF.Sigmoid, reads=["R"], writes=["R"], scale=1.5957691216057308)
            sc.tt("pool", H[0][:], H[0][:], G[:], ALU.mult, reads=[("H", 0), "G"], writes=[("H", 0)])
            sc.tt("dve", H[0][:], H[0][:], R[:], ALU.mult, reads=[("H", 0), "R"], writes=[("H", 0)])
            sc.dma("pool", c.yT[512 + cc * 128:512 + (cc + 1) * 128, :], H[0][:], reads=[("H", 0)])
        sc.flush()


def phase_mlstm(nc, sc, c, l):
    with ExitStack() as es:
        T = lambda name, shape, dt=F32: es.enter_context(nc.sbuf_tensor("%s_L%d" % (name, l), shape, dt))
        P = lambda name, shape, dt=F32: es.enter_context(nc.psum_tensor("%s_L%d" % (name, l), shape, dt))
        ident = T("d_id", [128, 128]); ones = T("d_ones", [128, 128])
        mask = T("d_mask", [128, 2, 128])
        fb = T("d_fb", [128, 8]); ng = T("d_ng", [128, 256])
        qT = T("d_qT", [128, 2, S]); kT = T("d_kT", [128, 2, S])
        tm = T("d_tm", [128, NT, 784])
        vp = T("d_vp", [128, NT, 8, 65])
        wA = T("d_wA", [128, NT, 8]); wB = T("d_wB", [128, NT, 8]); eg = T("d_eg", [128, NT, 8])
        nlf = T("d_nlf", [128, NT, 8]); tg = T("d_tg", [128, NT, 8])
        hs = T("d_hs", [128, NT, 256])
        Cst = T("d_C", [128, 2, 4, 65])
        tC = T("d_tC", [128, 65])
        PT = [T("d_PT%d" % i, [128, 128]) for i in range(3)]
        s4 = [T("d_s4%d" % i, [128, 4, 4]) for i in range(2)]
        sq = T("d_sq", [128, 256]); st = T("d_st", [128, 4, 4]); ym = [T("d_ym%d" % i, [128, 256]) for i in range(2)]
        yo = [T("d_yo%d" % i, [128, 256]) for i in range(2)]
        sg = T("d_sg", [128, 256])
        pg = P("d_pg", [128, 512])
        psS = [P("d_pS%d" % i, [128, 512]) for i in range(2)]
        pacc = [P("d_pa%d" % i, [128, 512]) for i in range(2)]
        pkv = [P("d_pk%d" % i, [128, 512]) for i in range(2)]
        ptr = P("d_ptr", [128, 512])

        sc.dma("sp", ident[:], c.ident, writes=["ident"])
        sc.dma("sp", ones[:], c.ones, writes=["ones"])
        sc.dma("sp", mask[:, 0, :], c.triu, writes=["mask"])
        sc.dma("sp", mask[:, 1, :], c.tril, writes=["mask"])
        sc.dma("sp", fb[:], c.fbrep[l], writes=["fb"])
        sc.dma("sp", ng[:], c.ngrep[l], writes=["ng"])
        for ch in range(2):
            sc.dma("sp", qT[:, ch, :], c.p_fm[512 + ch * 128:512 + (ch + 1) * 128, :], writes=["qT"])
            sc.dma("sp", kT[:, ch, :], c.p_fm[768 + ch * 128:768 + (ch + 1) * 128, :], writes=["kT"])
        for i in range(NT):
            sc.dma("sp", tm[:, i, :], c.p_tm[i * 128:(i + 1) * 128, 1536:2320], writes=[("tm", i)])
        sc.add("pool", lambda e: e.memset(Cst[:], 0.0), writes=["C"])
        for i in range(NT):
            gt = tm[:, i, 768:784]
            fview = gt.rearrange("p (d k h) -> p d k h", d=2, k=2)[:, :, 1, :]
            iview = gt.rearrange("p (d k h) -> p d k h", d=2, k=2)[:, :, 0, :]
            tk = ("tg", i)
            sc.tt("dve", tg[:, i, :].rearrange("p (d h) -> p d h", d=2), fview, fb[:].rearrange("p (d h) -> p d h", d=2),
                  ALU.add, reads=[("tm", i), "fb"], writes=[tk])
            sc.act(tg[:, i, :], tg[:, i, :], AF.Exp, reads=[tk], writes=[tk], scale=-1.0)
            sc.act(nlf[:, i, :], tg[:, i, :], AF.Ln, reads=[tk], writes=[("nlf", i)], bias=1.0)
            sc.mm(pg[:, 0:4], mask[:, 0, :], nlf[:, i, 0:4], True, True, reads=["mask", ("nlf", i)], writes=["pg"])
            sc.mm(pg[:, 4:8], mask[:, 1, :], nlf[:, i, 4:8], True, True, reads=["mask", ("nlf", i)], writes=["pg"])
            sc.mm(pg[:, 8:16], ones[:, :], nlf[:, i, :], True, True, reads=["ones", ("nlf", i)], writes=["pg"])
            sc.act(wA[:, i, :], pg[:, 0:8], AF.Exp, reads=["pg"], writes=[("wA", i)], scale=-1.0)
            sc.act(eg[:, i, :], pg[:, 8:16], AF.Exp, reads=["pg"], writes=[("eg", i)], scale=-1.0)
            sc.tt("dve", wB[:, i, :].rearrange("p (d h) -> p d h", d=2), iview, pg[:, 0:8].rearrange("p (d h) -> p d h", d=2),
                  ALU.add, reads=[("tm", i), "pg"], writes=[("wB", i)])
            sc.act(wB[:, i, :], wB[:, i, :], AF.Exp, reads=[("wB", i)], writes=[("wB", i)])
            v3 = tm[:, i, 256:512].rearrange("p (h d) -> p h d", d=64)
            for d in range(2):
                eng = "dve" if d == 0 else "pool"
                sc.tt(eng, vp[:, i, d * 4:(d + 1) * 4, 0:64], v3, wB[:, i, d * 4:(d + 1) * 4].unsqueeze(2).broadcast_to([128, 4, 64]),
                      ALU.mult, reads=[("tm", i), ("wB", i)], writes=[("vp", i)])
            sc.copy("pool", vp[:, i, :, 64:65], wB[:, i, :].unsqueeze(2), reads=[("wB", i)], writes=[("vp", i)])
        ui = 0
        for s_ in range(NT):
            for d in range(2):
                i = s_ if d == 0 else NT - 1 - s_
                tsl = slice(i * 128, (i + 1) * 128)
                acc = pacc[d]; ak = ("pacc", d)
                for h in range(4):
                    p0 = (h % 2) * 64; ch = h // 2
                    pS = psS[ui % 2]; pk_ = ("pS", ui % 2)
                    pt = PT[ui % 3]; ptk = ("PT", ui % 3)
                    kv = pkv[ui % 2]; kvk = ("pkv", ui % 2)
                    ui += 1
                    sc.mm(pS[:, 0:128], kT[p0:p0 + 64, ch, tsl], qT[p0:p0 + 64, ch, tsl], True, True,
                          reads=["kT", "qT"], writes=[pk_])
                    sc.stt("dve", pt[:], pS[:, 0:128], 0.125, mask[:, d, :], ALU.mult, ALU.mult,
                           reads=[pk_, "mask"], writes=[ptk])
                    sc.mm(acc[:, h * 65:(h + 1) * 65], pt[:], vp[:, i, d * 4 + h, :], True, False,
                          reads=[ptk, ("vp", i)], writes=[ak])
                    sc.mm(acc[:, h * 65:(h + 1) * 65], qT[p0:p0 + 64, ch, tsl], Cst[p0:p0 + 64, d, h, :], False, True,
                          reads=["qT", ("C", d, h)], writes=[ak])
                    if s_ < NT - 1:
                        sc.mm(kv[p0:p0 + 64, 0:65], tm[:, i, h * 64:(h + 1) * 64], vp[:, i, d * 4 + h, :], True, True,
                              reads=[("tm", i), ("vp", i)], writes=[kvk])
                        sc.stt("dve", tC[p0:p0 + 64, :], kv[p0:p0 + 64, 0:65], 0.125, Cst[p0:p0 + 64, d, h, :], ALU.mult, ALU.add,
                               reads=[kvk, ("C", d, h)], writes=["tC"])
                        sc.ts("dve", Cst[p0:p0 + 64, d, h, :], tC[p0:p0 + 64, :], eg[p0:p0 + 64, i, d * 4 + h:d * 4 + h + 1], None, ALU.mult,
                              reads=["tC", ("eg", i)], writes=[("C", d, h)])
                a3 = acc[:, 0:260].rearrange("p (h e) -> p h e", e=65)
                sb = s4[d]; sk = ("s4", d)
                sc.tt("dve", sb[:, 0, :], a3[:, :, 64], wA[:, i, d * 4:(d + 1) * 4], ALU.mult, reads=[ak, ("wA", i)], writes=[sk])
                sc.act(sb[:, 1, :], sb[:, 0, :], AF.Abs, reads=[sk], writes=[sk])
                sc.ts("dve", sb[:, 1, :], sb[:, 1, :], 1.0, None, ALU.max, reads=[sk], writes=[sk])
                sc.add("dve", lambda e, o=sb[:, 2, :], i_=sb[:, 1, :]: e.reciprocal(o, i_), reads=[sk], writes=[sk])
                sc.tt("dve", sb[:, 3, :], sb[:, 2, :], wA[:, i, d * 4:(d + 1) * 4], ALU.mult, reads=[sk, ("wA", i)], writes=[sk])
                h3 = hs[:, i, :].rearrange("p (h e) -> p h e", e=64)
                if s_ < NT // 2:
                    sc.tt("dve", h3, a3[:, :, 0:64], sb[:, 3, :].unsqueeze(2).broadcast_to([128, 4, 64]), ALU.mult,
                          reads=[ak, sk], writes=[("hs", i)])
                else:
                    sc.tt("dve", sq[:].rearrange("p (h e) -> p h e", e=64), a3[:, :, 0:64],
                          sb[:, 3, :].unsqueeze(2).broadcast_to([128, 4, 64]), ALU.mult, reads=[ak, sk], writes=["sq"])
                    sc.tt("pool", hs[:, i, :], hs[:, i, :], sq[:], ALU.add, reads=[("hs", i), "sq"], writes=[("hs", i)])
        for i in range(NT):
            y = ym[i % 2]; yk = ("ym", i % 2)
            sc.tt("pool", sq[:], hs[:, i, :], hs[:, i, :], ALU.mult, reads=[("hs", i)], writes=["sq"])
            sc.add("dve", lambda e, o=st[:, 0, :], i_=sq[:].rearrange("p (h d) -> p h d", d=64):
                   e.tensor_reduce(o, i_, AX.X, ALU.add), reads=["sq"], writes=["st"])
            sc.ts("dve", st[:, 1, :], st[:, 0, :], 1.0 / 64, EPS, ALU.mult, ALU.add, reads=["st"], writes=["st"])
            sc.act(st[:, 2, :], st[:, 1, :], AF.Sqrt, reads=["st"], writes=["st"])
            sc.add("dve", lambda e, o=st[:, 3, :], i_=st[:, 2, :]: e.reciprocal(o, i_), reads=["st"], writes=["st"])
            sc.tt("dve", y[:].rearrange("p (h d) -> p h d", d=64), hs[:, i, :].rearrange("p (h d) -> p h d", d=64),
                  st[:, 3, :].unsqueeze(2).broadcast_to([128, 4, 64]), ALU.mult, reads=[("hs", i), "st"], writes=[yk])
            sc.tt("pool", y[:], y[:], ng[:], ALU.mult, reads=[yk, "ng"], writes=[yk])
            sc.act(sg[:], tm[:, i, 512:768], AF.Sigmoid, reads=[("tm", i)], writes=["sg"])
            sc.tt("dve", y[:], y[:], sg[:], ALU.mult, reads=[yk, "sg"], writes=[yk])
            for ch in range(2):
                sc.tr(ptr[:, ch * 128:(ch + 1) * 128], y[:, ch * 128:(ch + 1) * 128], ident[:], reads=[yk, "ident"], writes=["ptr"])
            o = yo[i % 2]; ok = ("yo", i % 2)
            sc.copy("act", o[:], ptr[:, 0:256], reads=["ptr"], writes=[ok])
            sc.dma("pool", c.yT[768:1024, i * 128:(i + 1) * 128].rearrange("(ch p) t -> p ch t", p=128),
                   o[:].rearrange("p (ch t) -> p ch t", ch=2), reads=[ok])
        sc.flush()


BF16 = mybir.dt.bfloat16
F16 = mybir.dt.float16
NE = 16
CAP = 256
WQ = "pool"


def phase_outproj(nc, sc, c, l, x_src):
    with ExitStack() as es:
        T = lambda name, shape, dt=F32: es.enter_context(nc.sbuf_tensor("%s_L%d" % (name, l), shape, dt))
        P = lambda name, shape, dt=F32: es.enter_context(nc.psum_tensor("%s_L%d" % (name, l), shape, dt))
        wt = T("e_wt", [128, 8, D], BF16); yh = [T("e_yh%d" % i, [128, 8, 512], BF16) for i in range(2)]; g = T("e_g", [128, 8]); ones = T("e_ones", [128, 128])
        yb = [T("e_yb%d" % i, [128, 8, 512]) for i in range(2)]
        ysq = T("e_ysq", [128, 6, 512])
        xt = [T("e_xt%d" % i, [128, D]) for i in range(2)]
        st = [T("e_st%d" % i, [128, 8]) for i in range(2)]
        pm = [P("e_pm%d" % i, [128, 512]) for i in range(6)]
        pst = P("e_pst", [128, 512])
        sc.dma("sp", ones[:], c.ones, writes=["ones"])
        sc.dma("sp", g[:], c.gout[l], writes=["g"])
        for ch in range(8):
            sc.dma("pool", wt[:, ch, :], c.w_out[l][ch * 128:(ch + 1) * 128, :], writes=[("wt", ch)])
        pi = 0
        for b in range(4):
            y = yb[b % 2]; ykey = ("yb", b % 2)
            for ch in range(8):
                sc.dma("sp", y[:, ch, :], c.yT[ch * 128:(ch + 1) * 128, b * 512:(b + 1) * 512], writes=[ykey])
            sc.act(ysq[:], y[:, 0:6, :], AF.Square, reads=[ykey], writes=["ysq"])
            yhb = yh[b % 2]; yhk = ("yh", b % 2)
            for ch in range(8):
                sc.act(yhb[:, ch, :], y[:, ch, :], AF.Copy, reads=[ykey, "g"], writes=[yhk], scale=g[:, ch:ch + 1])
            for tl in range(4):
                i = b * 4 + tl
                tsl = slice(tl * 128, (tl + 1) * 128)
                x = xt[i % 2]; xk = ("xt", i % 2); sb = st[i % 2]; sk = ("st", i % 2)
                sc.dma("sp", x[:], x_src[i * 128:(i + 1) * 128, :], writes=[xk])
                for ch in range(6):
                    col = 0 if ch < 4 else 1
                    sc.mm(pst[:, col:col + 1], ysq[:, ch, tsl], ones[:, 0:1], ch in (0, 4), ch in (3, 5), reads=["ysq", "ones"], writes=["pst"])
                sc.ts("dve", sb[:, 0:1], pst[:, 0:1], 1.0 / 512, EPS, ALU.mult, ALU.add, reads=["pst"], writes=[sk])
                sc.ts("dve", sb[:, 1:2], pst[:, 1:2], 1.0 / 256, EPS, ALU.mult, ALU.add, reads=["pst"], writes=[sk])
                sc.act(sb[:, 2:4], sb[:, 0:2], AF.Sqrt, reads=[sk], writes=[sk])
                sc.add("dve", lambda e, o=sb[:, 4:6], i_=sb[:, 2:4]: e.reciprocal(o, i_), reads=[sk], writes=[sk])
                for n in range(2):
                    nsl = slice(n * 512, (n + 1) * 512)
                    ps = []
                    for (c0, c1) in ((0, 4), (4, 6), (6, 8)):
                        p_ = pm[pi % 6]; pk = ("pm", pi % 6); pi += 1
                        for ch in range(c0, c1):
                            sc.mm(p_[:, :], yhb[:, ch, tsl], wt[:, ch, nsl], ch == c0, ch == c1 - 1,
                                  reads=[yhk, ("wt", ch)], writes=[pk])
                        ps.append((p_, pk))
                    sc.stt("dve", x[:, nsl], ps[0][0][:, :], sb[:, 4:5], x[:, nsl], ALU.mult, ALU.add, reads=[ps[0][1], sk, xk], writes=[xk])
                    sc.stt("dve", x[:, nsl], ps[1][0][:, :], sb[:, 5:6], x[:, nsl], ALU.mult, ALU.add, reads=[ps[1][1], sk, xk], writes=[xk])
                    sc.tt("dve", x[:, nsl], x[:, nsl], ps[2][0][:, :], ALU.add, reads=[ps[2][1], xk], writes=[xk])
                sc.dma("pool", c.out[i * 128:(i + 1) * 128, :], x[:], reads=[xk])
        sc.flush()


def phase_router(nc, sc, c, l):
    with ExitStack() as es:
        T = lambda name, shape, dt=F32: es.enter_context(nc.sbuf_tensor("%s_L%d" % (name, l), shape, dt))
        P = lambda name, shape, dt=F32: es.enter_context(nc.psum_tensor("%s_L%d" % (name, l), shape, dt))
        ident = T("f_id", [128, 128]); g2 = T("f_g2", [128, 8]); wr = T("f_wr", [128, 8, NE])
        xt = [T("f_xt%d" % i, [128, D]) for i in range(2)]
        junk = T("f_junk", [128, D])
        hb = [T("f_hb%d" % i, [128, D], BF16) for i in range(2)]
        hT = [T("f_hT%d" % i, [128, 8, 128]) for i in range(2)]
        st = [T("f_st%d" % i, [128, 8]) for i in range(2)]
        aff = T("f_aff", [128, NT, NE]); ex = T("f_ex", [128, NE])
        affT = T("f_affT", [NE, S]); work = T("f_work", [NE, S]); msk = T("f_msk", [NE, S]); pos = T("f_pos", [NE, S])
        mx8 = T("f_mx8", [NE, 8]); onesr = T("f_onesr", [NE, S])
        slot = T("f_slot", [128, NT, NE])
        ga = T("f_ga", [128, NT, NE, 4], BF16); gf = T("f_gf", [128, NT, NE]); tki = T("f_tki", [128, NT, 2], BF16)
        pT = [P("f_pT%d" % i, [128, 512]) for i in range(4)]
        pl = [P("f_pl%d" % i, [128, 512]) for i in range(2)]
        pa = [P("f_pa%d" % i, [128, 512]) for i in range(2)]
        sc.dma("sp", ident[:], c.ident, writes=["ident"])
        sc.dma("sp", g2[:], c.g2rep[l], writes=["g2"])
        sc.dma("sp", wr[:], c.w_router[l].rearrange("(c p) e -> p c e", p=128), writes=["wr"])
        sc.dma("sp", tki[:], c.tokhl, writes=["tki"])
        sc.tt("dve", wr[:], wr[:], g2[:].unsqueeze(2).broadcast_to([128, 8, NE]), ALU.mult, reads=["wr", "g2"], writes=["wr"])
        sc.add("pool", lambda e: e.memset(onesr[:], 1.0), writes=["onesr"])
        for i in range(NT):
            x = xt[i % 2]; xk = ("xt", i % 2); sb = st[i % 2]; sk = ("st", i % 2)
            sc.dma("sp", x[:], c.out[i * 128:(i + 1) * 128, :], writes=[xk])
            sc.act(junk[:], x[:], AF.Square, reads=[xk], writes=["junk", sk], accum_out=sb[:, 0:1])
            sc.ts("dve", sb[:, 1:2], sb[:, 0:1], 1.0 / D, EPS, ALU.mult, ALU.add, reads=[sk], writes=[sk])
            sc.act(sb[:, 2:3], sb[:, 1:2], AF.Sqrt, reads=[sk], writes=[sk])
            sc.add("dve", lambda e, o=sb[:, 3:4], i_=sb[:, 2:3]: e.reciprocal(o, i_), reads=[sk], writes=[sk])
            sc.ts("dve", x[:], x[:], sb[:, 3:4], None, ALU.mult, reads=[xk, sk], writes=[xk])
            sc.copy("pool", hb[i % 2][:], x[:], reads=[xk], writes=[("hb", i % 2)])
            sc.dma("pool", c.h2n[i * 128:(i + 1) * 128, :], hb[i % 2][:], reads=[("hb", i % 2)])
            for half in range(2):
                pt = pT[(i % 2) * 2 + half]; pk = ("pT", (i % 2) * 2 + half)
                for cc in range(4):
                    ch = half * 4 + cc
                    sc.tr(pt[:, cc * 128:(cc + 1) * 128], x[:, ch * 128:(ch + 1) * 128], ident[:], reads=[xk, "ident"], writes=[pk])
                sc.copy("act" if half == 0 else "dve", hT[i % 2][:, half * 4:(half + 1) * 4, :],
                        pt[:].rearrange("p (c t) -> p c t", c=4), reads=[pk], writes=[("hT", i % 2)])
            lg = pl[i % 2]; lk = ("pl", i % 2)
            for ch in range(8):
                sc.mm(lg[:, 0:NE], hT[i % 2][:, ch, :], wr[:, ch, :], ch == 0, ch == 7, reads=[("hT", i % 2), "wr"], writes=[lk])
            sc.add("dve", lambda e, o=sb[:, 4:5], i_=lg[:, 0:NE]: e.tensor_reduce(o, i_, AX.X, ALU.max), reads=[lk], writes=[sk])
            sc.ts("dve", sb[:, 5:6], sb[:, 4:5], -1.0, None, ALU.mult, reads=[sk], writes=[sk])
            sc.act(ex[:], lg[:, 0:NE], AF.Exp, reads=[lk, sk], writes=["ex", sk], bias=sb[:, 5:6], accum_out=sb[:, 6:7])
            sc.add("dve", lambda e, o=sb[:, 7:8], i_=sb[:, 6:7]: e.reciprocal(o, i_), reads=[sk], writes=[sk])
            sc.ts("dve", aff[:, i, :], ex[:], sb[:, 7:8], None, ALU.mult, reads=["ex", sk], writes=[("aff", i)])
            pa_ = pa[i % 2]; pak = ("pa", i % 2)
            sc.tr(pa_[0:NE, 0:128], aff[:, i, :], ident[:], reads=[("aff", i), "ident"], writes=[pak])
            sc.copy("act", affT[:, i * 128:(i + 1) * 128], pa_[0:NE, 0:128], reads=[pak], writes=["affT"])
        sc.copy("dve", work[:], affT[:], reads=["affT"], writes=["work"])
        for r in range(CAP // 8):
            sc.add("dve", lambda e: e.max(mx8[:], work[:]), reads=["work"], writes=["mx8"])
            if r < CAP // 8 - 1:
                sc.add("dve", lambda e: e.match_replace(work[:], mx8[:], work[:], -1e30), reads=["mx8", "work"], writes=["work"])
        sc.ts("dve", msk[:], affT[:], mx8[:, 7:8], None, ALU.is_ge, reads=["affT", "mx8"], writes=["msk"])
        sc.add("dve", lambda e: e.tensor_tensor_scan(pos[:], onesr[:], msk[:], 0.0, ALU.mult, ALU.add), reads=["onesr", "msk"], writes=["pos"])
        sc.tt("dve", pos[:], pos[:], msk[:], ALU.mult, reads=["pos", "msk"], writes=["pos"])
        sc.ts("dve", pos[:], pos[:], -1.0, None, ALU.add, reads=["pos"], writes=["pos"])
        for i in range(NT):
            pa_ = pa[i % 2]; pak = ("pa", i % 2)
            sc.tr(pa_[:, 0:NE], pos[:, i * 128:(i + 1) * 128], ident[0:NE, 0:NE], reads=["pos", "ident"], writes=[pak])
            sc.copy("act", slot[:, i, :], pa_[:, 0:NE], reads=[pak], writes=["slot"])
        sc.copy("dve", ga[:, :, :, 0], aff[:], reads=[("aff", i) for i in range(NT)], writes=["ga"])
        sc.copy("dve", gf[:], ga[:, :, :, 0], reads=["ga"], writes=["gf"])
        sc.tt("dve", gf[:], aff[:], gf[:], ALU.subtract, reads=["gf"] + [("aff", i) for i in range(NT)], writes=["gf"])
        sc.copy("dve", ga[:, :, :, 1], gf[:], reads=["gf"], writes=["ga"])
        sc.copy("pool", ga[:, :, :, 2], tki[:, :, 0:1].broadcast_to([128, NT, NE]), reads=["tki"], writes=["ga"])
        sc.copy("pool", ga[:, :, :, 3], tki[:, :, 1:2].broadcast_to([128, NT, NE]), reads=["tki"], writes=["ga"])
        sc.dma("pool", c.slot_d, slot[:].rearrange("p a b -> p (a b)"), reads=["slot"])
        sc.dma("pool", c.ga_d, ga[:].rearrange("p a b c -> p (a b c)"), reads=["ga"])
        sc.flush()


def phase_experts(nc, sc, c, l):
    with ExitStack() as es:
        T = lambda name, shape, dt=F32: es.enter_context(nc.sbuf_tensor("%s_L%d" % (name, l), shape, dt))
        P = lambda name, shape, dt=F32: es.enter_context(nc.psum_tensor("%s_L%d" % (name, l), shape, dt))
        h2 = T("g_h2", [128, NT, D], BF16)
        acc = T("g_acc", [128, NT, D])
        Wg = [T("g_Wg%d" % i, [128, 8, 256], BF16) for i in range(2)]
        Wu = [T("g_Wu%d" % i, [128, 8, 256], BF16) for i in range(2)]
        Wd = [T("g_Wd%d" % i, [128, 2, D], BF16) for i in range(2)]
        Sel = T("g_Sel", [128, NT, CAP], BF16)
        SelT = T("g_SelT", [128, 2, S], BF16)
        xg = T("g_xg", [128, 8, CAP], BF16)
        H = [T("g_H%d" % i, [128, 2, CAP], BF16) for i in range(2)]
        sil = [T("g_sil%d" % i, [128, CAP]) for i in range(2)]
        ye = T("g_ye", [128, 2, D], BF16)
        slot = T("g_slot", [128, NT * NE]); ga = T("g_ga", [128, NT * NE * 4], BF16)
        io256 = T("g_io256", [128, CAP], F16); ioT = T("g_ioT", [128, S], F16)
        g2 = T("g_g2", [128, 8]); gt2 = T("g_gt2", [128, 2, 2]); tks = T("g_tks", [128, 8])
        yacc = [P("g_ya%d" % i, [128, 512]) for i in range(4)]
        au = [P("g_au%d" % i, [128, 512]) for i in range(2)]
        gs = [P("g_gs%d" % i, [128, 512]) for i in range(2)]
        sc.dma("sp", slot[:], c.slot_d, writes=["slot"])
        sc.dma("sp", ga[:], c.ga_d, writes=["ga"])
        sc.dma("sp", io256[:], c.iota256, writes=["io256"])
        sc.dma("sp", ioT[:], c.iotaT, writes=["ioT"])
        sc.dma("sp", g2[:], c.g2rep[l], writes=["g2"])
        for i in range(NT):
            sc.dma("sp", h2[:, i, :], c.h2n[i * 128:(i + 1) * 128, :], writes=[("h2", i)])
            sc.dma("sp", acc[:, i, :], c.out[i * 128:(i + 1) * 128, :], writes=[("acc", i)])
        ga4 = ga[:].rearrange("p (i e k) -> p i e k", i=NT, e=NE)
        wq = [0]
        def load_w(e, wb):
            b = wq[0] % 2; wq[0] += 1
            hs_ = slice(wb * 256, (wb + 1) * 256)
            sc.dma(WQ, Wg[b][:], c.w_gate[l, e][:, hs_].rearrange("(c p) h -> p c h", p=128), writes=[("Wg", b)])
            sc.dma(WQ, Wu[b][:], c.w_up[l, e][:, hs_].rearrange("(c p) h -> p c h", p=128), writes=[("Wu", b)])
            sc.dma(WQ, Wd[b][:], c.w_down[l, e][wb * 256:(wb + 1) * 256, :].rearrange("(c p) f -> p c f", p=128), writes=[("Wd", b)])
            return b
        def build_sel(e):
            for i in range(NT):
                sc.ts("dve", Sel[:, i, :], io256[:], slot[:, i * NE + e:i * NE + e + 1], None, ALU.is_equal,
                      reads=["io256", "slot"], writes=[("Sel", i)])
        build_sel(0)
        seq = [(e, wb) for e in range(NE) for wb in range(8)]
        nxt = load_w(0, 0)
        gi = 0; ai = 0; hi = 0
        for e in range(NE):
            tk = gs[gi % 2]; tkk = ("gs", gi % 2); gi += 1
            for ct in range(2):
                for i in range(NT):
                    sc.mm(tk[:, ct * 4:(ct + 1) * 4], Sel[:, i, ct * 128:(ct + 1) * 128], ga4[:, i, e, :], i == 0, i == NT - 1,
                          reads=[("Sel", i), "ga"], writes=[tkk])
            sc.copy("act", tks[:], tk[:, 0:8], reads=[tkk], writes=["tks"])
            t4 = tks[:].rearrange("p (ct k two) -> p ct k two", ct=2, k=2)
            sc.tt("dve", gt2[:], t4[:, :, :, 0], t4[:, :, :, 1], ALU.add, reads=["tks"], writes=["gt2"])
            for fp in range(4):
                gp = gs[gi % 2]; gk = ("gs", gi % 2); gi += 1
                for k in range(2):
                    fc = fp * 2 + k
                    for i in range(NT):
                        sc.mm(gp[:, k * 256:(k + 1) * 256], h2[:, i, fc * 128:(fc + 1) * 128], Sel[:, i, :], i == 0, i == NT - 1,
                              reads=[("h2", i), ("Sel", i)], writes=[gk])
                    sc.act(xg[:, fc, :], gp[:, k * 256:(k + 1) * 256], AF.Copy, reads=[gk, "g2"], writes=[("xg", fc)], scale=g2[:, fc:fc + 1])
            if e + 1 < NE:
                build_sel(e + 1)
            for wb in range(8):
                b = nxt
                si = e * 8 + wb + 1
                if si < len(seq):
                    nxt = load_w(*seq[si])
                hb_ = H[hi % 2]; hk = ("H", hi % 2); hi += 1
                for jj in range(2):
                    p_ = au[ai % 2]; pk = ("au", ai % 2)
                    sl_ = sil[ai % 2]; slk = ("sil", ai % 2); ai += 1
                    for ch in range(8):
                        sc.mm(p_[:, 0:256], Wg[b][:, ch, jj * 128:(jj + 1) * 128], xg[:, ch, :], ch == 0, ch == 7,
                              reads=[("Wg", b), ("xg", ch)], writes=[pk])
                    for ch in range(8):
                        sc.mm(p_[:, 256:512], Wu[b][:, ch, jj * 128:(jj + 1) * 128], xg[:, ch, :], ch == 0, ch == 7,
                              reads=[("Wu", b), ("xg", ch)], writes=[pk])
                    sc.act(sl_[:], p_[:, 0:256], AF.Silu, reads=[pk], writes=[slk])
                    sc.tt("dve", hb_[:, jj, :], sl_[:], p_[:, 256:512], ALU.mult, reads=[slk, pk], writes=[hk])
                for ct in range(2):
                    for fb in range(2):
                        for jj in range(2):
                            sc.mm(yacc[ct * 2 + fb][:, :], hb_[:, jj, ct * 128:(ct + 1) * 128], Wd[b][:, jj, fb * 512:(fb + 1) * 512],
                                  wb == 0 and jj == 0, wb == 7 and jj == 1, reads=[hk, ("Wd", b)], writes=[("ya", ct * 2 + fb)])
            for ct in range(2):
                for fb in range(2):
                    sc.act(ye[:, ct, fb * 512:(fb + 1) * 512], yacc[ct * 2 + fb][:, :], AF.Copy, reads=[("ya", ct * 2 + fb), "gt2"],
                           writes=[("ye", ct)], scale=gt2[:, ct, 0:1])
                sc.ts("dve", SelT[:, ct, :], ioT[:], gt2[:, ct, 1:2], None, ALU.is_equal,
                      reads=["ioT", "gt2"], writes=[("SelT", ct)])
            for i in range(NT):
                for fb in range(2):
                    sp_ = gs[gi % 2]; spk = ("gs", gi % 2); gi += 1
                    for ct in range(2):
                        sc.mm(sp_[:, :], SelT[:, ct, i * 128:(i + 1) * 128], ye[:, ct, fb * 512:(fb + 1) * 512], ct == 0, ct == 1,
                              reads=[("SelT", ct), ("ye", ct)], writes=[spk])
                    sc.tt("dve", acc[:, i, fb * 512:(fb + 1) * 512], acc[:, i, fb * 512:(fb + 1) * 512], sp_[:, :], ALU.add,
                          reads=[spk, ("acc", i)], writes=[("acc", i)])
        for i in range(NT):
            sc.dma("sp", c.out[i * 128:(i + 1) * 128, :], acc[:, i, :], reads=[("acc", i)])
        sc.flush()


def build(n_layers=DEPTH, debug=None):
    nc = bass.Bass("TRN2", target_bir_lowering=False)
    nc.dge_precook = False
    c = Ctx()
    def inp(name, shape):
        return nc.dram_tensor(name, list(shape), F32, kind="ExternalInput").ap()
    c.x = inp("x", [S, D])
    c.norm1_g = inp("norm1_g", [DEPTH, D])
    c.w_in = inp("w_in", [DEPTH, D, D_IN])
    c.ident = inp("ident", [128, 128])
    c.ones = inp("ones", [128, 128])
    c.cos_rep = inp("cos_rep", [S, 320])
    c.sin_rep = inp("sin_rep", [S, 320])
    c.ggrep = inp("ggrep", [DEPTH, 128, 640])
    c.lru_small = inp("lru_small", [DEPTH, 128, 2, 11])
    c.lru_w = inp("lru_w", [DEPTH, 128, 8, 128])
    c.w_out = inp("w_out", [DEPTH, D, D])
    c.gout = inp("gout", [DEPTH, 128, 8])
    c.g2rep = inp("g2rep", [DEPTH, 128, 8])
    c.w_router = inp("w_router", [DEPTH, D, NE])
    c.w_gate = inp("w_expert_gate", [DEPTH, NE, D, 2 * D])
    c.w_up = inp("w_expert_up", [DEPTH, NE, D, 2 * D])
    c.w_down = inp("w_expert_down", [DEPTH, NE, 2 * D, D])
    c.tokhl = nc.dram_tensor("tokhl", [128, NT, 2], BF16, kind="ExternalInput").ap()
    c.iota256 = nc.dram_tensor("iota256", [128, CAP], F16, kind="ExternalInput").ap()
    c.iotaT = nc.dram_tensor("iotaT", [128, S], F16, kind="ExternalInput").ap()
    c.h2n = nc.dram_tensor("h2n", [S, D], BF16, kind="Internal").ap()
    c.slot_d = nc.dram_tensor("slot_d", [128, NT * NE], F32, kind="ExternalOutput" if debug == "R" else "Internal").ap()
    c.ga_d = nc.dram_tensor("ga_d", [128, NT * NE * 4], BF16, kind="Internal").ap()
    c.triu = inp("triu", [128, 128])
    c.tril = inp("tril", [128, 128])
    c.fbrep = inp("fbrep", [DEPTH, 128, 8])
    c.ngrep = inp("ngrep", [DEPTH, 128, 256])
    c.yT = nc.dram_tensor("yT", [1024, S], F32, kind="ExternalOutput" if debug in ("B", "C", "D") else "Internal").ap()
    c.out = nc.dram_tensor("out", [S, D], F32, kind="ExternalOutput").ap()
    c.p_tm = nc.dram_tensor("p_tm", [S, D_IN], F32, kind="ExternalOutput" if debug == "A" else "Internal").ap()
    c.p_fm = nc.dram_tensor("p_fm", [1024, S], F32, kind="ExternalOutput" if debug == "A" else "Internal").ap()
    with ExitStack() as es:
        sc = Sched(nc, es)
        for l in range(n_layers):
            phase_inproj(nc, sc, c, l, c.x if l == 0 else c.out)
            if debug == "A":
                continue
            if debug != "C" and debug != "D":
                phase_attn(nc, sc, c, l)
            if debug == "B":
                continue
            if debug != "D":
                phase_lru(nc, sc, c, l)
            if debug == "C":
                continue
            phase_mlstm(nc, sc, c, l)
            if debug == "D":
                continue
            phase_outproj(nc, sc, c, l, c.x if l == 0 else c.out)
            if debug == "E":
                continue
            phase_router(nc, sc, c, l)
            if debug == "R":
                continue
            phase_experts(nc, sc, c, l)
    return nc


def consts():
    t = np.arange(S)
    row = (t // 64).astype(np.float32)
    col = (t % 64).astype(np.float32)
    inv = (1.0 / (np.float32(10000.0) ** (np.arange(16, dtype=np.float32) / np.float32(16)))).astype(np.float32)
    ang = np.concatenate([row[:, None] * inv, col[:, None] * inv], axis=-1).astype(np.float32)
    cos = np.cos(ang).astype(np.float32)
    sin = np.sin(ang).astype(np.float32)
    import ml_dtypes
    tok = (np.arange(NT)[None, :] * 128 + np.arange(128)[:, None]).astype(np.float32)
    thi = tok.astype(ml_dtypes.bfloat16)
    tlo = (tok - thi.astype(np.float32)).astype(ml_dtypes.bfloat16)
    return {"tokhl": np.ascontiguousarray(np.stack([thi, tlo], axis=-1)),
            "iota256": np.ascontiguousarray(np.broadcast_to(np.arange(CAP, dtype=np.float16)[None, :], (128, CAP))),
            "iotaT": np.ascontiguousarray(np.broadcast_to(np.arange(S, dtype=np.float16)[None, :], (128, S))),
            "ident": np.eye(128, dtype=np.float32), "ones": np.ones((128, 128), np.float32),
            "triu": np.triu(np.ones((128, 128), np.float32)), "tril": np.tril(np.ones((128, 128), np.float32)),
            "cos_rep": np.ascontiguousarray(np.tile(cos, (1, 10))), "sin_rep": np.ascontiguousarray(np.tile(sin, (1, 10)))}


def layout_small(inputs):
    o = {}
    qg = inputs["q_norm_g"]; kg = inputs["k_norm_g"]
    gg = np.concatenate([np.tile(qg, (1, 8)), np.tile(kg, (1, 2))], axis=1)
    o["ggrep"] = np.ascontiguousarray(np.broadcast_to(gg[:, None, :], (DEPTH, 128, 640))).astype(np.float32)
    go = np.concatenate([inputs["att_out_g"], inputs["lru_out_g"], np.ones((DEPTH, 256), np.float32)], axis=1)
    o["gout"] = np.ascontiguousarray(go.reshape(DEPTH, 8, 128).transpose(0, 2, 1)).astype(np.float32)
    o["g2rep"] = np.ascontiguousarray(inputs["norm2_g"].reshape(DEPTH, 8, 128).transpose(0, 2, 1)).astype(np.float32)
    o["fbrep"] = np.ascontiguousarray(np.broadcast_to(inputs["mlstm_f_bias"].reshape(DEPTH, 1, 8), (DEPTH, 128, 8))).astype(np.float32)
    o["ngrep"] = np.ascontiguousarray(np.broadcast_to(inputs["mlstm_norm_g"].reshape(DEPTH, 1, 256), (DEPTH, 128, 256))).astype(np.float32)
    cols = [inputs["conv_w"][:, j, :] for j in range(4)] + [inputs["conv_b"]]
    cols += [inputs["lru_ba"][:, d, :] for d in range(2)] + [inputs["lru_bx"][:, d, :] for d in range(2)]
    cols += [inputs["lru_lambda"][:, d, :] for d in range(2)]
    sm = np.stack(cols, axis=-1)
    o["lru_small"] = np.ascontiguousarray(sm.reshape(DEPTH, 2, 128, 11).transpose(0, 2, 1, 3)).astype(np.float32)
    lw = np.zeros((DEPTH, 128, 2, 2, 2, 128), np.float32)
    for kind, nm in enumerate(["lru_wa", "lru_wx"]):
        wsrc = inputs[nm]
        for d in range(2):
            for cc in range(2):
                for b in range(2):
                    lw[:, b * 64:(b + 1) * 64, d, kind, cc, b * 64:(b + 1) * 64] = wsrc[:, d, 2 * cc + b]
    o["lru_w"] = lw.reshape(DEPTH, 128, 8, 128)
    return o


SMALL = ["norm1_g", "q_norm_g", "k_norm_g", "conv_w", "conv_b", "lru_wa", "lru_ba", "lru_wx", "lru_bx", "lru_lambda",
         "mlstm_f_bias", "mlstm_norm_g", "att_out_g", "lru_out_g", "norm2_g"]
BIG = ["w_in", "w_out", "w_router", "w_expert_gate", "w_expert_up", "w_expert_down", "norm1_g"]


def kernel(**inputs):
    inputs = {k: np.asarray(v) for k, v in inputs.items()}
    nc = build()
    shared = {k: np.ascontiguousarray(inputs[k], dtype=np.float32) for k in BIG}
    shared.update(consts())
    shared.update(layout_small(inputs))
    x = np.ascontiguousarray(inputs["x"], dtype=np.float32)
    in_maps = []
    for b in range(8):
        m = dict(shared)
        m["x"] = x[b]
        in_maps.append(m)
    res = run_bass_kernel_spmd(nc, in_maps, core_ids=list(range(8)))
    return np.stack([r["out"] for r in res.results], axis=0).astype(np.float32)
```

```python
import numpy as np
from contextlib import ExitStack
import concourse.bass as bass
import concourse.mybir as mybir
from concourse.bass_utils import run_bass_kernel_spmd

F32 = mybir.dt.float32
F32R = mybir.dt.float32r
BF16 = mybir.dt.bfloat16
F16 = mybir.dt.float16
U32 = mybir.dt.uint32
I32 = mybir.dt.int32
AF = mybir.ActivationFunctionType
ALU = mybir.AluOpType
AX = mybir.AxisListType

S = 2048
D = 1024
DEPTH = 4
D_IN = 2320
NT = S // 128
EPS = 1e-6

ENG = ["pe", "act", "dve", "pool", "sp"]
BLK = {"pe": "tensor", "act": "scalar", "dve": "vector", "pool": "gpsimd", "sp": "sync"}


class Sched:
    def __init__(self, nc, es, n_dma=24):
        self.nc = nc
        self.sem = {e: es.enter_context(nc.semaphore("s_" + e)) for e in ENG}
        self.dsem = [es.enter_context(nc.semaphore("s_dma%d" % i)) for i in range(n_dma)]
        self.cnt = {e: 0 for e in ENG}
        self.duse = [0] * n_dma
        self.drr = 0
        self.ops = {e: [] for e in ENG}
        self.waited = {e: {} for e in ENG}
        self.last_w = {}
        self.readers = {}
        self.nops = 0

    def semobj(self, s):
        return self.sem[s] if isinstance(s, str) else self.dsem[s[1]]

    def add(self, eng, fn, reads=(), writes=(), dma=False):
        deps = {}
        def need(tok):
            s, v = tok
            if deps.get(s, 0) < v:
                deps[s] = v
        for r in reads:
            if r in self.last_w:
                need(self.last_w[r])
        for w in writes:
            if w in self.last_w:
                need(self.last_w[w])
            for t in self.readers.get(w, ()):
                need(t)
        if dma:
            k = self.drr
            self.drr = (self.drr + 1) % len(self.dsem)
            self.duse[k] += 1
            n = self.duse[k]
            if n > 1:
                need((("dma", k), 16 * (n - 1)))
            token = (("dma", k), 16 * n)
        else:
            self.cnt[eng] += 1
            token = (eng, self.cnt[eng])
        waits = []
        wd = self.waited[eng]
        for s, v in deps.items():
            if s == "pe" and eng == "pe" and not dma:
                continue
            if wd.get(s, 0) < v:
                wd[s] = v
                waits.append((s, v))
        self.ops[eng].append((waits, fn, token, dma))
        self.nops += 1
        for r in reads:
            self.readers.setdefault(r, []).append(token)
        for w in writes:
            self.last_w[w] = token
            self.readers[w] = []
        return token

    def flush(self):
        waits = []
        for k, n in enumerate(self.duse):
            if n and self.waited["sp"].get(("dma", k), 0) < 16 * n:
                waits.append((("dma", k), 16 * n))
        self.ops["sp"].append((waits, None, None, False))
        ops = self.ops
        self.ops = {e: [] for e in ENG}
        sched = self
        with self.nc.Block() as block:
            for e in ENG:
                lst = ops[e]

                def body(eng, lst=lst):
                    for waits, fn, token, isdma in lst:
                        for (s, v) in waits:
                            eng.wait_ge(sched.semobj(s), v)
                        if fn is None:
                            continue
                        inst = fn(eng)
                        inst.then_inc(sched.semobj(token[0]), 16 if isdma else 1)

                getattr(block, BLK[e])(body)
        for e in ENG:
            for e2 in ENG:
                self.waited[e][e2] = self.cnt[e2]
            for k, n in enumerate(self.duse):
                self.waited[e][("dma", k)] = 16 * n
        self.last_w = {}
        self.readers = {}

    def dma(self, q, out, in_, reads=(), writes=(), slow=False):
        if slow:
            return self.add(q, lambda e: e.dma_start(out=out, in_=in_, allow_slow_non_contiguous=True), reads, writes, dma=True)
        return self.add(q, lambda e: e.dma_start(out=out, in_=in_), reads, writes, dma=True)

    def mm(self, out, lhsT, rhs, start, stop, reads=(), writes=()):
        return self.add("pe", lambda e: e.matmul(out, lhsT, rhs, start=start, stop=stop), reads, writes)

    def tr(self, out, in_, ident, reads=(), writes=()):
        return self.add("pe", lambda e: e.transpose(out, in_, ident), reads, writes)

    def act(self, out, in_, func, reads=(), writes=(), eng="act", **kw):
        return self.add(eng, lambda e: e.activation(out, in_, func, **kw), reads, writes)

    def ts(self, eng, out, in0, s1, s2, op0, op1=None, reads=(), writes=(), **kw):
        if op1 is None:
            return self.add(eng, lambda e: e.tensor_scalar(out, in0, s1, None, op0, **kw), reads, writes)
        return self.add(eng, lambda e: e.tensor_scalar(out, in0, s1, s2, op0, op1, **kw), reads, writes)

    def tt(self, eng, out, in0, in1, op, reads=(), writes=()):
        return self.add(eng, lambda e: e.tensor_tensor(out, in0, in1, op), reads, writes)

    def stt(self, eng, out, in0, scalar, in1, op0, op1, reads=(), writes=()):
        return self.add(eng, lambda e: e.scalar_tensor_tensor(out, in0, scalar, in1, op0, op1), reads, writes)

    def copy(self, eng, out, in_, reads=(), writes=()):
        if eng == "act":
            return self.add(eng, lambda e: e.copy(out, in_), reads, writes)
        return self.add(eng, lambda e: e.tensor_copy(out, in_), reads, writes)


def r32(ap):
    return ap.bitcast(F32R)


class Ctx:
    pass


def phase_inproj(nc, sc, c, l, x_src):
    with ExitStack() as es:
        T = lambda name, shape, dt=F32: es.enter_context(nc.sbuf_tensor("%s_L%d" % (name, l), shape, dt))
        P = lambda name, shape, dt=F32: es.enter_context(nc.psum_tensor("%s_L%d" % (name, l), shape, dt))
        wt = T("a_wt", [128, 8, D_IN], BF16)
        g = T("a_g", [128, 8])
        ident = T("a_id", [128, 128])
        xt = [T("a_xt%d" % i, [128, D]) for i in range(2)]
        junk = T("a_junk", [128, D])
        st = [T("a_st%d" % i, [128, 4]) for i in range(2)]
        hT = [T("a_hT%d" % i, [128, 8, 512], BF16) for i in range(2)]
        og = [T("a_og%d" % i, [128, D_IN]) for i in range(2)]
        of = [T("a_of%d" % i, [128, 512]) for i in range(2)]
        pT = [P("a_pT%d" % i, [128, 512]) for i in range(4)]
        pm = [P("a_pm%d" % i, [128, 512]) for i in range(4)]

        sc.dma("sp", ident[:], c.ident, writes=["ident"])
        sc.dma("sp", g[:], c.norm1_g[l].rearrange("(c p) -> p c", p=128), writes=["g"], slow=True)
        for ch in range(8):
            sc.dma("pool", wt[:, ch, :], c.w_in[l][ch * 128:(ch + 1) * 128, :], writes=[("wt", ch)])

        pmi = 0
        for b in range(4):
            hb = hT[b % 2]
            hkey = ("hT", b % 2)
            for tl in range(4):
                i = b * 4 + tl
                xb = xt[i % 2]
                sb = st[i % 2]
                xk = ("xt", i % 2)
                sk = ("st", i % 2)
                sc.dma("sp", xb[:], x_src[i * 128:(i + 1) * 128, :], writes=[xk])
                sc.act(junk[:], xb[:], AF.Square, reads=[xk], writes=["junk", sk], accum_out=sb[:, 0:1])
                sc.ts("dve", sb[:, 1:2], sb[:, 0:1], 1.0 / D, EPS, ALU.mult, ALU.add, reads=[sk], writes=[sk])
                sc.act(sb[:, 2:3], sb[:, 1:2], AF.Sqrt, reads=[sk], writes=[sk])
                sc.add("dve", lambda e, o=sb[:, 3:4], i_=sb[:, 2:3]: e.reciprocal(o, i_), reads=[sk], writes=[sk])
                sc.ts("dve", xb[:], xb[:], sb[:, 3:4], None, ALU.mult, reads=[xk, sk], writes=[xk])
                for half in range(2):
                    pt = pT[(i % 2) * 2 + half]
                    pk = ("pT", (i % 2) * 2 + half)
                    for cc in range(4):
                        ch = half * 4 + cc
                        sc.tr(pt[:, cc * 128:(cc + 1) * 128], xb[:, ch * 128:(ch + 1) * 128], ident[:],
                              reads=[xk, "ident"], writes=[pk])
                    sc.tt("dve", hb[:, half * 4:(half + 1) * 4, tl * 128:(tl + 1) * 128],
                          pt[:].rearrange("p (c t) -> p c t", c=4),
                          g[:, half * 4:(half + 1) * 4].unsqueeze(2).broadcast_to([128, 4, 128]), ALU.mult,
                          reads=[pk, "g"], writes=[(hkey, tl)])
            for tl in range(4):
                i = b * 4 + tl
                ob = og[i % 2]
                ok = ("og", i % 2)
                for n in range(5):
                    n0 = n * 512
                    nw = min(512, D_IN - n0)
                    pmm = pm[pmi % 4]
                    pk = ("pm", pmi % 4)
                    pmi += 1
                    for ch in range(8):
                        sc.mm(pmm[:, 0:nw], hb[:, ch, tl * 128:(tl + 1) * 128], wt[:, ch, n0:n0 + nw],
                              ch == 0, ch == 7, reads=[(hkey, tl), ("wt", ch)], writes=[pk])
                    eng = "act" if n % 2 == 0 else "dve"
                    sc.copy(eng, ob[:, n0:n0 + nw], pmm[:, 0:nw], reads=[pk], writes=[ok])
                sc.dma("pool", c.p_tm[i * 128:(i + 1) * 128, :], ob[:], reads=[ok])
            for j in range(8):
                c0 = 768 + j * 128
                pmm = pm[pmi % 4]
                pk = ("pm", pmi % 4)
                pmi += 1
                ob = of[j % 2]
                ok = ("of", j % 2)
                for ch in range(8):
                    sc.mm(pmm[:, :], wt[:, ch, c0:c0 + 128], hb[:, ch, :], ch == 0, ch == 7,
                          reads=[(hkey, 0), (hkey, 1), (hkey, 2), (hkey, 3), ("wt", ch)], writes=[pk])
                eng = "act" if j % 2 == 0 else "dve"
                sc.copy(eng, ob[:], pmm[:], reads=[pk], writes=[ok])
                sc.dma("pool", c.p_fm[j * 128:(j + 1) * 128, b * 512:(b + 1) * 512], ob[:], reads=[ok])
        sc.flush()


def phase_attn(nc, sc, c, l):
    with ExitStack() as es:
        T = lambda name, shape, dt=F32: es.enter_context(nc.sbuf_tensor("%s_L%d" % (name, l), shape, dt))
        P = lambda name, shape, dt=F32: es.enter_context(nc.psum_tensor("%s_L%d" % (name, l), shape, dt))
        ident = T("b_id", [128, 128])
        ones = T("b_ones", [128, 128])
        gg = T("b_gg", [128, 640])
        qT = T("b_qT", [128, 4, S], BF16)
        kT = T("b_kT", [128, S], BF16)
        vx = T("b_vx", [128, NT, 2, 65], BF16)
        qkv = [T("b_qkv%d" % i, [128, 768]) for i in range(2)]
        cs = [T("b_cs%d" % i, [128, 2, 320]) for i in range(2)]
        sq = T("b_sq", [128, 640])
        st = T("b_st", [128, 4, 10])
        qn = T("b_qn", [128, 640])
        tmp = T("b_tmp", [128, 4, 320])
        qr = T("b_qr", [128, 640])
        PT = [T("b_PT%d" % i, [128, 512], BF16) for i in range(3)]
        rd = [T("b_rd%d" % i, [128, 512]) for i in range(2)]
        bcs = [T("b_bcs%d" % i, [64, 512]) for i in range(2)]
        yo = [T("b_yo%d" % i, [64, 512]) for i in range(2)]
        ptA = [P("b_ptA%d" % i, [128, 512]) for i in range(1)]
        ptB = [P("b_ptB%d" % i, [128, 512]) for i in range(1)]
        sps = [P("b_sps%d" % i, [128, 512]) for i in range(3)]
        ops_ = [P("b_ops%d" % i, [128, 512]) for i in range(2)]
        bcp = P("b_bcp", [128, 512])

        sc.dma("sp", ident[:], c.ident, writes=["ident"])
        sc.dma("sp", ones[:], c.ones, writes=["ones"])
        sc.dma("sp", gg[:], c.ggrep[l], writes=["gg"])
        for i in range(NT):
            qb_ = qkv[i % 2]; qk = ("qkv", i % 2)
            cb = cs[i % 2]; ck = ("cs", i % 2)
            for half in range(2):
                sc.dma("sp", qb_[:, 0:512].rearrange("p (pr half d) -> p pr half d", pr=4, half=2)[:, :, half, :],
                       c.p_tm[i * 128:(i + 1) * 128, half * 256:(half + 1) * 256].rearrange("t (pr d) -> t pr d", pr=4),
                       writes=[qk])
            sc.dma("sp", qb_[:, 512:768], c.p_tm[i * 128:(i + 1) * 128, 512:768], writes=[qk])
            sc.dma("sp", cb[:, 0, :], c.cos_rep[i * 128:(i + 1) * 128, :], writes=[ck])
            sc.dma("sp", cb[:, 1, :], c.sin_rep[i * 128:(i + 1) * 128, :], writes=[ck])
            sc.tt("pool", sq[:], qb_[:, 0:640], qb_[:, 0:640], ALU.mult, reads=[qk], writes=["sq"])
            sc.add("dve", lambda e, o=st[:, 0, :], i_=sq[:].rearrange("p (h d) -> p h d", d=64):
                   e.tensor_reduce(o, i_, AX.X, ALU.add), reads=["sq"], writes=["st"])
            sc.ts("dve", st[:, 1, :], st[:, 0, :], 1.0 / 64, EPS, ALU.mult, ALU.add, reads=["st"], writes=["st"])
            sc.act(st[:, 2, :], st[:, 1, :], AF.Sqrt, reads=["st"], writes=["st"])
            sc.add("dve", lambda e, o=st[:, 3, :], i_=st[:, 2, :]: e.reciprocal(o, i_), reads=["st"], writes=["st"])
            sc.tt("dve", qn[:].rearrange("p (h d) -> p h d", d=64), qb_[:, 0:640].rearrange("p (h d) -> p h d", d=64),
                  st[:, 3, :].unsqueeze(2).broadcast_to([128, 10, 64]), ALU.mult, reads=[qk, "st"], writes=["qn"])
            sc.tt("pool", qn[:], qn[:], gg[:], ALU.mult, reads=["qn", "gg"], writes=["qn"])
            q3 = qn[:].rearrange("p (h d) -> p h d", d=64)
            x1 = q3[:, :, 0:32]; x2 = q3[:, :, 32:64]
            co = cb[:, 0, :].rearrange("p (h d) -> p h d", d=32); si = cb[:, 1, :].rearrange("p (h d) -> p h d", d=32)
            t = [tmp[:, k, :].rearrange("p (h d) -> p h d", d=32) for k in range(4)]
            r3 = qr[:].rearrange("p (h d) -> p h d", d=64)
            sc.tt("dve", t[0], x1, co, ALU.mult, reads=["qn", ck], writes=[("tmp", 0)])
            sc.tt("pool", t[1], x2, si, ALU.mult, reads=["qn", ck], writes=[("tmp", 1)])
            sc.tt("dve", t[2], x2, co, ALU.mult, reads=["qn", ck], writes=[("tmp", 2)])
            sc.tt("pool", t[3], x1, si, ALU.mult, reads=["qn", ck], writes=[("tmp", 3)])
            sc.tt("dve", r3[:, :, 0:32], t[0], t[1], ALU.subtract, reads=[("tmp", 0), ("tmp", 1)], writes=["qr"])
            sc.tt("pool", r3[:, :, 32:64], t[2], t[3], ALU.add, reads=[("tmp", 2), ("tmp", 3)], writes=["qr"])
            for pr in range(4):
                sc.tr(ptA[0][:, pr * 128:(pr + 1) * 128], qr[:, pr * 128:(pr + 1) * 128], ident[:], reads=["qr", "ident"], writes=["ptA"])
            sc.tr(ptB[0][:, 0:128], qr[:, 512:640], ident[:], reads=["qr", "ident"], writes=["ptB"])
            sc.copy("act", qT[:, :, i * 128:(i + 1) * 128], ptA[0][:].rearrange("p (c t) -> p c t", c=4),
                    reads=["ptA"], writes=["qT"])
            sc.copy("dve", kT[:, i * 128:(i + 1) * 128], ptB[0][:, 0:128], reads=["ptB"], writes=["kT"])
            sc.copy("act", vx[:, i, :, 0:64], qb_[:, 640:768].rearrange("p (g d) -> p g d", d=64),
                    reads=[qk], writes=["vx"])
            sc.ts("pool", vx[:, i, :, 64:65], qb_[:, 0:2].unsqueeze(2), 0.0, 1.0, ALU.mult, ALU.add,
                  reads=[qk], writes=["vx"])

        steps = [(h, qb, kt) for h in range(8) for qb in range(4) for kt in range(NT)]
        def fin(gi, h, qb):
            O = ops_[gi % 2]; ok = ("O", gi % 2)
            r = rd[gi % 2]; rk = ("rd", gi % 2)
            sc.act(r[64:65, :], O[64:65, :], AF.Ln, reads=[ok], writes=[rk])
            sc.act(r[64:65, :], r[64:65, :], AF.Exp, reads=[rk], writes=[rk], scale=-1.0)
            sc.mm(bcp[0:64, :], ones[64:65, 0:64], r[64:65, :], True, True, reads=[rk, "ones"], writes=["bcp"])
            sc.copy("act", bcs[gi % 2][:], bcp[0:64, :], reads=["bcp"], writes=[("bcs", gi % 2)])
            sc.tt("dve", yo[gi % 2][:], O[0:64, :], bcs[gi % 2][:], ALU.mult, reads=[ok, ("bcs", gi % 2)],
                  writes=[("yo", gi % 2)])
            sc.dma("pool", c.yT[h * 64:(h + 1) * 64, qb * 512:(qb + 1) * 512], yo[gi % 2][:], reads=[("yo", gi % 2)])
        for s_ in range(len(steps) + 1):
            if s_ < len(steps):
                h, qb, kt = steps[s_]
                pr = h % 4; half = h // 4
                p0 = half * 64
                sp_ = sps[s_ % 3]; sk = ("sps", s_ % 3)
                sc.mm(sp_[:, :], kT[p0:p0 + 64, kt * 128:(kt + 1) * 128],
                      qT[p0:p0 + 64, pr, qb * 512:(qb + 1) * 512], True, True,
                      reads=["kT", "qT"], writes=[sk])
                sc.act(PT[s_ % 3][:], sp_[:], AF.Exp, reads=[sk], writes=[("PT", s_ % 3)], scale=0.125)
            if s_ >= 1:
                h, qb, kt = steps[s_ - 1]
                gi = (s_ - 1) // NT
                half = h // 4
                O = ops_[gi % 2]; ok = ("O", gi % 2)
                sc.mm(O[0:65, :], vx[:, kt, half, :], PT[(s_ - 1) % 3][:], kt == 0, kt == NT - 1,
                      reads=["vx", ("PT", (s_ - 1) % 3)], writes=[ok])
                if kt == NT - 1:
                    fin(gi, h, qb)
        sc.flush()


def phase_lru(nc, sc, c, l):
    with ExitStack() as es:
        T = lambda name, shape, dt=F32: es.enter_context(nc.sbuf_tensor("%s_L%d" % (name, l), shape, dt))
        P = lambda name, shape, dt=F32: es.enter_context(nc.psum_tensor("%s_L%d" % (name, l), shape, dt))
        sm = T("c_sm", [128, 2, 11])
        W = T("c_W", [128, 8, 128])
        X = T("c_X", [128, S]); G = T("c_G", [128, S]); XF = T("c_XF", [128, S]); XC = T("c_XC", [128, S])
        R = T("c_R", [128, S]); I_ = T("c_I", [128, S]); A = T("c_A", [128, S]); U = T("c_U", [128, S])
        H = [T("c_H%d" % i, [128, S]) for i in range(2)]
        cf = T("c_cf", [128, 8])
        pp = [P("c_pp%d" % i, [128, 512]) for i in range(4)]
        sc.dma("sp", sm[:], c.lru_small[l], writes=["sm"])
        sc.dma("sp", r32(W[:]), r32(c.lru_w[l]), writes=["W"])
        ppi = 0
        for cc in range(2):
            sc.dma("sp", X[:], c.p_fm[cc * 128:(cc + 1) * 128, :], writes=["X"])
            sc.dma("sp", G[:], c.p_fm[256 + cc * 128:256 + (cc + 1) * 128, :], writes=["G"])
            w = lambda j: sm[:, cc, j:j + 1]
            sc.ts("dve", XF[:], X[:], w(2), w(4), ALU.mult, ALU.add, reads=["X", "sm"], writes=["XF"])
            sc.stt("dve", XF[:, 2:S], X[:, 0:S - 2], w(0), XF[:, 2:S], ALU.mult, ALU.add, reads=["X", "XF", "sm"], writes=["XF"])
            sc.stt("dve", XF[:, 1:S], X[:, 0:S - 1], w(1), XF[:, 1:S], ALU.mult, ALU.add, reads=["X", "XF", "sm"], writes=["XF"])
            sc.copy("pool", r32(XC[:, S - 1:S]), XF[:, S - 1:S], reads=["XF"], writes=["XC"])
            sc.stt("dve", r32(XC[:, 0:S - 1]), X[:, 1:S], w(3), XF[:, 0:S - 1], ALU.mult, ALU.add, reads=["X", "XF", "sm"], writes=["XC"])
            for d in range(2):
                ba = sm[:, cc, 5 + d:6 + d]; bx = sm[:, cc, 7 + d:8 + d]; lam = sm[:, cc, 9 + d:10 + d]
                ck = ("cf", d)
                c0 = cf[:, 4 * d:4 * d + 1]; c1 = cf[:, 4 * d + 1:4 * d + 2]; c2 = cf[:, 4 * d + 2:4 * d + 3]
                sc.act(c0, lam, AF.Exp, reads=["sm"], writes=[ck], scale=-1.0)
                sc.act(c0, c0, AF.Ln, reads=[ck], writes=[ck], bias=1.0)
                sc.ts("dve", c1, c0, -8.0, None, ALU.mult, reads=[ck], writes=[ck])
                sc.ts("dve", c2, c0, -16.0, None, ALU.mult, reads=[ck], writes=[ck])
                for tb in range(4):
                    sl = slice(tb * 512, (tb + 1) * 512)
                    pa = pp[ppi % 4]; pak = ("pp", ppi % 4); ppi += 1
                    px = pp[ppi % 4]; pxk = ("pp", ppi % 4); ppi += 1
                    sc.mm(pa[:, :], r32(W[:, d * 4 + 0 * 2 + cc, :]), r32(XC[:, sl]), True, True, reads=["W", "XC"], writes=[pak])
                    sc.mm(px[:, :], r32(W[:, d * 4 + 1 * 2 + cc, :]), r32(XC[:, sl]), True, True, reads=["W", "XC"], writes=[pxk])
                    sc.act(R[:, sl], pa[:, :], AF.Sigmoid, reads=[pak, "sm"], writes=["R"], bias=ba)
                    sc.act(I_[:, sl], px[:, :], AF.Sigmoid, reads=[pxk, "sm"], writes=["I"], bias=bx)
                sc.act(A[:], R[:], AF.Exp, reads=["R", ck], writes=["A"], scale=c1)
                sc.act(U[:], R[:], AF.Exp, reads=["R", ck], writes=["U"], scale=c2)
                sc.ts("pool", U[:], U[:], -1.0, 1.0, ALU.mult, ALU.add, reads=["U"], writes=["U"])
                sc.act(U[:], U[:], AF.Sqrt, reads=["U"], writes=["U"])
                sc.tt("pool", I_[:], I_[:], XC[:], ALU.mult, reads=["I", "XC"], writes=["I"])
                sc.tt("dve", U[:], U[:], I_[:], ALU.mult, reads=["U", "I"], writes=["U"])
                if d == 0:
                    sc.add("dve", lambda e, o=H[0][:], a=A[:], u=U[:]: e.tensor_tensor_scan(o, a, u, 0.0, ALU.mult, ALU.add),
                           reads=["A", "U"], writes=[("H", 0)])
                else:
                    sc.add("dve", lambda e, o=H[1][:, ::-1], a=A[:, ::-1], u=U[:, ::-1]: e.tensor_tensor_scan(o, a, u, 0.0, ALU.mult, ALU.add),
                           reads=["A", "U"], writes=[("H", 1)])
            sc.tt("pool", H[0][:], H[0][:], H[1][:], ALU.add, reads=[("H", 0), ("H", 1)], writes=[("H", 0)])
            sc.tt("dve", R[:], G[:], G[:], ALU.mult, reads=["G", "R"], writes=["R"])
            sc.ts("dve", R[:], R[:], 0.044715, 1.0, ALU.mult, ALU.add, reads=["R"], writes=["R"])
            sc.tt("dve", R[:], R[:], G[:], ALU.mult, reads=["R", "G"], writes=["R"])
            sc.act(R[:], R[:], AF.Sigmoid, reads=["R"], writes=["R"], scale=1.5957691216057308)
            sc.tt("pool", H[0][:], H[0][:], G[:], ALU.mult, reads=[("H", 0), "G"], writes=[("H", 0)])
            sc.tt("dve", H[0][:], H[0][:], R[:], ALU.mult, reads=[("H", 0), "R"], writes=[("H", 0)])
            sc.dma("pool", c.yT[512 + cc * 128:512 + (cc + 1) * 128, :], H[0][:], reads=[("H", 0)])
        sc.flush()


def phase_mlstm(nc, sc, c, l):
    with ExitStack() as es:
        T = lambda name, shape, dt=F32: es.enter_context(nc.sbuf_tensor("%s_L%d" % (name, l), shape, dt))
        P = lambda name, shape, dt=F32: es.enter_context(nc.psum_tensor("%s_L%d" % (name, l), shape, dt))
        ident = T("d_id", [128, 128]); ones = T("d_ones", [128, 128])
        mask = T("d_mask", [128, 2, 128])
        fb = T("d_fb", [128, 8]); ng = T("d_ng", [128, 256])
        qT = T("d_qT", [128, 2, S]); kT = T("d_kT", [128, 2, S])
        tm = T("d_tm", [128, NT, 784])
        vp = T("d_vp", [128, NT, 8, 65])
        wA = T("d_wA", [128, NT, 8]); wB = T("d_wB", [128, NT, 8]); eg = T("d_eg", [128, NT, 8])
        nlf = T("d_nlf", [128, NT, 8]); tg = T("d_tg", [128, NT, 8])
        hs = T("d_hs", [128, NT, 256])
        Cst = T("d_C", [128, 2, 4, 65])
        tC = T("d_tC", [128, 65])
        PT = [T("d_PT%d" % i, [128, 128]) for i in range(3)]
        s4 = [T("d_s4%d" % i, [128, 4, 4]) for i in range(2)]
        sq = T("d_sq", [128, 256]); st = T("d_st", [128, 4, 4]); ym = [T("d_ym%d" % i, [128, 256]) for i in range(2)]
        yo = [T("d_yo%d" % i, [128, 256]) for i in range(2)]
        sg = T("d_sg", [128, 256])
        pg = P("d_pg", [128, 512])
        psS = [P("d_pS%d" % i, [128, 512]) for i in range(2)]
        pacc = [P("d_pa%d" % i, [128, 512]) for i in range(2)]
        pkv = [P("d_pk%d" % i, [128, 512]) for i in range(2)]
        ptr = P("d_ptr", [128, 512])

        sc.dma("sp", ident[:], c.ident, writes=["ident"])
        sc.dma("sp", ones[:], c.ones, writes=["ones"])
        sc.dma("sp", mask[:, 0, :], c.triu, writes=["mask"])
        sc.dma("sp", mask[:, 1, :], c.tril, writes=["mask"])
        sc.dma("sp", fb[:], c.fbrep[l], writes=["fb"])
        sc.dma("sp", ng[:], c.ngrep[l], writes=["ng"])
        for ch in range(2):
            sc.dma("sp", qT[:, ch, :], c.p_fm[512 + ch * 128:512 + (ch + 1) * 128, :], writes=["qT"])
            sc.dma("sp", kT[:, ch, :], c.p_fm[768 + ch * 128:768 + (ch + 1) * 128, :], writes=["kT"])
        for i in range(NT):
            sc.dma("sp", tm[:, i, :], c.p_tm[i * 128:(i + 1) * 128, 1536:2320], writes=[("tm", i)])
        sc.add("pool", lambda e: e.memset(Cst[:], 0.0), writes=["C"])
        for i in range(NT):
            gt = tm[:, i, 768:784]
            fview = gt.rearrange("p (d k h) -> p d k h", d=2, k=2)[:, :, 1, :]
            iview = gt.rearrange("p (d k h) -> p d k h", d=2, k=2)[:, :, 0, :]
            tk = ("tg", i)
            sc.tt("dve", tg[:, i, :].rearrange("p (d h) -> p d h", d=2), fview, fb[:].rearrange("p (d h) -> p d h", d=2),
                  ALU.add, reads=[("tm", i), "fb"], writes=[tk])
            sc.act(tg[:, i, :], tg[:, i, :], AF.Exp, reads=[tk], writes=[tk], scale=-1.0)
            sc.act(nlf[:, i, :], tg[:, i, :], AF.Ln, reads=[tk], writes=[("nlf", i)], bias=1.0)
            sc.mm(pg[:, 0:4], mask[:, 0, :], nlf[:, i, 0:4], True, True, reads=["mask", ("nlf", i)], writes=["pg"])
            sc.mm(pg[:, 4:8], mask[:, 1, :], nlf[:, i, 4:8], True, True, reads=["mask", ("nlf", i)], writes=["pg"])
            sc.mm(pg[:, 8:16], ones[:, :], nlf[:, i, :], True, True, reads=["ones", ("nlf", i)], writes=["pg"])
            sc.act(wA[:, i, :], pg[:, 0:8], AF.Exp, reads=["pg"], writes=[("wA", i)], scale=-1.0)
            sc.act(eg[:, i, :], pg[:, 8:16], AF.Exp, reads=["pg"], writes=[("eg", i)], scale=-1.0)
            sc.tt("dve", wB[:, i, :].rearrange("p (d h) -> p d h", d=2), iview, pg[:, 0:8].rearrange("p (d h) -> p d h", d=2),
                  ALU.add, reads=[("tm", i), "pg"], writes=[("wB", i)])
            sc.act(wB[:, i, :], wB[:, i, :], AF.Exp, reads=[("wB", i)], writes=[("wB", i)])
            v3 = tm[:, i, 256:512].rearrange("p (h d) -> p h d", d=64)
            for d in range(2):
                eng = "dve" if d == 0 else "pool"
                sc.tt(eng, vp[:, i, d * 4:(d + 1) * 4, 0:64], v3, wB[:, i, d * 4:(d + 1) * 4].unsqueeze(2).broadcast_to([128, 4, 64]),
                      ALU.mult, reads=[("tm", i), ("wB", i)], writes=[("vp", i)])
            sc.copy("pool", vp[:, i, :, 64:65], wB[:, i, :].unsqueeze(2), reads=[("wB", i)], writes=[("vp", i)])
        ui = 0
        for s_ in range(NT):
            for d in range(2):
                i = s_ if d == 0 else NT - 1 - s_
                tsl = slice(i * 128, (i + 1) * 128)
                acc = pacc[d]; ak = ("pacc", d)
                for h in range(4):
                    p0 = (h % 2) * 64; ch = h // 2
                    pS = psS[ui % 2]; pk_ = ("pS", ui % 2)
                    pt = PT[ui % 3]; ptk = ("PT", ui % 3)
                    kv = pkv[ui % 2]; kvk = ("pkv", ui % 2)
                    ui += 1
                    sc.mm(pS[:, 0:128], kT[p0:p0 + 64, ch, tsl], qT[p0:p0 + 64, ch, tsl], True, True,
                          reads=["kT", "qT"], writes=[pk_])
                    sc.stt("dve", pt[:], pS[:, 0:128], 0.125, mask[:, d, :], ALU.mult, ALU.mult,
                           reads=[pk_, "mask"], writes=[ptk])
                    sc.mm(acc[:, h * 65:(h + 1) * 65], pt[:], vp[:, i, d * 4 + h, :], True, False,
                          reads=[ptk, ("vp", i)], writes=[ak])
                    sc.mm(acc[:, h * 65:(h + 1) * 65], qT[p0:p0 + 64, ch, tsl], Cst[p0:p0 + 64, d, h, :], False, True,
                          reads=["qT", ("C", d, h)], writes=[ak])
                    if s_ < NT - 1:
                        sc.mm(kv[p0:p0 + 64, 0:65], tm[:, i, h * 64:(h + 1) * 64], vp[:, i, d * 4 + h, :], True, True,
                              reads=[("tm", i), ("vp", i)], writes=[kvk])
                        sc.stt("dve", tC[p0:p0 + 64, :], kv[p0:p0 + 64, 0:65], 0.125, Cst[p0:p0 + 64, d, h, :], ALU.mult, ALU.add,
                               reads=[kvk, ("C", d, h)], writes=["tC"])
                        sc.ts("dve", Cst[p0:p0 + 64, d, h, :], tC[p0:p0 + 64, :], eg[p0:p0 + 64, i, d * 4 + h:d * 4 + h + 1], None, ALU.mult,
                              reads=["tC", ("eg", i)], writes=[("C", d, h)])
                a3 = acc[:, 0:260].rearrange("p (h e) -> p h e", e=65)
                sb = s4[d]; sk = ("s4", d)
                sc.tt("dve", sb[:, 0, :], a3[:, :, 64], wA[:, i, d * 4:(d + 1) * 4], ALU.mult, reads=[ak, ("wA", i)], writes=[sk])
                sc.act(sb[:, 1, :], sb[:, 0, :], AF.Abs, reads=[sk], writes=[sk])
                sc.ts("dve", sb[:, 1, :], sb[:, 1, :], 1.0, None, ALU.max, reads=[sk], writes=[sk])
                sc.add("dve", lambda e, o=sb[:, 2, :], i_=sb[:, 1, :]: e.reciprocal(o, i_), reads=[sk], writes=[sk])
                sc.tt("dve", sb[:, 3, :], sb[:, 2, :], wA[:, i, d * 4:(d + 1) * 4], ALU.mult, reads=[sk, ("wA", i)], writes=[sk])
                h3 = hs[:, i, :].rearrange("p (h e) -> p h e", e=64)
                if s_ < NT // 2:
                    sc.tt("dve", h3, a3[:, :, 0:64], sb[:, 3, :].unsqueeze(2).broadcast_to([128, 4, 64]), ALU.mult,
                          reads=[ak, sk], writes=[("hs", i)])
                else:
                    sc.tt("dve", sq[:].rearrange("p (h e) -> p h e", e=64), a3[:, :, 0:64],
                          sb[:, 3, :].unsqueeze(2).broadcast_to([128, 4, 64]), ALU.mult, reads=[ak, sk], writes=["sq"])
                    sc.tt("pool", hs[:, i, :], hs[:, i, :], sq[:], ALU.add, reads=[("hs", i), "sq"], writes=[("hs", i)])
        for i in range(NT):
            y = ym[i % 2]; yk = ("ym", i % 2)
            sc.tt("pool", sq[:], hs[:, i, :], hs[:, i, :], ALU.mult, reads=[("hs", i)], writes=["sq"])
            sc.add("dve", lambda e, o=st[:, 0, :], i_=sq[:].rearrange("p (h d) -> p h d", d=64):
                   e.tensor_reduce(o, i_, AX.X, ALU.add), reads=["sq"], writes=["st"])
            sc.ts("dve", st[:, 1, :], st[:, 0, :], 1.0 / 64, EPS, ALU.mult, ALU.add, reads=["st"], writes=["st"])
            sc.act(st[:, 2, :], st[:, 1, :], AF.Sqrt, reads=["st"], writes=["st"])
            sc.add("dve", lambda e, o=st[:, 3, :], i_=st[:, 2, :]: e.reciprocal(o, i_), reads=["st"], writes=["st"])
            sc.tt("dve", y[:].rearrange("p (h d) -> p h d", d=64), hs[:, i, :].rearrange("p (h d) -> p h d", d=64),
                  st[:, 3, :].unsqueeze(2).broadcast_to([128, 4, 64]), ALU.mult, reads=[("hs", i), "st"], writes=[yk])
            sc.tt("pool", y[:], y[:], ng[:], ALU.mult, reads=[yk, "ng"], writes=[yk])
            sc.act(sg[:], tm[:, i, 512:768], AF.Sigmoid, reads=[("tm", i)], writes=["sg"])
            sc.tt("dve", y[:], y[:], sg[:], ALU.mult, reads=[yk, "sg"], writes=[yk])
            for ch in range(2):
                sc.tr(ptr[:, ch * 128:(ch + 1) * 128], y[:, ch * 128:(ch + 1) * 128], ident[:], reads=[yk, "ident"], writes=["ptr"])
            o = yo[i % 2]; ok = ("yo", i % 2)
            sc.copy("act", o[:], ptr[:, 0:256], reads=["ptr"], writes=[ok])
            sc.dma("pool", c.yT[768:1024, i * 128:(i + 1) * 128].rearrange("(ch p) t -> p ch t", p=128),
                   o[:].rearrange("p (ch t) -> p ch t", ch=2), reads=[ok])
        sc.flush()


BF16 = mybir.dt.bfloat16
F16 = mybir.dt.float16
NE = 16
CAP = 256
WQ = "pool"


def phase_outproj(nc, sc, c, l, x_src):
    with ExitStack() as es:
        T = lambda name, shape, dt=F32: es.enter_context(nc.sbuf_tensor("%s_L%d" % (name, l), shape, dt))
        P = lambda name, shape, dt=F32: es.enter_context(nc.psum_tensor("%s_L%d" % (name, l), shape, dt))
        wt = T("e_wt", [128, 8, D], BF16); yh = [T("e_yh%d" % i, [128, 8, 512], BF16) for i in range(2)]; g = T("e_g", [128, 8]); ones = T("e_ones", [128, 128])
        yb = [T("e_yb%d" % i, [128, 8, 512]) for i in range(2)]
        ysq = T("e_ysq", [128, 6, 512])
        xt = [T("e_xt%d" % i, [128, D]) for i in range(2)]
        st = [T("e_st%d" % i, [128, 8]) for i in range(2)]
        pm = [P("e_pm%d" % i, [128, 512]) for i in range(6)]
        pst = P("e_pst", [128, 512])
        sc.dma("sp", ones[:], c.ones, writes=["ones"])
        sc.dma("sp", g[:], c.gout[l], writes=["g"])
        for ch in range(8):
            sc.dma("pool", wt[:, ch, :], c.w_out[l][ch * 128:(ch + 1) * 128, :], writes=[("wt", ch)])
        pi = 0
        for b in range(4):
            y = yb[b % 2]; ykey = ("yb", b % 2)
            for ch in range(8):
                sc.dma("sp", y[:, ch, :], c.yT[ch * 128:(ch + 1) * 128, b * 512:(b + 1) * 512], writes=[ykey])
            sc.act(ysq[:], y[:, 0:6, :], AF.Square, reads=[ykey], writes=["ysq"])
            yhb = yh[b % 2]; yhk = ("yh", b % 2)
            for ch in range(8):
                sc.act(yhb[:, ch, :], y[:, ch, :], AF.Copy, reads=[ykey, "g"], writes=[yhk], scale=g[:, ch:ch + 1])
            for tl in range(4):
                i = b * 4 + tl
                tsl = slice(tl * 128, (tl + 1) * 128)
                x = xt[i % 2]; xk = ("xt", i % 2); sb = st[i % 2]; sk = ("st", i % 2)
                sc.dma("sp", x[:], x_src[i * 128:(i + 1) * 128, :], writes=[xk])
                for ch in range(6):
                    col = 0 if ch < 4 else 1
                    sc.mm(pst[:, col:col + 1], ysq[:, ch, tsl], ones[:, 0:1], ch in (0, 4), ch in (3, 5), reads=["ysq", "ones"], writes=["pst"])
                sc.ts("dve", sb[:, 0:1], pst[:, 0:1], 1.0 / 512, EPS, ALU.mult, ALU.add, reads=["pst"], writes=[sk])
                sc.ts("dve", sb[:, 1:2], pst[:, 1:2], 1.0 / 256, EPS, ALU.mult, ALU.add, reads=["pst"], writes=[sk])
                sc.act(sb[:, 2:4], sb[:, 0:2], AF.Sqrt, reads=[sk], writes=[sk])
                sc.add("dve", lambda e, o=sb[:, 4:6], i_=sb[:, 2:4]: e.reciprocal(o, i_), reads=[sk], writes=[sk])
                for n in range(2):
                    nsl = slice(n * 512, (n + 1) * 512)
                    ps = []
                    for (c0, c1) in ((0, 4), (4, 6), (6, 8)):
                        p_ = pm[pi % 6]; pk = ("pm", pi % 6); pi += 1
                        for ch in range(c0, c1):
                            sc.mm(p_[:, :], yhb[:, ch, tsl], wt[:, ch, nsl], ch == c0, ch == c1 - 1,
                                  reads=[yhk, ("wt", ch)], writes=[pk])
                        ps.append((p_, pk))
                    sc.stt("dve", x[:, nsl], ps[0][0][:, :], sb[:, 4:5], x[:, nsl], ALU.mult, ALU.add, reads=[ps[0][1], sk, xk], writes=[xk])
                    sc.stt("dve", x[:, nsl], ps[1][0][:, :], sb[:, 5:6], x[:, nsl], ALU.mult, ALU.add, reads=[ps[1][1], sk, xk], writes=[xk])
                    sc.tt("dve", x[:, nsl], x[:, nsl], ps[2][0][:, :], ALU.add, reads=[ps[2][1], xk], writes=[xk])
                sc.dma("pool", c.out[i * 128:(i + 1) * 128, :], x[:], reads=[xk])
        sc.flush()


def phase_router(nc, sc, c, l):
    with ExitStack() as es:
        T = lambda name, shape, dt=F32: es.enter_context(nc.sbuf_tensor("%s_L%d" % (name, l), shape, dt))
        P = lambda name, shape, dt=F32: es.enter_context(nc.psum_tensor("%s_L%d" % (name, l), shape, dt))
        ident = T("f_id", [128, 128]); g2 = T("f_g2", [128, 8]); wr = T("f_wr", [128, 8, NE])
        xt = [T("f_xt%d" % i, [128, D]) for i in range(2)]
        junk = T("f_junk", [128, D])
        hb = [T("f_hb%d" % i, [128, D], BF16) for i in range(2)]
        hT = [T("f_hT%d" % i, [128, 8, 128]) for i in range(2)]
        st = [T("f_st%d" % i, [128, 8]) for i in range(2)]
        aff = T("f_aff", [128, NT, NE]); ex = T("f_ex", [128, NE])
        affT = T("f_affT", [NE, S]); work = T("f_work", [NE, S])
        gem = T("f_gem", [NE, CAP]); iem = T("f_iem", [NE, CAP], U32); ief = T("f_ief", [NE, CAP])
        idxs = T("f_idxs", [128, 2, NE], I32); gts = T("f_gts", [128, 2, NE])
        pT = [P("f_pT%d" % i, [128, 512]) for i in range(4)]
        pl = [P("f_pl%d" % i, [128, 512]) for i in range(2)]
        pa = [P("f_pa%d" % i, [128, 512]) for i in range(2)]
        sc.dma("sp", ident[:], c.ident, writes=["ident"])
        sc.dma("sp", g2[:], c.g2rep[l], writes=["g2"])
        sc.dma("sp", wr[:], c.w_router[l].rearrange("(c p) e -> p c e", p=128), writes=["wr"])
        sc.tt("dve", wr[:], wr[:], g2[:].unsqueeze(2).broadcast_to([128, 8, NE]), ALU.mult, reads=["wr", "g2"], writes=["wr"])
        for i in range(NT):
            x = xt[i % 2]; xk = ("xt", i % 2); sb = st[i % 2]; sk = ("st", i % 2)
            sc.dma("sp", x[:], c.out[i * 128:(i + 1) * 128, :], writes=[xk])
            sc.act(junk[:], x[:], AF.Square, reads=[xk], writes=["junk", sk], accum_out=sb[:, 0:1])
            sc.ts("dve", sb[:, 1:2], sb[:, 0:1], 1.0 / D, EPS, ALU.mult, ALU.add, reads=[sk], writes=[sk])
            sc.act(sb[:, 2:3], sb[:, 1:2], AF.Sqrt, reads=[sk], writes=[sk])
            sc.add("dve", lambda e, o=sb[:, 3:4], i_=sb[:, 2:3]: e.reciprocal(o, i_), reads=[sk], writes=[sk])
            sc.ts("dve", x[:], x[:], sb[:, 3:4], None, ALU.mult, reads=[xk, sk], writes=[xk])
            sc.copy("pool", hb[i % 2][:], x[:], reads=[xk], writes=[("hb", i % 2)])
            sc.dma("pool", c.h2n[i * 128:(i + 1) * 128, :], hb[i % 2][:], reads=[("hb", i % 2)])
            for half in range(2):
                pt = pT[(i % 2) * 2 + half]; pk = ("pT", (i % 2) * 2 + half)
                for cc in range(4):
                    ch = half * 4 + cc
                    sc.tr(pt[:, cc * 128:(cc + 1) * 128], x[:, ch * 128:(ch + 1) * 128], ident[:], reads=[xk, "ident"], writes=[pk])
                sc.copy("act" if half == 0 else "dve", hT[i % 2][:, half * 4:(half + 1) * 4, :],
                        pt[:].rearrange("p (c t) -> p c t", c=4), reads=[pk], writes=[("hT", i % 2)])
            lg = pl[i % 2]; lk = ("pl", i % 2)
            for ch in range(8):
                sc.mm(lg[:, 0:NE], hT[i % 2][:, ch, :], wr[:, ch, :], ch == 0, ch == 7, reads=[("hT", i % 2), "wr"], writes=[lk])
            sc.add("dve", lambda e, o=sb[:, 4:5], i_=lg[:, 0:NE]: e.tensor_reduce(o, i_, AX.X, ALU.max), reads=[lk], writes=[sk])
            sc.ts("dve", sb[:, 5:6], sb[:, 4:5], -1.0, None, ALU.mult, reads=[sk], writes=[sk])
            sc.act(ex[:], lg[:, 0:NE], AF.Exp, reads=[lk, sk], writes=["ex", sk], bias=sb[:, 5:6], accum_out=sb[:, 6:7])
            sc.add("dve", lambda e, o=sb[:, 7:8], i_=sb[:, 6:7]: e.reciprocal(o, i_), reads=[sk], writes=[sk])
            sc.ts("dve", aff[:, i, :], ex[:], sb[:, 7:8], None, ALU.mult, reads=["ex", sk], writes=[("aff", i)])
            pa_ = pa[i % 2]; pak = ("pa", i % 2)
            sc.tr(pa_[0:NE, 0:128], aff[:, i, :], ident[:], reads=[("aff", i), "ident"], writes=[pak])
            sc.copy("act", affT[:, i * 128:(i + 1) * 128], pa_[0:NE, 0:128], reads=[pak], writes=["affT"])
        sc.copy("dve", work[:], affT[:], reads=["affT"], writes=["work"])
        for r in range(CAP // 8):
            sl8 = slice(r * 8, (r + 1) * 8)
            sc.add("dve", lambda e, o=gem[:, sl8]: e.max(o, work[:]), reads=["work"], writes=["gem"])
            sc.add("dve", lambda e, o=iem[:, sl8], m=gem[:, sl8]: e.max_index(o, m, work[:]), reads=["gem", "work"], writes=["iem"])
            if r < CAP // 8 - 1:
                sc.add("dve", lambda e, m=gem[:, sl8]: e.match_replace(work[:], m, work[:], -1e30), reads=["gem", "work"], writes=["work"])
        sc.copy("dve", ief[:], iem[:], reads=["iem"], writes=["ief"])
        for ct in range(2):
            pa_ = pa[ct]; pak = ("pa", ct)
            sc.tr(pa_[:, 0:NE], ief[:, ct * 128:(ct + 1) * 128], ident[0:NE, 0:NE], reads=["ief", "ident"], writes=[pak])
            sc.tr(pa_[:, NE:2 * NE], gem[:, ct * 128:(ct + 1) * 128], ident[0:NE, 0:NE], reads=["gem", "ident"], writes=[pak])
            sc.copy("act", idxs[:, ct, :], pa_[:, 0:NE], reads=[pak], writes=["idxs"])
            sc.copy("act", gts[:, ct, :], pa_[:, NE:2 * NE], reads=[pak], writes=["gts"])
        sc.dma("sp", c.idx_d, idxs[:].rearrange("p a b -> p (a b)"), reads=["idxs"])
        sc.dma("sp", c.gate_d, gts[:].rearrange("p a b -> p (a b)"), reads=["gts"])
        sc.flush()


def phase_experts(nc, sc, c, l):
    NWB = 4
    with ExitStack() as es:
        T = lambda name, shape, dt=F32: es.enter_context(nc.sbuf_tensor("%s_L%d" % (name, l), shape, dt))
        P = lambda name, shape, dt=F32: es.enter_context(nc.psum_tensor("%s_L%d" % (name, l), shape, dt))
        identb = T("g_idb", [128, 128], BF16)
        Wg = [T("g_Wg%d" % i, [128, 8, 256], BF16) for i in range(NWB)]
        Wu = [T("g_Wu%d" % i, [128, 8, 256], BF16) for i in range(NWB)]
        Wd = [T("g_Wd%d" % i, [128, 2, D], BF16) for i in range(NWB)]
        xtok = [[T("g_xt%d_%d" % (i, ct), [128, D], BF16) for ct in range(2)] for i in range(2)]
        xg = T("g_xg", [128, 8, CAP], BF16)
        H = [T("g_H%d" % i, [128, 2, CAP], BF16) for i in range(2)]
        sil = [T("g_sil%d" % i, [128, CAP]) for i in range(2)]
        ye = [[T("g_ye%d_%d" % (i, ct), [128, D]) for ct in range(2)] for i in range(2)]
        idxs = T("g_idx", [128, 2 * NE], I32); gts = T("g_gts", [128, 2 * NE])
        g2 = T("g_g2", [128, 8])
        yacc = [P("g_ya%d" % i, [128, 512]) for i in range(4)]
        au = [P("g_au%d" % i, [128, 512]) for i in range(2)]
        tp = [P("g_tp%d" % i, [128, 1024], BF16) for i in range(2)]
        sc.dma("sp", idxs[:], c.idx_d, writes=["idxs"])
        sc.dma("sp", gts[:], c.gate_d, writes=["gts"])
        sc.dma("sp", identb[:], c.identb, writes=["identb"])
        sc.dma("sp", g2[:], c.g2rep[l], writes=["g2"])
        wq = [0]
        def load_w(e, wb):
            b = wq[0] % NWB; wq[0] += 1
            hs_ = slice(wb * 256, (wb + 1) * 256)
            sc.dma("pool", Wg[b][:], c.w_gate[l, e][:, hs_].rearrange("(c p) h -> p c h", p=128), writes=[("Wg", b)])
            sc.dma("pool", Wu[b][:], c.w_up[l, e][:, hs_].rearrange("(c p) h -> p c h", p=128), writes=[("Wu", b)])
            sc.dma("pool", Wd[b][:], c.w_down[l, e][wb * 256:(wb + 1) * 256, :].rearrange("(c p) f -> p c f", p=128), writes=[("Wd", b)])
            return b
        def gather(e):
            for ct in range(2):
                col = ct * NE + e
                sc.add("pool", lambda g, o=xtok[e % 2][ct][:, :], ix=idxs[:, col:col + 1]:
                       g.indirect_dma_start(out=o, out_offset=None, in_=c.h2n[:, :],
                                            in_offset=bass.IndirectOffsetOnAxis(ap=ix, axis=0)),
                       reads=["idxs", "h2n_d"], writes=[("xtok", e % 2, ct)], dma=True)
        seq = [(e, wb) for e in range(NE) for wb in range(8)]
        pend = []
        for k in range(NWB - 1):
            pend.append(load_w(*seq[k]))
        gather(0)
        ai = 0; hi = 0
        for e in range(NE):
            for ct in range(2):
                tpp = tp[ct]; tk = ("tp", ct)
                for fc in range(8):
                    sc.tr(tpp[:, fc * 128:(fc + 1) * 128], xtok[e % 2][ct][:, fc * 128:(fc + 1) * 128], identb[:],
                          reads=[("xtok", e % 2, ct), "identb"], writes=[tk])
                for fc in range(8):
                    sc.act(xg[:, fc, ct * 128:(ct + 1) * 128], tpp[:, fc * 128:(fc + 1) * 128], AF.Copy,
                           reads=[tk, "g2"], writes=[("xg", fc)], scale=g2[:, fc:fc + 1])
            if e + 1 < NE:
                gather(e + 1)
            for wb in range(8):
                b = pend.pop(0)
                si = e * 8 + wb + NWB - 1
                if si < len(seq):
                    pend.append(load_w(*seq[si]))
                hb_ = H[hi % 2]; hk = ("H", hi % 2); hi += 1
                for jj in range(2):
                    p_ = au[ai % 2]; pk = ("au", ai % 2)
                    sl_ = sil[ai % 2]; slk = ("sil", ai % 2); ai += 1
                    for ch in range(8):
                        sc.mm(p_[:, 0:256], Wg[b][:, ch, jj * 128:(jj + 1) * 128], xg[:, ch, :], ch == 0, ch == 7,
                              reads=[("Wg", b), ("xg", ch)], writes=[pk])
                    for ch in range(8):
                        sc.mm(p_[:, 256:512], Wu[b][:, ch, jj * 128:(jj + 1) * 128], xg[:, ch, :], ch == 0, ch == 7,
                              reads=[("Wu", b), ("xg", ch)], writes=[pk])
                    sc.act(sl_[:], p_[:, 0:256], AF.Silu, reads=[pk], writes=[slk])
                    sc.tt("dve", hb_[:, jj, :], sl_[:], p_[:, 256:512], ALU.mult, reads=[slk, pk], writes=[hk])
                for ct in range(2):
                    for fb in range(2):
                        for jj in range(2):
                            sc.mm(yacc[ct * 2 + fb][:, :], hb_[:, jj, ct * 128:(ct + 1) * 128], Wd[b][:, jj, fb * 512:(fb + 1) * 512],
                                  wb == 0 and jj == 0, wb == 7 and jj == 1, reads=[hk, ("Wd", b)], writes=[("ya", ct * 2 + fb)])
            for ct in range(2):
                col = ct * NE + e
                yb_ = ye[e % 2][ct]; yk = ("ye", e % 2, ct)
                for fb in range(2):
                    if fb == 0:
                        sc.act(yb_[:, fb * 512:(fb + 1) * 512], yacc[ct * 2 + fb][:, :], AF.Copy, reads=[("ya", ct * 2 + fb), "gts"],
                               writes=[yk], scale=gts[:, col:col + 1])
                    else:
                        sc.ts("dve", yb_[:, fb * 512:(fb + 1) * 512], yacc[ct * 2 + fb][:, :], gts[:, col:col + 1], None, ALU.mult,
                              reads=[("ya", ct * 2 + fb), "gts"], writes=[yk])
                sc.add("pool", lambda g, i_=yb_[:, :], ix=idxs[:, col:col + 1]:
                       g.indirect_dma_start(out=c.out[:, :], out_offset=bass.IndirectOffsetOnAxis(ap=ix, axis=0),
                                            in_=i_, in_offset=None, compute_op=ALU.add),
                       reads=[yk, "idxs"], writes=["xout"], dma=True)
        sc.flush()


def build(n_layers=DEPTH, debug=None):
    nc = bass.Bass("TRN2", target_bir_lowering=False)
    nc.dge_precook = False
    c = Ctx()
    def inp(name, shape):
        return nc.dram_tensor(name, list(shape), F32, kind="ExternalInput").ap()
    c.x = inp("x", [S, D])
    c.norm1_g = inp("norm1_g", [DEPTH, D])
    c.w_in = inp("w_in", [DEPTH, D, D_IN])
    c.ident = inp("ident", [128, 128])
    c.ones = inp("ones", [128, 128])
    c.cos_rep = inp("cos_rep", [S, 320])
    c.sin_rep = inp("sin_rep", [S, 320])
    c.ggrep = inp("ggrep", [DEPTH, 128, 640])
    c.lru_small = inp("lru_small", [DEPTH, 128, 2, 11])
    c.lru_w = inp("lru_w", [DEPTH, 128, 8, 128])
    c.w_out = inp("w_out", [DEPTH, D, D])
    c.gout = inp("gout", [DEPTH, 128, 8])
    c.g2rep = inp("g2rep", [DEPTH, 128, 8])
    c.w_router = inp("w_router", [DEPTH, D, NE])
    c.w_gate = inp("w_expert_gate", [DEPTH, NE, D, 2 * D])
    c.w_up = inp("w_expert_up", [DEPTH, NE, D, 2 * D])
    c.w_down = inp("w_expert_down", [DEPTH, NE, 2 * D, D])
    c.h2n = nc.dram_tensor("h2n", [S, D], BF16, kind="Internal").ap()
    c.idx_d = nc.dram_tensor("idx_d", [128, 2 * NE], I32, kind="ExternalOutput" if debug == "R" else "Internal").ap()
    c.gate_d = nc.dram_tensor("gate_d", [128, 2 * NE], F32, kind="ExternalOutput" if debug == "R" else "Internal").ap()
    c.identb = nc.dram_tensor("identb", [128, 128], BF16, kind="ExternalInput").ap()
    c.triu = inp("triu", [128, 128])
    c.tril = inp("tril", [128, 128])
    c.fbrep = inp("fbrep", [DEPTH, 128, 8])
    c.ngrep = inp("ngrep", [DEPTH, 128, 256])
    c.yT = nc.dram_tensor("yT", [1024, S], F32, kind="ExternalOutput" if debug in ("B", "C", "D") else "Internal").ap()
    c.out = nc.dram_tensor("out", [S, D], F32, kind="ExternalOutput").ap()
    c.p_tm = nc.dram_tensor("p_tm", [S, D_IN], F32, kind="ExternalOutput" if debug == "A" else "Internal").ap()
    c.p_fm = nc.dram_tensor("p_fm", [1024, S], F32, kind="ExternalOutput" if debug == "A" else "Internal").ap()
    with ExitStack() as es:
        sc = Sched(nc, es)
        for l in range(n_layers):
            phase_inproj(nc, sc, c, l, c.x if l == 0 else c.out)
            if debug == "A":
                continue
            if debug != "C" and debug != "D":
                phase_attn(nc, sc, c, l)
            if debug == "B":
                continue
            if debug != "D":
                phase_lru(nc, sc, c, l)
            if debug == "C":
                continue
            phase_mlstm(nc, sc, c, l)
            if debug == "D":
                continue
            phase_outproj(nc, sc, c, l, c.x if l == 0 else c.out)
            if debug == "E":
                continue
            phase_router(nc, sc, c, l)
            if debug == "R":
                continue
            phase_experts(nc, sc, c, l)
    return nc


def consts():
    t = np.arange(S)
    row = (t // 64).astype(np.float32)
    col = (t % 64).astype(np.float32)
    inv = (1.0 / (np.float32(10000.0) ** (np.arange(16, dtype=np.float32) / np.float32(16)))).astype(np.float32)
    ang = np.concatenate([row[:, None] * inv, col[:, None] * inv], axis=-1).astype(np.float32)
    cos = np.cos(ang).astype(np.float32)
    sin = np.sin(ang).astype(np.float32)
    import ml_dtypes
    return {"identb": np.eye(128, dtype=np.float32).astype(ml_dtypes.bfloat16),
            "ident": np.eye(128, dtype=np.float32), "ones": np.ones((128, 128), np.float32),
            "triu": np.triu(np.ones((128, 128), np.float32)), "tril": np.tril(np.ones((128, 128), np.float32)),
            "cos_rep": np.ascontiguousarray(np.tile(cos, (1, 10))), "sin_rep": np.ascontiguousarray(np.tile(sin, (1, 10)))}


def layout_small(inputs):
    o = {}
    qg = inputs["q_norm_g"]; kg = inputs["k_norm_g"]
    gg = np.concatenate([np.tile(qg, (1, 8)), np.tile(kg, (1, 2))], axis=1)
    o["ggrep"] = np.ascontiguousarray(np.broadcast_to(gg[:, None, :], (DEPTH, 128, 640))).astype(np.float32)
    go = np.concatenate([inputs["att_out_g"], inputs["lru_out_g"], np.ones((DEPTH, 256), np.float32)], axis=1)
    o["gout"] = np.ascontiguousarray(go.reshape(DEPTH, 8, 128).transpose(0, 2, 1)).astype(np.float32)
    o["g2rep"] = np.ascontiguousarray(inputs["norm2_g"].reshape(DEPTH, 8, 128).transpose(0, 2, 1)).astype(np.float32)
    o["fbrep"] = np.ascontiguousarray(np.broadcast_to(inputs["mlstm_f_bias"].reshape(DEPTH, 1, 8), (DEPTH, 128, 8))).astype(np.float32)
    o["ngrep"] = np.ascontiguousarray(np.broadcast_to(inputs["mlstm_norm_g"].reshape(DEPTH, 1, 256), (DEPTH, 128, 256))).astype(np.float32)
    cols = [inputs["conv_w"][:, j, :] for j in range(4)] + [inputs["conv_b"]]
    cols += [inputs["lru_ba"][:, d, :] for d in range(2)] + [inputs["lru_bx"][:, d, :] for d in range(2)]
    cols += [inputs["lru_lambda"][:, d, :] for d in range(2)]
    sm = np.stack(cols, axis=-1)
    o["lru_small"] = np.ascontiguousarray(sm.reshape(DEPTH, 2, 128, 11).transpose(0, 2, 1, 3)).astype(np.float32)
    lw = np.zeros((DEPTH, 128, 2, 2, 2, 128), np.float32)
    for kind, nm in enumerate(["lru_wa", "lru_wx"]):
        wsrc = inputs[nm]
        for d in range(2):
            for cc in range(2):
                for b in range(2):
                    lw[:, b * 64:(b + 1) * 64, d, kind, cc, b * 64:(b + 1) * 64] = wsrc[:, d, 2 * cc + b]
    o["lru_w"] = lw.reshape(DEPTH, 128, 8, 128)
    return o


SMALL = ["norm1_g", "q_norm_g", "k_norm_g", "conv_w", "conv_b", "lru_wa", "lru_ba", "lru_wx", "lru_bx", "lru_lambda",
         "mlstm_f_bias", "mlstm_norm_g", "att_out_g", "lru_out_g", "norm2_g"]
BIG = ["w_in", "w_out", "w_router", "w_expert_gate", "w_expert_up", "w_expert_down", "norm1_g"]


def kernel(**inputs):
    inputs = {k: np.asarray(v) for k, v in inputs.items()}
    nc = build()
    shared = {k: np.ascontiguousarray(inputs[k], dtype=np.float32) for k in BIG}
    shared.update(consts())
    shared.update(layout_small(inputs))
    x = np.ascontiguousarray(inputs["x"], dtype=np.float32)
    in_maps = []
    for b in range(8):
        m = dict(shared)
        m["x"] = x[b]
        in_maps.append(m)
    res = run_bass_kernel_spmd(nc, in_maps, core_ids=list(range(8)))
    return np.stack([r["out"] for r in res.results], axis=0).astype(np.float32)
```

```python
import numpy as np
from contextlib import ExitStack
import concourse.bass as bass
import concourse.mybir as mybir
from concourse.bass_utils import run_bass_kernel_spmd

F32 = mybir.dt.float32
F32R = mybir.dt.float32r
BF16 = mybir.dt.bfloat16
F16 = mybir.dt.float16
U32 = mybir.dt.uint32
I32 = mybir.dt.int32
AF = mybir.ActivationFunctionType
ALU = mybir.AluOpType
AX = mybir.AxisListType

S = 2048
D = 1024
DEPTH = 4
D_IN = 2320
NT = S // 128
EPS = 1e-6

ENG = ["pe", "act", "dve", "pool", "sp"]
BLK = {"pe": "tensor", "act": "scalar", "dve": "vector", "pool": "gpsimd", "sp": "sync"}


class Sched:
    def __init__(self, nc, es, n_dma=24):
        self.nc = nc
        self.sem = {e: es.enter_context(nc.semaphore("s_" + e)) for e in ENG}
        self.dsem = [es.enter_context(nc.semaphore("s_dma%d" % i)) for i in range(n_dma)]
        self.cnt = {e: 0 for e in ENG}
        self.duse = [0] * n_dma
        self.drr = 0
        self.ops = {e: [] for e in ENG}
        self.waited = {e: {} for e in ENG}
        self.last_w = {}
        self.readers = {}
        self.nops = 0

    def semobj(self, s):
        return self.sem[s] if isinstance(s, str) else self.dsem[s[1]]

    def add(self, eng, fn, reads=(), writes=(), dma=False):
        deps = {}
        def need(tok):
            s, v = tok
            if deps.get(s, 0) < v:
                deps[s] = v
        for r in reads:
            if r in self.last_w:
                need(self.last_w[r])
        for w in writes:
            if w in self.last_w:
                need(self.last_w[w])
            for t in self.readers.get(w, ()):
                need(t)
        if dma:
            k = self.drr
            self.drr = (self.drr + 1) % len(self.dsem)
            self.duse[k] += 1
            n = self.duse[k]
            if n > 1:
                need((("dma", k), 16 * (n - 1)))
            token = (("dma", k), 16 * n)
        else:
            self.cnt[eng] += 1
            token = (eng, self.cnt[eng])
        waits = []
        wd = self.waited[eng]
        for s, v in deps.items():
            if s == "pe" and eng == "pe" and not dma:
                continue
            if wd.get(s, 0) < v:
                wd[s] = v
                waits.append((s, v))
        self.ops[eng].append((waits, fn, token, dma))
        self.nops += 1
        for r in reads:
            self.readers.setdefault(r, []).append(token)
        for w in writes:
            self.last_w[w] = token
            self.readers[w] = []
        return token

    def flush(self):
        waits = []
        for k, n in enumerate(self.duse):
            if n and self.waited["sp"].get(("dma", k), 0) < 16 * n:
                waits.append((("dma", k), 16 * n))
        self.ops["sp"].append((waits, None, None, False))
        ops = self.ops
        self.ops = {e: [] for e in ENG}
        sched = self
        with self.nc.Block() as block:
            for e in ENG:
                lst = ops[e]

                def body(eng, lst=lst):
                    for waits, fn, token, isdma in lst:
                        for (s, v) in waits:
                            eng.wait_ge(sched.semobj(s), v)
                        if fn is None:
                            continue
                        inst = fn(eng)
                        inst.then_inc(sched.semobj(token[0]), 16 if isdma else 1)

                getattr(block, BLK[e])(body)
        for e in ENG:
            for e2 in ENG:
                self.waited[e][e2] = self.cnt[e2]
            for k, n in enumerate(self.duse):
                self.waited[e][("dma", k)] = 16 * n
        self.last_w = {}
        self.readers = {}

    def dma(self, q, out, in_, reads=(), writes=(), slow=False):
        if slow:
            return self.add(q, lambda e: e.dma_start(out=out, in_=in_, allow_slow_non_contiguous=True), reads, writes, dma=True)
        return self.add(q, lambda e: e.dma_start(out=out, in_=in_), reads, writes, dma=True)

    def mm(self, out, lhsT, rhs, start, stop, reads=(), writes=()):
        return self.add("pe", lambda e: e.matmul(out, lhsT, rhs, start=start, stop=stop), reads, writes)

    def tr(self, out, in_, ident, reads=(), writes=()):
        return self.add("pe", lambda e: e.transpose(out, in_, ident), reads, writes)

    def act(self, out, in_, func, reads=(), writes=(), eng="act", **kw):
        return self.add(eng, lambda e: e.activation(out, in_, func, **kw), reads, writes)

    def ts(self, eng, out, in0, s1, s2, op0, op1=None, reads=(), writes=(), **kw):
        if op1 is None:
            return self.add(eng, lambda e: e.tensor_scalar(out, in0, s1, None, op0, **kw), reads, writes)
        return self.add(eng, lambda e: e.tensor_scalar(out, in0, s1, s2, op0, op1, **kw), reads, writes)

    def tt(self, eng, out, in0, in1, op, reads=(), writes=()):
        return self.add(eng, lambda e: e.tensor_tensor(out, in0, in1, op), reads, writes)

    def stt(self, eng, out, in0, scalar, in1, op0, op1, reads=(), writes=()):
        return self.add(eng, lambda e: e.scalar_tensor_tensor(out, in0, scalar, in1, op0, op1), reads, writes)

    def copy(self, eng, out, in_, reads=(), writes=()):
        if eng == "act":
            return self.add(eng, lambda e: e.copy(out, in_), reads, writes)
        return self.add(eng, lambda e: e.tensor_copy(out, in_), reads, writes)


def r32(ap):
    return ap.bitcast(F32R)


class Ctx:
    pass


def phase_inproj(nc, sc, c, l, x_src):
    with ExitStack() as es:
        T = lambda name, shape, dt=F32: es.enter_context(nc.sbuf_tensor("%s_L%d" % (name, l), shape, dt))
        P = lambda name, shape, dt=F32: es.enter_context(nc.psum_tensor("%s_L%d" % (name, l), shape, dt))
        wt = T("a_wt", [128, 8, D_IN], BF16)
        g = T("a_g", [128, 8])
        ident = T("a_id", [128, 128])
        xt = [T("a_xt%d" % i, [128, D]) for i in range(2)]
        junk = T("a_junk", [128, D])
        st = [T("a_st%d" % i, [128, 4]) for i in range(2)]
        hT = [T("a_hT%d" % i, [128, 8, 512], BF16) for i in range(2)]
        og = [T("a_og%d" % i, [128, D_IN]) for i in range(2)]
        of = [T("a_of%d" % i, [128, 512]) for i in range(2)]
        pT = [P("a_pT%d" % i, [128, 512]) for i in range(4)]
        pm = [P("a_pm%d" % i, [128, 512]) for i in range(4)]

        sc.dma("sp", ident[:], c.ident, writes=["ident"])
        sc.dma("sp", g[:], c.norm1_g[l].rearrange("(c p) -> p c", p=128), writes=["g"], slow=True)
        for ch in range(8):
            sc.dma("pool", wt[:, ch, :], c.w_in[l][ch * 128:(ch + 1) * 128, :], writes=[("wt", ch)])

        pmi = 0
        for b in range(4):
            hb = hT[b % 2]
            hkey = ("hT", b % 2)
            for tl in range(4):
                i = b * 4 + tl
                xb = xt[i % 2]
                sb = st[i % 2]
                xk = ("xt", i % 2)
                sk = ("st", i % 2)
                sc.dma("sp", xb[:], x_src[i * 128:(i + 1) * 128, :], writes=[xk])
                sc.act(junk[:], xb[:], AF.Square, reads=[xk], writes=["junk", sk], accum_out=sb[:, 0:1])
                sc.ts("dve", sb[:, 1:2], sb[:, 0:1], 1.0 / D, EPS, ALU.mult, ALU.add, reads=[sk], writes=[sk])
                sc.act(sb[:, 2:3], sb[:, 1:2], AF.Sqrt, reads=[sk], writes=[sk])
                sc.add("dve", lambda e, o=sb[:, 3:4], i_=sb[:, 2:3]: e.reciprocal(o, i_), reads=[sk], writes=[sk])
                sc.ts("dve", xb[:], xb[:], sb[:, 3:4], None, ALU.mult, reads=[xk, sk], writes=[xk])
                for half in range(2):
                    pt = pT[(i % 2) * 2 + half]
                    pk = ("pT", (i % 2) * 2 + half)
                    for cc in range(4):
                        ch = half * 4 + cc
                        sc.tr(pt[:, cc * 128:(cc + 1) * 128], xb[:, ch * 128:(ch + 1) * 128], ident[:],
                              reads=[xk, "ident"], writes=[pk])
                    sc.tt("dve", hb[:, half * 4:(half + 1) * 4, tl * 128:(tl + 1) * 128],
                          pt[:].rearrange("p (c t) -> p c t", c=4),
                          g[:, half * 4:(half + 1) * 4].unsqueeze(2).broadcast_to([128, 4, 128]), ALU.mult,
                          reads=[pk, "g"], writes=[(hkey, tl)])
            for tl in range(4):
                i = b * 4 + tl
                ob = og[i % 2]
                ok = ("og", i % 2)
                for n in range(5):
                    n0 = n * 512
                    nw = min(512, D_IN - n0)
                    pmm = pm[pmi % 4]
                    pk = ("pm", pmi % 4)
                    pmi += 1
                    for ch in range(8):
                        sc.mm(pmm[:, 0:nw], hb[:, ch, tl * 128:(tl + 1) * 128], wt[:, ch, n0:n0 + nw],
                              ch == 0, ch == 7, reads=[(hkey, tl), ("wt", ch)], writes=[pk])
                    eng = "act" if n % 2 == 0 else "dve"
                    sc.copy(eng, ob[:, n0:n0 + nw], pmm[:, 0:nw], reads=[pk], writes=[ok])
                sc.dma("pool", c.p_tm[i * 128:(i + 1) * 128, :], ob[:], reads=[ok])
            for j in range(8):
                c0 = 768 + j * 128
                pmm = pm[pmi % 4]
                pk = ("pm", pmi % 4)
                pmi += 1
                ob = of[j % 2]
                ok = ("of", j % 2)
                for ch in range(8):
                    sc.mm(pmm[:, :], wt[:, ch, c0:c0 + 128], hb[:, ch, :], ch == 0, ch == 7,
                          reads=[(hkey, 0), (hkey, 1), (hkey, 2), (hkey, 3), ("wt", ch)], writes=[pk])
                eng = "act" if j % 2 == 0 else "dve"
                sc.copy(eng, ob[:], pmm[:], reads=[pk], writes=[ok])
                sc.dma("pool", c.p_fm[j * 128:(j + 1) * 128, b * 512:(b + 1) * 512], ob[:], reads=[ok])
        sc.flush()


def phase_attn(nc, sc, c, l):
    with ExitStack() as es:
        T = lambda name, shape, dt=F32: es.enter_context(nc.sbuf_tensor("%s_L%d" % (name, l), shape, dt))
        P = lambda name, shape, dt=F32: es.enter_context(nc.psum_tensor("%s_L%d" % (name, l), shape, dt))
        ident = T("b_id", [128, 128])
        ones = T("b_ones", [128, 128])
        gg = T("b_gg", [128, 640])
        qT = T("b_qT", [128, 4, S], BF16)
        kT = T("b_kT", [128, 2, S], BF16)
        vx = T("b_vx", [128, NT, 2, 65], BF16)
        qkv = [T("b_qkv%d" % i, [128, 768]) for i in range(2)]
        cs = [T("b_cs%d" % i, [128, 2, 320]) for i in range(2)]
        sq2 = [T("b_sq%d" % i, [128, 640]) for i in range(2)]
        st2 = [T("b_st%d" % i, [128, 4, 10]) for i in range(2)]
        qn2 = [T("b_qn%d" % i, [128, 640]) for i in range(2)]
        tmp2 = [T("b_tmp%d" % i, [128, 4, 320]) for i in range(2)]
        qr2 = [T("b_qr%d" % i, [128, 640]) for i in range(2)]
        Osb = [T("b_Osb%d" % i, [65, 512]) for i in range(2)]
        PT = [T("b_PT%d" % i, [128, 512], BF16) for i in range(3)]
        rd = [T("b_rd%d" % i, [128, 512]) for i in range(2)]
        bcs = [T("b_bcs%d" % i, [64, 512]) for i in range(2)]
        yo = [T("b_yo%d" % i, [64, 512]) for i in range(2)]
        ptA = [P("b_ptA%d" % i, [128, 512]) for i in range(1)]
        ptB = [P("b_ptB%d" % i, [128, 512]) for i in range(1)]
        sps = [P("b_sps%d" % i, [128, 512]) for i in range(3)]
        ops_ = [P("b_ops%d" % i, [128, 512]) for i in range(2)]
        bcp = P("b_bcp", [128, 512])

        sc.dma("sp", ident[:], c.ident, writes=["ident"])
        sc.dma("sp", ones[:], c.ones, writes=["ones"])
        sc.dma("sp", gg[:], c.ggrep[l], writes=["gg"])
        sc.add("pool", lambda e: e.memset(kT[:], 0.0), writes=["kT"])
        for i in range(NT):
            qb_ = qkv[i % 2]; qk = ("qkv", i % 2)
            sq = sq2[i % 2]; st = st2[i % 2]; qn = qn2[i % 2]; tmp = tmp2[i % 2]; qr = qr2[i % 2]
            K_ = lambda nm: (nm, i % 2)
            cb = cs[i % 2]; ck = ("cs", i % 2)
            for half in range(2):
                sc.dma("sp", qb_[:, 0:512].rearrange("p (pr half d) -> p pr half d", pr=4, half=2)[:, :, half, :],
                       c.p_tm[i * 128:(i + 1) * 128, half * 256:(half + 1) * 256].rearrange("t (pr d) -> t pr d", pr=4),
                       writes=[qk])
            sc.dma("sp", qb_[:, 512:768], c.p_tm[i * 128:(i + 1) * 128, 512:768], writes=[qk])
            sc.dma("sp", cb[:, 0, :], c.cos_rep[i * 128:(i + 1) * 128, :], writes=[ck])
            sc.dma("sp", cb[:, 1, :], c.sin_rep[i * 128:(i + 1) * 128, :], writes=[ck])
            sc.tt("pool", sq[:], qb_[:, 0:640], qb_[:, 0:640], ALU.mult, reads=[qk], writes=[K_("sq")])
            sc.add("dve", lambda e, o=st[:, 0, :], i_=sq[:].rearrange("p (h d) -> p h d", d=64):
                   e.tensor_reduce(o, i_, AX.X, ALU.add), reads=[K_("sq")], writes=[K_("st")])
            sc.ts("dve", st[:, 1, :], st[:, 0, :], 1.0 / 64, EPS, ALU.mult, ALU.add, reads=[K_("st")], writes=[K_("st")])
            sc.act(st[:, 2, :], st[:, 1, :], AF.Sqrt, reads=[K_("st")], writes=[K_("st")])
            sc.add("dve", lambda e, o=st[:, 3, :], i_=st[:, 2, :]: e.reciprocal(o, i_), reads=[K_("st")], writes=[K_("st")])
            sc.tt("dve", qn[:].rearrange("p (h d) -> p h d", d=64), qb_[:, 0:640].rearrange("p (h d) -> p h d", d=64),
                  st[:, 3, :].unsqueeze(2).broadcast_to([128, 10, 64]), ALU.mult, reads=[qk, K_("st")], writes=[K_("qn")])
            sc.tt("pool", qn[:], qn[:], gg[:], ALU.mult, reads=[K_("qn"), "gg"], writes=[K_("qn")])
            q3 = qn[:].rearrange("p (h d) -> p h d", d=64)
            x1 = q3[:, :, 0:32]; x2 = q3[:, :, 32:64]
            co = cb[:, 0, :].rearrange("p (h d) -> p h d", d=32); si = cb[:, 1, :].rearrange("p (h d) -> p h d", d=32)
            t = [tmp[:, k, :].rearrange("p (h d) -> p h d", d=32) for k in range(4)]
            r3 = qr[:].rearrange("p (h d) -> p h d", d=64)
            sc.tt("dve", t[0], x1, co, ALU.mult, reads=[K_("qn"), ck], writes=[K_("tmp0")])
            sc.tt("pool", t[1], x2, si, ALU.mult, reads=[K_("qn"), ck], writes=[K_("tmp1")])
            sc.tt("dve", t[2], x2, co, ALU.mult, reads=[K_("qn"), ck], writes=[K_("tmp2")])
            sc.tt("pool", t[3], x1, si, ALU.mult, reads=[K_("qn"), ck], writes=[K_("tmp3")])
            sc.tt("dve", r3[:, :, 0:32], t[0], t[1], ALU.subtract, reads=[K_("tmp0"), K_("tmp1")], writes=[K_("qr")])
            sc.tt("pool", r3[:, :, 32:64], t[2], t[3], ALU.add, reads=[K_("tmp2"), K_("tmp3")], writes=[K_("qr")])
            for pr in range(4):
                sc.tr(ptA[0][:, pr * 128:(pr + 1) * 128], qr[:, pr * 128:(pr + 1) * 128], ident[:], reads=[K_("qr"), "ident"], writes=["ptA"])
            sc.tr(ptB[0][:, 0:128], qr[:, 512:640], ident[:], reads=[K_("qr"), "ident"], writes=["ptB"])
            sc.copy("act", qT[:, :, i * 128:(i + 1) * 128], ptA[0][:].rearrange("p (c t) -> p c t", c=4),
                    reads=["ptA"], writes=["qT"])
            sc.copy("dve", kT[0:64, 0, i * 128:(i + 1) * 128], ptB[0][0:64, 0:128], reads=["ptB"], writes=["kT"])
            sc.copy("dve", kT[64:128, 1, i * 128:(i + 1) * 128], ptB[0][64:128, 0:128], reads=["ptB"], writes=["kT"])
            sc.copy("act", vx[:, i, :, 0:64], qb_[:, 640:768].rearrange("p (g d) -> p g d", d=64),
                    reads=[qk], writes=["vx"])
            sc.ts("pool", vx[:, i, :, 64:65], qb_[:, 0:2].unsqueeze(2), 0.0, 1.0, ALU.mult, ALU.add,
                  reads=[qk], writes=["vx"])

        steps = [(pr + 4 * half, qb, kt) for pr in range(4) for qb in range(4) for kt in range(NT) for half in range(2)]
        def fin(half, h, qb):
            O = ops_[half]; ok = ("O", half)
            osb = Osb[half]; osk = ("Osb", half)
            r = rd[half]; rk = ("rd", half)
            sc.copy("dve", osb[:, :], O[0:65, :], reads=[ok], writes=[osk])
            sc.act(r[64:65, :], osb[64:65, :], AF.Ln, reads=[osk], writes=[rk])
            sc.act(r[64:65, :], r[64:65, :], AF.Exp, reads=[rk], writes=[rk], scale=-1.0)
            sc.mm(bcp[0:64, :], ones[64:65, 0:64], r[64:65, :], True, True, reads=[rk, "ones"], writes=["bcp"])
            sc.tt("dve", yo[half][:], osb[0:64, :], bcp[0:64, :], ALU.mult, reads=[osk, "bcp"], writes=[("yo", half)])
            sc.dma("sp", c.yT[h * 64:(h + 1) * 64, qb * 512:(qb + 1) * 512], yo[half][:], reads=[("yo", half)])
        for s_ in range(len(steps) + 2):
            if s_ < len(steps):
                h, qb, kt = steps[s_]
                pr = h % 4; half = h // 4
                p0 = half * 64
                sp_ = sps[s_ % 3]; sk = ("sps", s_ % 3)
                sc.mm(sp_[:, :], kT[:, half, kt * 128:(kt + 1) * 128],
                      qT[:, pr, qb * 512:(qb + 1) * 512], True, True,
                      reads=["kT", "qT"], writes=[sk])
                sc.act(PT[s_ % 3][:], sp_[:], AF.Exp, reads=[sk], writes=[("PT", s_ % 3)], scale=0.125)
            if s_ >= 2:
                h, qb, kt = steps[s_ - 2]
                half = h // 4
                O = ops_[half]; ok = ("O", half)
                sc.mm(O[0:65, :], vx[:, kt, half, :], PT[(s_ - 2) % 3][:], kt == 0, kt == NT - 1,
                      reads=["vx", ("PT", (s_ - 2) % 3)], writes=[ok])
                if kt == NT - 1:
                    fin(half, h, qb)
        sc.flush()


def phase_lru(nc, sc, c, l):
    with ExitStack() as es:
        T = lambda name, shape, dt=F32: es.enter_context(nc.sbuf_tensor("%s_L%d" % (name, l), shape, dt))
        P = lambda name, shape, dt=F32: es.enter_context(nc.psum_tensor("%s_L%d" % (name, l), shape, dt))
        sm = T("c_sm", [128, 2, 11])
        W = T("c_W", [128, 8, 128])
        X = T("c_X", [128, S]); G = T("c_G", [128, S]); XF = T("c_XF", [128, S]); XC = T("c_XC", [128, S])
        R = T("c_R", [128, S]); I_ = T("c_I", [128, S]); A = T("c_A", [128, S]); U = T("c_U", [128, S])
        H = [T("c_H%d" % i, [128, S]) for i in range(2)]
        cf = T("c_cf", [128, 8])
        pp = [P("c_pp%d" % i, [128, 512]) for i in range(4)]
        sc.dma("sp", sm[:], c.lru_small[l], writes=["sm"])
        sc.dma("sp", r32(W[:]), r32(c.lru_w[l]), writes=["W"])
        ppi = 0
        for cc in range(2):
            sc.dma("sp", X[:], c.p_fm[cc * 128:(cc + 1) * 128, :], writes=["X"])
            sc.dma("sp", G[:], c.p_fm[256 + cc * 128:256 + (cc + 1) * 128, :], writes=["G"])
            w = lambda j: sm[:, cc, j:j + 1]
            sc.ts("dve", XF[:], X[:], w(2), w(4), ALU.mult, ALU.add, reads=["X", "sm"], writes=["XF"])
            sc.stt("dve", XF[:, 2:S], X[:, 0:S - 2], w(0), XF[:, 2:S], ALU.mult, ALU.add, reads=["X", "XF", "sm"], writes=["XF"])
            sc.stt("dve", XF[:, 1:S], X[:, 0:S - 1], w(1), XF[:, 1:S], ALU.mult, ALU.add, reads=["X", "XF", "sm"], writes=["XF"])
            sc.copy("pool", r32(XC[:, S - 1:S]), XF[:, S - 1:S], reads=["XF"], writes=["XC"])
            sc.stt("dve", r32(XC[:, 0:S - 1]), X[:, 1:S], w(3), XF[:, 0:S - 1], ALU.mult, ALU.add, reads=["X", "XF", "sm"], writes=["XC"])
            for d in range(2):
                ba = sm[:, cc, 5 + d:6 + d]; bx = sm[:, cc, 7 + d:8 + d]; lam = sm[:, cc, 9 + d:10 + d]
                ck = ("cf", d)
                c0 = cf[:, 4 * d:4 * d + 1]; c1 = cf[:, 4 * d + 1:4 * d + 2]; c2 = cf[:, 4 * d + 2:4 * d + 3]
                sc.act(c0, lam, AF.Exp, reads=["sm"], writes=[ck], scale=-1.0)
                sc.act(c0, c0, AF.Ln, reads=[ck], writes=[ck], bias=1.0)
                sc.ts("dve", c1, c0, -8.0, None, ALU.mult, reads=[ck], writes=[ck])
                sc.ts("dve", c2, c0, -16.0, None, ALU.mult, reads=[ck], writes=[ck])
                for tb in range(4):
                    sl = slice(tb * 512, (tb + 1) * 512)
                    pa = pp[ppi % 4]; pak = ("pp", ppi % 4); ppi += 1
                    px = pp[ppi % 4]; pxk = ("pp", ppi % 4); ppi += 1
                    sc.mm(pa[:, :], r32(W[:, d * 4 + 0 * 2 + cc, :]), r32(XC[:, sl]), True, True, reads=["W", "XC"], writes=[pak])
                    sc.mm(px[:, :], r32(W[:, d * 4 + 1 * 2 + cc, :]), r32(XC[:, sl]), True, True, reads=["W", "XC"], writes=[pxk])
                    sc.act(R[:, sl], pa[:, :], AF.Sigmoid, reads=[pak, "sm"], writes=["R"], bias=ba)
                    sc.act(I_[:, sl], px[:, :], AF.Sigmoid, reads=[pxk, "sm"], writes=["I"], bias=bx)
                sc.act(A[:], R[:], AF.Exp, reads=["R", ck], writes=["A"], scale=c1)
                sc.act(U[:], R[:], AF.Exp, reads=["R", ck], writes=["U"], scale=c2)
                sc.ts("pool", U[:], U[:], -1.0, 1.0, ALU.mult, ALU.add, reads=["U"], writes=["U"])
                sc.act(U[:], U[:], AF.Sqrt, reads=["U"], writes=["U"])
                sc.tt("pool", I_[:], I_[:], XC[:], ALU.mult, reads=["I", "XC"], writes=["I"])
                sc.tt("dve", U[:], U[:], I_[:], ALU.mult, reads=["U", "I"], writes=["U"])
                if d == 0:
                    sc.add("dve", lambda e, o=H[0][:], a=A[:], u=U[:]: e.tensor_tensor_scan(o, a, u, 0.0, ALU.mult, ALU.add),
                           reads=["A", "U"], writes=[("H", 0)])
                else:
                    sc.add("dve", lambda e, o=H[1][:, ::-1], a=A[:, ::-1], u=U[:, ::-1]: e.tensor_tensor_scan(o, a, u, 0.0, ALU.mult, ALU.add),
                           reads=["A", "U"], writes=[("H", 1)])
            sc.tt("pool", H[0][:], H[0][:], H[1][:], ALU.add, reads=[("H", 0), ("H", 1)], writes=[("H", 0)])
            sc.tt("dve", R[:], G[:], G[:], ALU.mult, reads=["G", "R"], writes=["R"])
            sc.ts("dve", R[:], R[:], 0.044715, 1.0, ALU.mult, ALU.add, reads=["R"], writes=["R"])
            sc.tt("dve", R[:], R[:], G[:], ALU.mult, reads=["R", "G"], writes=["R"])
            sc.act(R[:], R[:], AF.Sigmoid, reads=["R"], writes=["R"], scale=1.5957691216057308)
            sc.tt("pool", H[0][:], H[0][:], G[:], ALU.mult, reads=[("H", 0), "G"], writes=[("H", 0)])
            sc.tt("dve", H[0][:], H[0][:], R[:], ALU.mult, reads=[("H", 0), "R"], writes=[("H", 0)])
            sc.dma("pool", c.yT[512 + cc * 128:512 + (cc + 1) * 128, :], H[0][:], reads=[("H", 0)])
        sc.flush()


def phase_mlstm(nc, sc, c, l):
    with ExitStack() as es:
        T = lambda name, shape, dt=F32: es.enter_context(nc.sbuf_tensor("%s_L%d" % (name, l), shape, dt))
        P = lambda name, shape, dt=F32: es.enter_context(nc.psum_tensor("%s_L%d" % (name, l), shape, dt))
        ident = T("d_id", [128, 128]); ones = T("d_ones", [128, 128])
        mask = T("d_mask", [128, 2, 128])
        fb = T("d_fb", [128, 8]); ng = T("d_ng", [128, 256])
        qT = T("d_qT", [128, 4, S], BF16); kT = T("d_kT", [128, 2, S], BF16)
        tmk = T("d_tmk", [128, NT, 256], BF16)
        tm = T("d_tm", [128, NT, 784])
        vp = T("d_vp", [128, NT, 8, 65], BF16)
        Cb = T("d_Cb", [128, 2, 4, 65], BF16)
        wA = T("d_wA", [128, NT, 8]); wB = T("d_wB", [128, NT, 8]); eg = T("d_eg", [128, NT, 8])
        nlf = T("d_nlf", [128, NT, 8]); tg = T("d_tg", [128, NT, 8])
        hs = T("d_hs", [128, NT, 256])
        Cst = T("d_C", [128, 2, 4, 65])
        tC = T("d_tC", [128, 65])
        PT = [T("d_PT%d" % i, [128, 128], BF16) for i in range(3)]
        s4 = [T("d_s4%d" % i, [128, 4, 4]) for i in range(2)]
        sq = T("d_sq", [128, 256]); st = T("d_st", [128, 4, 4]); ym = [T("d_ym%d" % i, [128, 256]) for i in range(2)]
        yo = [T("d_yo%d" % i, [128, 256]) for i in range(2)]
        sg = T("d_sg", [128, 256])
        pg = P("d_pg", [128, 512])
        psS = [P("d_pS%d" % i, [128, 512]) for i in range(2)]
        pacc = [P("d_pa%d" % i, [128, 512]) for i in range(2)]
        pkv = [P("d_pk%d" % i, [128, 512]) for i in range(2)]
        ptr = P("d_ptr", [128, 512])

        sc.dma("sp", ident[:], c.ident, writes=["ident"])
        sc.dma("sp", ones[:], c.ones, writes=["ones"])
        sc.dma("sp", mask[:, 0, :], c.triu, writes=["mask"])
        sc.dma("sp", mask[:, 1, :], c.tril, writes=["mask"])
        sc.dma("sp", fb[:], c.fbrep[l], writes=["fb"])
        sc.dma("sp", ng[:], c.ngrep[l], writes=["ng"])
        sc.add("pool", lambda e: e.memset(qT[:], 0.0), writes=["qT"])
        for h in range(4):
            p0 = (h % 2) * 64
            sc.dma("pool", qT[p0:p0 + 64, h, :], c.p_fm[512 + h * 64:512 + (h + 1) * 64, :], reads=["qT"], writes=[("qTh", h)])
        for ch in range(2):
            sc.dma("pool", kT[:, ch, :], c.p_fm[768 + ch * 128:768 + (ch + 1) * 128, :], writes=["kT"])
        for i in range(NT):
            sc.dma("sp", tm[:, i, 256:784], c.p_tm[i * 128:(i + 1) * 128, 1792:2320], writes=[("tm", i)])
            sc.dma("pool", tmk[:, i, :], c.p_tm[i * 128:(i + 1) * 128, 1536:1792], writes=[("tmk", i)])
        sc.add("dve", lambda e: e.memset(Cst[:], 0.0), writes=["C"])
        sc.add("dve", lambda e: e.memset(Cb[:], 0.0), writes=["Cb"])
        for i in range(NT):
            gt = tm[:, i, 768:784]
            fview = gt.rearrange("p (d k h) -> p d k h", d=2, k=2)[:, :, 1, :]
            iview = gt.rearrange("p (d k h) -> p d k h", d=2, k=2)[:, :, 0, :]
            tk = ("tg", i)
            sc.tt("dve", tg[:, i, :].rearrange("p (d h) -> p d h", d=2), fview, fb[:].rearrange("p (d h) -> p d h", d=2),
                  ALU.add, reads=[("tm", i), "fb"], writes=[tk])
            sc.act(tg[:, i, :], tg[:, i, :], AF.Exp, reads=[tk], writes=[tk], scale=-1.0)
            sc.act(nlf[:, i, :], tg[:, i, :], AF.Ln, reads=[tk], writes=[("nlf", i)], bias=1.0)
            sc.mm(pg[:, 0:4], mask[:, 0, :], nlf[:, i, 0:4], True, True, reads=["mask", ("nlf", i)], writes=["pg"])
            sc.mm(pg[:, 4:8], mask[:, 1, :], nlf[:, i, 4:8], True, True, reads=["mask", ("nlf", i)], writes=["pg"])
            sc.mm(pg[:, 8:16], ones[:, :], nlf[:, i, :], True, True, reads=["ones", ("nlf", i)], writes=["pg"])
            sc.act(wA[:, i, :], pg[:, 0:8], AF.Exp, reads=["pg"], writes=[("wA", i)], scale=-1.0)
            sc.act(eg[:, i, :], pg[:, 8:16], AF.Exp, reads=["pg"], writes=[("eg", i)], scale=-1.0)
            sc.tt("dve", wB[:, i, :].rearrange("p (d h) -> p d h", d=2), iview, pg[:, 0:8].rearrange("p (d h) -> p d h", d=2),
                  ALU.add, reads=[("tm", i), "pg"], writes=[("wB", i)])
            sc.act(wB[:, i, :], wB[:, i, :], AF.Exp, reads=[("wB", i)], writes=[("wB", i)])
            v3 = tm[:, i, 256:512].rearrange("p (h d) -> p h d", d=64)
            for d in range(2):
                eng = "dve" if d == 0 else "pool"
                sc.tt(eng, vp[:, i, d * 4:(d + 1) * 4, 0:64], v3, wB[:, i, d * 4:(d + 1) * 4].unsqueeze(2).broadcast_to([128, 4, 64]),
                      ALU.mult, reads=[("tm", i), ("wB", i)], writes=[("vp", i)])
            sc.copy("pool", vp[:, i, :, 64:65], wB[:, i, :].unsqueeze(2), reads=[("wB", i)], writes=[("vp", i)])
        ui = 0
        for s_ in range(NT):
            for d in range(2):
                i = s_ if d == 0 else NT - 1 - s_
                tsl = slice(i * 128, (i + 1) * 128)
                acc = pacc[d]; ak = ("pacc", d)
                for h in range(4):
                    p0 = (h % 2) * 64; ch = h // 2
                    pS = psS[ui % 2]; pk_ = ("pS", ui % 2)
                    pt = PT[ui % 3]; ptk = ("PT", ui % 3)
                    kv = pkv[ui % 2]; kvk = ("pkv", ui % 2)
                    ui += 1
                    sc.mm(pS[:, 0:128], kT[:, ch, tsl], qT[:, h, tsl], True, True,
                          reads=["kT", ("qTh", h)], writes=[pk_])
                    sc.stt("dve", pt[:], pS[:, 0:128], 0.125, mask[:, d, :], ALU.mult, ALU.mult,
                           reads=[pk_, "mask"], writes=[ptk])
                    sc.mm(acc[:, h * 65:(h + 1) * 65], pt[:], vp[:, i, d * 4 + h, :], True, False,
                          reads=[ptk, ("vp", i)], writes=[ak])
                    sc.mm(acc[:, h * 65:(h + 1) * 65], qT[:, h, tsl], Cb[:, d, h, :], False, True,
                          reads=[("qTh", h), ("Cb", d, h), "Cb"], writes=[ak])
                    if s_ < NT - 1:
                        sc.mm(kv[:, 0:65], tmk[:, i, ch * 128:(ch + 1) * 128], vp[:, i, d * 4 + h, :], True, True,
                              reads=[("tmk", i), ("vp", i)], writes=[kvk])
                        sc.stt("dve", tC[p0:p0 + 64, :], kv[p0:p0 + 64, 0:65], 0.125, Cst[p0:p0 + 64, d, h, :], ALU.mult, ALU.add,
                               reads=[kvk, ("C", d, h)], writes=["tC"])
                        sc.ts("dve", Cb[p0:p0 + 64, d, h, :], tC[p0:p0 + 64, :], eg[p0:p0 + 64, i, d * 4 + h:d * 4 + h + 1], None, ALU.mult,
                              reads=["tC", ("eg", i)], writes=[("Cb", d, h)])
                        sc.ts("pool", Cst[p0:p0 + 64, d, h, :], tC[p0:p0 + 64, :], eg[p0:p0 + 64, i, d * 4 + h:d * 4 + h + 1], None, ALU.mult,
                              reads=["tC", ("eg", i)], writes=[("C", d, h)])
                a3 = acc[:, 0:260].rearrange("p (h e) -> p h e", e=65)
                sb = s4[d]; sk = ("s4", d)
                sc.tt("dve", sb[:, 0, :], a3[:, :, 64], wA[:, i, d * 4:(d + 1) * 4], ALU.mult, reads=[ak, ("wA", i)], writes=[sk])
                sc.act(sb[:, 1, :], sb[:, 0, :], AF.Abs, reads=[sk], writes=[sk])
                sc.ts("dve", sb[:, 1, :], sb[:, 1, :], 1.0, None, ALU.max, reads=[sk], writes=[sk])
                sc.add("dve", lambda e, o=sb[:, 2, :], i_=sb[:, 1, :]: e.reciprocal(o, i_), reads=[sk], writes=[sk])
                sc.tt("dve", sb[:, 3, :], sb[:, 2, :], wA[:, i, d * 4:(d + 1) * 4], ALU.mult, reads=[sk, ("wA", i)], writes=[sk])
                h3 = hs[:, i, :].rearrange("p (h e) -> p h e", e=64)
                if s_ < NT // 2:
                    sc.tt("dve", h3, a3[:, :, 0:64], sb[:, 3, :].unsqueeze(2).broadcast_to([128, 4, 64]), ALU.mult,
                          reads=[ak, sk], writes=[("hs", i)])
                else:
                    sc.tt("dve", sq[:].rearrange("p (h e) -> p h e", e=64), a3[:, :, 0:64],
                          sb[:, 3, :].unsqueeze(2).broadcast_to([128, 4, 64]), ALU.mult, reads=[ak, sk], writes=["sq"])
                    sc.tt("pool", hs[:, i, :], hs[:, i, :], sq[:], ALU.add, reads=[("hs", i), "sq"], writes=[("hs", i)])
        for i in range(NT):
            y = ym[i % 2]; yk = ("ym", i % 2)
            sc.tt("pool", sq[:], hs[:, i, :], hs[:, i, :], ALU.mult, reads=[("hs", i)], writes=["sq"])
            sc.add("dve", lambda e, o=st[:, 0, :], i_=sq[:].rearrange("p (h d) -> p h d", d=64):
                   e.tensor_reduce(o, i_, AX.X, ALU.add), reads=["sq"], writes=["st"])
            sc.ts("dve", st[:, 1, :], st[:, 0, :], 1.0 / 64, EPS, ALU.mult, ALU.add, reads=["st"], writes=["st"])
            sc.act(st[:, 2, :], st[:, 1, :], AF.Sqrt, reads=["st"], writes=["st"])
            sc.add("dve", lambda e, o=st[:, 3, :], i_=st[:, 2, :]: e.reciprocal(o, i_), reads=["st"], writes=["st"])
            sc.tt("dve", y[:].rearrange("p (h d) -> p h d", d=64), hs[:, i, :].rearrange("p (h d) -> p h d", d=64),
                  st[:, 3, :].unsqueeze(2).broadcast_to([128, 4, 64]), ALU.mult, reads=[("hs", i), "st"], writes=[yk])
            sc.tt("pool", y[:], y[:], ng[:], ALU.mult, reads=[yk, "ng"], writes=[yk])
            sc.act(sg[:], tm[:, i, 512:768], AF.Sigmoid, reads=[("tm", i)], writes=["sg"])
            sc.tt("dve", y[:], y[:], sg[:], ALU.mult, reads=[yk, "sg"], writes=[yk])
            for ch in range(2):
                sc.tr(ptr[:, ch * 128:(ch + 1) * 128], y[:, ch * 128:(ch + 1) * 128], ident[:], reads=[yk, "ident"], writes=["ptr"])
            o = yo[i % 2]; ok = ("yo", i % 2)
            sc.copy("act", o[:], ptr[:, 0:256], reads=["ptr"], writes=[ok])
            sc.dma("pool", c.yT[768:1024, i * 128:(i + 1) * 128].rearrange("(ch p) t -> p ch t", p=128),
                   o[:].rearrange("p (ch t) -> p ch t", ch=2), reads=[ok])
        sc.flush()


BF16 = mybir.dt.bfloat16
F16 = mybir.dt.float16
NE = 16
CAP = 256
WQ = "pool"


def phase_outproj(nc, sc, c, l, x_src):
    with ExitStack() as es:
        T = lambda name, shape, dt=F32: es.enter_context(nc.sbuf_tensor("%s_L%d" % (name, l), shape, dt))
        P = lambda name, shape, dt=F32: es.enter_context(nc.psum_tensor("%s_L%d" % (name, l), shape, dt))
        wt = T("e_wt", [128, 8, D], BF16); yh = [T("e_yh%d" % i, [128, 8, 512], BF16) for i in range(2)]; g = T("e_g", [128, 8]); ones = T("e_ones", [128, 128])
        yb = [T("e_yb%d" % i, [128, 8, 512]) for i in range(2)]
        ysq = T("e_ysq", [128, 6, 512])
        xt = [T("e_xt%d" % i, [128, D]) for i in range(2)]
        st = [T("e_st%d" % i, [128, 8]) for i in range(2)]
        pm = [P("e_pm%d" % i, [128, 512]) for i in range(6)]
        pst = P("e_pst", [128, 512])
        sc.dma("sp", ones[:], c.ones, writes=["ones"])
        sc.dma("sp", g[:], c.gout[l], writes=["g"])
        for ch in range(8):
            sc.dma("pool", wt[:, ch, :], c.w_out[l][ch * 128:(ch + 1) * 128, :], writes=[("wt", ch)])
        pi = 0
        for b in range(4):
            y = yb[b % 2]; ykey = ("yb", b % 2)
            for ch in range(8):
                sc.dma("sp", y[:, ch, :], c.yT[ch * 128:(ch + 1) * 128, b * 512:(b + 1) * 512], writes=[ykey])
            sc.act(ysq[:], y[:, 0:6, :], AF.Square, reads=[ykey], writes=["ysq"])
            yhb = yh[b % 2]; yhk = ("yh", b % 2)
            for ch in range(8):
                sc.act(yhb[:, ch, :], y[:, ch, :], AF.Copy, reads=[ykey, "g"], writes=[yhk], scale=g[:, ch:ch + 1])
            for tl in range(4):
                i = b * 4 + tl
                tsl = slice(tl * 128, (tl + 1) * 128)
                x = xt[i % 2]; xk = ("xt", i % 2); sb = st[i % 2]; sk = ("st", i % 2)
                sc.dma("sp", x[:], x_src[i * 128:(i + 1) * 128, :], writes=[xk])
                for ch in range(6):
                    col = 0 if ch < 4 else 1
                    sc.mm(pst[:, col:col + 1], ysq[:, ch, tsl], ones[:, 0:1], ch in (0, 4), ch in (3, 5), reads=["ysq", "ones"], writes=["pst"])
                sc.ts("dve", sb[:, 0:1], pst[:, 0:1], 1.0 / 512, EPS, ALU.mult, ALU.add, reads=["pst"], writes=[sk])
                sc.ts("dve", sb[:, 1:2], pst[:, 1:2], 1.0 / 256, EPS, ALU.mult, ALU.add, reads=["pst"], writes=[sk])
                sc.act(sb[:, 2:4], sb[:, 0:2], AF.Sqrt, reads=[sk], writes=[sk])
                sc.add("dve", lambda e, o=sb[:, 4:6], i_=sb[:, 2:4]: e.reciprocal(o, i_), reads=[sk], writes=[sk])
                for n in range(2):
                    nsl = slice(n * 512, (n + 1) * 512)
                    ps = []
                    for (c0, c1) in ((0, 4), (4, 6), (6, 8)):
                        p_ = pm[pi % 6]; pk = ("pm", pi % 6); pi += 1
                        for ch in range(c0, c1):
                            sc.mm(p_[:, :], yhb[:, ch, tsl], wt[:, ch, nsl], ch == c0, ch == c1 - 1,
                                  reads=[yhk, ("wt", ch)], writes=[pk])
                        ps.append((p_, pk))
                    sc.stt("dve", x[:, nsl], ps[0][0][:, :], sb[:, 4:5], x[:, nsl], ALU.mult, ALU.add, reads=[ps[0][1], sk, xk], writes=[xk])
                    sc.stt("dve", x[:, nsl], ps[1][0][:, :], sb[:, 5:6], x[:, nsl], ALU.mult, ALU.add, reads=[ps[1][1], sk, xk], writes=[xk])
                    sc.tt("dve", x[:, nsl], x[:, nsl], ps[2][0][:, :], ALU.add, reads=[ps[2][1], xk], writes=[xk])
                sc.dma("pool", c.out[i * 128:(i + 1) * 128, :], x[:], reads=[xk])
        sc.flush()


def phase_router(nc, sc, c, l):
    with ExitStack() as es:
        T = lambda name, shape, dt=F32: es.enter_context(nc.sbuf_tensor("%s_L%d" % (name, l), shape, dt))
        P = lambda name, shape, dt=F32: es.enter_context(nc.psum_tensor("%s_L%d" % (name, l), shape, dt))
        ident = T("f_id", [128, 128]); g2 = T("f_g2", [128, 8]); wr = T("f_wr", [128, 8, NE])
        xt = [T("f_xt%d" % i, [128, D]) for i in range(2)]
        junk = T("f_junk", [128, D])
        hb = [T("f_hb%d" % i, [128, D], BF16) for i in range(2)]
        hT = [T("f_hT%d" % i, [128, 8, 128]) for i in range(2)]
        st = [T("f_st%d" % i, [128, 8]) for i in range(2)]
        aff = T("f_aff", [128, NT, NE]); ex = T("f_ex", [128, NE])
        affT = T("f_affT", [NE, S]); work = T("f_work", [NE, S])
        gem = T("f_gem", [NE, CAP]); iem = T("f_iem", [NE, CAP], U32); ief = T("f_ief", [NE, CAP])
        idxs = T("f_idxs", [128, 2, NE], I32); gts = T("f_gts", [128, 2, NE])
        pT = [P("f_pT%d" % i, [128, 512]) for i in range(4)]
        pl = [P("f_pl%d" % i, [128, 512]) for i in range(2)]
        pa = [P("f_pa%d" % i, [128, 512]) for i in range(2)]
        sc.dma("sp", ident[:], c.ident, writes=["ident"])
        sc.dma("sp", g2[:], c.g2rep[l], writes=["g2"])
        sc.dma("sp", wr[:], c.w_router[l].rearrange("(c p) e -> p c e", p=128), writes=["wr"])
        sc.tt("dve", wr[:], wr[:], g2[:].unsqueeze(2).broadcast_to([128, 8, NE]), ALU.mult, reads=["wr", "g2"], writes=["wr"])
        for i in range(NT):
            x = xt[i % 2]; xk = ("xt", i % 2); sb = st[i % 2]; sk = ("st", i % 2)
            sc.dma("sp", x[:], c.out[i * 128:(i + 1) * 128, :], writes=[xk])
            sc.act(junk[:], x[:], AF.Square, reads=[xk], writes=["junk", sk], accum_out=sb[:, 0:1])
            sc.ts("dve", sb[:, 1:2], sb[:, 0:1], 1.0 / D, EPS, ALU.mult, ALU.add, reads=[sk], writes=[sk])
            sc.act(sb[:, 2:3], sb[:, 1:2], AF.Sqrt, reads=[sk], writes=[sk])
            sc.add("dve", lambda e, o=sb[:, 3:4], i_=sb[:, 2:3]: e.reciprocal(o, i_), reads=[sk], writes=[sk])
            sc.ts("dve", x[:], x[:], sb[:, 3:4], None, ALU.mult, reads=[xk, sk], writes=[xk])
            sc.copy("pool", hb[i % 2][:], x[:], reads=[xk], writes=[("hb", i % 2)])
            sc.dma("pool", c.h2n[i * 128:(i + 1) * 128, :], hb[i % 2][:], reads=[("hb", i % 2)])
            for half in range(2):
                pt = pT[(i % 2) * 2 + half]; pk = ("pT", (i % 2) * 2 + half)
                for cc in range(4):
                    ch = half * 4 + cc
                    sc.tr(pt[:, cc * 128:(cc + 1) * 128], x[:, ch * 128:(ch + 1) * 128], ident[:], reads=[xk, "ident"], writes=[pk])
                sc.copy("act" if half == 0 else "dve", hT[i % 2][:, half * 4:(half + 1) * 4, :],
                        pt[:].rearrange("p (c t) -> p c t", c=4), reads=[pk], writes=[("hT", i % 2)])
            lg = pl[i % 2]; lk = ("pl", i % 2)
            for ch in range(8):
                sc.mm(lg[:, 0:NE], hT[i % 2][:, ch, :], wr[:, ch, :], ch == 0, ch == 7, reads=[("hT", i % 2), "wr"], writes=[lk])
            sc.add("dve", lambda e, o=sb[:, 4:5], i_=lg[:, 0:NE]: e.tensor_reduce(o, i_, AX.X, ALU.max), reads=[lk], writes=[sk])
            sc.ts("dve", sb[:, 5:6], sb[:, 4:5], -1.0, None, ALU.mult, reads=[sk], writes=[sk])
            sc.act(ex[:], lg[:, 0:NE], AF.Exp, reads=[lk, sk], writes=["ex", sk], bias=sb[:, 5:6], accum_out=sb[:, 6:7])
            sc.add("dve", lambda e, o=sb[:, 7:8], i_=sb[:, 6:7]: e.reciprocal(o, i_), reads=[sk], writes=[sk])
            sc.ts("dve", aff[:, i, :], ex[:], sb[:, 7:8], None, ALU.mult, reads=["ex", sk], writes=[("aff", i)])
            pa_ = pa[i % 2]; pak = ("pa", i % 2)
            sc.tr(pa_[0:NE, 0:128], aff[:, i, :], ident[:], reads=[("aff", i), "ident"], writes=[pak])
            sc.copy("act", affT[:, i * 128:(i + 1) * 128], pa_[0:NE, 0:128], reads=[pak], writes=["affT"])
        sc.copy("dve", work[:], affT[:], reads=["affT"], writes=["work"])
        for r in range(CAP // 8):
            sl8 = slice(r * 8, (r + 1) * 8)
            sc.add("dve", lambda e, o=gem[:, sl8]: e.max(o, work[:]), reads=["work"], writes=["gem"])
            sc.add("dve", lambda e, o=iem[:, sl8], m=gem[:, sl8]: e.max_index(o, m, work[:]), reads=["gem", "work"], writes=["iem"])
            if r < CAP // 8 - 1:
                sc.add("dve", lambda e, m=gem[:, sl8]: e.match_replace(work[:], m, work[:], -1e30), reads=["gem", "work"], writes=["work"])
        sc.copy("dve", ief[:], iem[:], reads=["iem"], writes=["ief"])
        for ct in range(2):
            pa_ = pa[ct]; pak = ("pa", ct)
            sc.tr(pa_[:, 0:NE], ief[:, ct * 128:(ct + 1) * 128], ident[0:NE, 0:NE], reads=["ief", "ident"], writes=[pak])
            sc.tr(pa_[:, NE:2 * NE], gem[:, ct * 128:(ct + 1) * 128], ident[0:NE, 0:NE], reads=["gem", "ident"], writes=[pak])
            sc.copy("act", idxs[:, ct, :], pa_[:, 0:NE], reads=[pak], writes=["idxs"])
            sc.copy("act", gts[:, ct, :], pa_[:, NE:2 * NE], reads=[pak], writes=["gts"])
        sc.dma("sp", c.idx_d, idxs[:].rearrange("p a b -> p (a b)"), reads=["idxs"])
        sc.dma("sp", c.gate_d, gts[:].rearrange("p a b -> p (a b)"), reads=["gts"])
        sc.flush()


def phase_experts(nc, sc, c, l):
    NWB = 4
    with ExitStack() as es:
        T = lambda name, shape, dt=F32: es.enter_context(nc.sbuf_tensor("%s_L%d" % (name, l), shape, dt))
        P = lambda name, shape, dt=F32: es.enter_context(nc.psum_tensor("%s_L%d" % (name, l), shape, dt))
        identb = T("g_idb", [128, 128], BF16)
        Wg = [T("g_Wg%d" % i, [128, 8, 256], BF16) for i in range(NWB)]
        Wu = [T("g_Wu%d" % i, [128, 8, 256], BF16) for i in range(NWB)]
        Wd = [T("g_Wd%d" % i, [128, 2, D], BF16) for i in range(NWB)]
        xtok = [[T("g_xt%d_%d" % (i, ct), [128, D], BF16) for ct in range(2)] for i in range(2)]
        xg = T("g_xg", [128, 8, CAP], BF16)
        H = [T("g_H%d" % i, [128, 2, CAP], BF16) for i in range(2)]
        sil = [T("g_sil%d" % i, [128, CAP]) for i in range(2)]
        ye = [[T("g_ye%d_%d" % (i, ct), [128, D]) for ct in range(2)] for i in range(2)]
        idxs = T("g_idx", [128, 2 * NE], I32); gts = T("g_gts", [128, 2 * NE])
        g2 = T("g_g2", [128, 8])
        yacc = [P("g_ya%d" % i, [128, 512]) for i in range(4)]
        au = [P("g_au%d" % i, [128, 512]) for i in range(2)]
        tp = [P("g_tp%d" % i, [128, 1024], BF16) for i in range(2)]
        sc.dma("sp", idxs[:], c.idx_d, writes=["idxs"])
        sc.dma("sp", gts[:], c.gate_d, writes=["gts"])
        sc.dma("sp", identb[:], c.identb, writes=["identb"])
        sc.dma("sp", g2[:], c.g2rep[l], writes=["g2"])
        wq = [0]
        def load_w(e, wb):
            b = wq[0] % NWB; wq[0] += 1
            hs_ = slice(wb * 256, (wb + 1) * 256)
            sc.dma("pool", Wg[b][:], c.w_gate[l, e][:, hs_].rearrange("(c p) h -> p c h", p=128), writes=[("Wg", b)])
            sc.dma("pool", Wu[b][:], c.w_up[l, e][:, hs_].rearrange("(c p) h -> p c h", p=128), writes=[("Wu", b)])
            sc.dma("pool", Wd[b][:], c.w_down[l, e][wb * 256:(wb + 1) * 256, :].rearrange("(c p) f -> p c f", p=128), writes=[("Wd", b)])
            return b
        def gather(e):
            for ct in range(2):
                col = ct * NE + e
                sc.add("pool", lambda g, o=xtok[e % 2][ct][:, :], ix=idxs[:, col:col + 1]:
                       g.indirect_dma_start(out=o, out_offset=None, in_=c.h2n[:, :],
                                            in_offset=bass.IndirectOffsetOnAxis(ap=ix, axis=0)),
                       reads=["idxs", "h2n_d"], writes=[("xtok", e % 2, ct)], dma=True)
        seq = [(e, wb) for e in range(NE) for wb in range(8)]
        pend = []
        for k in range(NWB - 1):
            pend.append(load_w(*seq[k]))
        gather(0)
        ai = 0; hi = 0
        for e in range(NE):
            for ct in range(2):
                tpp = tp[ct]; tk = ("tp", ct)
                for fc in range(8):
                    sc.tr(tpp[:, fc * 128:(fc + 1) * 128], xtok[e % 2][ct][:, fc * 128:(fc + 1) * 128], identb[:],
                          reads=[("xtok", e % 2, ct), "identb"], writes=[tk])
                for fc in range(8):
                    sc.act(xg[:, fc, ct * 128:(ct + 1) * 128], tpp[:, fc * 128:(fc + 1) * 128], AF.Copy,
                           reads=[tk, "g2"], writes=[("xg", fc)], scale=g2[:, fc:fc + 1])
            if e + 1 < NE:
                gather(e + 1)
            for wb in range(8):
                b = pend.pop(0)
                si = e * 8 + wb + NWB - 1
                if si < len(seq):
                    pend.append(load_w(*seq[si]))
                hb_ = H[hi % 2]; hk = ("H", hi % 2); hi += 1
                for jj in range(2):
                    p_ = au[ai % 2]; pk = ("au", ai % 2)
                    sl_ = sil[ai % 2]; slk = ("sil", ai % 2); ai += 1
                    for ch in range(8):
                        sc.mm(p_[:, 0:256], Wg[b][:, ch, jj * 128:(jj + 1) * 128], xg[:, ch, :], ch == 0, ch == 7,
                              reads=[("Wg", b), ("xg", ch)], writes=[pk])
                    for ch in range(8):
                        sc.mm(p_[:, 256:512], Wu[b][:, ch, jj * 128:(jj + 1) * 128], xg[:, ch, :], ch == 0, ch == 7,
                              reads=[("Wu", b), ("xg", ch)], writes=[pk])
                    sc.act(sl_[:], p_[:, 0:256], AF.Silu, reads=[pk], writes=[slk])
                    sc.tt("dve", hb_[:, jj, :], sl_[:], p_[:, 256:512], ALU.mult, reads=[slk, pk], writes=[hk])
                for ct in range(2):
                    for fb in range(2):
                        for jj in range(2):
                            sc.mm(yacc[ct * 2 + fb][:, :], hb_[:, jj, ct * 128:(ct + 1) * 128], Wd[b][:, jj, fb * 512:(fb + 1) * 512],
                                  wb == 0 and jj == 0, wb == 7 and jj == 1, reads=[hk, ("Wd", b)], writes=[("ya", ct * 2 + fb)])
            for ct in range(2):
                col = ct * NE + e
                yb_ = ye[e % 2][ct]; yk = ("ye", e % 2, ct)
                for fb in range(2):
                    if fb == 0:
                        sc.act(yb_[:, fb * 512:(fb + 1) * 512], yacc[ct * 2 + fb][:, :], AF.Copy, reads=[("ya", ct * 2 + fb), "gts"],
                               writes=[yk], scale=gts[:, col:col + 1])
                    else:
                        sc.ts("dve", yb_[:, fb * 512:(fb + 1) * 512], yacc[ct * 2 + fb][:, :], gts[:, col:col + 1], None, ALU.mult,
                              reads=[("ya", ct * 2 + fb), "gts"], writes=[yk])
                sc.add("pool", lambda g, i_=yb_[:, :], ix=idxs[:, col:col + 1]:
                       g.indirect_dma_start(out=c.out[:, :], out_offset=bass.IndirectOffsetOnAxis(ap=ix, axis=0),
                                            in_=i_, in_offset=None, compute_op=ALU.add),
                       reads=[yk, "idxs"], writes=["xout"], dma=True)
        sc.flush()


def build(n_layers=DEPTH, debug=None):
    nc = bass.Bass("TRN2", target_bir_lowering=False)
    nc.dge_precook = False
    c = Ctx()
    def inp(name, shape):
        return nc.dram_tensor(name, list(shape), F32, kind="ExternalInput").ap()
    c.x = inp("x", [S, D])
    c.norm1_g = inp("norm1_g", [DEPTH, D])
    c.w_in = inp("w_in", [DEPTH, D, D_IN])
    c.ident = inp("ident", [128, 128])
    c.ones = inp("ones", [128, 128])
    c.cos_rep = inp("cos_rep", [S, 320])
    c.sin_rep = inp("sin_rep", [S, 320])
    c.ggrep = inp("ggrep", [DEPTH, 128, 640])
    c.lru_small = inp("lru_small", [DEPTH, 128, 2, 11])
    c.lru_w = inp("lru_w", [DEPTH, 128, 8, 128])
    c.w_out = inp("w_out", [DEPTH, D, D])
    c.gout = inp("gout", [DEPTH, 128, 8])
    c.g2rep = inp("g2rep", [DEPTH, 128, 8])
    c.w_router = inp("w_router", [DEPTH, D, NE])
    c.w_gate = inp("w_expert_gate", [DEPTH, NE, D, 2 * D])
    c.w_up = inp("w_expert_up", [DEPTH, NE, D, 2 * D])
    c.w_down = inp("w_expert_down", [DEPTH, NE, 2 * D, D])
    c.h2n = nc.dram_tensor("h2n", [S, D], BF16, kind="Internal").ap()
    c.idx_d = nc.dram_tensor("idx_d", [128, 2 * NE], I32, kind="ExternalOutput" if debug == "R" else "Internal").ap()
    c.gate_d = nc.dram_tensor("gate_d", [128, 2 * NE], F32, kind="ExternalOutput" if debug == "R" else "Internal").ap()
    c.identb = nc.dram_tensor("identb", [128, 128], BF16, kind="ExternalInput").ap()
    c.triu = inp("triu", [128, 128])
    c.tril = inp("tril", [128, 128])
    c.fbrep = inp("fbrep", [DEPTH, 128, 8])
    c.ngrep = inp("ngrep", [DEPTH, 128, 256])
    c.yT = nc.dram_tensor("yT", [1024, S], F32, kind="ExternalOutput" if debug in ("B", "C", "D") else "Internal").ap()
    c.out = nc.dram_tensor("out", [S, D], F32, kind="ExternalOutput").ap()
    c.p_tm = nc.dram_tensor("p_tm", [S, D_IN], F32, kind="ExternalOutput" if debug == "A" else "Internal").ap()
    c.p_fm = nc.dram_tensor("p_fm", [1024, S], F32, kind="ExternalOutput" if debug == "A" else "Internal").ap()
    with ExitStack() as es:
        sc = Sched(nc, es)
        for l in range(n_layers):
            phase_inproj(nc, sc, c, l, c.x if l == 0 else c.out)
            if debug == "A":
                continue
            if debug != "C" and debug != "D":
                phase_attn(nc, sc, c, l)
            if debug == "B":
                continue
            if debug != "D":
                phase_lru(nc, sc, c, l)
            if debug == "C":
                continue
            phase_mlstm(nc, sc, c, l)
            if debug == "D":
                continue
            phase_outproj(nc, sc, c, l, c.x if l == 0 else c.out)
            if debug == "E":
                continue
            phase_router(nc, sc, c, l)
            if debug == "R":
                continue
            phase_experts(nc, sc, c, l)
    return nc


def consts():
    t = np.arange(S)
    row = (t // 64).astype(np.float32)
    col = (t % 64).astype(np.float32)
    inv = (1.0 / (np.float32(10000.0) ** (np.arange(16, dtype=np.float32) / np.float32(16)))).astype(np.float32)
    ang = np.concatenate([row[:, None] * inv, col[:, None] * inv], axis=-1).astype(np.float32)
    cos = np.cos(ang).astype(np.float32)
    sin = np.sin(ang).astype(np.float32)
    import ml_dtypes
    return {"identb": np.eye(128, dtype=np.float32).astype(ml_dtypes.bfloat16),
            "ident": np.eye(128, dtype=np.float32), "ones": np.ones((128, 128), np.float32),
            "triu": np.triu(np.ones((128, 128), np.float32)), "tril": np.tril(np.ones((128, 128), np.float32)),
            "cos_rep": np.ascontiguousarray(np.tile(cos, (1, 10))), "sin_rep": np.ascontiguousarray(np.tile(sin, (1, 10)))}


def layout_small(inputs):
    o = {}
    qg = inputs["q_norm_g"]; kg = inputs["k_norm_g"]
    gg = np.concatenate([np.tile(qg, (1, 8)), np.tile(kg, (1, 2))], axis=1)
    o["ggrep"] = np.ascontiguousarray(np.broadcast_to(gg[:, None, :], (DEPTH, 128, 640))).astype(np.float32)
    go = np.concatenate([inputs["att_out_g"], inputs["lru_out_g"], np.ones((DEPTH, 256), np.float32)], axis=1)
    o["gout"] = np.ascontiguousarray(go.reshape(DEPTH, 8, 128).transpose(0, 2, 1)).astype(np.float32)
    o["g2rep"] = np.ascontiguousarray(inputs["norm2_g"].reshape(DEPTH, 8, 128).transpose(0, 2, 1)).astype(np.float32)
    o["fbrep"] = np.ascontiguousarray(np.broadcast_to(inputs["mlstm_f_bias"].reshape(DEPTH, 1, 8), (DEPTH, 128, 8))).astype(np.float32)
    o["ngrep"] = np.ascontiguousarray(np.broadcast_to(inputs["mlstm_norm_g"].reshape(DEPTH, 1, 256), (DEPTH, 128, 256))).astype(np.float32)
    cols = [inputs["conv_w"][:, j, :] for j in range(4)] + [inputs["conv_b"]]
    cols += [inputs["lru_ba"][:, d, :] for d in range(2)] + [inputs["lru_bx"][:, d, :] for d in range(2)]
    cols += [inputs["lru_lambda"][:, d, :] for d in range(2)]
    sm = np.stack(cols, axis=-1)
    o["lru_small"] = np.ascontiguousarray(sm.reshape(DEPTH, 2, 128, 11).transpose(0, 2, 1, 3)).astype(np.float32)
    lw = np.zeros((DEPTH, 128, 2, 2, 2, 128), np.float32)
    for kind, nm in enumerate(["lru_wa", "lru_wx"]):
        wsrc = inputs[nm]
        for d in range(2):
            for cc in range(2):
                for b in range(2):
                    lw[:, b * 64:(b + 1) * 64, d, kind, cc, b * 64:(b + 1) * 64] = wsrc[:, d, 2 * cc + b]
    o["lru_w"] = lw.reshape(DEPTH, 128, 8, 128)
    return o


SMALL = ["norm1_g", "q_norm_g", "k_norm_g", "conv_w", "conv_b", "lru_wa", "lru_ba", "lru_wx", "lru_bx", "lru_lambda",
         "mlstm_f_bias", "mlstm_norm_g", "att_out_g", "lru_out_g", "norm2_g"]
BIG = ["w_in", "w_out", "w_router", "w_expert_gate", "w_expert_up", "w_expert_down", "norm1_g"]


def kernel(**inputs):
    inputs = {k: np.asarray(v) for k, v in inputs.items()}
    nc = build()
    shared = {k: np.ascontiguousarray(inputs[k], dtype=np.float32) for k in BIG}
    shared.update(consts())
    shared.update(layout_small(inputs))
    x = np.ascontiguousarray(inputs["x"], dtype=np.float32)
    in_maps = []
    for b in range(8):
        m = dict(shared)
        m["x"] = x[b]
        in_maps.append(m)
    res = run_bass_kernel_spmd(nc, in_maps, core_ids=list(range(8)))
    return np.stack([r["out"] for r in res.results], axis=0).astype(np.float32)
```

```python
import numpy as np
from contextlib import ExitStack
import concourse.bass as bass
import concourse.mybir as mybir
from concourse.bass_utils import run_bass_kernel_spmd

F32 = mybir.dt.float32
F32R = mybir.dt.float32r
BF16 = mybir.dt.bfloat16
F16 = mybir.dt.float16
U32 = mybir.dt.uint32
I32 = mybir.dt.int32
AF = mybir.ActivationFunctionType
ALU = mybir.AluOpType
AX = mybir.AxisListType

S = 2048
D = 1024
DEPTH = 4
D_IN = 2320
NT = S // 128
EPS = 1e-6

ENG = ["pe", "act", "dve", "pool", "sp"]
BLK = {"pe": "tensor", "act": "scalar", "dve": "vector", "pool": "gpsimd", "sp": "sync"}


class Sched:
    def __init__(self, nc, es, n_dma=24):
        self.nc = nc
        self.sem = {e: es.enter_context(nc.semaphore("s_" + e)) for e in ENG}
        self.dsem = [es.enter_context(nc.semaphore("s_dma%d" % i)) for i in range(n_dma)]
        self.cnt = {e: 0 for e in ENG}
        self.duse = [0] * n_dma
        self.drr = 0
        self.ops = {e: [] for e in ENG}
        self.waited = {e: {} for e in ENG}
        self.last_w = {}
        self.readers = {}
        self.nops = 0

    def semobj(self, s):
        return self.sem[s] if isinstance(s, str) else self.dsem[s[1]]

    def add(self, eng, fn, reads=(), writes=(), dma=False):
        deps = {}
        def need(tok):
            s, v = tok
            if deps.get(s, 0) < v:
                deps[s] = v
        for r in reads:
            if r in self.last_w:
                need(self.last_w[r])
        for w in writes:
            if w in self.last_w:
                need(self.last_w[w])
            for t in self.readers.get(w, ()):
                need(t)
        if dma:
            k = self.drr
            self.drr = (self.drr + 1) % len(self.dsem)
            self.duse[k] += 1
            n = self.duse[k]
            if n > 1:
                need((("dma", k), 16 * (n - 1)))
            token = (("dma", k), 16 * n)
        else:
            self.cnt[eng] += 1
            token = (eng, self.cnt[eng])
        waits = []
        wd = self.waited[eng]
        for s, v in deps.items():
            if s == "pe" and eng == "pe" and not dma:
                continue
            if wd.get(s, 0) < v:
                wd[s] = v
                waits.append((s, v))
        self.ops[eng].append((waits, fn, token, dma))
        self.nops += 1
        for r in reads:
            self.readers.setdefault(r, []).append(token)
        for w in writes:
            self.last_w[w] = token
            self.readers[w] = []
        return token

    def flush(self):
        waits = []
        for k, n in enumerate(self.duse):
            if n and self.waited["sp"].get(("dma", k), 0) < 16 * n:
                waits.append((("dma", k), 16 * n))
        self.ops["sp"].append((waits, None, None, False))
        ops = self.ops
        self.ops = {e: [] for e in ENG}
        sched = self
        with self.nc.Block() as block:
            for e in ENG:
                lst = ops[e]

                def body(eng, lst=lst):
                    for waits, fn, token, isdma in lst:
                        for (s, v) in waits:
                            eng.wait_ge(sched.semobj(s), v)
                        if fn is None:
                            continue
                        inst = fn(eng)
                        inst.then_inc(sched.semobj(token[0]), 16 if isdma else 1)

                getattr(block, BLK[e])(body)
        for e in ENG:
            for e2 in ENG:
                self.waited[e][e2] = self.cnt[e2]
            for k, n in enumerate(self.duse):
                self.waited[e][("dma", k)] = 16 * n
        self.last_w = {}
        self.readers = {}

    def dma(self, q, out, in_, reads=(), writes=(), slow=False):
        if slow:
            return self.add(q, lambda e: e.dma_start(out=out, in_=in_, allow_slow_non_contiguous=True), reads, writes, dma=True)
        return self.add(q, lambda e: e.dma_start(out=out, in_=in_), reads, writes, dma=True)

    def mm(self, out, lhsT, rhs, start, stop, reads=(), writes=()):
        return self.add("pe", lambda e: e.matmul(out, lhsT, rhs, start=start, stop=stop), reads, writes)

    def tr(self, out, in_, ident, reads=(), writes=()):
        return self.add("pe", lambda e: e.transpose(out, in_, ident), reads, writes)

    def act(self, out, in_, func, reads=(), writes=(), eng="act", **kw):
        return self.add(eng, lambda e: e.activation(out, in_, func, **kw), reads, writes)

    def ts(self, eng, out, in0, s1, s2, op0, op1=None, reads=(), writes=(), **kw):
        if op1 is None:
            return self.add(eng, lambda e: e.tensor_scalar(out, in0, s1, None, op0, **kw), reads, writes)
        return self.add(eng, lambda e: e.tensor_scalar(out, in0, s1, s2, op0, op1, **kw), reads, writes)

    def tt(self, eng, out, in0, in1, op, reads=(), writes=()):
        return self.add(eng, lambda e: e.tensor_tensor(out, in0, in1, op), reads, writes)

    def stt(self, eng, out, in0, scalar, in1, op0, op1, reads=(), writes=()):
        return self.add(eng, lambda e: e.scalar_tensor_tensor(out, in0, scalar, in1, op0, op1), reads, writes)

    def copy(self, eng, out, in_, reads=(), writes=()):
        if eng == "act":
            return self.add(eng, lambda e: e.copy(out, in_), reads, writes)
        return self.add(eng, lambda e: e.tensor_copy(out, in_), reads, writes)


def r32(ap):
    return ap.bitcast(F32R)


class Ctx:
    pass


def phase_inproj(nc, sc, c, l, x_src):
    with ExitStack() as es:
        T = lambda name, shape, dt=F32: es.enter_context(nc.sbuf_tensor("%s_L%d" % (name, l), shape, dt))
        P = lambda name, shape, dt=F32: es.enter_context(nc.psum_tensor("%s_L%d" % (name, l), shape, dt))
        wt = T("a_wt", [128, 8, D_IN], BF16)
        g = T("a_g", [128, 8])
        ident = T("a_id", [128, 128])
        xt = [T("a_xt%d" % i, [128, D]) for i in range(2)]
        junk = T("a_junk", [128, D])
        st = [T("a_st%d" % i, [128, 4]) for i in range(2)]
        hT = [T("a_hT%d" % i, [128, 8, 512], BF16) for i in range(2)]
        og = [T("a_og%d" % i, [128, D_IN]) for i in range(2)]
        of = [T("a_of%d" % i, [128, 512]) for i in range(2)]
        pT = [P("a_pT%d" % i, [128, 512]) for i in range(4)]
        pm = [P("a_pm%d" % i, [128, 512]) for i in range(4)]

        sc.dma("sp", ident[:], c.ident, writes=["ident"])
        sc.dma("sp", g[:], c.norm1_g[l].rearrange("(c p) -> p c", p=128), writes=["g"], slow=True)
        for ch in range(8):
            sc.dma("pool", wt[:, ch, :], c.w_in[l][ch * 128:(ch + 1) * 128, :], writes=[("wt", ch)])

        pmi = 0
        for b in range(4):
            hb = hT[b % 2]
            hkey = ("hT", b % 2)
            for tl in range(4):
                i = b * 4 + tl
                xb = xt[i % 2]
                sb = st[i % 2]
                xk = ("xt", i % 2)
                sk = ("st", i % 2)
                sc.dma("sp", xb[:], x_src[i * 128:(i + 1) * 128, :], writes=[xk])
                sc.act(junk[:], xb[:], AF.Square, reads=[xk], writes=["junk", sk], accum_out=sb[:, 0:1])
                sc.ts("dve", sb[:, 1:2], sb[:, 0:1], 1.0 / D, EPS, ALU.mult, ALU.add, reads=[sk], writes=[sk])
                sc.act(sb[:, 2:3], sb[:, 1:2], AF.Sqrt, reads=[sk], writes=[sk])
                sc.add("dve", lambda e, o=sb[:, 3:4], i_=sb[:, 2:3]: e.reciprocal(o, i_), reads=[sk], writes=[sk])
                sc.ts("dve", xb[:], xb[:], sb[:, 3:4], None, ALU.mult, reads=[xk, sk], writes=[xk])
                for half in range(2):
                    pt = pT[(i % 2) * 2 + half]
                    pk = ("pT", (i % 2) * 2 + half)
                    for cc in range(4):
                        ch = half * 4 + cc
                        sc.tr(pt[:, cc * 128:(cc + 1) * 128], xb[:, ch * 128:(ch + 1) * 128], ident[:],
                              reads=[xk, "ident"], writes=[pk])
                    sc.tt("dve", hb[:, half * 4:(half + 1) * 4, tl * 128:(tl + 1) * 128],
                          pt[:].rearrange("p (c t) -> p c t", c=4),
                          g[:, half * 4:(half + 1) * 4].unsqueeze(2).broadcast_to([128, 4, 128]), ALU.mult,
                          reads=[pk, "g"], writes=[(hkey, tl)])
            for tl in range(4):
                i = b * 4 + tl
                ob = og[i % 2]
                ok = ("og", i % 2)
                for n in range(5):
                    n0 = n * 512
                    nw = min(512, D_IN - n0)
                    pmm = pm[pmi % 4]
                    pk = ("pm", pmi % 4)
                    pmi += 1
                    for ch in range(8):
                        sc.mm(pmm[:, 0:nw], hb[:, ch, tl * 128:(tl + 1) * 128], wt[:, ch, n0:n0 + nw],
                              ch == 0, ch == 7, reads=[(hkey, tl), ("wt", ch)], writes=[pk])
                    eng = "act" if n % 2 == 0 else "dve"
                    sc.copy(eng, ob[:, n0:n0 + nw], pmm[:, 0:nw], reads=[pk], writes=[ok])
                sc.dma("pool", c.p_tm[i * 128:(i + 1) * 128, :], ob[:], reads=[ok])
            for j in range(8):
                c0 = 768 + j * 128
                pmm = pm[pmi % 4]
                pk = ("pm", pmi % 4)
                pmi += 1
                ob = of[j % 2]
                ok = ("of", j % 2)
                for ch in range(8):
                    sc.mm(pmm[:, :], wt[:, ch, c0:c0 + 128], hb[:, ch, :], ch == 0, ch == 7,
                          reads=[(hkey, 0), (hkey, 1), (hkey, 2), (hkey, 3), ("wt", ch)], writes=[pk])
                eng = "act" if j % 2 == 0 else "dve"
                sc.copy(eng, ob[:], pmm[:], reads=[pk], writes=[ok])
                sc.dma("pool", c.p_fm[j * 128:(j + 1) * 128, b * 512:(b + 1) * 512], ob[:], reads=[ok])
        sc.flush()


def phase_attn(nc, sc, c, l):
    with ExitStack() as es:
        T = lambda name, shape, dt=F32: es.enter_context(nc.sbuf_tensor("%s_L%d" % (name, l), shape, dt))
        P = lambda name, shape, dt=F32: es.enter_context(nc.psum_tensor("%s_L%d" % (name, l), shape, dt))
        ident = T("b_id", [128, 128])
        ones = T("b_ones", [128, 128])
        gg = T("b_gg", [128, 640])
        qT = T("b_qT", [128, 4, S], BF16)
        kT = T("b_kT", [128, 2, S], BF16)
        vx = T("b_vx", [128, NT, 2, 65], BF16)
        qkv = [T("b_qkv%d" % i, [128, 768]) for i in range(2)]
        cs = [T("b_cs%d" % i, [128, 2, 320]) for i in range(2)]
        sq2 = [T("b_sq%d" % i, [128, 640]) for i in range(2)]
        st2 = [T("b_st%d" % i, [128, 4, 10]) for i in range(2)]
        qn2 = [T("b_qn%d" % i, [128, 640]) for i in range(2)]
        tmp2 = [T("b_tmp%d" % i, [128, 4, 320]) for i in range(2)]
        qr2 = [T("b_qr%d" % i, [128, 640]) for i in range(2)]
        Osb = [T("b_Osb%d" % i, [65, 512]) for i in range(2)]
        PT = [T("b_PT%d" % i, [128, 512], BF16) for i in range(3)]
        rd = [T("b_rd%d" % i, [128, 512]) for i in range(2)]
        bcs = [T("b_bcs%d" % i, [64, 512]) for i in range(2)]
        yo = [T("b_yo%d" % i, [64, 512]) for i in range(2)]
        ptA = [P("b_ptA%d" % i, [128, 512]) for i in range(1)]
        ptB = [P("b_ptB%d" % i, [128, 512]) for i in range(1)]
        sps = [P("b_sps%d" % i, [128, 512]) for i in range(3)]
        ops_ = [P("b_ops%d" % i, [128, 512]) for i in range(2)]
        bcp = P("b_bcp", [128, 512])

        sc.dma("sp", ident[:], c.ident, writes=["ident"])
        sc.dma("sp", ones[:], c.ones, writes=["ones"])
        sc.dma("sp", gg[:], c.ggrep[l], writes=["gg"])
        sc.add("pool", lambda e: e.memset(kT[:], 0.0), writes=["kT"])
        for i in range(NT):
            qb_ = qkv[i % 2]; qk = ("qkv", i % 2)
            sq = sq2[i % 2]; st = st2[i % 2]; qn = qn2[i % 2]; tmp = tmp2[i % 2]; qr = qr2[i % 2]
            K_ = lambda nm: (nm, i % 2)
            cb = cs[i % 2]; ck = ("cs", i % 2)
            for half in range(2):
                sc.dma("sp", qb_[:, 0:512].rearrange("p (pr half d) -> p pr half d", pr=4, half=2)[:, :, half, :],
                       c.p_tm[i * 128:(i + 1) * 128, half * 256:(half + 1) * 256].rearrange("t (pr d) -> t pr d", pr=4),
                       writes=[qk])
            sc.dma("sp", qb_[:, 512:768], c.p_tm[i * 128:(i + 1) * 128, 512:768], writes=[qk])
            sc.dma("sp", cb[:, 0, :], c.cos_rep[i * 128:(i + 1) * 128, :], writes=[ck])
            sc.dma("sp", cb[:, 1, :], c.sin_rep[i * 128:(i + 1) * 128, :], writes=[ck])
            sc.tt("pool", sq[:], qb_[:, 0:640], qb_[:, 0:640], ALU.mult, reads=[qk], writes=[K_("sq")])
            sc.add("dve", lambda e, o=st[:, 0, :], i_=sq[:].rearrange("p (h d) -> p h d", d=64):
                   e.tensor_reduce(o, i_, AX.X, ALU.add), reads=[K_("sq")], writes=[K_("st")])
            sc.ts("dve", st[:, 1, :], st[:, 0, :], 1.0 / 64, EPS, ALU.mult, ALU.add, reads=[K_("st")], writes=[K_("st")])
            sc.act(st[:, 2, :], st[:, 1, :], AF.Sqrt, reads=[K_("st")], writes=[K_("st")])
            sc.add("dve", lambda e, o=st[:, 3, :], i_=st[:, 2, :]: e.reciprocal(o, i_), reads=[K_("st")], writes=[K_("st")])
            sc.tt("dve", qn[:].rearrange("p (h d) -> p h d", d=64), qb_[:, 0:640].rearrange("p (h d) -> p h d", d=64),
                  st[:, 3, :].unsqueeze(2).broadcast_to([128, 10, 64]), ALU.mult, reads=[qk, K_("st")], writes=[K_("qn")])
            sc.tt("pool", qn[:], qn[:], gg[:], ALU.mult, reads=[K_("qn"), "gg"], writes=[K_("qn")])
            q3 = qn[:].rearrange("p (h d) -> p h d", d=64)
            x1 = q3[:, :, 0:32]; x2 = q3[:, :, 32:64]
            co = cb[:, 0, :].rearrange("p (h d) -> p h d", d=32); si = cb[:, 1, :].rearrange("p (h d) -> p h d", d=32)
            t = [tmp[:, k, :].rearrange("p (h d) -> p h d", d=32) for k in range(4)]
            r3 = qr[:].rearrange("p (h d) -> p h d", d=64)
            sc.tt("dve", t[0], x1, co, ALU.mult, reads=[K_("qn"), ck], writes=[K_("tmp0")])
            sc.tt("pool", t[1], x2, si, ALU.mult, reads=[K_("qn"), ck], writes=[K_("tmp1")])
            sc.tt("dve", t[2], x2, co, ALU.mult, reads=[K_("qn"), ck], writes=[K_("tmp2")])
            sc.tt("pool", t[3], x1, si, ALU.mult, reads=[K_("qn"), ck], writes=[K_("tmp3")])
            sc.tt("dve", r3[:, :, 0:32], t[0], t[1], ALU.subtract, reads=[K_("tmp0"), K_("tmp1")], writes=[K_("qr")])
            sc.tt("pool", r3[:, :, 32:64], t[2], t[3], ALU.add, reads=[K_("tmp2"), K_("tmp3")], writes=[K_("qr")])
            for pr in range(4):
                sc.tr(ptA[0][:, pr * 128:(pr + 1) * 128], qr[:, pr * 128:(pr + 1) * 128], ident[:], reads=[K_("qr"), "ident"], writes=["ptA"])
            sc.tr(ptB[0][:, 0:128], qr[:, 512:640], ident[:], reads=[K_("qr"), "ident"], writes=["ptB"])
            sc.copy("act", qT[:, :, i * 128:(i + 1) * 128], ptA[0][:].rearrange("p (c t) -> p c t", c=4),
                    reads=["ptA"], writes=["qT"])
            sc.copy("dve", kT[0:64, 0, i * 128:(i + 1) * 128], ptB[0][0:64, 0:128], reads=["ptB"], writes=["kT"])
            sc.copy("dve", kT[64:128, 1, i * 128:(i + 1) * 128], ptB[0][64:128, 0:128], reads=["ptB"], writes=["kT"])
            sc.copy("act", vx[:, i, :, 0:64], qb_[:, 640:768].rearrange("p (g d) -> p g d", d=64),
                    reads=[qk], writes=["vx"])
            sc.ts("pool", vx[:, i, :, 64:65], qb_[:, 0:2].unsqueeze(2), 0.0, 1.0, ALU.mult, ALU.add,
                  reads=[qk], writes=["vx"])

        steps = [(pr + 4 * half, qb, kt) for pr in range(4) for qb in range(4) for kt in range(NT) for half in range(2)]
        def fin(half, h, qb):
            O = ops_[half]; ok = ("O", half)
            osb = Osb[half]; osk = ("Osb", half)
            r = rd[half]; rk = ("rd", half)
            sc.copy("dve", osb[:, :], O[0:65, :], reads=[ok], writes=[osk])
            sc.act(r[64:65, :], osb[64:65, :], AF.Ln, reads=[osk], writes=[rk])
            sc.act(r[64:65, :], r[64:65, :], AF.Exp, reads=[rk], writes=[rk], scale=-1.0)
            sc.mm(bcp[0:64, :], ones[64:65, 0:64], r[64:65, :], True, True, reads=[rk, "ones"], writes=["bcp"])
            sc.tt("dve", yo[half][:], osb[0:64, :], bcp[0:64, :], ALU.mult, reads=[osk, "bcp"], writes=[("yo", half)])
            sc.dma("sp", c.yT[h * 64:(h + 1) * 64, qb * 512:(qb + 1) * 512], yo[half][:], reads=[("yo", half)])
        for s_ in range(len(steps) + 2):
            if s_ < len(steps):
                h, qb, kt = steps[s_]
                pr = h % 4; half = h // 4
                p0 = half * 64
                sp_ = sps[s_ % 3]; sk = ("sps", s_ % 3)
                sc.mm(sp_[:, :], kT[:, half, kt * 128:(kt + 1) * 128],
                      qT[:, pr, qb * 512:(qb + 1) * 512], True, True,
                      reads=["kT", "qT"], writes=[sk])
                sc.act(PT[s_ % 3][:], sp_[:], AF.Exp, reads=[sk], writes=[("PT", s_ % 3)], scale=0.125)
            if s_ >= 2:
                h, qb, kt = steps[s_ - 2]
                half = h // 4
                O = ops_[half]; ok = ("O", half)
                sc.mm(O[0:65, :], vx[:, kt, half, :], PT[(s_ - 2) % 3][:], kt == 0, kt == NT - 1,
                      reads=["vx", ("PT", (s_ - 2) % 3)], writes=[ok])
                if kt == NT - 1:
                    fin(half, h, qb)
        sc.flush()


def phase_lru(nc, sc, c, l):
    with ExitStack() as es:
        T = lambda name, shape, dt=F32: es.enter_context(nc.sbuf_tensor("%s_L%d" % (name, l), shape, dt))
        P = lambda name, shape, dt=F32: es.enter_context(nc.psum_tensor("%s_L%d" % (name, l), shape, dt))
        sm = T("c_sm", [128, 2, 11])
        W = T("c_W", [128, 8, 128])
        X = T("c_X", [128, S]); G = T("c_G", [128, S]); XF = T("c_XF", [128, S]); XC = T("c_XC", [128, S])
        R = T("c_R", [128, S]); I_ = T("c_I", [128, S]); A = T("c_A", [128, S]); U = T("c_U", [128, S])
        H = [T("c_H%d" % i, [128, S]) for i in range(2)]
        cf = T("c_cf", [128, 8])
        pp = [P("c_pp%d" % i, [128, 512]) for i in range(4)]
        sc.dma("sp", sm[:], c.lru_small[l], writes=["sm"])
        sc.dma("sp", r32(W[:]), r32(c.lru_w[l]), writes=["W"])
        ppi = 0
        for cc in range(2):
            sc.dma("sp", X[:], c.p_fm[cc * 128:(cc + 1) * 128, :], writes=["X"])
            sc.dma("sp", G[:], c.p_fm[256 + cc * 128:256 + (cc + 1) * 128, :], writes=["G"])
            w = lambda j: sm[:, cc, j:j + 1]
            sc.ts("dve", XF[:], X[:], w(2), w(4), ALU.mult, ALU.add, reads=["X", "sm"], writes=["XF"])
            sc.stt("dve", XF[:, 2:S], X[:, 0:S - 2], w(0), XF[:, 2:S], ALU.mult, ALU.add, reads=["X", "XF", "sm"], writes=["XF"])
            sc.stt("dve", XF[:, 1:S], X[:, 0:S - 1], w(1), XF[:, 1:S], ALU.mult, ALU.add, reads=["X", "XF", "sm"], writes=["XF"])
            sc.copy("pool", r32(XC[:, S - 1:S]), XF[:, S - 1:S], reads=["XF"], writes=["XC"])
            sc.stt("dve", r32(XC[:, 0:S - 1]), X[:, 1:S], w(3), XF[:, 0:S - 1], ALU.mult, ALU.add, reads=["X", "XF", "sm"], writes=["XC"])
            for d in range(2):
                ba = sm[:, cc, 5 + d:6 + d]; bx = sm[:, cc, 7 + d:8 + d]; lam = sm[:, cc, 9 + d:10 + d]
                ck = ("cf", d)
                c0 = cf[:, 4 * d:4 * d + 1]; c1 = cf[:, 4 * d + 1:4 * d + 2]; c2 = cf[:, 4 * d + 2:4 * d + 3]
                sc.act(c0, lam, AF.Exp, reads=["sm"], writes=[ck], scale=-1.0)
                sc.act(c0, c0, AF.Ln, reads=[ck], writes=[ck], bias=1.0)
                sc.ts("dve", c1, c0, -8.0, None, ALU.mult, reads=[ck], writes=[ck])
                sc.ts("dve", c2, c0, -16.0, None, ALU.mult, reads=[ck], writes=[ck])
                for tb in range(4):
                    sl = slice(tb * 512, (tb + 1) * 512)
                    pa = pp[ppi % 4]; pak = ("pp", ppi % 4); ppi += 1
                    px = pp[ppi % 4]; pxk = ("pp", ppi % 4); ppi += 1
                    sc.mm(pa[:, :], r32(W[:, d * 4 + 0 * 2 + cc, :]), r32(XC[:, sl]), True, True, reads=["W", "XC"], writes=[pak])
                    sc.mm(px[:, :], r32(W[:, d * 4 + 1 * 2 + cc, :]), r32(XC[:, sl]), True, True, reads=["W", "XC"], writes=[pxk])
                    sc.act(R[:, sl], pa[:, :], AF.Sigmoid, reads=[pak, "sm"], writes=["R"], bias=ba)
                    sc.act(I_[:, sl], px[:, :], AF.Sigmoid, reads=[pxk, "sm"], writes=["I"], bias=bx)
                sc.act(A[:], R[:], AF.Exp, reads=["R", ck], writes=["A"], scale=c1)
                sc.act(U[:], R[:], AF.Exp, reads=["R", ck], writes=["U"], scale=c2)
                sc.ts("pool", U[:], U[:], -1.0, 1.0, ALU.mult, ALU.add, reads=["U"], writes=["U"])
                sc.act(U[:], U[:], AF.Sqrt, reads=["U"], writes=["U"])
                sc.tt("pool", I_[:], I_[:], XC[:], ALU.mult, reads=["I", "XC"], writes=["I"])
                sc.tt("dve", U[:], U[:], I_[:], ALU.mult, reads=["U", "I"], writes=["U"])
                if d == 0:
                    sc.add("dve", lambda e, o=H[0][:], a=A[:], u=U[:]: e.tensor_tensor_scan(o, a, u, 0.0, ALU.mult, ALU.add),
                           reads=["A", "U"], writes=[("H", 0)])
                else:
                    sc.add("dve", lambda e, o=H[1][:, ::-1], a=A[:, ::-1], u=U[:, ::-1]: e.tensor_tensor_scan(o, a, u, 0.0, ALU.mult, ALU.add),
                           reads=["A", "U"], writes=[("H", 1)])
            sc.tt("pool", H[0][:], H[0][:], H[1][:], ALU.add, reads=[("H", 0), ("H", 1)], writes=[("H", 0)])
            sc.tt("dve", R[:], G[:], G[:], ALU.mult, reads=["G", "R"], writes=["R"])
            sc.ts("dve", R[:], R[:], 0.044715, 1.0, ALU.mult, ALU.add, reads=["R"], writes=["R"])
            sc.tt("dve", R[:], R[:], G[:], ALU.mult, reads=["R", "G"], writes=["R"])
            sc.act(R[:], R[:], AF.Sigmoid, reads=["R"], writes=["R"], scale=1.5957691216057308)
            sc.tt("pool", H[0][:], H[0][:], G[:], ALU.mult, reads=[("H", 0), "G"], writes=[("H", 0)])
            sc.tt("dve", H[0][:], H[0][:], R[:], ALU.mult, reads=[("H", 0), "R"], writes=[("H", 0)])
            sc.dma("pool", c.yT[512 + cc * 128:512 + (cc + 1) * 128, :], H[0][:], reads=[("H", 0)])
        sc.flush()


def phase_mlstm(nc, sc, c, l):
    with ExitStack() as es:
        T = lambda name, shape, dt=F32: es.enter_context(nc.sbuf_tensor("%s_L%d" % (name, l), shape, dt))
        P = lambda name, shape, dt=F32: es.enter_context(nc.psum_tensor("%s_L%d" % (name, l), shape, dt))
        ident = T("d_id", [128, 128]); ones = T("d_ones", [128, 128])
        mask = T("d_mask", [128, 2, 128])
        fb = T("d_fb", [128, 8]); ng = T("d_ng", [128, 256])
        qT = T("d_qT", [128, 4, S], BF16); kT = T("d_kT", [128, 2, S], BF16)
        tmk = T("d_tmk", [128, NT, 256], BF16)
        tm = T("d_tm", [128, NT, 784])
        vp = T("d_vp", [128, NT, 8, 65], BF16)
        Cb = T("d_Cb", [128, 2, 4, 65], BF16)
        wA = T("d_wA", [128, NT, 8]); wB = T("d_wB", [128, NT, 8]); eg = T("d_eg", [128, NT, 8])
        nlf = T("d_nlf", [128, NT, 8]); tg = T("d_tg", [128, NT, 8])
        hs = T("d_hs", [128, NT, 256])
        Cst = T("d_C", [128, 2, 4, 65])
        tC = T("d_tC", [128, 65])
        PT = [T("d_PT%d" % i, [128, 128], BF16) for i in range(3)]
        s4 = [T("d_s4%d" % i, [128, 4, 4]) for i in range(2)]
        sq = T("d_sq", [128, 256]); st = T("d_st", [128, 4, 4]); ym = [T("d_ym%d" % i, [128, 256]) for i in range(2)]
        yo = [T("d_yo%d" % i, [128, 256]) for i in range(2)]
        sg = T("d_sg", [128, 256])
        pg = P("d_pg", [128, 512])
        psS = [P("d_pS%d" % i, [128, 512]) for i in range(2)]
        pacc = [P("d_pa%d" % i, [128, 512]) for i in range(2)]
        pkv = [P("d_pk%d" % i, [128, 512]) for i in range(2)]
        ptr = P("d_ptr", [128, 512])

        sc.dma("sp", ident[:], c.ident, writes=["ident"])
        sc.dma("sp", ones[:], c.ones, writes=["ones"])
        sc.dma("sp", mask[:, 0, :], c.triu, writes=["mask"])
        sc.dma("sp", mask[:, 1, :], c.tril, writes=["mask"])
        sc.dma("sp", fb[:], c.fbrep[l], writes=["fb"])
        sc.dma("sp", ng[:], c.ngrep[l], writes=["ng"])
        sc.add("pool", lambda e: e.memset(qT[:], 0.0), writes=["qT"])
        for h in range(4):
            p0 = (h % 2) * 64
            sc.dma("pool", qT[p0:p0 + 64, h, :], c.p_fm[512 + h * 64:512 + (h + 1) * 64, :], reads=["qT"], writes=[("qTh", h)])
        for ch in range(2):
            sc.dma("pool", kT[:, ch, :], c.p_fm[768 + ch * 128:768 + (ch + 1) * 128, :], writes=["kT"])
        for i in range(NT):
            sc.dma("sp", tm[:, i, 256:784], c.p_tm[i * 128:(i + 1) * 128, 1792:2320], writes=[("tm", i)])
            sc.dma("pool", tmk[:, i, :], c.p_tm[i * 128:(i + 1) * 128, 1536:1792], writes=[("tmk", i)])
        sc.add("dve", lambda e: e.memset(Cst[:], 0.0), writes=["C"])
        sc.add("dve", lambda e: e.memset(Cb[:], 0.0), writes=["Cb"])
        for i in range(NT):
            gt = tm[:, i, 768:784]
            fview = gt.rearrange("p (d k h) -> p d k h", d=2, k=2)[:, :, 1, :]
            iview = gt.rearrange("p (d k h) -> p d k h", d=2, k=2)[:, :, 0, :]
            tk = ("tg", i)
            sc.tt("dve", tg[:, i, :].rearrange("p (d h) -> p d h", d=2), fview, fb[:].rearrange("p (d h) -> p d h", d=2),
                  ALU.add, reads=[("tm", i), "fb"], writes=[tk])
            sc.act(tg[:, i, :], tg[:, i, :], AF.Exp, reads=[tk], writes=[tk], scale=-1.0)
            sc.act(nlf[:, i, :], tg[:, i, :], AF.Ln, reads=[tk], writes=[("nlf", i)], bias=1.0)
            sc.mm(pg[:, 0:4], mask[:, 0, :], nlf[:, i, 0:4], True, True, reads=["mask", ("nlf", i)], writes=["pg"])
            sc.mm(pg[:, 4:8], mask[:, 1, :], nlf[:, i, 4:8], True, True, reads=["mask", ("nlf", i)], writes=["pg"])
            sc.mm(pg[:, 8:16], ones[:, :], nlf[:, i, :], True, True, reads=["ones", ("nlf", i)], writes=["pg"])
            sc.act(wA[:, i, :], pg[:, 0:8], AF.Exp, reads=["pg"], writes=[("wA", i)], scale=-1.0)
            sc.act(eg[:, i, :], pg[:, 8:16], AF.Exp, reads=["pg"], writes=[("eg", i)], scale=-1.0)
            sc.tt("dve", wB[:, i, :].rearrange("p (d h) -> p d h", d=2), iview, pg[:, 0:8].rearrange("p (d h) -> p d h", d=2),
                  ALU.add, reads=[("tm", i), "pg"], writes=[("wB", i)])
            sc.act(wB[:, i, :], wB[:, i, :], AF.Exp, reads=[("wB", i)], writes=[("wB", i)])
            v3 = tm[:, i, 256:512].rearrange("p (h d) -> p h d", d=64)
            for d in range(2):
                eng = "dve" if d == 0 else "pool"
                sc.tt(eng, vp[:, i, d * 4:(d + 1) * 4, 0:64], v3, wB[:, i, d * 4:(d + 1) * 4].unsqueeze(2).broadcast_to([128, 4, 64]),
                      ALU.mult, reads=[("tm", i), ("wB", i)], writes=[("vp", i)])
            sc.copy("pool", vp[:, i, :, 64:65], wB[:, i, :].unsqueeze(2), reads=[("wB", i)], writes=[("vp", i)])
        ui = 0
        for s_ in range(NT):
            for d in range(2):
                i = s_ if d == 0 else NT - 1 - s_
                tsl = slice(i * 128, (i + 1) * 128)
                acc = pacc[d]; ak = ("pacc", d)
                for h in range(4):
                    p0 = (h % 2) * 64; ch = h // 2
                    pS = psS[ui % 2]; pk_ = ("pS", ui % 2)
                    pt = PT[ui % 3]; ptk = ("PT", ui % 3)
                    kv = pkv[ui % 2]; kvk = ("pkv", ui % 2)
                    ui += 1
                    sc.mm(pS[:, 0:128], kT[:, ch, tsl], qT[:, h, tsl], True, True,
                          reads=["kT", ("qTh", h)], writes=[pk_])
                    sc.stt("dve", pt[:], pS[:, 0:128], 0.125, mask[:, d, :], ALU.mult, ALU.mult,
                           reads=[pk_, "mask"], writes=[ptk])
                    sc.mm(acc[:, h * 65:(h + 1) * 65], pt[:], vp[:, i, d * 4 + h, :], True, False,
                          reads=[ptk, ("vp", i)], writes=[ak])
                    sc.mm(acc[:, h * 65:(h + 1) * 65], qT[:, h, tsl], Cb[:, d, h, :], False, True,
                          reads=[("qTh", h), ("Cb", d, h), "Cb"], writes=[ak])
                    if s_ < NT - 1:
                        sc.mm(kv[:, 0:65], tmk[:, i, ch * 128:(ch + 1) * 128], vp[:, i, d * 4 + h, :], True, True,
                              reads=[("tmk", i), ("vp", i)], writes=[kvk])
                        sc.stt("dve", tC[p0:p0 + 64, :], kv[p0:p0 + 64, 0:65], 0.125, Cst[p0:p0 + 64, d, h, :], ALU.mult, ALU.add,
                               reads=[kvk, ("C", d, h)], writes=["tC"])
                        sc.ts("dve", Cb[p0:p0 + 64, d, h, :], tC[p0:p0 + 64, :], eg[p0:p0 + 64, i, d * 4 + h:d * 4 + h + 1], None, ALU.mult,
                              reads=["tC", ("eg", i)], writes=[("Cb", d, h)])
                        sc.ts("pool", Cst[p0:p0 + 64, d, h, :], tC[p0:p0 + 64, :], eg[p0:p0 + 64, i, d * 4 + h:d * 4 + h + 1], None, ALU.mult,
                              reads=["tC", ("eg", i)], writes=[("C", d, h)])
                a3 = acc[:, 0:260].rearrange("p (h e) -> p h e", e=65)
                sb = s4[d]; sk = ("s4", d)
                sc.tt("dve", sb[:, 0, :], a3[:, :, 64], wA[:, i, d * 4:(d + 1) * 4], ALU.mult, reads=[ak, ("wA", i)], writes=[sk])
                sc.act(sb[:, 1, :], sb[:, 0, :], AF.Abs, reads=[sk], writes=[sk])
                sc.ts("dve", sb[:, 1, :], sb[:, 1, :], 1.0, None, ALU.max, reads=[sk], writes=[sk])
                sc.add("dve", lambda e, o=sb[:, 2, :], i_=sb[:, 1, :]: e.reciprocal(o, i_), reads=[sk], writes=[sk])
                sc.tt("dve", sb[:, 3, :], sb[:, 2, :], wA[:, i, d * 4:(d + 1) * 4], ALU.mult, reads=[sk, ("wA", i)], writes=[sk])
                h3 = hs[:, i, :].rearrange("p (h e) -> p h e", e=64)
                if s_ < NT // 2:
                    sc.tt("dve", h3, a3[:, :, 0:64], sb[:, 3, :].unsqueeze(2).broadcast_to([128, 4, 64]), ALU.mult,
                          reads=[ak, sk], writes=[("hs", i)])
                else:
                    sc.tt("dve", sq[:].rearrange("p (h e) -> p h e", e=64), a3[:, :, 0:64],
                          sb[:, 3, :].unsqueeze(2).broadcast_to([128, 4, 64]), ALU.mult, reads=[ak, sk], writes=["sq"])
                    sc.tt("pool", hs[:, i, :], hs[:, i, :], sq[:], ALU.add, reads=[("hs", i), "sq"], writes=[("hs", i)])
        for i in range(NT):
            y = ym[i % 2]; yk = ("ym", i % 2)
            sc.tt("pool", sq[:], hs[:, i, :], hs[:, i, :], ALU.mult, reads=[("hs", i)], writes=["sq"])
            sc.add("dve", lambda e, o=st[:, 0, :], i_=sq[:].rearrange("p (h d) -> p h d", d=64):
                   e.tensor_reduce(o, i_, AX.X, ALU.add), reads=["sq"], writes=["st"])
            sc.ts("dve", st[:, 1, :], st[:, 0, :], 1.0 / 64, EPS, ALU.mult, ALU.add, reads=["st"], writes=["st"])
            sc.act(st[:, 2, :], st[:, 1, :], AF.Sqrt, reads=["st"], writes=["st"])
            sc.add("dve", lambda e, o=st[:, 3, :], i_=st[:, 2, :]: e.reciprocal(o, i_), reads=["st"], writes=["st"])
            sc.tt("dve", y[:].rearrange("p (h d) -> p h d", d=64), hs[:, i, :].rearrange("p (h d) -> p h d", d=64),
                  st[:, 3, :].unsqueeze(2).broadcast_to([128, 4, 64]), ALU.mult, reads=[("hs", i), "st"], writes=[yk])
            sc.tt("pool", y[:], y[:], ng[:], ALU.mult, reads=[yk, "ng"], writes=[yk])
            sc.act(sg[:], tm[:, i, 512:768], AF.Sigmoid, reads=[("tm", i)], writes=["sg"])
            sc.tt("dve", y[:], y[:], sg[:], ALU.mult, reads=[yk, "sg"], writes=[yk])
            for ch in range(2):
                sc.tr(ptr[:, ch * 128:(ch + 1) * 128], y[:, ch * 128:(ch + 1) * 128], ident[:], reads=[yk, "ident"], writes=["ptr"])
            o = yo[i % 2]; ok = ("yo", i % 2)
            sc.copy("act", o[:], ptr[:, 0:256], reads=["ptr"], writes=[ok])
            sc.dma("pool", c.yT[768:1024, i * 128:(i + 1) * 128].rearrange("(ch p) t -> p ch t", p=128),
                   o[:].rearrange("p (ch t) -> p ch t", ch=2), reads=[ok])
        sc.flush()


BF16 = mybir.dt.bfloat16
F16 = mybir.dt.float16
NE = 16
CAP = 256
WQ = "pool"


def phase_outproj_router(nc, sc, c, l, x_src):
    with ExitStack() as es:
        T = lambda name, shape, dt=F32: es.enter_context(nc.sbuf_tensor("%s_L%d" % (name, l), shape, dt))
        P = lambda name, shape, dt=F32: es.enter_context(nc.psum_tensor("%s_L%d" % (name, l), shape, dt))
        wt = T("e_wt", [128, 8, D], BF16); yh = [T("e_yh%d" % i, [128, 8, 512], BF16) for i in range(2)]
        g = T("e_g", [128, 8]); ones = T("e_ones", [128, 128])
        yb = [T("e_yb%d" % i, [128, 8, 512]) for i in range(2)]
        ysq = T("e_ysq", [128, 6, 512])
        xt = [T("e_xt%d" % i, [128, D]) for i in range(3)]
        stb = [T("e_stb%d" % i, [128, 4, 8]) for i in range(2)]
        pm = [P("e_pm%d" % i, [128, 512]) for i in range(3)]
        pst = P("e_pst", [128, 512])
        ident = T("f_id", [128, 128]); g2 = T("f_g2", [128, 8]); wr = T("f_wr", [128, 8, NE])
        xn = [T("f_xn%d" % i, [128, D]) for i in range(2)]
        junk = T("f_junk", [128, D])
        hb = [T("f_hb%d" % i, [128, D], BF16) for i in range(2)]
        hT = [T("f_hT%d" % i, [128, 8, 128]) for i in range(2)]
        rs = [T("f_st%d" % i, [128, 8]) for i in range(6)]
        aff = T("f_aff", [128, NT, NE]); ex = [T("f_ex%d" % i, [128, NE]) for i in range(2)]
        affT = T("f_affT", [NE, S]); work = T("f_work", [NE, S])
        gem = T("f_gem", [NE, CAP]); iem = T("f_iem", [NE, CAP], U32); ief = T("f_ief", [NE, CAP])
        idxs = T("f_idxs", [128, 2, NE], I32); gts = T("f_gts", [128, 2, NE])
        pT = [P("f_pT%d" % i, [128, 512]) for i in range(2)]
        pla = P("f_pla", [128, 512]); pla2 = P("f_pla2", [128, 512])

        sc.dma("sp", ones[:], c.ones, writes=["ones"])
        sc.dma("sp", g[:], c.gout[l], writes=["g"])
        for ch in range(8):
            sc.dma("pool", wt[:, ch, :], c.w_out[l][ch * 128:(ch + 1) * 128, :], writes=[("wt", ch)])
        sc.dma("sp", ident[:], c.ident, writes=["ident"])
        sc.dma("sp", g2[:], c.g2rep[l], writes=["g2"])
        sc.dma("sp", wr[:], c.w_router[l].rearrange("(c p) e -> p c e", p=128), writes=["wr"])
        sc.tt("dve", wr[:], wr[:], g2[:].unsqueeze(2).broadcast_to([128, 8, NE]), ALU.mult, reads=["wr", "g2"], writes=["wr"])

        NRS = 6
        def router_stage(k, i):
            if i < 0 or i >= NT:
                return
            x = xt[i % 3]; xk = ("xt", i % 3)
            xs = xn[i % 2]; xsk = ("xn", i % 2); sb = rs[i % NRS]; sk = ("rs", i % NRS)
            if k == 0:
                sc.act(junk[:], x[:], AF.Square, reads=[xk], writes=["junk", sk], accum_out=sb[:, 0:1])
                sc.ts("dve", sb[:, 1:2], sb[:, 0:1], 1.0 / D, EPS, ALU.mult, ALU.add, reads=[sk], writes=[sk])
                sc.act(sb[:, 2:3], sb[:, 1:2], AF.Sqrt, reads=[sk], writes=[sk])
            elif k == 1:
                sc.add("dve", lambda e, o=sb[:, 3:4], i_=sb[:, 2:3]: e.reciprocal(o, i_), reads=[sk], writes=[sk])
                sc.ts("dve", xs[:], x[:], sb[:, 3:4], None, ALU.mult, reads=[xk, sk], writes=[xsk])
                sc.copy("act", hb[i % 2][:], xs[:], reads=[xsk], writes=[("hb", i % 2)])
                sc.dma("sp", c.h2n[i * 128:(i + 1) * 128, :], hb[i % 2][:], reads=[("hb", i % 2)])
                for half in range(2):
                    pt = pT[half]; pk = ("pT", half)
                    for cc in range(4):
                        ch = half * 4 + cc
                        sc.tr(pt[:, cc * 128:(cc + 1) * 128], xs[:, ch * 128:(ch + 1) * 128], ident[:], reads=[xsk, "ident"], writes=[pk])
            elif k == 2:
                for half in range(2):
                    pt = pT[half]; pk = ("pT", half)
                    sc.copy("act", hT[i % 2][:, half * 4:(half + 1) * 4, :],
                            pt[:].rearrange("p (c t) -> p c t", c=4), reads=[pk], writes=[("hT", i % 2)])
                for ch in range(8):
                    sc.mm(pla[:, 0:NE], hT[i % 2][:, ch, :], wr[:, ch, :], ch == 0, ch == 7, reads=[("hT", i % 2), "wr"], writes=["pl"])
            elif k == 3:
                sc.add("dve", lambda e, o=sb[:, 4:5], i_=pla[:, 0:NE]: e.tensor_reduce(o, i_, AX.X, ALU.max), reads=["pl"], writes=[sk])
                sc.ts("dve", sb[:, 5:6], sb[:, 4:5], -1.0, None, ALU.mult, reads=[sk], writes=[sk])
                sc.act(ex[i % 2][:], pla[:, 0:NE], AF.Exp, reads=["pl", sk], writes=[("ex", i % 2), sk], bias=sb[:, 5:6], accum_out=sb[:, 6:7])
            elif k == 4:
                sc.add("dve", lambda e, o=sb[:, 7:8], i_=sb[:, 6:7]: e.reciprocal(o, i_), reads=[sk], writes=[sk])
                sc.ts("dve", aff[:, i, :], ex[i % 2][:], sb[:, 7:8], None, ALU.mult, reads=[("ex", i % 2), sk], writes=[("aff", i)])
                sc.tr(pla2[0:NE, 128:256], aff[:, i, :], ident[:], reads=[("aff", i), "ident"], writes=["pa"])
            elif k == 5:
                sc.copy("act", affT[:, i * 128:(i + 1) * 128], pla2[0:NE, 128:256], reads=["pa"], writes=["affT"])
        def router_iter(it):
            for k in (5, 4, 3, 2, 1, 0):
                router_stage(k, it - 1 - k)

        pi = 0
        for b in range(4):
            y = yb[b % 2]; ykey = ("yb", b % 2)
            for ch in range(8):
                sc.dma("sp", y[:, ch, :], c.yT[ch * 128:(ch + 1) * 128, b * 512:(b + 1) * 512], writes=[ykey])
            sc.act(ysq[:], y[:, 0:6, :], AF.Square, reads=[ykey], writes=["ysq"])
            yhb = yh[b % 2]; yhk = ("yh", b % 2)
            for ch in range(8):
                sc.act(yhb[:, ch, :], y[:, ch, :], AF.Copy, reads=[ykey, "g"], writes=[yhk], scale=g[:, ch:ch + 1])
            sbb = stb[b % 2]; sbk = ("stb", b % 2)
            for tl in range(4):
                tsl = slice(tl * 128, (tl + 1) * 128)
                for ch in range(6):
                    col = tl * 2 + (0 if ch < 4 else 1)
                    sc.mm(pst[:, col:col + 1], ysq[:, ch, tsl], ones[:, 0:1], ch in (0, 4), ch in (3, 5), reads=["ysq", "ones"], writes=["pst"])
            p3 = pst[:, 0:8].rearrange("p (t k) -> p t k", k=2)
            sc.ts("dve", sbb[:, :, 0:1], p3[:, :, 0:1], 1.0 / 512, EPS, ALU.mult, ALU.add, reads=["pst"], writes=[sbk])
            sc.ts("dve", sbb[:, :, 1:2], p3[:, :, 1:2], 1.0 / 256, EPS, ALU.mult, ALU.add, reads=["pst"], writes=[sbk])
            sc.act(sbb[:, :, 2:4], sbb[:, :, 0:2], AF.Sqrt, reads=[sbk], writes=[sbk])
            sc.add("dve", lambda e, o=sbb[:, :, 4:6], i_=sbb[:, :, 2:4]: e.reciprocal(o, i_), reads=[sbk], writes=[sbk])
            for tl in range(4):
                i = b * 4 + tl
                tsl = slice(tl * 128, (tl + 1) * 128)
                x = xt[i % 3]; xk = ("xt", i % 3)
                sc.dma("sp", x[:], x_src[i * 128:(i + 1) * 128, :], writes=[xk])
                sb = stb[b % 2][:, tl, :]; sk = ("stb", b % 2)
                for n in range(2):
                    nsl = slice(n * 512, (n + 1) * 512)
                    ps = []
                    for (c0, c1) in ((0, 4), (4, 6), (6, 8)):
                        p_ = pm[pi % 3]; pk = ("pm", pi % 3); pi += 1
                        for ch in range(c0, c1):
                            sc.mm(p_[:, :], yhb[:, ch, tsl], wt[:, ch, nsl], ch == c0, ch == c1 - 1,
                                  reads=[yhk, ("wt", ch)], writes=[pk])
                        ps.append((p_, pk))
                    sc.stt("dve", x[:, nsl], ps[0][0][:, :], sb[:, 4:5], x[:, nsl], ALU.mult, ALU.add, reads=[ps[0][1], sk, xk], writes=[xk])
                    sc.stt("dve", x[:, nsl], ps[1][0][:, :], sb[:, 5:6], x[:, nsl], ALU.mult, ALU.add, reads=[ps[1][1], sk, xk], writes=[xk])
                    sc.tt("dve", x[:, nsl], x[:, nsl], ps[2][0][:, :], ALU.add, reads=[ps[2][1], xk], writes=[xk])
                sc.dma("sp", c.out[i * 128:(i + 1) * 128, :], x[:], reads=[xk])
                router_iter(i)
        for it in range(NT, NT + 7):
            router_iter(it)
        sc.copy("dve", work[:], affT[:], reads=["affT"], writes=["work"])
        for r in range(CAP // 8):
            sl8 = slice(r * 8, (r + 1) * 8)
            sc.add("dve", lambda e, o=gem[:, sl8]: e.max(o, work[:]), reads=["work"], writes=["gem"])
            sc.add("dve", lambda e, o=iem[:, sl8], m=gem[:, sl8]: e.max_index(o, m, work[:]), reads=["gem", "work"], writes=["iem"])
            if r < CAP // 8 - 1:
                sc.add("dve", lambda e, m=gem[:, sl8]: e.match_replace(work[:], m, work[:], -1e30), reads=["gem", "work"], writes=["work"])
        sc.copy("dve", ief[:], iem[:], reads=["iem"], writes=["ief"])
        for ct in range(2):
            pa_ = pT[ct]; pak = ("pT", ct)
            sc.tr(pa_[:, 0:NE], ief[:, ct * 128:(ct + 1) * 128], ident[0:NE, 0:NE], reads=["ief", "ident"], writes=[pak])
            sc.tr(pa_[:, NE:2 * NE], gem[:, ct * 128:(ct + 1) * 128], ident[0:NE, 0:NE], reads=["gem", "ident"], writes=[pak])
            sc.copy("act", idxs[:, ct, :], pa_[:, 0:NE], reads=[pak], writes=["idxs"])
            sc.copy("act", gts[:, ct, :], pa_[:, NE:2 * NE], reads=[pak], writes=["gts"])
        sc.dma("sp", c.idx_d, idxs[:].rearrange("p a b -> p (a b)"), reads=["idxs"])
        sc.dma("sp", c.gate_d, gts[:].rearrange("p a b -> p (a b)"), reads=["gts"])
        sc.flush()


def phase_experts(nc, sc, c, l):
    NWB = 3
    NBLK = 4
    with ExitStack() as es:
        T = lambda name, shape, dt=F32: es.enter_context(nc.sbuf_tensor("%s_L%d" % (name, l), shape, dt))
        P = lambda name, shape, dt=F32: es.enter_context(nc.psum_tensor("%s_L%d" % (name, l), shape, dt))
        identb = T("g_idb", [128, 128], BF16)
        Wg = [T("g_Wg%d" % i, [128, 8, 512], BF16) for i in range(NWB)]
        Wu = [T("g_Wu%d" % i, [128, 8, 512], BF16) for i in range(NWB)]
        Wd = [T("g_Wd%d" % i, [128, 4, D], BF16) for i in range(NWB)]
        xtok = [[T("g_xt%d_%d" % (i, ct), [128, D], BF16) for ct in range(2)] for i in range(2)]
        xg = T("g_xg", [128, 8, CAP], BF16)
        H = [T("g_H%d" % i, [128, 4, CAP], BF16) for i in range(2)]
        sil = [T("g_sil%d" % i, [128, CAP]) for i in range(2)]
        ye = [[T("g_ye%d_%d" % (i, ct), [128, D]) for ct in range(2)] for i in range(2)]
        idxs = T("g_idx", [128, 2 * NE], I32); gts = T("g_gts", [128, 2 * NE])
        g2 = T("g_g2", [128, 8])
        yacc = [P("g_ya%d" % i, [128, 512]) for i in range(4)]
        au = [P("g_au%d" % i, [128, 512]) for i in range(2)]
        tp = [P("g_tp%d" % i, [128, 1024], BF16) for i in range(2)]
        sc.dma("sp", idxs[:], c.idx_d, writes=["idxs"])
        sc.dma("sp", gts[:], c.gate_d, writes=["gts"])
        sc.dma("sp", identb[:], c.identb, writes=["identb"])
        sc.dma("sp", g2[:], c.g2rep[l], writes=["g2"])
        wq = [0]
        def load_w(e, wb):
            b = wq[0] % NWB; wq[0] += 1
            hs_ = slice(wb * 512, (wb + 1) * 512)
            sc.dma("pool", Wg[b][:], c.w_gate[l, e][:, hs_].rearrange("(c p) h -> p c h", p=128), writes=[("Wg", b)])
            sc.dma("pool", Wu[b][:], c.w_up[l, e][:, hs_].rearrange("(c p) h -> p c h", p=128), writes=[("Wu", b)])
            sc.dma("pool", Wd[b][:], c.w_down[l, e][wb * 512:(wb + 1) * 512, :].rearrange("(c p) f -> p c f", p=128), writes=[("Wd", b)])
            return b
        def gather(e):
            for ct in range(2):
                col = ct * NE + e
                sc.add("pool", lambda g, o=xtok[e % 2][ct][:, :], ix=idxs[:, col:col + 1]:
                       g.indirect_dma_start(out=o, out_offset=None, in_=c.h2n[:, :],
                                            in_offset=bass.IndirectOffsetOnAxis(ap=ix, axis=0)),
                       reads=["idxs", "h2n_d"], writes=[("xtok", e % 2, ct)], dma=True)
        seq = [(e, wb) for e in range(NE) for wb in range(NBLK)]
        pend = []
        for k in range(NWB - 1):
            pend.append(load_w(*seq[k]))
        gather(0)
        ai = 0; hi = 0
        for e in range(NE):
            for ct in range(2):
                tpp = tp[ct]; tk = ("tp", ct)
                for fc in range(8):
                    sc.tr(tpp[:, fc * 128:(fc + 1) * 128], xtok[e % 2][ct][:, fc * 128:(fc + 1) * 128], identb[:],
                          reads=[("xtok", e % 2, ct), "identb"], writes=[tk])
                for fc in range(8):
                    sc.act(xg[:, fc, ct * 128:(ct + 1) * 128], tpp[:, fc * 128:(fc + 1) * 128], AF.Copy,
                           reads=[tk, "g2"], writes=[("xg", fc)], scale=g2[:, fc:fc + 1])
            if e + 1 < NE:
                gather(e + 1)
            for wb in range(NBLK):
                b = pend.pop(0)
                si = e * NBLK + wb + NWB - 1
                if si < len(seq):
                    pend.append(load_w(*seq[si]))
                hb_ = H[hi % 2]; hk = ("H", hi % 2); hi += 1
                for jj in range(4):
                    p_ = au[ai % 2]; pk = ("au", ai % 2)
                    sl_ = sil[ai % 2]; slk = ("sil", ai % 2); ai += 1
                    for ch in range(8):
                        sc.mm(p_[:, 0:256], Wg[b][:, ch, jj * 128:(jj + 1) * 128], xg[:, ch, :], ch == 0, ch == 7,
                              reads=[("Wg", b), ("xg", ch)], writes=[pk])
                    for ch in range(8):
                        sc.mm(p_[:, 256:512], Wu[b][:, ch, jj * 128:(jj + 1) * 128], xg[:, ch, :], ch == 0, ch == 7,
                              reads=[("Wu", b), ("xg", ch)], writes=[pk])
                    sc.act(sl_[:], p_[:, 0:256], AF.Silu, reads=[pk], writes=[slk])
                    sc.tt("dve", hb_[:, jj, :], sl_[:], p_[:, 256:512], ALU.mult, reads=[slk, pk], writes=[hk])
                for ct in range(2):
                    for fb in range(2):
                        for jj in range(4):
                            sc.mm(yacc[ct * 2 + fb][:, :], hb_[:, jj, ct * 128:(ct + 1) * 128], Wd[b][:, jj, fb * 512:(fb + 1) * 512],
                                  wb == 0 and jj == 0, wb == NBLK - 1 and jj == 3, reads=[hk, ("Wd", b)], writes=[("ya", ct * 2 + fb)])
            for ct in range(2):
                col = ct * NE + e
                yb_ = ye[e % 2][ct]; yk = ("ye", e % 2, ct)
                for fb in range(2):
                    if fb == 0:
                        sc.act(yb_[:, fb * 512:(fb + 1) * 512], yacc[ct * 2 + fb][:, :], AF.Copy, reads=[("ya", ct * 2 + fb), "gts"],
                               writes=[yk], scale=gts[:, col:col + 1])
                    else:
                        sc.ts("dve", yb_[:, fb * 512:(fb + 1) * 512], yacc[ct * 2 + fb][:, :], gts[:, col:col + 1], None, ALU.mult,
                              reads=[("ya", ct * 2 + fb), "gts"], writes=[yk])
                sc.add("pool", lambda g, i_=yb_[:, :], ix=idxs[:, col:col + 1]:
                       g.indirect_dma_start(out=c.out[:, :], out_offset=bass.IndirectOffsetOnAxis(ap=ix, axis=0),
                                            in_=i_, in_offset=None, compute_op=ALU.add),
                       reads=[yk, "idxs"], writes=["xout"], dma=True)
        sc.flush()


def build(n_layers=DEPTH, debug=None):
    nc = bass.Bass("TRN2", target_bir_lowering=False)
    nc.dge_precook = False
    c = Ctx()
    def inp(name, shape):
        return nc.dram_tensor(name, list(shape), F32, kind="ExternalInput").ap()
    c.x = inp("x", [S, D])
    c.norm1_g = inp("norm1_g", [DEPTH, D])
    c.w_in = inp("w_in", [DEPTH, D, D_IN])
    c.ident = inp("ident", [128, 128])
    c.ones = inp("ones", [128, 128])
    c.cos_rep = inp("cos_rep", [S, 320])
    c.sin_rep = inp("sin_rep", [S, 320])
    c.ggrep = inp("ggrep", [DEPTH, 128, 640])
    c.lru_small = inp("lru_small", [DEPTH, 128, 2, 11])
    c.lru_w = inp("lru_w", [DEPTH, 128, 8, 128])
    c.w_out = inp("w_out", [DEPTH, D, D])
    c.gout = inp("gout", [DEPTH, 128, 8])
    c.g2rep = inp("g2rep", [DEPTH, 128, 8])
    c.w_router = inp("w_router", [DEPTH, D, NE])
    c.w_gate = inp("w_expert_gate", [DEPTH, NE, D, 2 * D])
    c.w_up = inp("w_expert_up", [DEPTH, NE, D, 2 * D])
    c.w_down = inp("w_expert_down", [DEPTH, NE, 2 * D, D])
    c.h2n = nc.dram_tensor("h2n", [S, D], BF16, kind="Internal").ap()
    c.idx_d = nc.dram_tensor("idx_d", [128, 2 * NE], I32, kind="ExternalOutput" if debug == "R" else "Internal").ap()
    c.gate_d = nc.dram_tensor("gate_d", [128, 2 * NE], F32, kind="ExternalOutput" if debug == "R" else "Internal").ap()
    c.identb = nc.dram_tensor("identb", [128, 128], BF16, kind="ExternalInput").ap()
    c.triu = inp("triu", [128, 128])
    c.tril = inp("tril", [128, 128])
    c.fbrep = inp("fbrep", [DEPTH, 128, 8])
    c.ngrep = inp("ngrep", [DEPTH, 128, 256])
    c.yT = nc.dram_tensor("yT", [1024, S], F32, kind="ExternalOutput" if debug in ("B", "C", "D") else "Internal").ap()
    c.out = nc.dram_tensor("out", [S, D], F32, kind="ExternalOutput").ap()
    c.p_tm = nc.dram_tensor("p_tm", [S, D_IN], F32, kind="ExternalOutput" if debug == "A" else "Internal").ap()
    c.p_fm = nc.dram_tensor("p_fm", [1024, S], F32, kind="ExternalOutput" if debug == "A" else "Internal").ap()
    with ExitStack() as es:
        sc = Sched(nc, es)
        for l in range(n_layers):
            phase_inproj(nc, sc, c, l, c.x if l == 0 else c.out)
            if debug == "A":
                continue
            if debug != "C" and debug != "D":
                phase_attn(nc, sc, c, l)
            if debug == "B":
                continue
            if debug != "D":
                phase_lru(nc, sc, c, l)
            if debug == "C":
                continue
            phase_mlstm(nc, sc, c, l)
            if debug == "D":
                continue
            phase_outproj_router(nc, sc, c, l, c.x if l == 0 else c.out)
            if debug in ("E", "R"):
                continue
            phase_experts(nc, sc, c, l)
    return nc


def consts():
    t = np.arange(S)
    row = (t // 64).astype(np.float32)
    col = (t % 64).astype(np.float32)
    inv = (1.0 / (np.float32(10000.0) ** (np.arange(16, dtype=np.float32) / np.float32(16)))).astype(np.float32)
    ang = np.concatenate([row[:, None] * inv, col[:, None] * inv], axis=-1).astype(np.float32)
    cos = np.cos(ang).astype(np.float32)
    sin = np.sin(ang).astype(np.float32)
    import ml_dtypes
    return {"identb": np.eye(128, dtype=np.float32).astype(ml_dtypes.bfloat16),
            "ident": np.eye(128, dtype=np.float32), "ones": np.ones((128, 128), np.float32),
            "triu": np.triu(np.ones((128, 128), np.float32)), "tril": np.tril(np.ones((128, 128), np.float32)),
            "cos_rep": np.ascontiguousarray(np.tile(cos, (1, 10))), "sin_rep": np.ascontiguousarray(np.tile(sin, (1, 10)))}


def layout_small(inputs):
    o = {}
    qg = inputs["q_norm_g"]; kg = inputs["k_norm_g"]
    gg = np.concatenate([np.tile(qg, (1, 8)), np.tile(kg, (1, 2))], axis=1)
    o["ggrep"] = np.ascontiguousarray(np.broadcast_to(gg[:, None, :], (DEPTH, 128, 640))).astype(np.float32)
    go = np.concatenate([inputs["att_out_g"], inputs["lru_out_g"], np.ones((DEPTH, 256), np.float32)], axis=1)
    o["gout"] = np.ascontiguousarray(go.reshape(DEPTH, 8, 128).transpose(0, 2, 1)).astype(np.float32)
    o["g2rep"] = np.ascontiguousarray(inputs["norm2_g"].reshape(DEPTH, 8, 128).transpose(0, 2, 1)).astype(np.float32)
    o["fbrep"] = np.ascontiguousarray(np.broadcast_to(inputs["mlstm_f_bias"].reshape(DEPTH, 1, 8), (DEPTH, 128, 8))).astype(np.float32)
    o["ngrep"] = np.ascontiguousarray(np.broadcast_to(inputs["mlstm_norm_g"].reshape(DEPTH, 1, 256), (DEPTH, 128, 256))).astype(np.float32)
    cols = [inputs["conv_w"][:, j, :] for j in range(4)] + [inputs["conv_b"]]
    cols += [inputs["lru_ba"][:, d, :] for d in range(2)] + [inputs["lru_bx"][:, d, :] for d in range(2)]
    cols += [inputs["lru_lambda"][:, d, :] for d in range(2)]
    sm = np.stack(cols, axis=-1)
    o["lru_small"] = np.ascontiguousarray(sm.reshape(DEPTH, 2, 128, 11).transpose(0, 2, 1, 3)).astype(np.float32)
    lw = np.zeros((DEPTH, 128, 2, 2, 2, 128), np.float32)
    for kind, nm in enumerate(["lru_wa", "lru_wx"]):
        wsrc = inputs[nm]
        for d in range(2):
            for cc in range(2):
                for b in range(2):
                    lw[:, b * 64:(b + 1) * 64, d, kind, cc, b * 64:(b + 1) * 64] = wsrc[:, d, 2 * cc + b]
    o["lru_w"] = lw.reshape(DEPTH, 128, 8, 128)
    return o


SMALL = ["norm1_g", "q_norm_g", "k_norm_g", "conv_w", "conv_b", "lru_wa", "lru_ba", "lru_wx", "lru_bx", "lru_lambda",
         "mlstm_f_bias", "mlstm_norm_g", "att_out_g", "lru_out_g", "norm2_g"]
BIG = ["w_in", "w_out", "w_router", "w_expert_gate", "w_expert_up", "w_expert_down", "norm1_g"]


def kernel(**inputs):
    inputs = {k: np.asarray(v) for k, v in inputs.items()}
    nc = build()
    shared = {k: np.ascontiguousarray(inputs[k], dtype=np.float32) for k in BIG}
    shared.update(consts())
    shared.update(layout_small(inputs))
    x = np.ascontiguousarray(inputs["x"], dtype=np.float32)
    in_maps = []
    for b in range(8):
        m = dict(shared)
        m["x"] = x[b]
        in_maps.append(m)
    res = run_bass_kernel_spmd(nc, in_maps, core_ids=list(range(8)))
    return np.stack([r["out"] for r in res.results], axis=0).astype(np.float32)
```

```python
import numpy as np
from contextlib import ExitStack
import concourse.bass as bass
import concourse.mybir as mybir
from concourse.bass_utils import run_bass_kernel_spmd

F32 = mybir.dt.float32
F32R = mybir.dt.float32r
BF16 = mybir.dt.bfloat16
F16 = mybir.dt.float16
U32 = mybir.dt.uint32
I32 = mybir.dt.int32
AF = mybir.ActivationFunctionType
ALU = mybir.AluOpType
AX = mybir.AxisListType

S = 2048
D = 1024
DEPTH = 4
D_IN = 2320
NT = S // 128
EPS = 1e-6

ENG = ["pe", "act", "dve", "pool", "sp"]
BLK = {"pe": "tensor", "act": "scalar", "dve": "vector", "pool": "gpsimd", "sp": "sync"}


class Sched:
    def __init__(self, nc, es, n_dma=24):
        self.nc = nc
        self.sem = {e: es.enter_context(nc.semaphore("s_" + e)) for e in ENG}
        self.dsem = [es.enter_context(nc.semaphore("s_dma%d" % i)) for i in range(n_dma)]
        self.cnt = {e: 0 for e in ENG}
        self.duse = [0] * n_dma
        self.drr = 0
        self.ops = {e: [] for e in ENG}
        self.waited = {e: {} for e in ENG}
        self.last_w = {}
        self.readers = {}
        self.nops = 0

    def semobj(self, s):
        return self.sem[s] if isinstance(s, str) else self.dsem[s[1]]

    def add(self, eng, fn, reads=(), writes=(), dma=False):
        deps = {}
        def need(tok):
            s, v = tok
            if deps.get(s, 0) < v:
                deps[s] = v
        for r in reads:
            if r in self.last_w:
                need(self.last_w[r])
        for w in writes:
            if w in self.last_w:
                need(self.last_w[w])
            for t in self.readers.get(w, ()):
                need(t)
        if dma:
            k = self.drr
            self.drr = (self.drr + 1) % len(self.dsem)
            self.duse[k] += 1
            n = self.duse[k]
            if n > 1:
                need((("dma", k), 16 * (n - 1)))
            token = (("dma", k), 16 * n)
        else:
            self.cnt[eng] += 1
            token = (eng, self.cnt[eng])
        waits = []
        wd = self.waited[eng]
        for s, v in deps.items():
            if s == "pe" and eng == "pe" and not dma:
                continue
            if wd.get(s, 0) < v:
                wd[s] = v
                waits.append((s, v))
        self.ops[eng].append((waits, fn, token, dma))
        self.nops += 1
        for r in reads:
            self.readers.setdefault(r, []).append(token)
        for w in writes:
            self.last_w[w] = token
            self.readers[w] = []
        return token

    def flush(self):
        waits = []
        for k, n in enumerate(self.duse):
            if n and self.waited["sp"].get(("dma", k), 0) < 16 * n:
                waits.append((("dma", k), 16 * n))
        self.ops["sp"].append((waits, None, None, False))
        ops = self.ops
        self.ops = {e: [] for e in ENG}
        sched = self
        with self.nc.Block() as block:
            for e in ENG:
                lst = ops[e]

                def body(eng, lst=lst):
                    for waits, fn, token, isdma in lst:
                        for (s, v) in waits:
                            eng.wait_ge(sched.semobj(s), v)
                        if fn is None:
                            continue
                        inst = fn(eng)
                        inst.then_inc(sched.semobj(token[0]), 16 if isdma else 1)

                getattr(block, BLK[e])(body)
        for e in ENG:
            for e2 in ENG:
                self.waited[e][e2] = self.cnt[e2]
            for k, n in enumerate(self.duse):
                self.waited[e][("dma", k)] = 16 * n
        self.last_w = {}
        self.readers = {}

    def dma(self, q, out, in_, reads=(), writes=(), slow=False):
        if slow:
            return self.add(q, lambda e: e.dma_start(out=out, in_=in_, allow_slow_non_contiguous=True), reads, writes, dma=True)
        return self.add(q, lambda e: e.dma_start(out=out, in_=in_), reads, writes, dma=True)

    def mm(self, out, lhsT, rhs, start, stop, reads=(), writes=()):
        return self.add("pe", lambda e: e.matmul(out, lhsT, rhs, start=start, stop=stop), reads, writes)

    def tr(self, out, in_, ident, reads=(), writes=()):
        return self.add("pe", lambda e: e.transpose(out, in_, ident), reads, writes)

    def act(self, out, in_, func, reads=(), writes=(), eng="act", **kw):
        return self.add(eng, lambda e: e.activation(out, in_, func, **kw), reads, writes)

    def ts(self, eng, out, in0, s1, s2, op0, op1=None, reads=(), writes=(), **kw):
        if op1 is None:
            return self.add(eng, lambda e: e.tensor_scalar(out, in0, s1, None, op0, **kw), reads, writes)
        return self.add(eng, lambda e: e.tensor_scalar(out, in0, s1, s2, op0, op1, **kw), reads, writes)

    def tt(self, eng, out, in0, in1, op, reads=(), writes=()):
        return self.add(eng, lambda e: e.tensor_tensor(out, in0, in1, op), reads, writes)

    def stt(self, eng, out, in0, scalar, in1, op0, op1, reads=(), writes=()):
        return self.add(eng, lambda e: e.scalar_tensor_tensor(out, in0, scalar, in1, op0, op1), reads, writes)

    def copy(self, eng, out, in_, reads=(), writes=()):
        if eng == "act":
            return self.add(eng, lambda e: e.copy(out, in_), reads, writes)
        return self.add(eng, lambda e: e.tensor_copy(out, in_), reads, writes)


def r32(ap):
    return ap.bitcast(F32R)


class Ctx:
    pass


def phase_inproj(nc, sc, c, l, x_src):
    with ExitStack() as es:
        T = lambda name, shape, dt=F32: es.enter_context(nc.sbuf_tensor("%s_L%d" % (name, l), shape, dt))
        P = lambda name, shape, dt=F32: es.enter_context(nc.psum_tensor("%s_L%d" % (name, l), shape, dt))
        wt = T("a_wt", [128, 8, D_IN], BF16)
        g = T("a_g", [128, 8])
        ident = T("a_id", [128, 128])
        xt = [T("a_xt%d" % i, [128, D]) for i in range(2)]
        junk = T("a_junk", [128, D])
        st = [T("a_st%d" % i, [128, 4]) for i in range(2)]
        hT = [T("a_hT%d" % i, [128, 8, 512], BF16) for i in range(2)]
        og = [T("a_og%d" % i, [128, D_IN]) for i in range(2)]
        of = [T("a_of%d" % i, [128, 512]) for i in range(2)]
        pT = [P("a_pT%d" % i, [128, 512]) for i in range(4)]
        pm = [P("a_pm%d" % i, [128, 512]) for i in range(4)]

        sc.dma("sp", ident[:], c.ident, writes=["ident"])
        sc.dma("sp", g[:], c.norm1_g[l].rearrange("(c p) -> p c", p=128), writes=["g"], slow=True)
        for ch in range(8):
            sc.dma("pool", wt[:, ch, :], c.w_in[l][ch * 128:(ch + 1) * 128, :], writes=[("wt", ch)])

        pmi = 0
        for b in range(4):
            hb = hT[b % 2]
            hkey = ("hT", b % 2)
            for tl in range(4):
                i = b * 4 + tl
                xb = xt[i % 2]
                sb = st[i % 2]
                xk = ("xt", i % 2)
                sk = ("st", i % 2)
                sc.dma("sp", xb[:], x_src[i * 128:(i + 1) * 128, :], writes=[xk])
                sc.act(junk[:], xb[:], AF.Square, reads=[xk], writes=["junk", sk], accum_out=sb[:, 0:1])
                sc.ts("dve", sb[:, 1:2], sb[:, 0:1], 1.0 / D, EPS, ALU.mult, ALU.add, reads=[sk], writes=[sk])
                sc.act(sb[:, 2:3], sb[:, 1:2], AF.Sqrt, reads=[sk], writes=[sk])
                sc.add("dve", lambda e, o=sb[:, 3:4], i_=sb[:, 2:3]: e.reciprocal(o, i_), reads=[sk], writes=[sk])
                sc.ts("dve", xb[:], xb[:], sb[:, 3:4], None, ALU.mult, reads=[xk, sk], writes=[xk])
                for half in range(2):
                    pt = pT[(i % 2) * 2 + half]
                    pk = ("pT", (i % 2) * 2 + half)
                    for cc in range(4):
                        ch = half * 4 + cc
                        sc.tr(pt[:, cc * 128:(cc + 1) * 128], xb[:, ch * 128:(ch + 1) * 128], ident[:],
                              reads=[xk, "ident"], writes=[pk])
                    sc.tt("dve", hb[:, half * 4:(half + 1) * 4, tl * 128:(tl + 1) * 128],
                          pt[:].rearrange("p (c t) -> p c t", c=4),
                          g[:, half * 4:(half + 1) * 4].unsqueeze(2).broadcast_to([128, 4, 128]), ALU.mult,
                          reads=[pk, "g"], writes=[(hkey, tl)])
            for tl in range(4):
                i = b * 4 + tl
                ob = og[i % 2]
                ok = ("og", i % 2)
                for n in range(5):
                    n0 = n * 512
                    nw = min(512, D_IN - n0)
                    pmm = pm[pmi % 4]
                    pk = ("pm", pmi % 4)
                    pmi += 1
                    for ch in range(8):
                        sc.mm(pmm[:, 0:nw], hb[:, ch, tl * 128:(tl + 1) * 128], wt[:, ch, n0:n0 + nw],
                              ch == 0, ch == 7, reads=[(hkey, tl), ("wt", ch)], writes=[pk])
                    eng = "act" if n % 2 == 0 else "dve"
                    sc.copy(eng, ob[:, n0:n0 + nw], pmm[:, 0:nw], reads=[pk], writes=[ok])
                sc.dma("pool", c.p_tm[i * 128:(i + 1) * 128, :], ob[:], reads=[ok])
            for j in range(8):
                c0 = 768 + j * 128
                pmm = pm[pmi % 4]
                pk = ("pm", pmi % 4)
                pmi += 1
                ob = of[j % 2]
                ok = ("of", j % 2)
                for ch in range(8):
                    sc.mm(pmm[:, :], wt[:, ch, c0:c0 + 128], hb[:, ch, :], ch == 0, ch == 7,
                          reads=[(hkey, 0), (hkey, 1), (hkey, 2), (hkey, 3), ("wt", ch)], writes=[pk])
                eng = "act" if j % 2 == 0 else "dve"
                sc.copy(eng, ob[:], pmm[:], reads=[pk], writes=[ok])
                sc.dma("pool", c.p_fm[j * 128:(j + 1) * 128, b * 512:(b + 1) * 512], ob[:], reads=[ok])
        sc.flush()


def phase_attn(nc, sc, c, l):
    with ExitStack() as es:
        T = lambda name, shape, dt=F32: es.enter_context(nc.sbuf_tensor("%s_L%d" % (name, l), shape, dt))
        P = lambda name, shape, dt=F32: es.enter_context(nc.psum_tensor("%s_L%d" % (name, l), shape, dt))
        ident = T("b_id", [128, 128])
        ones = T("b_ones", [128, 128])
        gg = T("b_gg", [128, 640])
        qT = T("b_qT", [128, 4, S], BF16)
        kT = T("b_kT", [128, 2, S], BF16)
        vx = T("b_vx", [128, NT, 2, 65], BF16)
        qkv = [T("b_qkv%d" % i, [128, 768]) for i in range(2)]
        cs = [T("b_cs%d" % i, [128, 2, 320]) for i in range(2)]
        sq2 = [T("b_sq%d" % i, [128, 640]) for i in range(2)]
        st2 = [T("b_st%d" % i, [128, 4, 10]) for i in range(2)]
        qn2 = [T("b_qn%d" % i, [128, 640]) for i in range(2)]
        tmp2 = [T("b_tmp%d" % i, [128, 4, 320]) for i in range(2)]
        qr2 = [T("b_qr%d" % i, [128, 640]) for i in range(2)]
        Osb = [T("b_Osb%d" % i, [65, 512]) for i in range(2)]
        PT = [T("b_PT%d" % i, [128, 512], BF16) for i in range(3)]
        rd = [T("b_rd%d" % i, [128, 512]) for i in range(2)]
        bcs = [T("b_bcs%d" % i, [64, 512]) for i in range(2)]
        yo = [T("b_yo%d" % i, [64, 512]) for i in range(2)]
        ptA = [P("b_ptA%d" % i, [128, 512]) for i in range(1)]
        ptB = [P("b_ptB%d" % i, [128, 512]) for i in range(1)]
        sps = [P("b_sps%d" % i, [128, 512]) for i in range(3)]
        ops_ = [P("b_ops%d" % i, [128, 512]) for i in range(2)]
        bcp = P("b_bcp", [128, 512])

        sc.dma("sp", ident[:], c.ident, writes=["ident"])
        sc.dma("sp", ones[:], c.ones, writes=["ones"])
        sc.dma("sp", gg[:], c.ggrep[l], writes=["gg"])
        sc.add("pool", lambda e: e.memset(kT[:], 0.0), writes=["kT"])
        for i in range(NT):
            qb_ = qkv[i % 2]; qk = ("qkv", i % 2)
            sq = sq2[i % 2]; st = st2[i % 2]; qn = qn2[i % 2]; tmp = tmp2[i % 2]; qr = qr2[i % 2]
            K_ = lambda nm: (nm, i % 2)
            cb = cs[i % 2]; ck = ("cs", i % 2)
            for half in range(2):
                sc.dma("sp", qb_[:, 0:512].rearrange("p (pr half d) -> p pr half d", pr=4, half=2)[:, :, half, :],
                       c.p_tm[i * 128:(i + 1) * 128, half * 256:(half + 1) * 256].rearrange("t (pr d) -> t pr d", pr=4),
                       writes=[qk])
            sc.dma("sp", qb_[:, 512:768], c.p_tm[i * 128:(i + 1) * 128, 512:768], writes=[qk])
            sc.dma("sp", cb[:, 0, :], c.cos_rep[i * 128:(i + 1) * 128, :], writes=[ck])
            sc.dma("sp", cb[:, 1, :], c.sin_rep[i * 128:(i + 1) * 128, :], writes=[ck])
            sc.tt("pool", sq[:], qb_[:, 0:640], qb_[:, 0:640], ALU.mult, reads=[qk], writes=[K_("sq")])
            sc.add("dve", lambda e, o=st[:, 0, :], i_=sq[:].rearrange("p (h d) -> p h d", d=64):
                   e.tensor_reduce(o, i_, AX.X, ALU.add), reads=[K_("sq")], writes=[K_("st")])
            sc.ts("dve", st[:, 1, :], st[:, 0, :], 1.0 / 64, EPS, ALU.mult, ALU.add, reads=[K_("st")], writes=[K_("st")])
            sc.act(st[:, 2, :], st[:, 1, :], AF.Sqrt, reads=[K_("st")], writes=[K_("st")])
            sc.add("dve", lambda e, o=st[:, 3, :], i_=st[:, 2, :]: e.reciprocal(o, i_), reads=[K_("st")], writes=[K_("st")])
            sc.tt("dve", qn[:].rearrange("p (h d) -> p h d", d=64), qb_[:, 0:640].rearrange("p (h d) -> p h d", d=64),
                  st[:, 3, :].unsqueeze(2).broadcast_to([128, 10, 64]), ALU.mult, reads=[qk, K_("st")], writes=[K_("qn")])
            sc.tt("pool", qn[:], qn[:], gg[:], ALU.mult, reads=[K_("qn"), "gg"], writes=[K_("qn")])
            q3 = qn[:].rearrange("p (h d) -> p h d", d=64)
            x1 = q3[:, :, 0:32]; x2 = q3[:, :, 32:64]
            co = cb[:, 0, :].rearrange("p (h d) -> p h d", d=32); si = cb[:, 1, :].rearrange("p (h d) -> p h d", d=32)
            t = [tmp[:, k, :].rearrange("p (h d) -> p h d", d=32) for k in range(4)]
            r3 = qr[:].rearrange("p (h d) -> p h d", d=64)
            sc.tt("dve", t[0], x1, co, ALU.mult, reads=[K_("qn"), ck], writes=[K_("tmp0")])
            sc.tt("pool", t[1], x2, si, ALU.mult, reads=[K_("qn"), ck], writes=[K_("tmp1")])
            sc.tt("dve", t[2], x2, co, ALU.mult, reads=[K_("qn"), ck], writes=[K_("tmp2")])
            sc.tt("pool", t[3], x1, si, ALU.mult, reads=[K_("qn"), ck], writes=[K_("tmp3")])
            sc.tt("dve", r3[:, :, 0:32], t[0], t[1], ALU.subtract, reads=[K_("tmp0"), K_("tmp1")], writes=[K_("qr")])
            sc.tt("pool", r3[:, :, 32:64], t[2], t[3], ALU.add, reads=[K_("tmp2"), K_("tmp3")], writes=[K_("qr")])
            for pr in range(4):
                sc.tr(ptA[0][:, pr * 128:(pr + 1) * 128], qr[:, pr * 128:(pr + 1) * 128], ident[:], reads=[K_("qr"), "ident"], writes=["ptA"])
            sc.tr(ptB[0][:, 0:128], qr[:, 512:640], ident[:], reads=[K_("qr"), "ident"], writes=["ptB"])
            sc.copy("act", qT[:, :, i * 128:(i + 1) * 128], ptA[0][:].rearrange("p (c t) -> p c t", c=4),
                    reads=["ptA"], writes=["qT"])
            sc.copy("dve", kT[0:64, 0, i * 128:(i + 1) * 128], ptB[0][0:64, 0:128], reads=["ptB"], writes=["kT"])
            sc.copy("dve", kT[64:128, 1, i * 128:(i + 1) * 128], ptB[0][64:128, 0:128], reads=["ptB"], writes=["kT"])
            sc.copy("act", vx[:, i, :, 0:64], qb_[:, 640:768].rearrange("p (g d) -> p g d", d=64),
                    reads=[qk], writes=["vx"])
            sc.ts("pool", vx[:, i, :, 64:65], qb_[:, 0:2].unsqueeze(2), 0.0, 1.0, ALU.mult, ALU.add,
                  reads=[qk], writes=["vx"])

        steps = [(pr + 4 * half, qb, kt) for pr in range(4) for qb in range(4) for kt in range(NT) for half in range(2)]
        def fin(half, h, qb):
            O = ops_[half]; ok = ("O", half)
            osb = Osb[half]; osk = ("Osb", half)
            r = rd[half]; rk = ("rd", half)
            sc.copy("dve", osb[:, :], O[0:65, :], reads=[ok], writes=[osk])
            sc.act(r[64:65, :], osb[64:65, :], AF.Ln, reads=[osk], writes=[rk])
            sc.act(r[64:65, :], r[64:65, :], AF.Exp, reads=[rk], writes=[rk], scale=-1.0)
            sc.mm(bcp[0:64, :], ones[64:65, 0:64], r[64:65, :], True, True, reads=[rk, "ones"], writes=["bcp"])
            sc.tt("dve", yo[half][:], osb[0:64, :], bcp[0:64, :], ALU.mult, reads=[osk, "bcp"], writes=[("yo", half)])
            sc.dma("sp", c.yT[h * 64:(h + 1) * 64, qb * 512:(qb + 1) * 512], yo[half][:], reads=[("yo", half)])
        for s_ in range(len(steps) + 2):
            if s_ < len(steps):
                h, qb, kt = steps[s_]
                pr = h % 4; half = h // 4
                p0 = half * 64
                sp_ = sps[s_ % 3]; sk = ("sps", s_ % 3)
                sc.mm(sp_[:, :], kT[:, half, kt * 128:(kt + 1) * 128],
                      qT[:, pr, qb * 512:(qb + 1) * 512], True, True,
                      reads=["kT", "qT"], writes=[sk])
                sc.act(PT[s_ % 3][:], sp_[:], AF.Exp, reads=[sk], writes=[("PT", s_ % 3)], scale=0.125)
            if s_ >= 2:
                h, qb, kt = steps[s_ - 2]
                half = h // 4
                O = ops_[half]; ok = ("O", half)
                sc.mm(O[0:65, :], vx[:, kt, half, :], PT[(s_ - 2) % 3][:], kt == 0, kt == NT - 1,
                      reads=["vx", ("PT", (s_ - 2) % 3)], writes=[ok])
                if kt == NT - 1:
                    fin(half, h, qb)
        sc.flush()


def phase_lru(nc, sc, c, l):
    with ExitStack() as es:
        T = lambda name, shape, dt=F32: es.enter_context(nc.sbuf_tensor("%s_L%d" % (name, l), shape, dt))
        P = lambda name, shape, dt=F32: es.enter_context(nc.psum_tensor("%s_L%d" % (name, l), shape, dt))
        sm = T("c_sm", [128, 2, 11])
        W = T("c_W", [128, 8, 128])
        X = T("c_X", [128, S]); G = T("c_G", [128, S]); XF = T("c_XF", [128, S]); XC = T("c_XC", [128, S])
        R = T("c_R", [128, S]); I_ = T("c_I", [128, S]); A = T("c_A", [128, S]); U = T("c_U", [128, S])
        H = [T("c_H%d" % i, [128, S]) for i in range(2)]
        cf = T("c_cf", [128, 8])
        pp = [P("c_pp%d" % i, [128, 512]) for i in range(4)]
        sc.dma("sp", sm[:], c.lru_small[l], writes=["sm"])
        sc.dma("sp", r32(W[:]), r32(c.lru_w[l]), writes=["W"])
        ppi = 0
        for cc in range(2):
            sc.dma("sp", X[:], c.p_fm[cc * 128:(cc + 1) * 128, :], writes=["X"])
            sc.dma("sp", G[:], c.p_fm[256 + cc * 128:256 + (cc + 1) * 128, :], writes=["G"])
            w = lambda j: sm[:, cc, j:j + 1]
            sc.ts("dve", XF[:], X[:], w(2), w(4), ALU.mult, ALU.add, reads=["X", "sm"], writes=["XF"])
            sc.stt("dve", XF[:, 2:S], X[:, 0:S - 2], w(0), XF[:, 2:S], ALU.mult, ALU.add, reads=["X", "XF", "sm"], writes=["XF"])
            sc.stt("dve", XF[:, 1:S], X[:, 0:S - 1], w(1), XF[:, 1:S], ALU.mult, ALU.add, reads=["X", "XF", "sm"], writes=["XF"])
            sc.copy("pool", r32(XC[:, S - 1:S]), XF[:, S - 1:S], reads=["XF"], writes=["XC"])
            sc.stt("dve", r32(XC[:, 0:S - 1]), X[:, 1:S], w(3), XF[:, 0:S - 1], ALU.mult, ALU.add, reads=["X", "XF", "sm"], writes=["XC"])
            for d in range(2):
                ba = sm[:, cc, 5 + d:6 + d]; bx = sm[:, cc, 7 + d:8 + d]; lam = sm[:, cc, 9 + d:10 + d]
                ck = ("cf", d)
                c0 = cf[:, 4 * d:4 * d + 1]; c1 = cf[:, 4 * d + 1:4 * d + 2]; c2 = cf[:, 4 * d + 2:4 * d + 3]
                sc.act(c0, lam, AF.Exp, reads=["sm"], writes=[ck], scale=-1.0)
                sc.act(c0, c0, AF.Ln, reads=[ck], writes=[ck], bias=1.0)
                sc.ts("dve", c1, c0, -8.0, None, ALU.mult, reads=[ck], writes=[ck])
                sc.ts("dve", c2, c0, -16.0, None, ALU.mult, reads=[ck], writes=[ck])
                for tb in range(4):
                    sl = slice(tb * 512, (tb + 1) * 512)
                    pa = pp[ppi % 4]; pak = ("pp", ppi % 4); ppi += 1
                    px = pp[ppi % 4]; pxk = ("pp", ppi % 4); ppi += 1
                    sc.mm(pa[:, :], r32(W[:, d * 4 + 0 * 2 + cc, :]), r32(XC[:, sl]), True, True, reads=["W", "XC"], writes=[pak])
                    sc.mm(px[:, :], r32(W[:, d * 4 + 1 * 2 + cc, :]), r32(XC[:, sl]), True, True, reads=["W", "XC"], writes=[pxk])
                    sc.act(R[:, sl], pa[:, :], AF.Sigmoid, reads=[pak, "sm"], writes=["R"], bias=ba)
                    sc.act(I_[:, sl], px[:, :], AF.Sigmoid, reads=[pxk, "sm"], writes=["I"], bias=bx)
                sc.act(A[:], R[:], AF.Exp, reads=["R", ck], writes=["A"], scale=c1)
                sc.act(U[:], R[:], AF.Exp, reads=["R", ck], writes=["U"], scale=c2)
                sc.ts("pool", U[:], U[:], -1.0, 1.0, ALU.mult, ALU.add, reads=["U"], writes=["U"])
                sc.act(U[:], U[:], AF.Sqrt, reads=["U"], writes=["U"])
                sc.tt("pool", I_[:], I_[:], XC[:], ALU.mult, reads=["I", "XC"], writes=["I"])
                sc.tt("dve", U[:], U[:], I_[:], ALU.mult, reads=["U", "I"], writes=["U"])
                if d == 0:
                    sc.add("dve", lambda e, o=H[0][:], a=A[:], u=U[:]: e.tensor_tensor_scan(o, a, u, 0.0, ALU.mult, ALU.add),
                           reads=["A", "U"], writes=[("H", 0)])
                else:
                    sc.add("dve", lambda e, o=H[1][:, ::-1], a=A[:, ::-1], u=U[:, ::-1]: e.tensor_tensor_scan(o, a, u, 0.0, ALU.mult, ALU.add),
                           reads=["A", "U"], writes=[("H", 1)])
            sc.tt("pool", H[0][:], H[0][:], H[1][:], ALU.add, reads=[("H", 0), ("H", 1)], writes=[("H", 0)])
            sc.tt("dve", R[:], G[:], G[:], ALU.mult, reads=["G", "R"], writes=["R"])
            sc.ts("dve", R[:], R[:], 0.044715, 1.0, ALU.mult, ALU.add, reads=["R"], writes=["R"])
            sc.tt("dve", R[:], R[:], G[:], ALU.mult, reads=["R", "G"], writes=["R"])
            sc.act(R[:], R[:], AF.Sigmoid, reads=["R"], writes=["R"], scale=1.5957691216057308)
            sc.tt("pool", H[0][:], H[0][:], G[:], ALU.mult, reads=[("H", 0), "G"], writes=[("H", 0)])
            sc.tt("dve", H[0][:], H[0][:], R[:], ALU.mult, reads=[("H", 0), "R"], writes=[("H", 0)])
            sc.dma("pool", c.yT[512 + cc * 128:512 + (cc + 1) * 128, :], H[0][:], reads=[("H", 0)])
        sc.flush()


def phase_mlstm(nc, sc, c, l):
    with ExitStack() as es:
        T = lambda name, shape, dt=F32: es.enter_context(nc.sbuf_tensor("%s_L%d" % (name, l), shape, dt))
        P = lambda name, shape, dt=F32: es.enter_context(nc.psum_tensor("%s_L%d" % (name, l), shape, dt))
        ident = T("d_id", [128, 128]); ones = T("d_ones", [128, 128])
        mask = T("d_mask", [128, 2, 128])
        fb = T("d_fb", [128, 8]); ng = T("d_ng", [128, 256])
        qT = T("d_qT", [128, 4, S], BF16); kT = T("d_kT", [128, 2, S], BF16)
        tmk = T("d_tmk", [128, NT, 256], BF16)
        tm = T("d_tm", [128, NT, 784])
        vp = T("d_vp", [128, NT, 8, 65], BF16)
        Cb = T("d_Cb", [128, 2, 4, 65], BF16)
        wA = T("d_wA", [128, NT, 8]); wB = T("d_wB", [128, NT, 8]); eg = T("d_eg", [128, NT, 8])
        nlf = T("d_nlf", [128, NT, 8]); tg = T("d_tg", [128, NT, 8])
        hs = T("d_hs", [128, NT, 256])
        Cst = T("d_C", [128, 2, 4, 65])
        tC8 = T("d_tC", [128, 8, 65])
        PT = [T("d_PT%d" % i, [128, 128], BF16) for i in range(3)]
        s4 = [T("d_s4%d" % i, [128, 4, 4]) for i in range(2)]
        sq = T("d_sq", [128, 256]); sqe = [T("d_sqe%d" % i, [128, 256]) for i in range(2)]; ste = [T("d_ste%d" % i, [128, 4, 4]) for i in range(2)]; sge = [T("d_sge%d" % i, [128, 256]) for i in range(2)]; ym = [T("d_ym%d" % i, [128, 256]) for i in range(2)]
        yo = [T("d_yo%d" % i, [128, 256]) for i in range(2)]
        sg = T("d_sg", [128, 256])
        pg = P("d_pg", [128, 512])
        psS = [P("d_pS%d" % i, [128, 512]) for i in range(2)]
        pacc = [P("d_pa%d" % i, [128, 512]) for i in range(2)]
        pkv = [P("d_pk%d" % i, [128, 512]) for i in range(2)]
        ptr = P("d_ptr", [128, 512])

        sc.dma("sp", ident[:], c.ident, writes=["ident"])
        sc.dma("sp", ones[:], c.ones, writes=["ones"])
        sc.dma("sp", mask[:, 0, :], c.triu, writes=["mask"])
        sc.dma("sp", mask[:, 1, :], c.tril, writes=["mask"])
        sc.dma("sp", fb[:], c.fbrep[l], writes=["fb"])
        sc.dma("sp", ng[:], c.ngrep[l], writes=["ng"])
        sc.add("pool", lambda e: e.memset(qT[:], 0.0), writes=["qT"])
        for h in range(4):
            p0 = (h % 2) * 64
            sc.dma("pool", qT[p0:p0 + 64, h, :], c.p_fm[512 + h * 64:512 + (h + 1) * 64, :], reads=["qT"], writes=[("qTh", h)])
        for ch in range(2):
            sc.dma("pool", kT[:, ch, :], c.p_fm[768 + ch * 128:768 + (ch + 1) * 128, :], writes=["kT"])
        for i in range(NT):
            sc.dma("sp", tm[:, i, 256:784], c.p_tm[i * 128:(i + 1) * 128, 1792:2320], writes=[("tm", i)])
            sc.dma("pool", tmk[:, i, :], c.p_tm[i * 128:(i + 1) * 128, 1536:1792], writes=[("tmk", i)])
        sc.add("dve", lambda e: e.memset(Cst[:], 0.0), writes=["C"])
        sc.add("dve", lambda e: e.memset(Cb[:], 0.0), writes=["Cb"])
        for i in range(NT):
            gt = tm[:, i, 768:784]
            fview = gt.rearrange("p (d k h) -> p d k h", d=2, k=2)[:, :, 1, :]
            iview = gt.rearrange("p (d k h) -> p d k h", d=2, k=2)[:, :, 0, :]
            tk = ("tg", i)
            sc.tt("dve", tg[:, i, :].rearrange("p (d h) -> p d h", d=2), fview, fb[:].rearrange("p (d h) -> p d h", d=2),
                  ALU.add, reads=[("tm", i), "fb"], writes=[tk])
            sc.act(tg[:, i, :], tg[:, i, :], AF.Exp, reads=[tk], writes=[tk], scale=-1.0)
            sc.act(nlf[:, i, :], tg[:, i, :], AF.Ln, reads=[tk], writes=[("nlf", i)], bias=1.0)
            sc.mm(pg[:, 0:4], mask[:, 0, :], nlf[:, i, 0:4], True, True, reads=["mask", ("nlf", i)], writes=["pg"])
            sc.mm(pg[:, 4:8], mask[:, 1, :], nlf[:, i, 4:8], True, True, reads=["mask", ("nlf", i)], writes=["pg"])
            sc.mm(pg[:, 8:16], ones[:, :], nlf[:, i, :], True, True, reads=["ones", ("nlf", i)], writes=["pg"])
            sc.act(wA[:, i, :], pg[:, 0:8], AF.Exp, reads=["pg"], writes=[("wA", i)], scale=-1.0)
            sc.act(eg[:, i, :], pg[:, 8:16], AF.Exp, reads=["pg"], writes=[("eg", i)], scale=-1.0)
            sc.tt("dve", wB[:, i, :].rearrange("p (d h) -> p d h", d=2), iview, pg[:, 0:8].rearrange("p (d h) -> p d h", d=2),
                  ALU.add, reads=[("tm", i), "pg"], writes=[("wB", i)])
            sc.act(wB[:, i, :], wB[:, i, :], AF.Exp, reads=[("wB", i)], writes=[("wB", i)])
            v3 = tm[:, i, 256:512].rearrange("p (h d) -> p h d", d=64)
            for d in range(2):
                eng = "dve" if d == 0 else "pool"
                sc.tt(eng, vp[:, i, d * 4:(d + 1) * 4, 0:64], v3, wB[:, i, d * 4:(d + 1) * 4].unsqueeze(2).broadcast_to([128, 4, 64]),
                      ALU.mult, reads=[("tm", i), ("wB", i)], writes=[("vp", i)])
            sc.copy("pool", vp[:, i, :, 64:65], wB[:, i, :].unsqueeze(2), reads=[("wB", i)], writes=[("vp", i)])
        ui = 0
        for s_ in range(NT):
            for d in range(2):
                i = s_ if d == 0 else NT - 1 - s_
                tsl = slice(i * 128, (i + 1) * 128)
                acc = pacc[d]; ak = ("pacc", d)
                for h in range(4):
                    p0 = (h % 2) * 64; ch = h // 2
                    pS = psS[ui % 2]; pk_ = ("pS", ui % 2)
                    pt = PT[ui % 3]; ptk = ("PT", ui % 3)
                    kv = pkv[ui % 2]; kvk = ("pkv", ui % 2)
                    ui += 1
                    sc.mm(pS[:, 0:128], kT[:, ch, tsl], qT[:, h, tsl], True, True,
                          reads=["kT", ("qTh", h)], writes=[pk_])
                    sc.stt("dve", pt[:], pS[:, 0:128], 0.125, mask[:, d, :], ALU.mult, ALU.mult,
                           reads=[pk_, "mask"], writes=[ptk])
                    sc.mm(acc[:, h * 65:(h + 1) * 65], pt[:], vp[:, i, d * 4 + h, :], True, False,
                          reads=[ptk, ("vp", i)], writes=[ak])
                    sc.mm(acc[:, h * 65:(h + 1) * 65], qT[:, h, tsl], Cb[:, d, h, :], False, True,
                          reads=[("qTh", h), ("Cb", d, h), "Cb"], writes=[ak])
                    if s_ < NT - 1:
                        sc.mm(kv[:, 0:65], tmk[:, i, ch * 128:(ch + 1) * 128], vp[:, i, d * 4 + h, :], True, True,
                              reads=[("tmk", i), ("vp", i)], writes=[kvk])
                        tC = tC8[:, d * 4 + h, :]; tck = ("tC", d, h)
                        sc.stt("dve", tC[p0:p0 + 64, :], kv[p0:p0 + 64, 0:65], 0.125, Cst[p0:p0 + 64, d, h, :], ALU.mult, ALU.add,
                               reads=[kvk, ("C", d, h)], writes=[tck])
                        sc.ts("dve", Cb[p0:p0 + 64, d, h, :], tC[p0:p0 + 64, :], eg[p0:p0 + 64, i, d * 4 + h:d * 4 + h + 1], None, ALU.mult,
                              reads=[tck, ("eg", i)], writes=[("Cb", d, h)])
                        sc.ts("pool", Cst[p0:p0 + 64, d, h, :], tC[p0:p0 + 64, :], eg[p0:p0 + 64, i, d * 4 + h:d * 4 + h + 1], None, ALU.mult,
                              reads=[tck, ("eg", i)], writes=[("C", d, h)])
                a3 = acc[:, 0:260].rearrange("p (h e) -> p h e", e=65)
                sb = s4[d]; sk = ("s4", d)
                sc.tt("dve", sb[:, 0, :], a3[:, :, 64], wA[:, i, d * 4:(d + 1) * 4], ALU.mult, reads=[ak, ("wA", i)], writes=[sk])
                sc.act(sb[:, 1, :], sb[:, 0, :], AF.Abs, reads=[sk], writes=[sk])
                sc.ts("dve", sb[:, 1, :], sb[:, 1, :], 1.0, None, ALU.max, reads=[sk], writes=[sk])
                sc.add("dve", lambda e, o=sb[:, 2, :], i_=sb[:, 1, :]: e.reciprocal(o, i_), reads=[sk], writes=[sk])
                sc.tt("dve", sb[:, 3, :], sb[:, 2, :], wA[:, i, d * 4:(d + 1) * 4], ALU.mult, reads=[sk, ("wA", i)], writes=[sk])
                h3 = hs[:, i, :].rearrange("p (h e) -> p h e", e=64)
                if s_ < NT // 2:
                    sc.tt("dve", h3, a3[:, :, 0:64], sb[:, 3, :].unsqueeze(2).broadcast_to([128, 4, 64]), ALU.mult,
                          reads=[ak, sk], writes=[("hs", i)])
                else:
                    sc.tt("dve", sq[:].rearrange("p (h e) -> p h e", e=64), a3[:, :, 0:64],
                          sb[:, 3, :].unsqueeze(2).broadcast_to([128, 4, 64]), ALU.mult, reads=[ak, sk], writes=["sq"])
                    sc.tt("pool", hs[:, i, :], hs[:, i, :], sq[:], ALU.add, reads=[("hs", i), "sq"], writes=[("hs", i)])
        for i in range(NT):
            y = ym[i % 2]; yk = ("ym", i % 2)
            sq = sqe[i % 2]; st = ste[i % 2]; sg = sge[i % 2]; KE = lambda nm: (nm, i % 2)
            sc.tt("pool", sq[:], hs[:, i, :], hs[:, i, :], ALU.mult, reads=[("hs", i)], writes=[KE("sqe")])
            sc.add("dve", lambda e, o=st[:, 0, :], i_=sq[:].rearrange("p (h d) -> p h d", d=64):
                   e.tensor_reduce(o, i_, AX.X, ALU.add), reads=[KE("sqe")], writes=[KE("ste")])
            sc.ts("dve", st[:, 1, :], st[:, 0, :], 1.0 / 64, EPS, ALU.mult, ALU.add, reads=[KE("ste")], writes=[KE("ste")])
            sc.act(st[:, 2, :], st[:, 1, :], AF.Sqrt, reads=[KE("ste")], writes=[KE("ste")])
            sc.add("dve", lambda e, o=st[:, 3, :], i_=st[:, 2, :]: e.reciprocal(o, i_), reads=[KE("ste")], writes=[KE("ste")])
            sc.tt("dve", y[:].rearrange("p (h d) -> p h d", d=64), hs[:, i, :].rearrange("p (h d) -> p h d", d=64),
                  st[:, 3, :].unsqueeze(2).broadcast_to([128, 4, 64]), ALU.mult, reads=[("hs", i), KE("ste")], writes=[yk])
            sc.tt("pool", y[:], y[:], ng[:], ALU.mult, reads=[yk, "ng"], writes=[yk])
            sc.act(sg[:], tm[:, i, 512:768], AF.Sigmoid, reads=[("tm", i)], writes=[KE("sge")])
            sc.tt("dve", y[:], y[:], sg[:], ALU.mult, reads=[yk, KE("sge")], writes=[yk])
            for ch in range(2):
                sc.tr(ptr[:, ch * 128:(ch + 1) * 128], y[:, ch * 128:(ch + 1) * 128], ident[:], reads=[yk, "ident"], writes=["ptr"])
            o = yo[i % 2]; ok = ("yo", i % 2)
            sc.copy("act", o[:], ptr[:, 0:256], reads=["ptr"], writes=[ok])
            sc.dma("pool", c.yT[768:1024, i * 128:(i + 1) * 128].rearrange("(ch p) t -> p ch t", p=128),
                   o[:].rearrange("p (ch t) -> p ch t", ch=2), reads=[ok])
        sc.flush()


BF16 = mybir.dt.bfloat16
F16 = mybir.dt.float16
NE = 16
CAP = 256
WQ = "pool"


def phase_outproj_router(nc, sc, c, l, x_src):
    with ExitStack() as es:
        T = lambda name, shape, dt=F32: es.enter_context(nc.sbuf_tensor("%s_L%d" % (name, l), shape, dt))
        P = lambda name, shape, dt=F32: es.enter_context(nc.psum_tensor("%s_L%d" % (name, l), shape, dt))
        wt = T("e_wt", [128, 8, D], BF16); yh = [T("e_yh%d" % i, [128, 8, 512], BF16) for i in range(2)]
        g = T("e_g", [128, 8]); ones = T("e_ones", [128, 128])
        yb = [T("e_yb%d" % i, [128, 8, 512]) for i in range(2)]
        ysq = T("e_ysq", [128, 6, 512])
        xt = [T("e_xt%d" % i, [128, D]) for i in range(3)]
        stb = [T("e_stb%d" % i, [128, 4, 8]) for i in range(2)]
        pm = [P("e_pm%d" % i, [128, 512]) for i in range(3)]
        pst = P("e_pst", [128, 512])
        ident = T("f_id", [128, 128]); g2 = T("f_g2", [128, 8]); wr = T("f_wr", [128, 8, NE])
        xn = [T("f_xn%d" % i, [128, D]) for i in range(2)]
        junk = T("f_junk", [128, D])
        hb = [T("f_hb%d" % i, [128, D], BF16) for i in range(2)]
        hT = [T("f_hT%d" % i, [128, 8, 128]) for i in range(2)]
        rs = [T("f_st%d" % i, [128, 8]) for i in range(6)]
        aff = T("f_aff", [128, NT, NE]); ex = [T("f_ex%d" % i, [128, NE]) for i in range(2)]
        affT = T("f_affT", [NE, S]); work = T("f_work", [NE, S])
        gem = T("f_gem", [NE, CAP]); iem = T("f_iem", [NE, CAP], U32); ief = T("f_ief", [NE, CAP])
        idxs = T("f_idxs", [128, 2, NE], I32); gts = T("f_gts", [128, 2, NE])
        pT = [P("f_pT%d" % i, [128, 512]) for i in range(2)]
        pla = P("f_pla", [128, 512]); pla2 = P("f_pla2", [128, 512])

        sc.dma("sp", ones[:], c.ones, writes=["ones"])
        sc.dma("sp", g[:], c.gout[l], writes=["g"])
        for ch in range(8):
            sc.dma("pool", wt[:, ch, :], c.w_out[l][ch * 128:(ch + 1) * 128, :], writes=[("wt", ch)])
        sc.dma("sp", ident[:], c.ident, writes=["ident"])
        sc.dma("sp", g2[:], c.g2rep[l], writes=["g2"])
        sc.dma("sp", wr[:], c.w_router[l].rearrange("(c p) e -> p c e", p=128), writes=["wr"])
        sc.tt("dve", wr[:], wr[:], g2[:].unsqueeze(2).broadcast_to([128, 8, NE]), ALU.mult, reads=["wr", "g2"], writes=["wr"])

        NRS = 6
        def router_stage(k, i):
            if i < 0 or i >= NT:
                return
            x = xt[i % 3]; xk = ("xt", i % 3)
            xs = xn[i % 2]; xsk = ("xn", i % 2); sb = rs[i % NRS]; sk = ("rs", i % NRS)
            if k == 0:
                sc.act(junk[:], x[:], AF.Square, reads=[xk], writes=["junk", sk], accum_out=sb[:, 0:1])
                sc.ts("dve", sb[:, 1:2], sb[:, 0:1], 1.0 / D, EPS, ALU.mult, ALU.add, reads=[sk], writes=[sk])
                sc.act(sb[:, 2:3], sb[:, 1:2], AF.Sqrt, reads=[sk], writes=[sk])
            elif k == 1:
                sc.add("dve", lambda e, o=sb[:, 3:4], i_=sb[:, 2:3]: e.reciprocal(o, i_), reads=[sk], writes=[sk])
                sc.ts("dve", xs[:], x[:], sb[:, 3:4], None, ALU.mult, reads=[xk, sk], writes=[xsk])
                sc.copy("act", hb[i % 2][:], xs[:], reads=[xsk], writes=[("hb", i % 2)])
                sc.dma("sp", c.h2n[i * 128:(i + 1) * 128, :], hb[i % 2][:], reads=[("hb", i % 2)])
                for half in range(2):
                    pt = pT[half]; pk = ("pT", half)
                    for cc in range(4):
                        ch = half * 4 + cc
                        sc.tr(pt[:, cc * 128:(cc + 1) * 128], xs[:, ch * 128:(ch + 1) * 128], ident[:], reads=[xsk, "ident"], writes=[pk])
            elif k == 2:
                for half in range(2):
                    pt = pT[half]; pk = ("pT", half)
                    sc.copy("act", hT[i % 2][:, half * 4:(half + 1) * 4, :],
                            pt[:].rearrange("p (c t) -> p c t", c=4), reads=[pk], writes=[("hT", i % 2)])
                for ch in range(8):
                    sc.mm(pla[:, 0:NE], hT[i % 2][:, ch, :], wr[:, ch, :], ch == 0, ch == 7, reads=[("hT", i % 2), "wr"], writes=["pl"])
            elif k == 3:
                sc.add("dve", lambda e, o=sb[:, 4:5], i_=pla[:, 0:NE]: e.tensor_reduce(o, i_, AX.X, ALU.max), reads=["pl"], writes=[sk])
                sc.ts("dve", sb[:, 5:6], sb[:, 4:5], -1.0, None, ALU.mult, reads=[sk], writes=[sk])
                sc.act(ex[i % 2][:], pla[:, 0:NE], AF.Exp, reads=["pl", sk], writes=[("ex", i % 2), sk], bias=sb[:, 5:6], accum_out=sb[:, 6:7])
            elif k == 4:
                sc.add("dve", lambda e, o=sb[:, 7:8], i_=sb[:, 6:7]: e.reciprocal(o, i_), reads=[sk], writes=[sk])
                sc.ts("dve", aff[:, i, :], ex[i % 2][:], sb[:, 7:8], None, ALU.mult, reads=[("ex", i % 2), sk], writes=[("aff", i)])
                sc.tr(pla2[0:NE, 128:256], aff[:, i, :], ident[:], reads=[("aff", i), "ident"], writes=["pa"])
            elif k == 5:
                sc.copy("act", affT[:, i * 128:(i + 1) * 128], pla2[0:NE, 128:256], reads=["pa"], writes=["affT"])
        def router_iter(it):
            for k in (5, 4, 3, 2, 1, 0):
                router_stage(k, it - 1 - k)

        pi = 0
        for b in range(4):
            y = yb[b % 2]; ykey = ("yb", b % 2)
            for ch in range(8):
                sc.dma("sp", y[:, ch, :], c.yT[ch * 128:(ch + 1) * 128, b * 512:(b + 1) * 512], writes=[ykey])
            sc.act(ysq[:], y[:, 0:6, :], AF.Square, reads=[ykey], writes=["ysq"])
            yhb = yh[b % 2]; yhk = ("yh", b % 2)
            for ch in range(8):
                sc.act(yhb[:, ch, :], y[:, ch, :], AF.Copy, reads=[ykey, "g"], writes=[yhk], scale=g[:, ch:ch + 1])
            sbb = stb[b % 2]; sbk = ("stb", b % 2)
            for tl in range(4):
                tsl = slice(tl * 128, (tl + 1) * 128)
                for ch in range(6):
                    col = tl * 2 + (0 if ch < 4 else 1)
                    sc.mm(pst[:, col:col + 1], ysq[:, ch, tsl], ones[:, 0:1], ch in (0, 4), ch in (3, 5), reads=["ysq", "ones"], writes=["pst"])
            p3 = pst[:, 0:8].rearrange("p (t k) -> p t k", k=2)
            sc.ts("dve", sbb[:, :, 0:1], p3[:, :, 0:1], 1.0 / 512, EPS, ALU.mult, ALU.add, reads=["pst"], writes=[sbk])
            sc.ts("dve", sbb[:, :, 1:2], p3[:, :, 1:2], 1.0 / 256, EPS, ALU.mult, ALU.add, reads=["pst"], writes=[sbk])
            sc.act(sbb[:, :, 2:4], sbb[:, :, 0:2], AF.Sqrt, reads=[sbk], writes=[sbk])
            sc.add("dve", lambda e, o=sbb[:, :, 4:6], i_=sbb[:, :, 2:4]: e.reciprocal(o, i_), reads=[sbk], writes=[sbk])
            for tl in range(4):
                i = b * 4 + tl
                tsl = slice(tl * 128, (tl + 1) * 128)
                x = xt[i % 3]; xk = ("xt", i % 3)
                sc.dma("sp", x[:], x_src[i * 128:(i + 1) * 128, :], writes=[xk])
                sb = stb[b % 2][:, tl, :]; sk = ("stb", b % 2)
                for n in range(2):
                    nsl = slice(n * 512, (n + 1) * 512)
                    ps = []
                    for (c0, c1) in ((0, 4), (4, 6), (6, 8)):
                        p_ = pm[pi % 3]; pk = ("pm", pi % 3); pi += 1
                        for ch in range(c0, c1):
                            sc.mm(p_[:, :], yhb[:, ch, tsl], wt[:, ch, nsl], ch == c0, ch == c1 - 1,
                                  reads=[yhk, ("wt", ch)], writes=[pk])
                        ps.append((p_, pk))
                    sc.stt("dve", x[:, nsl], ps[0][0][:, :], sb[:, 4:5], x[:, nsl], ALU.mult, ALU.add, reads=[ps[0][1], sk, xk], writes=[xk])
                    sc.stt("dve", x[:, nsl], ps[1][0][:, :], sb[:, 5:6], x[:, nsl], ALU.mult, ALU.add, reads=[ps[1][1], sk, xk], writes=[xk])
                    sc.tt("dve", x[:, nsl], x[:, nsl], ps[2][0][:, :], ALU.add, reads=[ps[2][1], xk], writes=[xk])
                sc.dma("sp", c.out[i * 128:(i + 1) * 128, :], x[:], reads=[xk])
                router_iter(i)
        for it in range(NT, NT + 7):
            router_iter(it)
        sc.copy("dve", work[:], affT[:], reads=["affT"], writes=["work"])
        for r in range(CAP // 8):
            sl8 = slice(r * 8, (r + 1) * 8)
            sc.add("dve", lambda e, o=gem[:, sl8]: e.max(o, work[:]), reads=["work"], writes=["gem"])
            sc.add("dve", lambda e, o=iem[:, sl8], m=gem[:, sl8]: e.max_index(o, m, work[:]), reads=["gem", "work"], writes=["iem"])
            if r < CAP // 8 - 1:
                sc.add("dve", lambda e, m=gem[:, sl8]: e.match_replace(work[:], m, work[:], -1e30), reads=["gem", "work"], writes=["work"])
        sc.copy("dve", ief[:], iem[:], reads=["iem"], writes=["ief"])
        for ct in range(2):
            pa_ = pT[ct]; pak = ("pT", ct)
            sc.tr(pa_[:, 0:NE], ief[:, ct * 128:(ct + 1) * 128], ident[0:NE, 0:NE], reads=["ief", "ident"], writes=[pak])
            sc.tr(pa_[:, NE:2 * NE], gem[:, ct * 128:(ct + 1) * 128], ident[0:NE, 0:NE], reads=["gem", "ident"], writes=[pak])
            sc.copy("act", idxs[:, ct, :], pa_[:, 0:NE], reads=[pak], writes=["idxs"])
            sc.copy("act", gts[:, ct, :], pa_[:, NE:2 * NE], reads=[pak], writes=["gts"])
        sc.dma("sp", c.idx_d, idxs[:].rearrange("p a b -> p (a b)"), reads=["idxs"])
        sc.dma("sp", c.gate_d, gts[:].rearrange("p a b -> p (a b)"), reads=["gts"])
        sc.flush()


def phase_experts(nc, sc, c, l):
    NWB = 4
    with ExitStack() as es:
        T = lambda name, shape, dt=F32: es.enter_context(nc.sbuf_tensor("%s_L%d" % (name, l), shape, dt))
        P = lambda name, shape, dt=F32: es.enter_context(nc.psum_tensor("%s_L%d" % (name, l), shape, dt))
        identb = T("g_idb", [128, 128], BF16)
        Wg = [T("g_Wg%d" % i, [128, 8, 256], BF16) for i in range(NWB)]
        Wu = [T("g_Wu%d" % i, [128, 8, 256], BF16) for i in range(NWB)]
        Wd = [T("g_Wd%d" % i, [128, 2, D], BF16) for i in range(NWB)]
        xtok = [[T("g_xt%d_%d" % (i, ct), [128, D], BF16) for ct in range(2)] for i in range(2)]
        xg = T("g_xg", [128, 8, CAP], BF16)
        H = [T("g_H%d" % i, [128, 2, CAP], BF16) for i in range(2)]
        sil = [T("g_sil%d" % i, [128, CAP]) for i in range(2)]
        ye = [[T("g_ye%d_%d" % (i, ct), [128, D]) for ct in range(2)] for i in range(2)]
        idxs = T("g_idx", [128, 2 * NE], I32); gts = T("g_gts", [128, 2 * NE])
        g2 = T("g_g2", [128, 8])
        yacc = [P("g_ya%d" % i, [128, 512]) for i in range(4)]
        au = [P("g_au%d" % i, [128, 512]) for i in range(2)]
        tp = [P("g_tp%d" % i, [128, 1024], BF16) for i in range(2)]
        sc.dma("sp", idxs[:], c.idx_d, writes=["idxs"])
        sc.dma("sp", gts[:], c.gate_d, writes=["gts"])
        sc.dma("sp", identb[:], c.identb, writes=["identb"])
        sc.dma("sp", g2[:], c.g2rep[l], writes=["g2"])
        wq = [0]
        def load_w(e, wb):
            b = wq[0] % NWB; wq[0] += 1
            hs_ = slice(wb * 256, (wb + 1) * 256)
            sc.dma("pool", Wg[b][:], c.w_gate[l, e][:, hs_].rearrange("(c p) h -> p c h", p=128), writes=[("Wg", b)])
            sc.dma("pool", Wu[b][:], c.w_up[l, e][:, hs_].rearrange("(c p) h -> p c h", p=128), writes=[("Wu", b)])
            sc.dma("pool", Wd[b][:], c.w_down[l, e][wb * 256:(wb + 1) * 256, :].rearrange("(c p) f -> p c f", p=128), writes=[("Wd", b)])
            return b
        def gather(e):
            for ct in range(2):
                col = ct * NE + e
                sc.add("pool", lambda g, o=xtok[e % 2][ct][:, :], ix=idxs[:, col:col + 1]:
                       g.indirect_dma_start(out=o, out_offset=None, in_=c.h2n[:, :],
                                            in_offset=bass.IndirectOffsetOnAxis(ap=ix, axis=0)),
                       reads=["idxs", "h2n_d"], writes=[("xtok", e % 2, ct)], dma=True)
        seq = [(e, wb) for e in range(NE) for wb in range(8)]
        pend = []
        for k in range(NWB - 1):
            pend.append(load_w(*seq[k]))
        gather(0)
        ai = 0; hi = 0
        for e in range(NE):
            for ct in range(2):
                tpp = tp[ct]; tk = ("tp", ct)
                for fc in range(8):
                    sc.tr(tpp[:, fc * 128:(fc + 1) * 128], xtok[e % 2][ct][:, fc * 128:(fc + 1) * 128], identb[:],
                          reads=[("xtok", e % 2, ct), "identb"], writes=[tk])
                for fc in range(8):
                    sc.act(xg[:, fc, ct * 128:(ct + 1) * 128], tpp[:, fc * 128:(fc + 1) * 128], AF.Copy,
                           reads=[tk, "g2"], writes=[("xg", fc)], scale=g2[:, fc:fc + 1])
            if e + 1 < NE:
                gather(e + 1)
            for wb in range(8):
                b = pend.pop(0)
                si = e * 8 + wb + NWB - 1
                if si < len(seq):
                    pend.append(load_w(*seq[si]))
                hb_ = H[hi % 2]; hk = ("H", hi % 2); hi += 1
                for jj in range(2):
                    p_ = au[ai % 2]; pk = ("au", ai % 2)
                    sl_ = sil[ai % 2]; slk = ("sil", ai % 2); ai += 1
                    for ch in range(8):
                        sc.mm(p_[:, 0:256], Wg[b][:, ch, jj * 128:(jj + 1) * 128], xg[:, ch, :], ch == 0, ch == 7,
                              reads=[("Wg", b), ("xg", ch)], writes=[pk])
                    for ch in range(8):
                        sc.mm(p_[:, 256:512], Wu[b][:, ch, jj * 128:(jj + 1) * 128], xg[:, ch, :], ch == 0, ch == 7,
                              reads=[("Wu", b), ("xg", ch)], writes=[pk])
                    sc.act(sl_[:], p_[:, 0:256], AF.Silu, reads=[pk], writes=[slk])
                    sc.tt("dve", hb_[:, jj, :], sl_[:], p_[:, 256:512], ALU.mult, reads=[slk, pk], writes=[hk])
                for ct in range(2):
                    for fb in range(2):
                        for jj in range(2):
                            sc.mm(yacc[ct * 2 + fb][:, :], hb_[:, jj, ct * 128:(ct + 1) * 128], Wd[b][:, jj, fb * 512:(fb + 1) * 512],
                                  wb == 0 and jj == 0, wb == 7 and jj == 1, reads=[hk, ("Wd", b)], writes=[("ya", ct * 2 + fb)])
            for ct in range(2):
                col = ct * NE + e
                yb_ = ye[e % 2][ct]; yk = ("ye", e % 2, ct)
                for fb in range(2):
                    if fb == 0:
                        sc.act(yb_[:, fb * 512:(fb + 1) * 512], yacc[ct * 2 + fb][:, :], AF.Copy, reads=[("ya", ct * 2 + fb), "gts"],
                               writes=[yk], scale=gts[:, col:col + 1])
                    else:
                        sc.ts("dve", yb_[:, fb * 512:(fb + 1) * 512], yacc[ct * 2 + fb][:, :], gts[:, col:col + 1], None, ALU.mult,
                              reads=[("ya", ct * 2 + fb), "gts"], writes=[yk])
                sc.add("pool", lambda g, i_=yb_[:, :], ix=idxs[:, col:col + 1]:
                       g.indirect_dma_start(out=c.out[:, :], out_offset=bass.IndirectOffsetOnAxis(ap=ix, axis=0),
                                            in_=i_, in_offset=None, compute_op=ALU.add),
                       reads=[yk, "idxs"], writes=["xout"], dma=True)
        sc.flush()


def build(n_layers=DEPTH, debug=None):
    nc = bass.Bass("TRN2", target_bir_lowering=False)
    nc.dge_precook = False
    c = Ctx()
    def inp(name, shape):
        return nc.dram_tensor(name, list(shape), F32, kind="ExternalInput").ap()
    c.x = inp("x", [S, D])
    c.norm1_g = inp("norm1_g", [DEPTH, D])
    c.w_in = inp("w_in", [DEPTH, D, D_IN])
    c.ident = inp("ident", [128, 128])
    c.ones = inp("ones", [128, 128])
    c.cos_rep = inp("cos_rep", [S, 320])
    c.sin_rep = inp("sin_rep", [S, 320])
    c.ggrep = inp("ggrep", [DEPTH, 128, 640])
    c.lru_small = inp("lru_small", [DEPTH, 128, 2, 11])
    c.lru_w = inp("lru_w", [DEPTH, 128, 8, 128])
    c.w_out = inp("w_out", [DEPTH, D, D])
    c.gout = inp("gout", [DEPTH, 128, 8])
    c.g2rep = inp("g2rep", [DEPTH, 128, 8])
    c.w_router = inp("w_router", [DEPTH, D, NE])
    c.w_gate = inp("w_expert_gate", [DEPTH, NE, D, 2 * D])
    c.w_up = inp("w_expert_up", [DEPTH, NE, D, 2 * D])
    c.w_down = inp("w_expert_down", [DEPTH, NE, 2 * D, D])
    c.h2n = nc.dram_tensor("h2n", [S, D], BF16, kind="Internal").ap()
    c.idx_d = nc.dram_tensor("idx_d", [128, 2 * NE], I32, kind="ExternalOutput" if debug == "R" else "Internal").ap()
    c.gate_d = nc.dram_tensor("gate_d", [128, 2 * NE], F32, kind="ExternalOutput" if debug == "R" else "Internal").ap()
    c.identb = nc.dram_tensor("identb", [128, 128], BF16, kind="ExternalInput").ap()
    c.triu = inp("triu", [128, 128])
    c.tril = inp("tril", [128, 128])
    c.fbrep = inp("fbrep", [DEPTH, 128, 8])
    c.ngrep = inp("ngrep", [DEPTH, 128, 256])
    c.yT = nc.dram_tensor("yT", [1024, S], F32, kind="ExternalOutput" if debug in ("B", "C", "D") else "Internal").ap()
    c.out = nc.dram_tensor("out", [S, D], F32, kind="ExternalOutput").ap()
    c.p_tm = nc.dram_tensor("p_tm", [S, D_IN], F32, kind="ExternalOutput" if debug == "A" else "Internal").ap()
    c.p_fm = nc.dram_tensor("p_fm", [1024, S], F32, kind="ExternalOutput" if debug == "A" else "Internal").ap()
    with ExitStack() as es:
        sc = Sched(nc, es)
        for l in range(n_layers):
            phase_inproj(nc, sc, c, l, c.x if l == 0 else c.out)
            if debug == "A":
                continue
            if debug != "C" and debug != "D":
                phase_attn(nc, sc, c, l)
            if debug == "B":
                continue
            if debug != "D":
                phase_lru(nc, sc, c, l)
            if debug == "C":
                continue
            phase_mlstm(nc, sc, c, l)
            if debug == "D":
                continue
            phase_outproj_router(nc, sc, c, l, c.x if l == 0 else c.out)
            if debug in ("E", "R"):
                continue
            phase_experts(nc, sc, c, l)
    return nc


def consts():
    t = np.arange(S)
    row = (t // 64).astype(np.float32)
    col = (t % 64).astype(np.float32)
    inv = (1.0 / (np.float32(10000.0) ** (np.arange(16, dtype=np.float32) / np.float32(16)))).astype(np.float32)
    ang = np.concatenate([row[:, None] * inv, col[:, None] * inv], axis=-1).astype(np.float32)
    cos = np.cos(ang).astype(np.float32)
    sin = np.sin(ang).astype(np.float32)
    import ml_dtypes
    return {"identb": np.eye(128, dtype=np.float32).astype(ml_dtypes.bfloat16),
            "ident": np.eye(128, dtype=np.float32), "ones": np.ones((128, 128), np.float32),
            "triu": np.triu(np.ones((128, 128), np.float32)), "tril": np.tril(np.ones((128, 128), np.float32)),
            "cos_rep": np.ascontiguousarray(np.tile(cos, (1, 10))), "sin_rep": np.ascontiguousarray(np.tile(sin, (1, 10)))}


def layout_small(inputs):
    o = {}
    qg = inputs["q_norm_g"]; kg = inputs["k_norm_g"]
    gg = np.concatenate([np.tile(qg, (1, 8)), np.tile(kg, (1, 2))], axis=1)
    o["ggrep"] = np.ascontiguousarray(np.broadcast_to(gg[:, None, :], (DEPTH, 128, 640))).astype(np.float32)
    go = np.concatenate([inputs["att_out_g"], inputs["lru_out_g"], np.ones((DEPTH, 256), np.float32)], axis=1)
    o["gout"] = np.ascontiguousarray(go.reshape(DEPTH, 8, 128).transpose(0, 2, 1)).astype(np.float32)
    o["g2rep"] = np.ascontiguousarray(inputs["norm2_g"].reshape(DEPTH, 8, 128).transpose(0, 2, 1)).astype(np.float32)
    o["fbrep"] = np.ascontiguousarray(np.broadcast_to(inputs["mlstm_f_bias"].reshape(DEPTH, 1, 8), (DEPTH, 128, 8))).astype(np.float32)
    o["ngrep"] = np.ascontiguousarray(np.broadcast_to(inputs["mlstm_norm_g"].reshape(DEPTH, 1, 256), (DEPTH, 128, 256))).astype(np.float32)
    cols = [inputs["conv_w"][:, j, :] for j in range(4)] + [inputs["conv_b"]]
    cols += [inputs["lru_ba"][:, d, :] for d in range(2)] + [inputs["lru_bx"][:, d, :] for d in range(2)]
    cols += [inputs["lru_lambda"][:, d, :] for d in range(2)]
    sm = np.stack(cols, axis=-1)
    o["lru_small"] = np.ascontiguousarray(sm.reshape(DEPTH, 2, 128, 11).transpose(0, 2, 1, 3)).astype(np.float32)
    lw = np.zeros((DEPTH, 128, 2, 2, 2, 128), np.float32)
    for kind, nm in enumerate(["lru_wa", "lru_wx"]):
        wsrc = inputs[nm]
        for d in range(2):
            for cc in range(2):
                for b in range(2):
                    lw[:, b * 64:(b + 1) * 64, d, kind, cc, b * 64:(b + 1) * 64] = wsrc[:, d, 2 * cc + b]
    o["lru_w"] = lw.reshape(DEPTH, 128, 8, 128)
    return o


SMALL = ["norm1_g", "q_norm_g", "k_norm_g", "conv_w", "conv_b", "lru_wa", "lru_ba", "lru_wx", "lru_bx", "lru_lambda",
         "mlstm_f_bias", "mlstm_norm_g", "att_out_g", "lru_out_g", "norm2_g"]
BIG = ["w_in", "w_out", "w_router", "w_expert_gate", "w_expert_up", "w_expert_down", "norm1_g"]


def kernel(**inputs):
    inputs = {k: np.asarray(v) for k, v in inputs.items()}
    nc = build()
    shared = {k: np.ascontiguousarray(inputs[k], dtype=np.float32) for k in BIG}
    shared.update(consts())
    shared.update(layout_small(inputs))
    x = np.ascontiguousarray(inputs["x"], dtype=np.float32)
    in_maps = []
    for b in range(8):
        m = dict(shared)
        m["x"] = x[b]
        in_maps.append(m)
    res = run_bass_kernel_spmd(nc, in_maps, core_ids=list(range(8)))
    return np.stack([r["out"] for r in res.results], axis=0).astype(np.float32)
```
